# Optimizing a Trainium2 kernel written in Bass

```python
import jax, jax.numpy as jnp
from jax import lax
import numpy as np

D_MODEL = 2048
BATCH = 8
SEQ = 2048
DEPTH = 1

MEM_LEN = 256
NORM_EPS = 1e-6

MIX_WIDTH = D_MODEL
POOL_WIDTH = MIX_WIDTH // 2
POOL_WINDOWS = (2, 4, 8, 16)
POOL_GROUP = POOL_WIDTH // len(POOL_WINDOWS)
RWKV_WIDTH = MIX_WIDTH - POOL_WIDTH
RWKV_HEAD = 64
RWKV_HEADS = RWKV_WIDTH // RWKV_HEAD
GN_EPS = 64e-5
DECAY_LORA = max(32, int(round(1.8 * RWKV_WIDTH ** 0.5 / 32)) * 32)
AAA_LORA = max(32, int(round(1.8 * RWKV_WIDTH ** 0.5 / 32)) * 32)
GATE_LORA = max(32, int(round(0.6 * RWKV_WIDTH ** 0.8 / 32)) * 32)
SHIFT_COLS = 3 * RWKV_WIDTH + DECAY_LORA + AAA_LORA + GATE_LORA
IN_COLS = POOL_WIDTH + SHIFT_COLS

XATTN_HEADS = 4
XATTN_HEAD_DIM = D_MODEL // XATTN_HEADS

MOE_GROUPS = 4
MOE_EXPERTS_PER_GROUP = 4
MOE_EXPERTS = MOE_GROUPS * MOE_EXPERTS_PER_GROUP
MOE_TOPK = 2
MOE_FF = D_MODEL // 4

kernel_name = "hybrid_pool_rwkv7_memxattn_hmoe"


def _rmsnorm(x, g):
    xf = x.astype(jnp.float32)
    y = xf * lax.rsqrt(jnp.mean(xf * xf, axis=-1, keepdims=True) + NORM_EPS)
    return (y * g.astype(jnp.float32)).astype(x.dtype)


def _token_shift(u):
    return jnp.pad(u, ((0, 0), (1, 0), (0, 0)))[:, :-1]


def _pool_mixer(u, w_pool, pool_scale):
    B, S, _ = u.shape
    uf = u.astype(jnp.float32)
    c = jnp.cumsum(uf, axis=1)
    maxw = max(POOL_WINDOWS)
    c_pad = jnp.pad(c, ((0, 0), (maxw, 0), (0, 0)))
    pos = jnp.arange(S)
    outs = []
    for gi, w in enumerate(POOL_WINDOWS):
        lo, hi = gi * POOL_GROUP, (gi + 1) * POOL_GROUP
        c_prev = c_pad[:, maxw - w: maxw - w + S, lo:hi]
        cnt = jnp.minimum(pos + 1, w).astype(jnp.float32)[None, :, None]
        outs.append((c[:, :, lo:hi] - c_prev) / cnt - uf[:, :, lo:hi])
    pooled = jnp.stack(outs, axis=2).astype(u.dtype)
    mixed = jnp.einsum('bsgc,gcd->bsgd', pooled, w_pool)
    return (mixed.reshape(B, S, POOL_WIDTH) * pool_scale).astype(u.dtype)


def _rwkv7_scan(r, decay, k, v, a_vec, b_vec):
    B, S, H, N = r.shape

    def step(state, inp):
        r_t, w_t, k_t, v_t, a_t, b_t = inp
        sa = jnp.einsum('bhij,bhj->bhi', state, a_t)
        state = (state * w_t[:, :, None, :] + sa[..., None] * b_t[:, :, None, :]
                 + v_t[..., None] * k_t[:, :, None, :])
        y_t = jnp.einsum('bhij,bhj->bhi', state, r_t)
        return state, y_t

    xs = tuple(jnp.moveaxis(t, 1, 0) for t in (r, decay, k, v, a_vec, b_vec))
    state0 = jnp.zeros((B, H, N, N), jnp.float32)
    _, ys = lax.scan(step, state0, xs)
    return jnp.moveaxis(ys, 0, 1)


def _rwkv7_mixer(p, mu, w0, w2, a0, a2, g2, k_k, k_a, r_k, ln_w, ln_b):
    B, S, _ = p.shape
    H, N, C = RWKV_HEADS, RWKV_HEAD, RWKV_WIDTH
    f32 = jnp.float32
    pf = p.astype(f32)
    z = pf + (_token_shift(pf) - pf) * mu.astype(f32)
    o1, o2 = 3 * C + DECAY_LORA, 3 * C + DECAY_LORA + AAA_LORA
    r, k, v = z[..., :C], z[..., C:2 * C], z[..., 2 * C:3 * C]
    w_lo, a_lo, g_lo = z[..., 3 * C:o1], z[..., o1:o2], z[..., o2:]
    w_log = -jax.nn.softplus(-(w0.astype(f32) + jnp.tanh(w_lo) @ w2.astype(f32))) - 0.5
    decay = jnp.exp(-jnp.exp(w_log))
    a = jax.nn.sigmoid(a0.astype(f32) + a_lo @ a2.astype(f32))
    g = jax.nn.sigmoid(g_lo) @ g2.astype(f32)
    heads = lambda t: t.reshape(B, S, H, N)
    kk = heads(k * k_k.astype(f32))
    kk = kk * lax.rsqrt(jnp.maximum(jnp.sum(kk * kk, axis=-1, keepdims=True), 1e-24))
    k = k * (1.0 + (a - 1.0) * k_a.astype(f32))
    r4, k4, v4, a4 = heads(r), heads(k), heads(v), heads(a)
    y = _rwkv7_scan(r4, heads(decay), k4, v4, -kk, kk * a4)
    mean = jnp.mean(y, axis=-1, keepdims=True)
    var = jnp.mean(jnp.square(y - mean), axis=-1, keepdims=True)
    y = ((y - mean) * lax.rsqrt(var + GN_EPS)).reshape(B, S, C) * ln_w.astype(f32) + ln_b.astype(f32)
    bonus = jnp.sum(r4 * k4 * r_k.astype(f32), axis=-1, keepdims=True) * v4
    y = y + bonus.reshape(B, S, C)
    return (y * g).astype(p.dtype)


def _memory_xattn(hn, mem_n, w_q, w_kv, w_o):
    B, S, D = hn.shape
    M = mem_n.shape[1]
    q = (hn @ w_q).reshape(B, S, XATTN_HEADS, XATTN_HEAD_DIM)
    kv = (mem_n @ w_kv).reshape(B, M, 2, XATTN_HEADS, XATTN_HEAD_DIM)
    k, v = kv[:, :, 0], kv[:, :, 1]
    s = jnp.einsum('bshd,bmhd->bhsm', q, k).astype(jnp.float32) * (XATTN_HEAD_DIM ** -0.5)
    prob = jax.nn.softmax(s, axis=-1).astype(hn.dtype)
    o = jnp.einsum('bhsm,bmhd->bshd', prob, v).reshape(B, S, D)
    return o @ w_o


def _hier_moe(hn, w_group, b_group, w_expert, b_expert, w_gate, w_up, w_down):
    B, S, D = hn.shape
    t = hn.reshape(B * S, D)
    f32 = jnp.float32
    g_logits = (t @ w_group).astype(f32) + b_group.astype(f32)
    g_prob = jax.nn.softmax(g_logits, axis=-1)
    g_idx = jnp.argmax(g_logits, axis=-1)
    g_w = jnp.take_along_axis(g_prob, g_idx[:, None], axis=-1)
    e_logits = ((t @ w_expert).astype(f32) + b_expert.astype(f32)).reshape(-1, MOE_GROUPS, MOE_EXPERTS_PER_GROUP)
    e_logits = jnp.take_along_axis(e_logits, g_idx[:, None, None], axis=1)[:, 0]
    top_v, top_i = lax.top_k(e_logits, MOE_TOPK)
    top_p = jax.nn.softmax(top_v, axis=-1) * g_w
    expert_id = g_idx[:, None] * MOE_EXPERTS_PER_GROUP + top_i
    gates = jnp.sum(jax.nn.one_hot(expert_id, MOE_EXPERTS, dtype=f32) * top_p[..., None], axis=1)
    gates = gates.astype(t.dtype)
    out = jnp.zeros_like(t)
    for e in range(MOE_EXPERTS):
        hid = jax.nn.silu(t @ w_gate[e]) * (t @ w_up[e])
        out = out + gates[:, e:e + 1] * (hid @ w_down[e])
    return out.reshape(B, S, D)


def setup_inputs(seed: int = 0) -> dict:
    key = jax.random.key(seed)
    ks = iter(jax.random.split(key, 40))
    f32 = jnp.float32
    L, D, C = DEPTH, D_MODEL, RWKV_WIDTH

    def nrm(shape, scale):
        return jax.random.normal(next(ks), shape, f32) * scale

    def gain(shape, base=1.0):
        return base + 0.02 * jax.random.normal(next(ks), shape, f32)

    def unif(shape, lo, hi):
        return jax.random.uniform(next(ks), shape, f32, lo, hi)

    return {
        'x': nrm((BATCH, SEQ, D), 1.0),
        'mem': nrm((BATCH, MEM_LEN, D), 1.0),
        'norm_mix_g': gain((L, D)),
        'w_in': nrm((L, D, IN_COLS), D ** -0.5),
        'pool_w': nrm((L, len(POOL_WINDOWS), POOL_GROUP, POOL_GROUP), POOL_GROUP ** -0.5),
        'pool_scale': gain((L, POOL_WIDTH)),
        'rwkv_mu': unif((L, SHIFT_COLS), 0.0, 1.0),
        'rwkv_w0': unif((L, C), -6.5, -1.5),
        'rwkv_w2': nrm((L, DECAY_LORA, C), 0.5 * DECAY_LORA ** -0.5),
        'rwkv_a0': nrm((L, C), 0.1),
        'rwkv_a2': nrm((L, AAA_LORA, C), AAA_LORA ** -0.5),
        'rwkv_g2': nrm((L, GATE_LORA, C), GATE_LORA ** -0.5),
        'rwkv_k_k': gain((L, C), 0.85),
        'rwkv_k_a': gain((L, C)),
        'rwkv_r_k': nrm((L, RWKV_HEADS, RWKV_HEAD), 0.1),
        'rwkv_ln_w': gain((L, C)),
        'rwkv_ln_b': nrm((L, C), 0.02),
        'w_out': nrm((L, MIX_WIDTH, D), MIX_WIDTH ** -0.5),
        'norm_xattn_g': gain((L, D)),
        'norm_mem_g': gain((L, D)),
        'xattn_w_q': nrm((L, D, D), D ** -0.5),
        'xattn_w_kv': nrm((L, D, 2 * D), D ** -0.5),
        'xattn_w_o': nrm((L, D, D), D ** -0.5),
        'norm_ffn_g': gain((L, D)),
        'moe_w_group': nrm((L, D, MOE_GROUPS), D ** -0.5),
        'moe_b_group': nrm((L, MOE_GROUPS), 0.01),
        'moe_w_expert': nrm((L, D, MOE_EXPERTS), D ** -0.5),
        'moe_b_expert': nrm((L, MOE_EXPERTS), 0.01),
        'moe_w_gate': nrm((L, MOE_EXPERTS, D, MOE_FF), D ** -0.5),
        'moe_w_up': nrm((L, MOE_EXPERTS, D, MOE_FF), D ** -0.5),
        'moe_w_down': nrm((L, MOE_EXPERTS, MOE_FF, D), MOE_FF ** -0.5),
        'norm_final_g': gain((D,)),
    }


def reference(x, mem, norm_mix_g, w_in, pool_w, pool_scale, rwkv_mu, rwkv_w0, rwkv_w2,
              rwkv_a0, rwkv_a2, rwkv_g2, rwkv_k_k, rwkv_k_a, rwkv_r_k, rwkv_ln_w, rwkv_ln_b,
              w_out, norm_xattn_g, norm_mem_g, xattn_w_q, xattn_w_kv, xattn_w_o,
              norm_ffn_g, moe_w_group, moe_b_group, moe_w_expert, moe_b_expert,
              moe_w_gate, moe_w_up, moe_w_down, norm_final_g):
    h = x
    for l in range(DEPTH):
        hn = _rmsnorm(h, norm_mix_g[l])
        proj = hn @ w_in[l]
        pool_out = _pool_mixer(proj[..., :POOL_WIDTH], pool_w[l], pool_scale[l])
        rwkv_out = _rwkv7_mixer(proj[..., POOL_WIDTH:], rwkv_mu[l], rwkv_w0[l], rwkv_w2[l],
                                rwkv_a0[l], rwkv_a2[l], rwkv_g2[l], rwkv_k_k[l], rwkv_k_a[l],
                                rwkv_r_k[l], rwkv_ln_w[l], rwkv_ln_b[l])
        mixed = jnp.concatenate([pool_out, rwkv_out], axis=-1)
        h = h + mixed @ w_out[l]
        hn = _rmsnorm(h, norm_xattn_g[l])
        mem_n = _rmsnorm(mem, norm_mem_g[l])
        h = h + _memory_xattn(hn, mem_n, xattn_w_q[l], xattn_w_kv[l], xattn_w_o[l])
        hn = _rmsnorm(h, norm_ffn_g[l])
        h = h + _hier_moe(hn, moe_w_group[l], moe_b_group[l], moe_w_expert[l], moe_b_expert[l],
                          moe_w_gate[l], moe_w_up[l], moe_w_down[l])
    return _rmsnorm(h, norm_final_g)
```

```python
import numpy as np
import concourse.bass as bass
import concourse.mybir as mybir
from concourse.bass_utils import run_bass_kernel_spmd
from contextlib import ExitStack

F32 = mybir.dt.float32
BF16 = mybir.dt.bfloat16
I32 = mybir.dt.int32
AF = mybir.ActivationFunctionType
ALU = mybir.AluOpType
AX = mybir.AxisListType

D = 2048
KC = 16
T = 2048
NTT = T // 128
NTQ = T // 512
NCH = T // 64
PAD = 16
CDEC = float(np.exp(-0.5))
WORDS = 51200
NSLOT, SL = 26, 384
NA = SL // 128


class Dep:
    __slots__ = ("w", "r")

    def __init__(self):
        self.w = None
        self.r = {}


class Eng:
    def __init__(self, fw, name, b, is_pe=False):
        self.fw, self.name, self.b, self.is_pe = fw, name, b, is_pe
        self.sem = fw.new_sem(name)
        self.cnt = 0
        self.waited = {}
        self.dma_slots = None
        self.dma_i = 0

    def _wait(self, tok):
        sem, val = tok
        if self.waited.get(id(sem), 0) < val:
            self.b.wait_ge(sem, val)
            self.waited[id(sem)] = val

    def _collect(self, reads, writes):
        for d in reads:
            if d.w is not None and not (self.is_pe and d.w[0] is self.sem):
                self._wait(d.w)
        for d in writes:
            if d.w is not None and not (self.is_pe and d.w[0] is self.sem):
                self._wait(d.w)
            for t in d.r.values():
                if not (self.is_pe and t[0] is self.sem):
                    self._wait(t)

    def op(self, fn, reads=(), writes=()):
        self._collect(reads, writes)
        inst = fn(self.b)
        self.cnt += 1
        inst.then_inc(self.sem, 1)
        tok = (self.sem, self.cnt)
        for d in reads:
            d.r[id(self.sem)] = tok
        for d in writes:
            d.w = tok
            d.r = {}
        return tok

    def dma(self, out, in_, reads=(), writes=(), **kw):
        return self.dma_fn(lambda e: e.dma_start(out=out, in_=in_, **kw), reads, writes)

    def dma_fn(self, fn, reads=(), writes=()):
        if self.dma_slots is None:
            self.dma_slots = [[self.fw.new_sem(f"{self.name}_d{i}"), 0] for i in range(8)]
        self._collect(reads, writes)
        slot = self.dma_slots[self.dma_i % len(self.dma_slots)]
        self.dma_i += 1
        if slot[1] > 0:
            self._wait((slot[0], slot[1]))
        inst = fn(self.b)
        slot[1] += 16
        inst.then_inc(slot[0], 16)
        tok = (slot[0], slot[1])
        for d in reads:
            d.r[id(slot[0])] = tok
        for d in writes:
            d.w = tok
            d.r = {}
        return tok


class FW:
    def __init__(self, nc, es):
        self.nc, self.es = nc, es
        self.pe = Eng(self, "pe", nc.tensor, True)
        self.act = Eng(self, "act", nc.scalar)
        self.dve = Eng(self, "dve", nc.vector)
        self.pool = Eng(self, "pool", nc.gpsimd)
        self.sp = Eng(self, "sp", nc.sync)
        self.engs = [self.pe, self.act, self.dve, self.pool, self.sp]

    def new_sem(self, name):
        return self.es.enter_context(self.nc.semaphore(name))

    def barrier(self):
        toks = []
        for e in self.engs:
            if e.cnt > 0:
                toks.append((e.sem, e.cnt))
            if e.dma_slots:
                for s in e.dma_slots:
                    if s[1] > 0:
                        toks.append((s[0], s[1]))
        for e in self.engs:
            for t in toks:
                if t[0] is not e.sem:
                    e._wait(t)


class Alloc:
    def __init__(self, big, start, end):
        self.big, self.top, self.end = big, start, end

    def f32(self, n):
        a = self.big[:, self.top:self.top + n]
        self.top += n
        assert self.top <= self.end, (self.top, self.end)
        return a

    def bf16(self, n):
        w = (n + 1) // 2
        a = self.big[:, self.top:self.top + w].bitcast(BF16)
        self.top += w
        assert self.top <= self.end, (self.top, self.end)
        return a[:, 0:n]


def r3(ap, a):
    return ap.rearrange("p (a b) -> p a b", a=a)


PV = dict(pool_scale=0, mu_rkv=8, mu_lo=32, w0=35, a0=43, k_k=51, k_a=59, ln_w=67, ln_b=75, r_k=83, omka=91)
NPV = 99


def build(stop=99, dbg=()):
    nc = bass.Bass("TRN2", target_bir_lowering=False)

    def din(name, shape):
        return nc.dram_tensor(name, list(shape), F32, kind="ExternalInput").ap()

    x = din("x", [T, D])
    mem = din("mem", [256, D])
    w_in = din("w_in", [D, 4384])
    pool_w = din("pool_w", [4, 256, 256])
    w2 = din("w2", [64, 1024])
    a2 = din("a2", [64, 1024])
    g2 = din("g2", [160, 1024])
    w_out = din("w_out", [D, D])
    w_q = din("w_q", [D, D])
    w_kv = din("w_kv", [D, 2 * D])
    w_o = din("w_o", [D, D])
    w_r = din("w_r", [D, 20])
    b_r = din("b_r", [1, 20])
    wg_l = din("wg_l", [8192, 2048])
    wu_l = din("wu_l", [8192, 2048])
    wd_l = din("wd_l", [8192, 2048])
    gB = din("gB", [5, 128, D])
    pvd = din("pv", [128, NPV])
    cst = din("cst", [128, 1024])
    rmask_d = din("rmask", [128, T])
    out = nc.dram_tensor("out", [T, D], F32, kind="ExternalOutput").ap()
    dbg_aps = {}
    for name, shape in dbg:
        dbg_aps[name] = nc.dram_tensor(name, list(shape), F32, kind="ExternalOutput").ap()
    rkv_s = nc.dram_tensor("rkv_s", [24, 128, T], F32, kind="Internal").ap()
    h1_s = nc.dram_tensor("h1_s", [T, D], F32, kind="Internal").ap()
    h2_s = nc.dram_tensor("h2_s", [T, D], F32, kind="Internal").ap()
    NROW = NSLOT * SL
    Xs = nc.dram_tensor("Xs", [NROW, D], BF16, kind="Internal").ap()
    Ys = nc.dram_tensor("Ys", [NROW, D], F32, kind="Internal").ap()

    with ExitStack() as es:
        fw = FW(nc, es)
        pe, act, dve, pool, sp = fw.pe, fw.act, fw.dve, fw.pool, fw.sp
        big = es.enter_context(nc.sbuf_tensor("big", [128, WORDS], F32))[:]
        banks = [(es.enter_context(nc.psum_tensor(f"bk{i}", [128, 512], F32))[:], Dep()) for i in range(8)]
        bki = [0]

        def nb():
            b = banks[bki[0] % 8]
            bki[0] += 1
            return b

        def mm(o, lhsT, rhs, start, stop, reads, writes):
            pe.op(lambda e: e.matmul(o, lhsT=lhsT, rhs=rhs, start=start, stop=stop), reads, writes)

        CONST_W = 7424
        ca = Alloc(big, 0, CONST_W)
        identf = ca.f32(128)
        blk1 = ca.f32(128)
        mSI = ca.f32(256)
        mL = ca.f32(128)
        i64 = ca.f32(64)
        rcnt = ca.f32(64)
        pv = ca.f32(NPV + 1)
        identb = ca.bf16(128)
        onesb = ca.bf16(128)
        rmask = ca.bf16(T)
        gBt = ca.f32(D)
        lo1 = ca.bf16(T)
        sg1 = ca.bf16(T)
        sg2 = ca.bf16(T)
        ones1 = ca.f32(128)
        brow = ca.f32(20)
        d_const, d_gB, d_lo1, d_sg1, d_sg2 = Dep(), Dep(), Dep(), Dep(), Dep()
        B0_OFF = CONST_W
        B1_OFF = B0_OFF + 8192
        M_OFF = B1_OFF + 8192
        A_OFF = WORDS - 16384
        bufA = r3(big[:, A_OFF:WORDS].bitcast(BF16), 16)
        bufB = r3(big[:, B0_OFF:M_OFF].bitcast(BF16), 16)
        d_A, d_B = Dep(), Dep()

        sp.dma(identf, cst[:, 0:128], writes=[d_const])
        sp.dma(blk1, cst[:, 128:256], writes=[d_const])
        sp.dma(mSI, cst[:, 256:512], writes=[d_const])
        sp.dma(mL, cst[:, 512:640], writes=[d_const])
        sp.dma(i64, cst[:, 640:704], writes=[d_const])
        sp.dma(rcnt, cst[:, 704:768], writes=[d_const])
        sp.dma(pv[:, 0:NPV], pvd, writes=[d_const])
        sp.dma(brow[0:1, :], b_r, writes=[d_const])
        pool.dma(identb, cst[:, 0:128], writes=[d_const])
        pool.dma(rmask, rmask_d, writes=[d_const])
        pool.op(lambda e: e.memset(onesb, 1.0), writes=[d_const])
        pool.op(lambda e: e.memset(ones1, 1.0), writes=[d_const])
        dve.op(lambda e: e.tensor_scalar(out=pv[:, PV["omka"]:PV["omka"] + 8], in0=pv[:, PV["k_a"]:PV["k_a"] + 8],
                                         scalar1=-1.0, scalar2=1.0, op0=ALU.mult, op1=ALU.add), reads=[d_const], writes=[d_const])
        fw.barrier()

        def pvc(name, j):
            c = PV[name] + j
            return pv[:, c:c + 1]

        d_Xs, d_Ys = Dep(), Dep()
        if stop >= 8:
            dve.op(lambda e: e.memset(sg1, 0.0), writes=[d_sg1])
            dve.op(lambda e: e.memset(sg2, 0.0), writes=[d_sg2])
            for c in range(NROW // 128):
                zt, dz = ((sg1, d_sg1), (sg2, d_sg2))[c % 2]
                pool.dma(Xs[c * 128:(c + 1) * 128, :], zt, reads=[dz])

        def load_gB(i):
            sp.dma(gBt, gB[i], writes=[d_gB])

        def norm_tiles(al, ntiles, src_fn, dstT, d_dst, tok_off=0, keep=None):
            xts = [(al.f32(D), Dep()) for _ in range(2)]
            xns = [(al.bf16(D), Dep()) for _ in range(2)]
            junk = al.bf16(D)
            d_junk = Dep()
            sts = [(al.f32(8), Dep()) for _ in range(4)]
            for i in range(ntiles):
                xt, dx = xts[i % 2]
                xn, dn = xns[i % 2]
                st, d_st = sts[i % 4]
                sp.dma(xt, src_fn(i), writes=[dx])
                if keep is not None:
                    keep(i, xt, dx)
                act.op(lambda e: e.activation(out=junk, in_=xt, func=AF.Square, accum_out=st[:, 0:1]), reads=[dx], writes=[d_junk, d_st])
                dve.op(lambda e: e.tensor_scalar(out=st[:, 1:2], in0=st[:, 0:1], scalar1=1.0 / D, scalar2=1e-6, op0=ALU.mult, op1=ALU.add),
                       reads=[d_st], writes=[d_st])
                act.op(lambda e: e.activation(out=st[:, 2:3], in_=st[:, 1:2], func=AF.Sqrt), reads=[d_st], writes=[d_st])
                dve.op(lambda e: e.reciprocal(out=st[:, 3:4], in_=st[:, 2:3]), reads=[d_st], writes=[d_st])
                dve.op(lambda e: e.scalar_tensor_tensor(out=xn, in0=xt, scalar=st[:, 3:4], in1=gBt, op0=ALU.mult, op1=ALU.mult),
                       reads=[dx, d_st, d_gB], writes=[dn])
                for q in range(4):
                    pb, pd = nb()
                    for j in range(4):
                        kc = q * 4 + j
                        mm(pb[:, j * 128:(j + 1) * 128], xn[:, kc * 128:(kc + 1) * 128], identb, True, True, [dn, d_const], [pd])
                    t0 = tok_off + i * 128
                    eng = act if q % 2 == 0 else dve
                    if eng is act:
                        act.op(lambda e: e.activation(out=dstT[:, q * 4:q * 4 + 4, t0:t0 + 128], in_=r3(pb, 4), func=AF.Copy), reads=[pd], writes=[d_dst])
                    else:
                        dve.op(lambda e: e.tensor_copy(out=dstT[:, q * 4:q * 4 + 4, t0:t0 + 128], in_=r3(pb, 4)), reads=[pd], writes=[d_dst])

        def dump(name, ap_sb, dep, dst=None):
            if name in dbg_aps:
                sp.dma(dbg_aps[name] if dst is None else dst, ap_sb, reads=[dep])

        load_gB(0)
        al = Alloc(big, M_OFF, A_OFF)
        norm_tiles(al, NTT, lambda i: x[i * 128:(i + 1) * 128, :], bufA, d_A)
        fw.barrier()
        if "hnT" in dbg_aps:
            tmp = Alloc(big, M_OFF, A_OFF).f32(T)
            dtmp = Dep()
            for kc in range(16):
                dve.op(lambda e: e.tensor_copy(out=tmp, in_=bufA[:, kc, :]), reads=[d_A], writes=[dtmp])
                sp.dma(dbg_aps["hnT"][kc * 128:(kc + 1) * 128, :], tmp, reads=[dtmp])
            fw.barrier()

        if stop >= 2:
            al = Alloc(big, B1_OFF, A_OFF)
            wbs = [(r3(al.bf16(16 * 128), 16), Dep()) for _ in range(4)]
            wbi = [0]
            pbufs = [(al.f32(PAD + T), Dep()) for _ in range(2)]
            fbs = [(al.f32(PAD + T), Dep()) for _ in range(3)]
            pooled = [(al.bf16(T), Dep()) for _ in range(2)]
            pwb = r3(al.bf16(8 * 256), 8)
            d_pw = Dep()
            for pbf, dp in pbufs + fbs:
                dve.op(lambda e: e.memset(pbf[:, 0:PAD], 0.0), writes=[dp])
            pool.dma(pwb, pool_w.rearrange("g (cc p) d -> p (g cc) d", p=128), writes=[d_pw])
            pbi = [0]

            def proj_block(col0, n):
                ws, dw = wbs[wbi[0] % 4]
                wbi[0] += 1
                pool.dma(ws[:, :, 0:n], w_in[:, col0:col0 + n].rearrange("(kc p) n -> p kc n", p=128), writes=[dw])
                pbf, dp = pbufs[pbi[0] % 2]
                pbi[0] += 1
                for tq in range(NTQ):
                    pb, pd = nb()
                    for kc in range(KC):
                        mm(pb[0:n, :], ws[:, kc, 0:n], bufA[:, kc, tq * 512:(tq + 1) * 512], kc == 0, kc == KC - 1, [dw, d_A], [pd])
                    act.op(lambda e: e.activation(out=pbf[0:n, PAD + tq * 512:PAD + (tq + 1) * 512], in_=pb[0:n, :], func=AF.Copy), reads=[pd], writes=[dp])
                return pbf, dp

            def tshift(pbf, dp, n, mu_ap, zout, dz):
                f0, df0 = fbs[0]
                dve.op(lambda e: e.tensor_tensor(out=f0[0:n, 0:T], in0=pbf[0:n, PAD - 1:PAD - 1 + T], in1=pbf[0:n, PAD:PAD + T], op=ALU.subtract),
                       reads=[dp], writes=[df0])
                dve.op(lambda e: e.scalar_tensor_tensor(out=zout, in0=f0[0:n, 0:T], scalar=mu_ap, in1=pbf[0:n, PAD:PAD + T], op0=ALU.mult, op1=ALU.add),
                       reads=[df0, dp, d_const], writes=[dz])

            z1f, dz1 = fbs[1]
            z1 = z1f[:, PAD:PAD + T]
            pbf, dp = proj_block(4096, 128)
            tshift(pbf, dp, 128, pv[:, PV["mu_lo"]:PV["mu_lo"] + 1], z1, dz1)
            act.op(lambda e: e.activation(out=lo1[0:64, :], in_=z1[0:64, :], func=AF.Tanh), reads=[dz1], writes=[d_lo1])
            act.op(lambda e: e.activation(out=lo1[64:128, :], in_=z1[64:128, :], func=AF.Copy), reads=[dz1], writes=[d_lo1])
            pbf, dp = proj_block(4224, 128)
            tshift(pbf, dp, 128, pv[:, PV["mu_lo"] + 1:PV["mu_lo"] + 2], z1, dz1)
            act.op(lambda e: e.activation(out=sg1, in_=z1, func=AF.Sigmoid), reads=[dz1], writes=[d_sg1])
            pbf, dp = proj_block(4352, 32)
            tshift(pbf, dp, 32, pv[0:32, PV["mu_lo"] + 2:PV["mu_lo"] + 3], z1[0:32, :], dz1)
            act.op(lambda e: e.activation(out=sg2[0:32, :], in_=z1[0:32, :], func=AF.Sigmoid), reads=[dz1], writes=[d_sg2])
            for j in range(24):
                pbf, dp = proj_block(1024 + j * 128, 128)
                tshift(pbf, dp, 128, pvc("mu_rkv", j), z1, dz1)
                sp.dma(rkv_s[j], z1, reads=[dz1])
            for cb in range(8):
                gi = cb // 2
                w = (2, 4, 8, 16)[gi]
                pbf, dp = proj_block(cb * 128, 128)
                (fa, dfa), (fb_, dfb) = fbs[1], fbs[2]
                src, dsrc = pbf, dp
                sh = 1
                k = 0
                while sh < w:
                    dst, ddst = (fa, dfa) if k % 2 == 0 else (fb_, dfb)
                    dve.op(lambda e: e.tensor_tensor(out=dst[:, PAD:PAD + T], in0=src[:, PAD:PAD + T], in1=src[:, PAD - sh:PAD - sh + T], op=ALU.add),
                           reads=[dsrc], writes=[ddst])
                    src, dsrc = dst, ddst
                    sh *= 2
                    k += 1
                po, dpo = pooled[cb % 2]
                dve.op(lambda e: e.scalar_tensor_tensor(out=po, in0=src[:, PAD:PAD + T], scalar=1.0 / w, in1=pbf[:, PAD:PAD + T], op0=ALU.mult, op1=ALU.subtract),
                       reads=[dsrc, dp], writes=[dpo])
                f0, df0 = fbs[0]
                dve.op(lambda e: e.tensor_tensor(out=f0[:, 0:16], in0=src[:, PAD:PAD + 16], in1=rcnt[:, gi * 16:(gi + 1) * 16], op=ALU.mult),
                       reads=[dsrc, d_const], writes=[df0])
                dve.op(lambda e: e.tensor_tensor(out=po[:, 0:16], in0=f0[:, 0:16], in1=pbf[:, PAD:PAD + 16], op=ALU.subtract),
                       reads=[df0, dp], writes=[dpo])
                if cb % 2 == 1:
                    for db in range(2):
                        for tq in range(NTQ):
                            pb, pd = nb()
                            for cc in range(2):
                                mm(pb, pwb[:, gi * 2 + cc, db * 128:(db + 1) * 128], pooled[cc][0][:, tq * 512:(tq + 1) * 512], cc == 0, cc == 1,
                                   [d_pw, pooled[cc][1]], [pd])
                            blk = gi * 2 + db
                            act.op(lambda e: e.activation(out=bufB[:, blk, tq * 512:(tq + 1) * 512], in_=pb, func=AF.Copy, scale=pvc("pool_scale", blk)),
                                   reads=[pd, d_const], writes=[d_B])
            fw.barrier()
            if "mixT" in dbg_aps:
                tmp = Alloc(big, B1_OFF, A_OFF).f32(T)
                dtmp = Dep()
                for kc in range(8):
                    dve.op(lambda e: e.tensor_copy(out=tmp, in_=bufB[:, kc, :]), reads=[d_B], writes=[dtmp])
                    sp.dma(dbg_aps["mixT"][kc * 128:(kc + 1) * 128, :], tmp, reads=[dtmp])
                fw.barrier()


        if stop >= 3:
            QTK = 512
            NQ = T // QTK
            al = Alloc(big, M_OFF, WORDS)
            lw = al.bf16(1024)
            g2a = al.bf16(1024)
            g2b = al.bf16(1024)
            S32 = al.f32(64)
            Sbf = al.bf16(64)
            d_lw, d_S32, d_Sbf = Dep(), Dep(), Dep()
            sets = []
            for i in range(2):
                sets.append(dict(AR=r3(al.bf16(8 * 128), 8), BK=r3(al.bf16(8 * 128), 8), BKh=r3(al.bf16(8 * 128), 8), vb=al.bf16(QTK),
                                 GL=al.f32(8), gbuf=al.bf16(QTK), bonus=al.f32(QTK), ybuf=al.f32(QTK),
                                 d_AR=Dep(), d_BK=Dep(), d_BKh=Dep(), d_vb=Dep(), d_GL=Dep(), d_g=Dep(), d_bonus=Dep(), d_y=Dep(),
                                 d_ARr=[Dep() for _ in range(4)]))
            Fq = [al.f32(QTK) for _ in range(8)]
            dFq = [Dep() for _ in range(8)]
            NBall = r3(al.bf16(8 * 128), 8)
            KBall = r3(al.bf16(8 * 128), 8)
            TT = r3(al.bf16(8 * 64), 8)
            TM = r3(al.bf16(8 * 320), 8)
            APU = r3(al.bf16(8 * 128), 8)
            Mc = r3(al.f32(8 * 64), 8)
            CcT = r3(al.f32(8 * 64), 8)
            Wg = [[r3(al.bf16(2 * 128), 2) for _ in range(2)] for _ in range(4)]
            NTg = [[r3(al.bf16(2 * 64), 2) for _ in range(2)] for _ in range(4)]
            d_NB = [Dep() for _ in range(4)]
            d_KB = [Dep() for _ in range(4)]
            d_TT = [Dep() for _ in range(4)]
            d_TM = [Dep() for _ in range(4)]
            d_APU = [Dep() for _ in range(4)]
            d_Mc = [Dep() for _ in range(4)]
            d_Cc = [Dep() for _ in range(4)]
            dWg = [[Dep(), Dep()] for _ in range(4)]
            dNTg = [[Dep(), Dep()] for _ in range(4)]
            yc, sq, rs = al.f32(QTK), al.f32(QTK), al.f32(QTK)
            dyc, dsq, drs = Dep(), Dep(), Dep()
            pool.dma(lw[0:64, :], w2, writes=[d_lw])
            pool.dma(lw[64:128, :], a2, writes=[d_lw])
            pool.dma(g2a, g2[0:128, :], writes=[d_lw])
            pool.dma(g2b[0:32, :], g2[128:160, :], writes=[d_lw])
            HS = [slice(0, 64), slice(64, 128)]
            i64b = i64.unsqueeze(1).to_broadcast([128, 2, 64])
            pyb, pdyb = banks[7]
            nbm = [0]

            def nb7():
                b = banks[nbm[0] % 7]
                nbm[0] += 1
                return b

            def prep_gen(u):
                hp, q = divmod(u, NQ)
                S_ = sets[u % 2]
                cs = slice(hp * 128, (hp + 1) * 128)
                tsl = slice(q * QTK, (q + 1) * QTK)
                k_, sgw, alr, cum, kk, f5, f6, f7 = Fq
                dk, dsgw, dalr, dcum, dkk, df5, df6, df7 = dFq
                AR, BK, BKh, vb, GL, gbuf, bonus = S_["AR"], S_["BK"], S_["BKh"], S_["vb"], S_["GL"], S_["gbuf"], S_["bonus"]
                d_AR, d_BK, d_BKh, d_vb, d_GL, d_g, d_bonus = S_["d_AR"], S_["d_BK"], S_["d_BKh"], S_["d_vb"], S_["d_GL"], S_["d_g"], S_["d_bonus"]
                sp.dma(k_, rkv_s[8 + hp][:, tsl], writes=[dk])
                pb, pd = nb7()
                mm(pb, lw[0:64, cs], lo1[0:64, tsl], True, True, [d_lw, d_lo1], [pd])
                act.op(lambda e: e.activation(out=sgw, in_=pb, func=AF.Sigmoid, bias=pvc("w0", hp)), reads=[pd, d_const], writes=[dsgw])
                pb, pd = nb7()
                mm(pb, lw[64:128, cs], lo1[64:128, tsl], True, True, [d_lw, d_lo1], [pd])
                act.op(lambda e: e.activation(out=alr, in_=pb, func=AF.Sigmoid, bias=pvc("a0", hp)), reads=[pd, d_const], writes=[dalr])
                yield
                pb, pd = nb7()
                mm(pb, g2a[:, cs], sg1[:, tsl], True, False, [d_lw, d_sg1], [pd])
                mm(pb, g2b[0:32, cs], sg2[0:32, tsl], False, True, [d_lw, d_sg2], [pd])
                act.op(lambda e: e.activation(out=gbuf, in_=pb, func=AF.Copy), reads=[pd], writes=[d_g])
                dve.op(lambda e: e.tensor_tensor_scan(out=cum, data0=rmask[:, 0:QTK], data1=sgw, initial=0.0, op0=ALU.mult, op1=ALU.add),
                       reads=[d_const, dsgw], writes=[dcum])
                yield
                act.op(lambda e: e.activation(out=kk, in_=k_, func=AF.Copy, scale=pvc("k_k", hp)), reads=[dk, d_const], writes=[dkk])
                pool.op(lambda e: e.tensor_tensor(out=f5, in0=kk, in1=kk, op=ALU.mult), reads=[dkk], writes=[df5])
                yield
                pb, pd = nb7()
                mm(pb, blk1, f5, True, True, [d_const, df5], [pd])
                dve.op(lambda e: e.tensor_scalar(out=f6, in0=pb, scalar1=1e-24, scalar2=None, op0=ALU.max), reads=[pd], writes=[df6])
                act.op(lambda e: e.activation(out=f6, in_=f6, func=AF.Sqrt), reads=[df6], writes=[df6])
                yield
                dve.op(lambda e: e.reciprocal(out=f6, in_=f6), reads=[df6], writes=[df6])
                yield
                pool.op(lambda e: e.tensor_tensor(out=kk, in0=kk, in1=f6, op=ALU.mult), reads=[dkk, df6], writes=[dkk])
                dve.op(lambda e: e.tensor_scalar(out=f5, in0=alr, scalar1=pvc("k_a", hp), scalar2=pvc("omka", hp), op0=ALU.mult, op1=ALU.add),
                       reads=[dalr, d_const], writes=[df5])
                yield
                pool.op(lambda e: e.tensor_tensor(out=f5, in0=f5, in1=k_, op=ALU.mult), reads=[df5, dk], writes=[df5])
                pool.op(lambda e: e.tensor_tensor(out=alr, in0=alr, in1=kk, op=ALU.mult), reads=[dalr, dkk], writes=[dalr])
                yield
                act.op(lambda e: e.activation(out=f6, in_=cum, func=AF.Exp, scale=CDEC), reads=[dcum], writes=[df6])
                dve.op(lambda e: e.tensor_tensor(out=BK[:, :, 0:64], in0=r3(alr, 8), in1=r3(f6, 8), op=ALU.mult), reads=[dalr, df6], writes=[d_BK])
                pool.op(lambda e: e.tensor_tensor(out=BK[:, :, 64:128], in0=r3(f5, 8), in1=r3(f6, 8), op=ALU.mult), reads=[df5, df6], writes=[d_BK])
                yield
                dve.op(lambda e: e.tensor_tensor(out=r3(f6, 8), in0=r3(cum, 8), in1=r3(cum, 8)[:, :, 63:64].to_broadcast([128, 8, 64]), op=ALU.subtract),
                       reads=[dcum], writes=[df6])
                act.op(lambda e: e.activation(out=f6, in_=f6, func=AF.Exp, scale=CDEC), reads=[df6], writes=[df6])
                yield
                dve.op(lambda e: e.tensor_tensor(out=BKh[:, :, 0:64], in0=r3(alr, 8), in1=r3(f6, 8), op=ALU.mult), reads=[dalr, df6], writes=[d_BKh])
                pool.op(lambda e: e.tensor_tensor(out=BKh[:, :, 64:128], in0=r3(f5, 8), in1=r3(f6, 8), op=ALU.mult), reads=[df5, df6], writes=[d_BKh])
                yield
                pool.op(lambda e: e.tensor_tensor(out=f6, in0=cum, in1=sgw, op=ALU.subtract), reads=[dcum, dsgw], writes=[df6])
                act.op(lambda e: e.activation(out=f6, in_=f6, func=AF.Exp, scale=-CDEC), reads=[df6], writes=[df6])
                yield
                dve.op(lambda e: e.scalar_tensor_tensor(out=AR[:, :, 0:64], in0=r3(kk, 8), scalar=-1.0, in1=r3(f6, 8), op0=ALU.mult, op1=ALU.mult),
                       reads=[dkk, df6], writes=[d_AR])
                act.op(lambda e: e.activation(out=GL, in_=r3(cum, 8)[:, :, 63], func=AF.Exp, scale=-CDEC), reads=[dcum], writes=[d_GL])
                yield
                act.op(lambda e: e.activation(out=f6, in_=cum, func=AF.Exp, scale=-CDEC), reads=[dcum], writes=[df6])
                sp.dma(k_, rkv_s[hp][:, tsl], writes=[dk])
                pool.op(lambda e: e.tensor_tensor(out=AR[:, :, 64:128], in0=r3(k_, 8), in1=r3(f6, 8), op=ALU.mult), reads=[dk, df6], writes=[d_AR] + S_["d_ARr"])
                yield
                dve.op(lambda e: e.scalar_tensor_tensor(out=f6, in0=k_, scalar=pvc("r_k", hp), in1=f5, op0=ALU.mult, op1=ALU.mult),
                       reads=[dk, df5, d_const], writes=[df6])
                sp.dma(sgw, rkv_s[16 + hp][:, tsl], writes=[dsgw])
                yield
                pb, pd = nb7()
                mm(pb, blk1, f6, True, True, [d_const, df6], [pd])
                dve.op(lambda e: e.tensor_tensor(out=bonus, in0=pb, in1=sgw, op=ALU.mult), reads=[pd, dsgw], writes=[d_bonus])
                act.op(lambda e: e.activation(out=vb, in_=sgw, func=AF.Copy), reads=[dsgw], writes=[d_vb])
                yield

            def chunk_gen(u):
                hp, q = divmod(u, NQ)
                S_ = sets[u % 2]
                tsl = slice(q * QTK, (q + 1) * QTK)
                AR, BK, BKh, vb, GL, gbuf, bonus, ybuf = S_["AR"], S_["BK"], S_["BKh"], S_["vb"], S_["GL"], S_["gbuf"], S_["bonus"], S_["ybuf"]
                d_AR, d_BK, d_BKh, d_vb, d_GL, d_g, d_bonus, dy = S_["d_AR"], S_["d_BK"], S_["d_BKh"], S_["d_vb"], S_["d_GL"], S_["d_g"], S_["d_bonus"], S_["d_y"]
                d_ARr = S_["d_ARr"]
                if q == 0:
                    dve.op(lambda e: e.memset(S32, 0.0), writes=[d_S32])
                    dve.op(lambda e: e.memset(Sbf, 0.0), writes=[d_Sbf])
                pool.op(lambda e: e.tensor_tensor(out=Mc, in0=i64.unsqueeze(1).to_broadcast([128, 8, 64]),
                                                  in1=GL.unsqueeze(2).to_broadcast([128, 8, 64]), op=ALU.mult),
                        reads=[d_const, d_GL], writes=d_Mc)
                for g in range(4):
                    l0 = 2 * g
                    pa, pda = nb7()
                    pb_, pdb = nb7()
                    pt, pdt = nb7()
                    pv_, pdv = nb7()
                    for ci in range(2):
                        c = l0 + ci
                        for h in range(2):
                            hs = HS[h]
                            mm(pa[hs, ci * 128:(ci + 1) * 128], BK[hs, c, 0:64], AR[hs, c, :], True, True, [d_BK, d_AR, d_ARr[g]], [pda])
                            mm(pb_[hs, ci * 128:(ci + 1) * 128], BK[hs, c, 64:128], AR[hs, c, :], True, True, [d_BK, d_AR, d_ARr[g]], [pdb])
                            mm(pt[hs, ci * 64:(ci + 1) * 64], AR[hs, c, 0:64], BK[hs, c, 0:64], True, True, [d_BK, d_AR], [pdt])
                            idh = identb[hs, 64 * h:64 * h + 64]
                            mm(pv_[hs, ci * 256:ci * 256 + 64], vb[hs, c * 64:(c + 1) * 64], idh, True, True, [d_vb, d_const], [pdv])
                            mm(pv_[hs, ci * 256 + 64:ci * 256 + 128], BKh[hs, c, 0:64], idh, True, True, [d_BKh, d_const], [pdv])
                            mm(pv_[hs, ci * 256 + 128:ci * 256 + 192], BKh[hs, c, 64:128], idh, True, True, [d_BKh, d_const], [pdv])
                            mm(pv_[hs, ci * 256 + 192:ci * 256 + 256], AR[hs, c, 0:64], idh, True, True, [d_AR, d_const], [pdv])
                    dve.op(lambda e: e.tensor_tensor(out=NBall[:, l0:l0 + 2, :], in0=r3(pa[:, 0:256], 2), in1=r3(mSI, 2), op=ALU.mult),
                           reads=[pda, d_const], writes=[d_NB[g]])
                    dve.op(lambda e: e.tensor_tensor(out=KBall[:, l0:l0 + 2, :], in0=r3(pb_[:, 0:256], 2), in1=r3(mSI, 2), op=ALU.mult),
                           reads=[pdb, d_const], writes=[d_KB[g]])
                    dve.op(lambda e: e.tensor_tensor(out=NTg[g][0], in0=r3(pt[:, 0:128], 2), in1=r3(mL, 2), op=ALU.mult),
                           reads=[pdt, d_const], writes=[dNTg[g][0]])
                    act.op(lambda e: e.activation(out=TM[:, l0:l0 + 2, 0:256], in_=r3(pv_, 2), func=AF.Copy), reads=[pdv], writes=[d_TM[g]])
                    pool.op(lambda e: e.tensor_tensor(out=Wg[g][0][:, :, 64:128], in0=NBall[:, l0:l0 + 2, 0:64], in1=i64b, op=ALU.add),
                            reads=[d_NB[g], d_const], writes=[dWg[g][0]])
                    yield
                for g in range(4):
                    l0 = 2 * g
                    p0, pd0 = nb7()
                    q0, qd0 = nb7()
                    NT, dNT = NTg[g][0], dNTg[g][0]
                    for ci in range(2):
                        for h in range(2):
                            hs = HS[h]
                            mm(p0[hs, ci * 64:(ci + 1) * 64], NT[hs, ci, :], NBall[hs, l0 + ci, 0:64], True, True, [dNT, d_NB[g]], [pd0])
                            mm(q0[hs, ci * 64:(ci + 1) * 64], NBall[hs, l0 + ci, 0:64], NT[hs, ci, :], True, True, [dNT, d_NB[g]], [qd0])
                    act.op(lambda e: e.activation(out=Wg[g][0][:, :, 0:64], in_=r3(p0[:, 0:128], 2), func=AF.Copy), reads=[pd0], writes=[dWg[g][0]])
                    act.op(lambda e: e.activation(out=NTg[g][1], in_=r3(q0[:, 0:128], 2), func=AF.Copy), reads=[qd0], writes=[dNTg[g][1]])
                    yield
                cur, ntc = 0, 1
                for lvl in range(1, 6):
                    last = lvl == 5
                    for g in range(4):
                        l0 = 2 * g
                        Wc, dWc = Wg[g][cur], dWg[g][cur]
                        NTc, dNTc = NTg[g][ntc], dNTg[g][ntc]
                        p1, pd1 = nb7()
                        if not last:
                            q1, qd1 = nb7()
                        for ci in range(2):
                            for h in range(2):
                                hs = HS[h]
                                if not last:
                                    mm(p1[hs, ci * 128:(ci + 1) * 128], NTc[hs, ci, :], Wc[hs, ci, :], True, True, [dNTc, dWc], [pd1])
                                    mm(q1[hs, ci * 64:(ci + 1) * 64], Wc[hs, ci, 0:64], NTc[hs, ci, :], True, True, [dNTc, dWc], [qd1])
                                else:
                                    mm(p1[hs, ci * 64:(ci + 1) * 64], NTc[hs, ci, :], Wc[hs, ci, 64:128], True, True, [dNTc, dWc], [pd1])
                        if not last:
                            Wn, dWn = Wg[g][1 - cur], dWg[g][1 - cur]
                            NTn, dNTn = NTg[g][1 - ntc], dNTg[g][1 - ntc]
                            act.op(lambda e: e.activation(out=Wn[:, :, 0:64], in_=r3(p1[:, 0:256], 2)[:, :, 0:64], func=AF.Copy), reads=[pd1], writes=[dWn])
                            dve.op(lambda e: e.tensor_tensor(out=Wn[:, :, 64:128], in0=r3(p1[:, 0:256], 2)[:, :, 64:128], in1=Wc[:, :, 64:128], op=ALU.add),
                                   reads=[pd1, dWc], writes=[dWn])
                            act.op(lambda e: e.activation(out=NTn, in_=r3(q1[:, 0:128], 2), func=AF.Copy), reads=[qd1], writes=[dNTn])
                        else:
                            dve.op(lambda e: e.tensor_tensor(out=TT[:, l0:l0 + 2, :], in0=r3(p1[:, 0:128], 2), in1=Wc[:, :, 64:128], op=ALU.add),
                                   reads=[pd1, dWc], writes=[d_TT[g]])
                        if g % 2 == 1:
                            yield
                    cur, ntc = 1 - cur, 1 - ntc
                for g in range(4):
                    l0 = 2 * g
                    pw, pdw = nb7()
                    for ci in range(2):
                        for h in range(2):
                            hs = HS[h]
                            mm(pw[hs, ci * 64:(ci + 1) * 64], KBall[hs, l0 + ci, 0:64], TM[hs, l0 + ci, 0:64], True, True, [d_KB[g], d_TM[g]], [pdw])
                    act.op(lambda e: e.activation(out=TM[:, l0:l0 + 2, 256:320], in_=r3(pw[:, 0:128], 2), func=AF.Copy), reads=[pdw], writes=[d_TM[g]])
                yield
                for g in range(4):
                    l0 = 2 * g
                    pq, pdq = nb7()
                    for ci in range(2):
                        for h in range(2):
                            hs = HS[h]
                            mm(pq[hs, ci * 128:(ci + 1) * 128], TT[hs, l0 + ci, :], TM[hs, l0 + ci, 192:320], True, True, [d_TT[g], d_TM[g]], [pdq])
                    dve.op(lambda e: e.tensor_copy(out=APU[:, l0:l0 + 2, :], in_=r3(pq[:, 0:256], 2)), reads=[pdq], writes=[d_APU[g]])
                yield
                for g in range(4):
                    l0 = 2 * g
                    pm, pdm = nb7()
                    pc, pdc = nb7()
                    pr, pdr = nb7()
                    for ci in range(2):
                        l = l0 + ci
                        for h in range(2):
                            hs = HS[h]
                            mm(pm[hs, ci * 64:(ci + 1) * 64], APU[hs, l, 0:64], TM[hs, l, 64:128], True, True, [d_APU[g], d_TM[g]], [pdm])
                            mm(pc[hs, ci * 64:(ci + 1) * 64], TM[hs, l, 64:128], APU[hs, l, 64:128], True, False, [d_APU[g], d_TM[g]], [pdc])
                            mm(pc[hs, ci * 64:(ci + 1) * 64], TM[hs, l, 128:192], TM[hs, l, 0:64], False, True, [d_TM[g]], [pdc])
                            mm(pr[hs, ci * 64:(ci + 1) * 64], APU[hs, l, 0:64], NBall[hs, l, 64:128], True, True, [d_APU[g], d_NB[g]], [pdr])
                    dve.op(lambda e: e.tensor_tensor(out=Mc[:, l0:l0 + 2, :], in0=r3(pm[:, 0:128], 2), in1=Mc[:, l0:l0 + 2, :], op=ALU.add),
                           reads=[pdm, d_Mc[g]], writes=[d_Mc[g]])
                    act.op(lambda e: e.activation(out=CcT[:, l0:l0 + 2, :], in_=r3(pc[:, 0:128], 2), func=AF.Copy), reads=[pdc], writes=[d_Cc[g]])
                    dve.op(lambda e: e.tensor_tensor(out=AR[:, l0:l0 + 2, 64:128], in0=r3(pr[:, 0:128], 2), in1=AR[:, l0:l0 + 2, 64:128], op=ALU.add),
                           reads=[pdr, d_ARr[g], d_AR], writes=[d_ARr[g]])
                    if g % 2 == 1:
                        yield
                for l in range(8):
                    g = l // 2
                    ps_, pds = nb7()
                    for h in range(2):
                        hs = HS[h]
                        mm(ps_[hs, 0:64], Mc[hs, l, :], S32[hs, :], True, True, [d_Mc[g], d_S32], [pds])
                    for h in range(2):
                        hs = HS[h]
                        mm(pyb[hs, l * 64:(l + 1) * 64], Sbf[hs, :], AR[hs, l, 64:128], True, False, [d_Sbf, d_ARr[g]], [pdyb])
                        mm(pyb[hs, l * 64:(l + 1) * 64], APU[hs, l, 64:128], NBall[hs, l, 64:128], False, False, [d_APU[g], d_NB[g]], [pdyb])
                        mm(pyb[hs, l * 64:(l + 1) * 64], TM[hs, l, 0:64], KBall[hs, l, 64:128], False, True, [d_TM[g], d_KB[g]], [pdyb])
                    dve.op(lambda e: e.tensor_tensor(out=S32, in0=ps_[:, 0:64], in1=CcT[:, l, :], op=ALU.add), reads=[pds, d_Cc[g], d_S32], writes=[d_S32])
                    act.op(lambda e: e.activation(out=Sbf, in_=S32, func=AF.Copy), reads=[d_S32], writes=[d_Sbf])
                    yield
                act.op(lambda e: e.activation(out=ybuf, in_=pyb, func=AF.Copy), reads=[pdyb], writes=[dy])
                pb, pd = nb7()
                mm(pb, blk1, ybuf, True, True, [d_const, dy], [pd])
                dve.op(lambda e: e.scalar_tensor_tensor(out=yc, in0=pb, scalar=-1.0 / 64, in1=ybuf, op0=ALU.mult, op1=ALU.add),
                       reads=[pd, dy], writes=[dyc])
                pool.op(lambda e: e.tensor_tensor(out=sq, in0=yc, in1=yc, op=ALU.mult), reads=[dyc], writes=[dsq])
                yield
                pb, pd = nb7()
                mm(pb, blk1, sq, True, True, [d_const, dsq], [pd])
                dve.op(lambda e: e.tensor_scalar(out=rs, in0=pb, scalar1=1.0 / 64, scalar2=64e-5, op0=ALU.mult, op1=ALU.add), reads=[pd], writes=[drs])
                act.op(lambda e: e.activation(out=rs, in_=rs, func=AF.Sqrt), reads=[drs], writes=[drs])
                yield
                dve.op(lambda e: e.reciprocal(out=rs, in_=rs), reads=[drs], writes=[drs])
                pool.op(lambda e: e.tensor_tensor(out=yc, in0=yc, in1=rs, op=ALU.mult), reads=[dyc, drs], writes=[dyc])
                yield
                dve.op(lambda e: e.tensor_scalar(out=yc, in0=yc, scalar1=pvc("ln_w", hp), scalar2=pvc("ln_b", hp), op0=ALU.mult, op1=ALU.add),
                       reads=[dyc, d_const], writes=[dyc])
                pool.op(lambda e: e.tensor_tensor(out=yc, in0=yc, in1=bonus, op=ALU.add), reads=[dyc, d_bonus], writes=[dyc])
                dve.op(lambda e: e.tensor_tensor(out=bufB[:, 8 + hp, tsl], in0=yc, in1=gbuf, op=ALU.mult), reads=[dyc, d_g], writes=[d_B])
                yield

            def run_interleaved(gens):
                gens = [g for g in gens if g is not None]
                while gens:
                    for g in list(gens):
                        try:
                            next(g)
                        except StopIteration:
                            gens.remove(g)

            NU = 8 * NQ
            run_interleaved([prep_gen(0)])
            for u in range(NU):
                run_interleaved([chunk_gen(u), prep_gen(u + 1) if u + 1 < NU else None])
            fw.barrier()
            if "rwT" in dbg_aps:
                tmp = Alloc(big, M_OFF, WORDS).f32(T)
                dtmp = Dep()
                for kc in range(8):
                    dve.op(lambda e: e.tensor_copy(out=tmp, in_=bufB[:, 8 + kc, :]), reads=[d_B], writes=[dtmp])
                    sp.dma(dbg_aps["rwT"][kc * 128:(kc + 1) * 128, :], tmp, reads=[dtmp])
                fw.barrier()

        def out_proj(srcT, d_src, wmat, res_fn, dst_dram, al):
            wbs2 = [(r3(al.bf16(16 * 512), 16), Dep()) for _ in range(2)]
            xts = [(al.f32(512), Dep()) for _ in range(2)]
            hos = [(al.f32(512), Dep()) for _ in range(2)]
            i = 0
            for dblk in range(4):
                ws, dw = wbs2[dblk % 2]
                ds_ = slice(dblk * 512, (dblk + 1) * 512)
                pool.dma(ws, wmat[:, ds_].rearrange("(kc p) n -> p kc n", p=128), writes=[dw])
                for tt in range(NTT):
                    rows = slice(tt * 128, (tt + 1) * 128)
                    xt, dx = xts[i % 2]
                    ho, dh = hos[i % 2]
                    i += 1
                    sp.dma(xt, res_fn(rows, ds_), writes=[dx])
                    pb, pd = nb()
                    for kc in range(KC):
                        mm(pb, srcT[:, kc, rows], ws[:, kc, :], kc == 0, kc == KC - 1, [d_src, dw], [pd])
                    dve.op(lambda e: e.tensor_tensor(out=ho, in0=pb, in1=xt, op=ALU.add), reads=[pd, dx], writes=[dh])
                    sp.dma(dst_dram[rows, ds_], ho, reads=[dh])

        if stop >= 4:
            out_proj(bufB, d_B, w_out, lambda rows, cols: x[rows, cols], h1_s, Alloc(big, M_OFF, A_OFF))
            fw.barrier()
            load_gB(1)
            norm_tiles(Alloc(big, M_OFF, A_OFF), NTT, lambda i: h1_s[i * 128:(i + 1) * 128, :], bufA, d_A)
            fw.barrier()
            if "h1" in dbg_aps:
                tmp = Alloc(big, M_OFF, A_OFF).f32(D)
                dtmp = Dep()
                for tt in range(NTT):
                    sp.dma(tmp, h1_s[tt * 128:(tt + 1) * 128, :], writes=[dtmp])
                    sp.dma(dbg_aps["h1"][tt * 128:(tt + 1) * 128, :], tmp, reads=[dtmp])
                fw.barrier()


        if stop >= 5:
            kv_al = Alloc(big, M_OFF, M_OFF + 4096)
            KT = r3(kv_al.bf16(16 * 256), 16)
            Vb = r3(kv_al.bf16(2 * D), 2)
            d_KT, d_Vb, d_memT = Dep(), Dep(), Dep()
            alB = Alloc(big, B0_OFF, M_OFF)
            alM = Alloc(big, M_OFF + 4096, A_OFF)
            memT = r3(alB.bf16(16 * 256), 16)
            wbs5 = [(r3(alB.bf16(16 * 512), 16), Dep()), (r3(alM.bf16(16 * 512), 16), Dep())]
            load_gB(3)
            norm_tiles(alB, 2, lambda i: mem[i * 128:(i + 1) * 128, :], memT, d_memT)
            for g in range(8):
                ws, dw = wbs5[g % 2]
                pool.dma(ws, w_kv[:, g * 512:(g + 1) * 512].rearrange("(kc p) n -> p kc n", p=128), writes=[dw])
                if g < 4:
                    for j in range(4):
                        cb = g * 4 + j
                        pb, pd = nb()
                        for kc in range(KC):
                            mm(pb[:, 0:256], ws[:, kc, j * 128:(j + 1) * 128], memT[:, kc, :], kc == 0, kc == KC - 1, [dw, d_memT], [pd])
                        act.op(lambda e: e.activation(out=KT[:, cb, :], in_=pb[:, 0:256], func=AF.Copy), reads=[pd], writes=[d_KT])
                else:
                    for mc in range(2):
                        pb, pd = nb()
                        for kc in range(KC):
                            mm(pb, memT[:, kc, mc * 128:(mc + 1) * 128], ws[:, kc, :], kc == 0, kc == KC - 1, [dw, d_memT], [pd])
                        dve.op(lambda e: e.tensor_copy(out=Vb[:, mc, (g - 4) * 512:(g - 3) * 512], in_=pb), reads=[pd], writes=[d_Vb])
            fw.barrier()
            alM = Alloc(big, M_OFF + 4096, A_OFF)
            wbs6 = [(r3(alM.bf16(16 * 256), 16), Dep()) for _ in range(2)]
            qscale = float(512 ** -0.5)
            for g in range(8):
                ws, dw = wbs6[g % 2]
                pool.dma(ws, w_q[:, g * 256:(g + 1) * 256].rearrange("(kc p) n -> p kc n", p=128), writes=[dw])
                for j in range(2):
                    cb = g * 2 + j
                    for tq in range(NTQ):
                        ts_ = slice(tq * 512, (tq + 1) * 512)
                        pb, pd = nb()
                        for kc in range(KC):
                            mm(pb, ws[:, kc, j * 128:(j + 1) * 128], bufA[:, kc, ts_], kc == 0, kc == KC - 1, [dw, d_A], [pd])
                        if tq % 2 == 0:
                            act.op(lambda e: e.activation(out=bufB[:, cb, ts_], in_=pb, func=AF.Copy, scale=qscale), reads=[pd], writes=[d_B])
                        else:
                            dve.op(lambda e: e.tensor_scalar(out=bufB[:, cb, ts_], in0=pb, scalar1=qscale, scalar2=None, op0=ALU.mult), reads=[pd], writes=[d_B])
            fw.barrier()
            alM = Alloc(big, M_OFF + 4096, A_OFF)
            Es = [(r3(alM.bf16(2 * 512), 2), Dep()) for _ in range(2)]
            rinvs = [(alM.f32(512), Dep()) for _ in range(2)]
            it = 0
            for h in range(4):
                for tq in range(NTQ):
                    ts_ = slice(tq * 512, (tq + 1) * 512)
                    E, dE = Es[it % 2]
                    rinv, dri = rinvs[it % 2]
                    it += 1
                    for mc in range(2):
                        pb, pd = nb()
                        for c in range(4):
                            mm(pb, KT[:, h * 4 + c, mc * 128:(mc + 1) * 128], bufB[:, h * 4 + c, ts_], c == 0, c == 3, [d_KT, d_B], [pd])
                        act.op(lambda e: e.activation(out=E[:, mc, :], in_=pb, func=AF.Exp), reads=[pd], writes=[dE])
                    pb, pd = nb()
                    for mc in range(2):
                        mm(pb, onesb, E[:, mc, :], mc == 0, mc == 1, [d_const, dE], [pd])
                    dve.op(lambda e: e.reciprocal(out=rinv, in_=pb), reads=[pd], writes=[dri])
                    for c in range(4):
                        pb, pd = nb()
                        for mc in range(2):
                            mm(pb, Vb[:, mc, h * 512 + c * 128:h * 512 + (c + 1) * 128], E[:, mc, :], mc == 0, mc == 1, [d_Vb, dE], [pd])
                        dve.op(lambda e: e.tensor_tensor(out=bufA[:, h * 4 + c, ts_], in0=pb, in1=rinv, op=ALU.mult), reads=[pd, dri], writes=[d_A])
            fw.barrier()
            out_proj(bufA, d_A, w_o, lambda rows, cols: h1_s[rows, cols], h2_s, Alloc(big, B0_OFF, M_OFF))
            fw.barrier()
            if "h2" in dbg_aps:
                tmp = Alloc(big, B0_OFF, M_OFF).f32(D)
                dtmp = Dep()
                for tt in range(NTT):
                    sp.dma(tmp, h2_s[tt * 128:(tt + 1) * 128, :], writes=[dtmp])
                    sp.dma(dbg_aps["h2"][tt * 128:(tt + 1) * 128, :], tmp, reads=[dtmp])
                fw.barrier()

        if stop >= 8:
            IOA = bass.IndirectOffsetOnAxis
            bc_reg = es.enter_context(nc.gpsimd.register("bc"))
            nc.gpsimd.reg_mov(bc_reg, NROW - 1)
            BCV = nc.gpsimd.snap(bc_reg)
            bw_reg = es.enter_context(nc.gpsimd.register("bw"))
            nc.gpsimd.reg_mov(bw_reg, 8191)
            BWV = nc.gpsimd.snap(bw_reg)
            al8 = Alloc(big, B0_OFF, WORDS)
            LT = al8.f32(128)
            iop = al8.f32(1)
            siota = al8.f32(32)
            thr8 = al8.f32(8)
            p1a, p2a = al8.f32(16), al8.f32(16)
            pos1i = al8.f32(16).bitcast(I32)
            pos2i = al8.f32(16).bitcast(I32)
            widx = al8.f32(NSLOT * 4).bitcast(I32)
            d_c8, d_pos, d_widx, d_pp = Dep(), Dep(), Dep(), Dep()
            P8_TOP = al8.top
            sp.dma(LT, cst[:, 768:896], writes=[d_c8])
            sp.dma(iop, cst[:, 896:897], writes=[d_c8], allow_slow_non_contiguous=True)
            sp.dma(siota, cst[:, 897:929], writes=[d_c8])
            sp.dma(thr8, cst[:, 929:937], writes=[d_c8])
            fw.barrier()
            xnb_all = r3(al8.bf16(16 * D), 16)
            d_xnb = [Dep() for _ in range(16)]
            xts = [(al8.f32(D), Dep()) for _ in range(2)]
            xn32s = [(al8.f32(D), Dep()) for _ in range(2)]
            junk = al8.bf16(D)
            d_junk = Dep()
            h32s = [(r3(al8.f32(16 * 128), 16), Dep()) for _ in range(2)]
            wr32 = r3(al8.f32(16 * 20), 16)
            d_wr = Dep()
            logits = r3(al8.f32(16 * 20), 16)
            d_log = Dep()
            sts = [(al8.f32(8), Dep()) for _ in range(4)]
            sp.dma(wr32, w_r.rearrange("(kc p) n -> p kc n", p=128), writes=[d_wr])
            load_gB(2)
            for tt in range(16):
                xt, dx = xts[tt % 2]
                xn32, dxn = xn32s[tt % 2]
                h32, d_h32 = h32s[tt % 2]
                st, d_st = sts[tt % 4]
                sp.dma(xt, h2_s[tt * 128:(tt + 1) * 128, :], writes=[dx])
                act.op(lambda e: e.activation(out=junk, in_=xt, func=AF.Square, accum_out=st[:, 0:1]), reads=[dx], writes=[d_junk, d_st])
                dve.op(lambda e: e.tensor_scalar(out=st[:, 1:2], in0=st[:, 0:1], scalar1=1.0 / D, scalar2=1e-6, op0=ALU.mult, op1=ALU.add),
                       reads=[d_st], writes=[d_st])
                act.op(lambda e: e.activation(out=st[:, 2:3], in_=st[:, 1:2], func=AF.Sqrt), reads=[d_st], writes=[d_st])
                dve.op(lambda e: e.reciprocal(out=st[:, 3:4], in_=st[:, 2:3]), reads=[d_st], writes=[d_st])
                dve.op(lambda e: e.scalar_tensor_tensor(out=xn32, in0=xt, scalar=st[:, 3:4], in1=gBt, op0=ALU.mult, op1=ALU.mult),
                       reads=[dx, d_st, d_gB], writes=[dxn])
                act.op(lambda e: e.activation(out=xnb_all[:, tt, :], in_=xn32, func=AF.Copy), reads=[dxn], writes=[d_xnb[tt]])
                for q in range(4):
                    pf, pdf = nb()
                    for j in range(4):
                        kc = q * 4 + j
                        mm(pf[:, j * 128:(j + 1) * 128], xn32[:, kc * 128:(kc + 1) * 128], identf, True, True, [dxn, d_const], [pdf])
                    if q % 2 == 0:
                        dve.op(lambda e: e.tensor_copy(out=h32[:, q * 4:q * 4 + 4, :], in_=r3(pf, 4)), reads=[pdf], writes=[d_h32])
                    else:
                        act.op(lambda e: e.activation(out=h32[:, q * 4:q * 4 + 4, :], in_=r3(pf, 4), func=AF.Copy), reads=[pdf], writes=[d_h32])
                pb, pd = nb()
                for kc in range(KC):
                    mm(pb[:, 0:20], h32[:, kc, :], wr32[:, kc, :], kc == 0, False, [d_h32, d_wr], [pd])
                mm(pb[:, 0:20], ones1[0:1, 0:128], brow[0:1, 0:20], False, True, [d_const], [pd])
                dve.op(lambda e: e.tensor_copy(out=logits[:, tt, :], in_=pb[:, 0:20]), reads=[pd], writes=[d_log])
            NT_ = 16
            rt = [al8.f32(NT_ * 4) for _ in range(12)]
            rt4 = al8.f32(NT_ * 16)
            sel1 = al8.f32(NT_ * 16)
            sel2 = al8.f32(NT_ * 16)
            ind = al8.f32(NT_ * 16)
            tot = r3(al8.f32(NT_ * 16), NT_)
            tcum = r3(al8.f32(NT_ * 16), NT_)
            posall = al8.f32(NT_ * 16)
            ptmp = al8.f32(NT_ * 16)
            c8 = al8.f32(16 * 8)
            cnt, nsl, bsl, bsl256 = al8.f32(16), al8.f32(16), al8.f32(16), al8.f32(16)
            total = al8.f32(1)
            es32 = al8.f32(NSLOT * 16)
            esf, unused, wbase = al8.f32(NSLOT), al8.f32(NSLOT), al8.f32(NSLOT)
            pos1f, pos2f = al8.f32(16), al8.f32(16)
            widxf = al8.f32(NSLOT * 4).rearrange("p (s q) -> p s q", q=4)
            d_rt = Dep()
            lg = logits[:, :, 0:4]
            le = logits[:, :, 4:20].rearrange("p t (g e) -> p t g e", g=4)
            gmax, gsum, gw, m1, m2 = [rt[i][:, 0:NT_] for i in range(5)]
            goh, gsh, esel, oh1, e2 = [r3(rt[7 + i], NT_) for i in range(5)]
            t4 = rt4.rearrange("p (t g e) -> p t g e", t=NT_, g=4)

            def bc3(v):
                return v.unsqueeze(2).to_broadcast([128, NT_, 4])

            def v4(a):
                return a.rearrange("p (t g e) -> p t g e", t=NT_, g=4)
            R = [d_log, d_rt, d_c8]
            W_ = [d_rt]
            dve.op(lambda e: e.tensor_reduce(out=gmax, in_=lg, axis=AX.X, op=ALU.max), R, W_)
            dve.op(lambda e: e.tensor_tensor(out=goh, in0=lg, in1=bc3(gmax), op=ALU.is_equal), R, W_)
            dve.op(lambda e: e.tensor_tensor(out=gsh, in0=lg, in1=bc3(gmax), op=ALU.subtract), R, W_)
            act.op(lambda e: e.activation(out=gsh, in_=gsh, func=AF.Exp), R, W_)
            dve.op(lambda e: e.tensor_reduce(out=gsum, in_=gsh, axis=AX.X, op=ALU.add), R, W_)
            dve.op(lambda e: e.reciprocal(out=gw, in_=gsum), R, W_)
            dve.op(lambda e: e.tensor_tensor(out=t4, in0=le, in1=goh.unsqueeze(3).to_broadcast([128, NT_, 4, 4]), op=ALU.mult), R, W_)
            dve.op(lambda e: e.tensor_reduce(out=esel, in_=t4.rearrange("p t g e -> p t e g"), axis=AX.X, op=ALU.add), R, W_)
            dve.op(lambda e: e.tensor_reduce(out=m1, in_=esel, axis=AX.X, op=ALU.max), R, W_)
            dve.op(lambda e: e.tensor_tensor(out=oh1, in0=esel, in1=bc3(m1), op=ALU.is_equal), R, W_)
            dve.op(lambda e: e.scalar_tensor_tensor(out=e2, in0=oh1, scalar=-1e30, in1=esel, op0=ALU.mult, op1=ALU.add), R, W_)
            dve.op(lambda e: e.tensor_reduce(out=m2, in_=e2, axis=AX.X, op=ALU.max), R, W_)
            dve.op(lambda e: e.tensor_tensor(out=e2, in0=e2, in1=bc3(m2), op=ALU.is_equal), R, W_)
            dve.op(lambda e: e.tensor_tensor(out=p1a, in0=m1, in1=m2, op=ALU.subtract), R, W_ + [d_pp])
            act.op(lambda e: e.activation(out=p1a, in_=p1a, func=AF.Sigmoid), R + [d_pp], W_ + [d_pp])
            dve.op(lambda e: e.tensor_scalar(out=p2a, in0=p1a, scalar1=-1.0, scalar2=1.0, op0=ALU.mult, op1=ALU.add), R + [d_pp], W_ + [d_pp])
            dve.op(lambda e: e.tensor_tensor(out=p1a, in0=p1a, in1=gw, op=ALU.mult), R + [d_pp], W_ + [d_pp])
            dve.op(lambda e: e.tensor_tensor(out=p2a, in0=p2a, in1=gw, op=ALU.mult), R + [d_pp], W_ + [d_pp])
            dve.op(lambda e: e.tensor_tensor(out=v4(sel1), in0=goh.unsqueeze(3).to_broadcast([128, NT_, 4, 4]),
                                             in1=oh1.unsqueeze(2).to_broadcast([128, NT_, 4, 4]), op=ALU.mult), R, W_)
            dve.op(lambda e: e.tensor_tensor(out=v4(sel2), in0=goh.unsqueeze(3).to_broadcast([128, NT_, 4, 4]),
                                             in1=e2.unsqueeze(2).to_broadcast([128, NT_, 4, 4]), op=ALU.mult), R, W_)
            dve.op(lambda e: e.tensor_tensor(out=ind, in0=sel1, in1=sel2, op=ALU.add), R, W_)
            pw, pdw = nb()
            mm(pw[:, 0:256], LT, ind, True, True, [d_rt, d_c8], [pdw])
            pt_, pdt = nb()
            mm(pt_[:, 0:256], ones1, ind, True, True, [d_rt, d_const], [pdt])
            dve.op(lambda e: e.tensor_copy(out=tot, in_=r3(pt_[:, 0:256], NT_)), R + [pdt], W_)
            dve.op(lambda e: e.memset(tcum[:, 0, :], 0.0), R, W_)
            for tt in range(1, NT_):
                dve.op(lambda e: e.tensor_tensor(out=tcum[:, tt, :], in0=tcum[:, tt - 1, :], in1=tot[:, tt - 1, :], op=ALU.add), R, W_)
            dve.op(lambda e: e.tensor_tensor(out=cnt, in0=tcum[:, NT_ - 1, :], in1=tot[:, NT_ - 1, :], op=ALU.add), R, W_)
            dve.op(lambda e: e.tensor_tensor(out=r3(c8, 16), in0=cnt.unsqueeze(2).to_broadcast([128, 16, 8]),
                                             in1=thr8.unsqueeze(1).to_broadcast([128, 16, 8]), op=ALU.is_gt), R, W_)
            dve.op(lambda e: e.tensor_reduce(out=nsl, in_=r3(c8, 16), axis=AX.X, op=ALU.add), R, W_)
            dve.op(lambda e: e.memset(bsl[:, 0:1], 0.0), R, W_)
            for ex in range(1, 16):
                dve.op(lambda e: e.tensor_tensor(out=bsl[:, ex:ex + 1], in0=bsl[:, ex - 1:ex], in1=nsl[:, ex - 1:ex], op=ALU.add), R, W_)
            dve.op(lambda e: e.tensor_tensor(out=total, in0=bsl[:, 15:16], in1=nsl[:, 15:16], op=ALU.add), R, W_)
            dve.op(lambda e: e.tensor_scalar(out=bsl256, in0=bsl, scalar1=float(SL), scalar2=None, op0=ALU.mult), R, W_)
            dve.op(lambda e: e.tensor_tensor(out=posall, in0=pw[:, 0:256], in1=tcum.rearrange("p t e -> p (t e)"), op=ALU.add), R + [pdw], W_)
            dve.op(lambda e: e.tensor_tensor(out=r3(posall, NT_), in0=r3(posall, NT_), in1=bsl256.unsqueeze(1).to_broadcast([128, NT_, 16]), op=ALU.add), R, W_)
            dve.op(lambda e: e.tensor_tensor(out=ptmp, in0=posall, in1=sel1, op=ALU.mult), R, W_)
            dve.op(lambda e: e.tensor_reduce(out=pos1f, in_=r3(ptmp, NT_), axis=AX.X, op=ALU.add), R, W_)
            dve.op(lambda e: e.tensor_tensor(out=ptmp, in0=posall, in1=sel2, op=ALU.mult), R, W_)
            dve.op(lambda e: e.tensor_reduce(out=pos2f, in_=r3(ptmp, NT_), axis=AX.X, op=ALU.add), R, W_)
            dve.op(lambda e: e.tensor_copy(out=pos1i, in_=pos1f), R, W_ + [d_pos])
            dve.op(lambda e: e.tensor_copy(out=pos2i, in_=pos2f), R, W_ + [d_pos])
            dve.op(lambda e: e.tensor_tensor(out=r3(es32, NSLOT), in0=bsl.unsqueeze(1).to_broadcast([128, NSLOT, 16]),
                                             in1=siota[:, 0:NSLOT].unsqueeze(2).to_broadcast([128, NSLOT, 16]), op=ALU.is_le), R, W_)
            dve.op(lambda e: e.tensor_reduce(out=esf, in_=r3(es32, NSLOT), axis=AX.X, op=ALU.add), R, W_)
            dve.op(lambda e: e.tensor_scalar(out=unused, in0=siota[:, 0:NSLOT], scalar1=total[:, 0:1], scalar2=1.0e6, op0=ALU.is_ge, op1=ALU.mult), R, W_)
            dve.op(lambda e: e.tensor_scalar(out=wbase, in0=esf, scalar1=-1.0, scalar2=512.0, op0=ALU.add, op1=ALU.mult), R, W_)
            dve.op(lambda e: e.tensor_tensor(out=wbase, in0=wbase, in1=unused, op=ALU.add), R, W_)
            dve.op(lambda e: e.tensor_scalar(out=wbase, in0=wbase, scalar1=iop[:, 0:1], scalar2=None, op0=ALU.add), R, W_)
            for q in range(4):
                dve.op(lambda e: e.tensor_scalar(out=widxf[:, :, q], in0=wbase, scalar1=float(128 * q), scalar2=None, op0=ALU.add), R, W_)
            dve.op(lambda e: e.tensor_copy(out=widx, in_=widxf.rearrange("p s q -> p (s q)")), R, W_ + [d_widx])
            if "route" in dbg_aps:
                sp.dma(dbg_aps["route"][:, 0:16], pos1f, reads=[d_rt])
                sp.dma(dbg_aps["route"][:, 16:32], pos2f, reads=[d_rt])
                sp.dma(dbg_aps["route"][:, 32:64], wbase, reads=[d_rt])
                sp.dma(dbg_aps["route"][:, 64:80], p1a, reads=[d_pp])
                sp.dma(dbg_aps["route"][:, 80:96], p2a, reads=[d_pp])
                sp.dma(dbg_aps["route"][:, 96:112], cnt, reads=[d_rt])
            for tt in range(16):
                for posi in (pos1i, pos2i):
                    pool.dma_fn(lambda e: e.indirect_dma_start(out=Xs, out_offset=IOA(ap=posi[:, tt:tt + 1], axis=0), in_=xnb_all[:, tt, :], in_offset=None,
                                                               bounds_check=BCV, oob_is_err=False),
                                reads=[d_xnb[tt], d_pos], writes=[d_Xs])
            fw.barrier()
            ald = Alloc(big, P8_TOP, WORDS)
            wbufs = [(ald.bf16(8192), [Dep() for _ in range(4)]) for _ in range(6)]
            xsls = [(r3(ald.bf16(NA * D), NA), Dep()) for _ in range(2)]
            XTs = [(r3(ald.bf16(16 * SL), 16), Dep()) for _ in range(2)]
            hids = [(r3(ald.bf16(4 * SL), 4), Dep()) for _ in range(2)]
            sbs = [(ald.bf16(SL), Dep()) for _ in range(2)]
            yos = [(ald.f32(D), Dep()) for _ in range(2)]
            cnt8 = dict(yi=0, ei=0)

            def wload(i, s):
                wsl = []
                for m, wl in enumerate((wg_l, wu_l, wd_l)):
                    buf, deps = wbufs[(3 * i + m) % 6]
                    for q in range(4):
                        pool.dma_fn(lambda e: e.indirect_dma_start(out=buf[:, q * 2048:(q + 1) * 2048], out_offset=None, in_=wl,
                                                                   in_offset=IOA(ap=widx[:, s * 4 + q:s * 4 + q + 1], axis=0), bounds_check=BWV, oob_is_err=False),
                                    reads=[d_widx], writes=[deps[q]])
                    wsl.append((buf, deps))
                return wsl

            def xload(i, s):
                xsl, dxs = xsls[i % 2]
                sp.dma(xsl, Xs[s * SL:(s + 1) * SL, :].rearrange("(a p) n -> p a n", p=128), reads=[d_Xs], writes=[dxs])

            def emit_T(i, s):
                xsl, dxs = xsls[i % 2]
                XT, dXT = XTs[i % 2]
                for a in range(NA):
                    for q4 in range(4):
                        pb, pd = nb()
                        for j in range(4):
                            kc = q4 * 4 + j
                            mm(pb[:, j * 128:(j + 1) * 128], xsl[:, a, kc * 128:(kc + 1) * 128], identb, True, True, [dxs, d_const], [pd])
                        cnt8["ei"] += 1
                        if cnt8["ei"] % 2 == 0:
                            act.op(lambda e: e.activation(out=XT[:, q4 * 4:q4 * 4 + 4, a * 128:(a + 1) * 128], in_=r3(pb, 4), func=AF.Copy), reads=[pd], writes=[dXT])
                        else:
                            dve.op(lambda e: e.tensor_copy(out=XT[:, q4 * 4:q4 * 4 + 4, a * 128:(a + 1) * 128], in_=r3(pb, 4)), reads=[pd], writes=[dXT])

            def emit_GU(i, s, wsl):
                wg, dwg = r3(wsl[0][0], 16), wsl[0][1]
                wu, dwu = r3(wsl[1][0], 16), wsl[1][1]
                XT, dXT = XTs[i % 2]
                hid, dhid = hids[i % 2]
                for ffc in range(4):
                    pg, pdg = nb()
                    for kc in range(KC):
                        mm(pg[:, 0:SL], wg[:, kc, ffc * 128:(ffc + 1) * 128], XT[:, kc, :], kc == 0, kc == KC - 1, [dwg[kc // 4], dXT], [pdg])
                    pu, pdu = nb()
                    for kc in range(KC):
                        mm(pu[:, 0:SL], wu[:, kc, ffc * 128:(ffc + 1) * 128], XT[:, kc, :], kc == 0, kc == KC - 1, [dwu[kc // 4], dXT], [pdu])
                    sb_, dsb = sbs[ffc % 2]
                    act.op(lambda e: e.activation(out=sb_, in_=pg[:, 0:SL], func=AF.Silu), reads=[pdg], writes=[dsb])
                    dve.op(lambda e: e.tensor_tensor(out=hid[:, ffc, :], in0=pu[:, 0:SL], in1=sb_, op=ALU.mult), reads=[pdu, dsb], writes=[dhid])

            def emit_D(i, s, wsl):
                wd, dwd = r3(wsl[2][0], 4), wsl[2][1]
                hid, dhid = hids[i % 2]
                for a in range(NA):
                    yo, dyo = yos[cnt8["yi"] % 2]
                    cnt8["yi"] += 1
                    for dblk in range(4):
                        ds_ = slice(dblk * 512, (dblk + 1) * 512)
                        pb, pd = nb()
                        for ffc in range(4):
                            mm(pb, hid[:, ffc, a * 128:(a + 1) * 128], wd[:, ffc, ds_], ffc == 0, ffc == 3, [dhid, dwd[ffc]], [pd])
                        if dblk % 2 == 0:
                            act.op(lambda e: e.activation(out=yo[:, ds_], in_=pb, func=AF.Copy), reads=[pd], writes=[dyo])
                        else:
                            dve.op(lambda e: e.tensor_copy(out=yo[:, ds_], in_=pb), reads=[pd], writes=[dyo])
                    r0 = s * SL + a * 128
                    sp.dma(Ys[r0:r0 + 128, :], yo, reads=[dyo], writes=[d_Ys])

            lo_n = NSLOT - NSLOT // 3
            lo, hi = list(range(lo_n)), list(range(NSLOT - 1, lo_n - 1, -1))
            order = []
            while lo or hi:
                order += lo[:2]
                lo = lo[2:]
                if hi:
                    order.append(hi.pop(0))
            assert sorted(order) == list(range(NSLOT))
            xload(0, order[0])
            emit_T(0, order[0])
            for i, s in enumerate(order):
                wsl = wload(i, s)
                if i + 1 < NSLOT:
                    xload(i + 1, order[i + 1])
                emit_GU(i, s, wsl)
                if i + 1 < NSLOT:
                    emit_T(i + 1, order[i + 1])
                emit_D(i, s, wsl)
            fw.barrier()
            ale = Alloc(big, P8_TOP, WORDS)
            cts = [(ale.f32(D), Dep()) for _ in range(2)]
            y1s = [(ale.f32(D), Dep()) for _ in range(2)]
            y2s = [(ale.f32(D), Dep()) for _ in range(2)]
            junk2 = ale.bf16(D)
            sts2 = [(ale.f32(8), Dep()) for _ in range(4)]
            load_gB(4)
            for tt in range(16):
                xt, dx = cts[tt % 2]
                y1, dy1 = y1s[tt % 2]
                y2, dy2 = y2s[tt % 2]
                st, d_st = sts2[tt % 4]
                sp.dma(xt, h2_s[tt * 128:(tt + 1) * 128, :], writes=[dx])
                pool.dma_fn(lambda e: e.indirect_dma_start(out=y1, out_offset=None, in_=Ys, in_offset=IOA(ap=pos1i[:, tt:tt + 1], axis=0),
                                                           bounds_check=BCV, oob_is_err=False), reads=[d_Ys, d_pos], writes=[dy1])
                pool.dma_fn(lambda e: e.indirect_dma_start(out=y2, out_offset=None, in_=Ys, in_offset=IOA(ap=pos2i[:, tt:tt + 1], axis=0),
                                                           bounds_check=BCV, oob_is_err=False), reads=[d_Ys, d_pos], writes=[dy2])
                dve.op(lambda e: e.scalar_tensor_tensor(out=xt, in0=y1, scalar=p1a[:, tt:tt + 1], in1=xt, op0=ALU.mult, op1=ALU.add),
                       reads=[dy1, dx, d_pp], writes=[dx])
                dve.op(lambda e: e.scalar_tensor_tensor(out=xt, in0=y2, scalar=p2a[:, tt:tt + 1], in1=xt, op0=ALU.mult, op1=ALU.add),
                       reads=[dy2, dx, d_pp], writes=[dx])
                if "h3" in dbg_aps:
                    sp.dma(dbg_aps["h3"][tt * 128:(tt + 1) * 128, :], xt, reads=[dx])
                act.op(lambda e: e.activation(out=junk2, in_=xt, func=AF.Square, accum_out=st[:, 0:1]), reads=[dx], writes=[d_junk, d_st])
                dve.op(lambda e: e.tensor_scalar(out=st[:, 1:2], in0=st[:, 0:1], scalar1=1.0 / D, scalar2=1e-6, op0=ALU.mult, op1=ALU.add),
                       reads=[d_st], writes=[d_st])
                act.op(lambda e: e.activation(out=st[:, 2:3], in_=st[:, 1:2], func=AF.Sqrt), reads=[d_st], writes=[d_st])
                dve.op(lambda e: e.reciprocal(out=st[:, 3:4], in_=st[:, 2:3]), reads=[d_st], writes=[d_st])
                dve.op(lambda e: e.scalar_tensor_tensor(out=xt, in0=xt, scalar=st[:, 3:4], in1=gBt, op0=ALU.mult, op1=ALU.mult),
                       reads=[dx, d_st, d_gB], writes=[dx])
                sp.dma(out[tt * 128:(tt + 1) * 128, :], xt, reads=[dx])
            fw.barrier()

        fw.barrier()
    return nc


def host_consts(inp):
    l = 0
    f = np.float32
    gBh = np.stack([np.broadcast_to(v, (128, D)) for v in (inp["norm_mix_g"][l], inp["norm_xattn_g"][l], inp["norm_ffn_g"][l],
                                                            inp["norm_mem_g"][l], inp["norm_final_g"])]).astype(f)
    pvh = np.zeros((128, NPV), f)

    def col(v, n):
        return np.ascontiguousarray(np.asarray(v, f).reshape(n, 128).T)
    pvh[:, 0:8] = col(inp["pool_scale"][l], 8)
    mu = np.asarray(inp["rwkv_mu"][l], f)
    pvh[:, 8:32] = col(mu[0:3072], 24)
    pvh[:, 32] = mu[3072:3200]
    pvh[:, 33] = mu[3200:3328]
    pvh[0:32, 34] = mu[3328:3360]
    pvh[:, 35:43] = col(inp["rwkv_w0"][l], 8)
    pvh[:, 43:51] = col(inp["rwkv_a0"][l], 8)
    pvh[:, 51:59] = col(inp["rwkv_k_k"][l], 8)
    pvh[:, 59:67] = col(inp["rwkv_k_a"][l], 8)
    pvh[:, 67:75] = col(inp["rwkv_ln_w"][l], 8)
    pvh[:, 75:83] = col(inp["rwkv_ln_b"][l], 8)
    pvh[:, 83:91] = col(np.asarray(inp["rwkv_r_k"][l]).reshape(-1), 8)
    cst = np.zeros((128, 1024), f)
    p = np.arange(128)
    cst[:, 0:128] = np.eye(128, dtype=f)
    cst[:, 128:256] = (p[:, None] // 64 == p[None, :] // 64).astype(f)
    s = p % 64
    tcol = np.arange(64)
    strict = (s[:, None] < tcol[None, :]).astype(f)
    incl = (s[:, None] <= tcol[None, :]).astype(f)
    one = np.concatenate([strict, incl], 1)
    cst[:, 256:512] = np.concatenate([one, one], 1)
    low = (s[:, None] > tcol[None, :]).astype(f)
    cst[:, 512:640] = np.concatenate([low, low], 1)
    cst[:, 640:704] = (s[:, None] == tcol[None, :]).astype(f)
    tt = np.arange(16)
    for gi, w in enumerate((2, 4, 8, 16)):
        cst[:, 704 + gi * 16:704 + (gi + 1) * 16] = (1.0 / np.minimum(tt + 1, w)).astype(f)[None, :]
    cst[:, 768:896] = (p[:, None] < p[None, :]).astype(f)
    cst[:, 896] = p.astype(f)
    cst[:, 897:929] = np.arange(32, dtype=f)[None, :]
    cst[:, 929:937] = (float(SL) * np.arange(8, dtype=f))[None, :]
    rm = np.ones((128, T), f)
    rm[:, ::64] = 0.0
    w_r = np.concatenate([inp["moe_w_group"][l], inp["moe_w_expert"][l]], 1).astype(f)
    b_r = np.concatenate([inp["moe_b_group"][l], inp["moe_b_expert"][l]])[None, :].astype(f)
    return dict(gB=gBh, pv=pvh, cst=cst, rmask=rm, w_r=np.ascontiguousarray(w_r), b_r=b_r)


def make_in_maps(inp, cores):
    l = 0
    c = host_consts(inp)
    shared = dict(
        w_in=inp["w_in"][l], pool_w=inp["pool_w"][l], w2=inp["rwkv_w2"][l], a2=inp["rwkv_a2"][l], g2=inp["rwkv_g2"][l],
        w_out=inp["w_out"][l], w_q=inp["xattn_w_q"][l], w_kv=inp["xattn_w_kv"][l], w_o=inp["xattn_w_o"][l],
        wg_l=np.asarray(inp["moe_w_gate"][l], np.float32).reshape(16, 4, 4, 128, 512).transpose(0, 1, 3, 2, 4).reshape(8192, 2048),
        wu_l=np.asarray(inp["moe_w_up"][l], np.float32).reshape(16, 4, 4, 128, 512).transpose(0, 1, 3, 2, 4).reshape(8192, 2048),
        wd_l=np.asarray(inp["moe_w_down"][l], np.float32).reshape(8192, 2048), **c)
    shared = {k: np.ascontiguousarray(np.asarray(v, np.float32)) for k, v in shared.items()}
    maps = []
    for b in cores:
        m = dict(shared)
        m["x"] = np.ascontiguousarray(inp["x"][b])
        m["mem"] = np.ascontiguousarray(inp["mem"][b])
        maps.append(m)
    return maps


def kernel(**inputs):
    inp = {k: np.asarray(v) for k, v in inputs.items()}
    nc = build()
    maps = make_in_maps(inp, list(range(8)))
    res = run_bass_kernel_spmd(nc, maps, core_ids=list(range(8)))
    return np.stack([np.asarray(r["out"]) for r in res.results], 0).astype(np.float32)
```

```python
import numpy as np
import concourse.bass as bass
import concourse.mybir as mybir
from concourse.bass_utils import run_bass_kernel_spmd
from contextlib import ExitStack

F32 = mybir.dt.float32
BF16 = mybir.dt.bfloat16
I32 = mybir.dt.int32
AF = mybir.ActivationFunctionType
ALU = mybir.AluOpType
AX = mybir.AxisListType

D = 2048
KC = 16
T = 2048
NTT = T // 128
NTQ = T // 512
NCH = T // 64
PAD = 16
CDEC = float(np.exp(-0.5))
WORDS = 51200
NSLOT, SL = 26, 384
NA = SL // 128


class Dep:
    __slots__ = ("w", "r")

    def __init__(self):
        self.w = None
        self.r = {}


class Eng:
    def __init__(self, fw, name, b, is_pe=False):
        self.fw, self.name, self.b, self.is_pe = fw, name, b, is_pe
        self.sem = fw.new_sem(name)
        self.cnt = 0
        self.waited = {}
        self.dma_slots = None
        self.dma_i = 0

    def _wait(self, tok):
        sem, val = tok
        if self.waited.get(id(sem), 0) < val:
            self.b.wait_ge(sem, val)
            self.waited[id(sem)] = val

    def _collect(self, reads, writes):
        for d in reads:
            if d.w is not None and not (self.is_pe and d.w[0] is self.sem):
                self._wait(d.w)
        for d in writes:
            if d.w is not None and not (self.is_pe and d.w[0] is self.sem):
                self._wait(d.w)
            for t in d.r.values():
                if not (self.is_pe and t[0] is self.sem):
                    self._wait(t)

    def op(self, fn, reads=(), writes=()):
        self._collect(reads, writes)
        inst = fn(self.b)
        self.cnt += 1
        inst.then_inc(self.sem, 1)
        tok = (self.sem, self.cnt)
        for d in reads:
            d.r[id(self.sem)] = tok
        for d in writes:
            d.w = tok
            d.r = {}
        return tok

    def dma(self, out, in_, reads=(), writes=(), **kw):
        return self.dma_fn(lambda e: e.dma_start(out=out, in_=in_, **kw), reads, writes)

    def dma_fn(self, fn, reads=(), writes=()):
        if self.dma_slots is None:
            self.dma_slots = [[self.fw.new_sem(f"{self.name}_d{i}"), 0] for i in range(8)]
        self._collect(reads, writes)
        slot = self.dma_slots[self.dma_i % len(self.dma_slots)]
        self.dma_i += 1
        if slot[1] > 0:
            self._wait((slot[0], slot[1]))
        inst = fn(self.b)
        slot[1] += 16
        inst.then_inc(slot[0], 16)
        tok = (slot[0], slot[1])
        for d in reads:
            d.r[id(slot[0])] = tok
        for d in writes:
            d.w = tok
            d.r = {}
        return tok


class FW:
    def __init__(self, nc, es):
        self.nc, self.es = nc, es
        self.pe = Eng(self, "pe", nc.tensor, True)
        self.act = Eng(self, "act", nc.scalar)
        self.dve = Eng(self, "dve", nc.vector)
        self.pool = Eng(self, "pool", nc.gpsimd)
        self.sp = Eng(self, "sp", nc.sync)
        self.engs = [self.pe, self.act, self.dve, self.pool, self.sp]

    def new_sem(self, name):
        return self.es.enter_context(self.nc.semaphore(name))

    def barrier(self):
        toks = []
        for e in self.engs:
            if e.cnt > 0:
                toks.append((e.sem, e.cnt))
            if e.dma_slots:
                for s in e.dma_slots:
                    if s[1] > 0:
                        toks.append((s[0], s[1]))
        for e in self.engs:
            for t in toks:
                if t[0] is not e.sem:
                    e._wait(t)


class Alloc:
    def __init__(self, big, start, end):
        self.big, self.top, self.end = big, start, end

    def f32(self, n):
        a = self.big[:, self.top:self.top + n]
        self.top += n
        assert self.top <= self.end, (self.top, self.end)
        return a

    def bf16(self, n):
        w = (n + 1) // 2
        a = self.big[:, self.top:self.top + w].bitcast(BF16)
        self.top += w
        assert self.top <= self.end, (self.top, self.end)
        return a[:, 0:n]


def r3(ap, a):
    return ap.rearrange("p (a b) -> p a b", a=a)


PV = dict(pool_scale=0, mu_rkv=8, mu_lo=32, w0=35, a0=43, k_k=51, k_a=59, ln_w=67, ln_b=75, r_k=83, omka=91)
NPV = 99


def build(stop=99, dbg=()):
    nc = bass.Bass("TRN2", target_bir_lowering=False)

    def din(name, shape):
        return nc.dram_tensor(name, list(shape), F32, kind="ExternalInput").ap()

    x = din("x", [T, D])
    mem = din("mem", [256, D])
    w_in = din("w_in", [D, 4384])
    pool_w = din("pool_w", [4, 256, 256])
    w2 = din("w2", [64, 1024])
    a2 = din("a2", [64, 1024])
    g2 = din("g2", [160, 1024])
    w_out = din("w_out", [D, D])
    w_q = din("w_q", [D, D])
    w_kv = din("w_kv", [D, 2 * D])
    w_o = din("w_o", [D, D])
    w_r = din("w_r", [D, 20])
    b_r = din("b_r", [1, 20])
    wg_l = din("wg_l", [8192, 2048])
    wu_l = din("wu_l", [8192, 2048])
    wd_l = din("wd_l", [8192, 2048])
    gB = din("gB", [5, 128, D])
    pvd = din("pv", [128, NPV])
    cst = din("cst", [128, 1024])
    rmask_d = din("rmask", [128, T])
    out = nc.dram_tensor("out", [T, D], F32, kind="ExternalOutput").ap()
    dbg_aps = {}
    for name, shape in dbg:
        dbg_aps[name] = nc.dram_tensor(name, list(shape), F32, kind="ExternalOutput").ap()
    rkv_s = nc.dram_tensor("rkv_s", [24, 128, T], F32, kind="Internal").ap()
    h1_s = nc.dram_tensor("h1_s", [T, D], F32, kind="Internal").ap()
    h2_s = nc.dram_tensor("h2_s", [T, D], F32, kind="Internal").ap()
    NROW = NSLOT * SL
    Xs = nc.dram_tensor("Xs", [NROW, D], BF16, kind="Internal").ap()
    Ys = nc.dram_tensor("Ys", [NROW, D], F32, kind="Internal").ap()

    with ExitStack() as es:
        fw = FW(nc, es)
        pe, act, dve, pool, sp = fw.pe, fw.act, fw.dve, fw.pool, fw.sp
        big = es.enter_context(nc.sbuf_tensor("big", [128, WORDS], F32))[:]
        banks = [(es.enter_context(nc.psum_tensor(f"bk{i}", [128, 512], F32))[:], Dep()) for i in range(8)]
        bki = [0]

        def nb():
            b = banks[bki[0] % 8]
            bki[0] += 1
            return b

        def mm(o, lhsT, rhs, start, stop, reads, writes):
            pe.op(lambda e: e.matmul(o, lhsT=lhsT, rhs=rhs, start=start, stop=stop), reads, writes)

        CONST_W = 7424
        ca = Alloc(big, 0, CONST_W)
        identf = ca.f32(128)
        blk1 = ca.f32(128)
        mSI = ca.f32(256)
        mL = ca.f32(128)
        i64 = ca.f32(64)
        rcnt = ca.f32(64)
        pv = ca.f32(NPV + 1)
        identb = ca.bf16(128)
        onesb = ca.bf16(128)
        rmask = ca.bf16(T)
        gBt = ca.f32(D)
        lo1 = ca.bf16(T)
        sg1 = ca.bf16(T)
        sg2 = ca.bf16(T)
        ones1 = ca.f32(128)
        brow = ca.f32(20)
        d_const, d_gB, d_lo1, d_sg1, d_sg2 = Dep(), Dep(), Dep(), Dep(), Dep()
        B0_OFF = CONST_W
        B1_OFF = B0_OFF + 8192
        M_OFF = B1_OFF + 8192
        A_OFF = WORDS - 16384
        bufA = r3(big[:, A_OFF:WORDS].bitcast(BF16), 16)
        bufB = r3(big[:, B0_OFF:M_OFF].bitcast(BF16), 16)
        d_A, d_B = Dep(), Dep()

        sp.dma(identf, cst[:, 0:128], writes=[d_const])
        sp.dma(blk1, cst[:, 128:256], writes=[d_const])
        sp.dma(mSI, cst[:, 256:512], writes=[d_const])
        sp.dma(mL, cst[:, 512:640], writes=[d_const])
        sp.dma(i64, cst[:, 640:704], writes=[d_const])
        sp.dma(rcnt, cst[:, 704:768], writes=[d_const])
        sp.dma(pv[:, 0:NPV], pvd, writes=[d_const])
        sp.dma(brow[0:1, :], b_r, writes=[d_const])
        pool.dma(identb, cst[:, 0:128], writes=[d_const])
        pool.dma(rmask, rmask_d, writes=[d_const])
        pool.op(lambda e: e.memset(onesb, 1.0), writes=[d_const])
        pool.op(lambda e: e.memset(ones1, 1.0), writes=[d_const])
        dve.op(lambda e: e.tensor_scalar(out=pv[:, PV["omka"]:PV["omka"] + 8], in0=pv[:, PV["k_a"]:PV["k_a"] + 8],
                                         scalar1=-1.0, scalar2=1.0, op0=ALU.mult, op1=ALU.add), reads=[d_const], writes=[d_const])
        fw.barrier()

        def pvc(name, j):
            c = PV[name] + j
            return pv[:, c:c + 1]

        d_Xs, d_Ys = Dep(), Dep()
        if stop >= 8:
            dve.op(lambda e: e.memset(sg1, 0.0), writes=[d_sg1])
            dve.op(lambda e: e.memset(sg2, 0.0), writes=[d_sg2])
            for c in range(NROW // 128):
                zt, dz = ((sg1, d_sg1), (sg2, d_sg2))[c % 2]
                pool.dma(Xs[c * 128:(c + 1) * 128, :], zt, reads=[dz])

        def load_gB(i):
            sp.dma(gBt, gB[i], writes=[d_gB])

        def norm_tiles(al, ntiles, src_fn, dstT, d_dst, tok_off=0, keep=None):
            xts = [(al.f32(D), Dep()) for _ in range(2)]
            xns = [(al.bf16(D), Dep()) for _ in range(2)]
            junk = al.bf16(D)
            d_junk = Dep()
            sts = [(al.f32(8), Dep()) for _ in range(4)]
            for i in range(ntiles):
                xt, dx = xts[i % 2]
                xn, dn = xns[i % 2]
                st, d_st = sts[i % 4]
                sp.dma(xt, src_fn(i), writes=[dx])
                if keep is not None:
                    keep(i, xt, dx)
                act.op(lambda e: e.activation(out=junk, in_=xt, func=AF.Square, accum_out=st[:, 0:1]), reads=[dx], writes=[d_junk, d_st])
                dve.op(lambda e: e.tensor_scalar(out=st[:, 1:2], in0=st[:, 0:1], scalar1=1.0 / D, scalar2=1e-6, op0=ALU.mult, op1=ALU.add),
                       reads=[d_st], writes=[d_st])
                act.op(lambda e: e.activation(out=st[:, 2:3], in_=st[:, 1:2], func=AF.Sqrt), reads=[d_st], writes=[d_st])
                dve.op(lambda e: e.reciprocal(out=st[:, 3:4], in_=st[:, 2:3]), reads=[d_st], writes=[d_st])
                dve.op(lambda e: e.scalar_tensor_tensor(out=xn, in0=xt, scalar=st[:, 3:4], in1=gBt, op0=ALU.mult, op1=ALU.mult),
                       reads=[dx, d_st, d_gB], writes=[dn])
                for q in range(4):
                    pb, pd = nb()
                    for j in range(4):
                        kc = q * 4 + j
                        mm(pb[:, j * 128:(j + 1) * 128], xn[:, kc * 128:(kc + 1) * 128], identb, True, True, [dn, d_const], [pd])
                    t0 = tok_off + i * 128
                    eng = act if q % 2 == 0 else dve
                    if eng is act:
                        act.op(lambda e: e.activation(out=dstT[:, q * 4:q * 4 + 4, t0:t0 + 128], in_=r3(pb, 4), func=AF.Copy), reads=[pd], writes=[d_dst])
                    else:
                        dve.op(lambda e: e.tensor_copy(out=dstT[:, q * 4:q * 4 + 4, t0:t0 + 128], in_=r3(pb, 4)), reads=[pd], writes=[d_dst])

        def dump(name, ap_sb, dep, dst=None):
            if name in dbg_aps:
                sp.dma(dbg_aps[name] if dst is None else dst, ap_sb, reads=[dep])

        load_gB(0)
        al = Alloc(big, M_OFF, A_OFF)
        norm_tiles(al, NTT, lambda i: x[i * 128:(i + 1) * 128, :], bufA, d_A)
        fw.barrier()
        if "hnT" in dbg_aps:
            tmp = Alloc(big, M_OFF, A_OFF).f32(T)
            dtmp = Dep()
            for kc in range(16):
                dve.op(lambda e: e.tensor_copy(out=tmp, in_=bufA[:, kc, :]), reads=[d_A], writes=[dtmp])
                sp.dma(dbg_aps["hnT"][kc * 128:(kc + 1) * 128, :], tmp, reads=[dtmp])
            fw.barrier()

        if stop >= 2:
            al = Alloc(big, B1_OFF, A_OFF)
            wbs = [(r3(al.bf16(16 * 128), 16), Dep()) for _ in range(4)]
            wbi = [0]
            pbufs = [(al.f32(PAD + T), Dep()) for _ in range(2)]
            fbs = [(al.f32(PAD + T), Dep()) for _ in range(3)]
            pooled = [(al.bf16(T), Dep()) for _ in range(2)]
            pwb = r3(al.bf16(8 * 256), 8)
            d_pw = Dep()
            for pbf, dp in pbufs + fbs:
                dve.op(lambda e: e.memset(pbf[:, 0:PAD], 0.0), writes=[dp])
            pool.dma(pwb, pool_w.rearrange("g (cc p) d -> p (g cc) d", p=128), writes=[d_pw])
            pbi = [0]

            def proj_block(col0, n):
                ws, dw = wbs[wbi[0] % 4]
                wbi[0] += 1
                pool.dma(ws[:, :, 0:n], w_in[:, col0:col0 + n].rearrange("(kc p) n -> p kc n", p=128), writes=[dw])
                pbf, dp = pbufs[pbi[0] % 2]
                pbi[0] += 1
                for tq in range(NTQ):
                    pb, pd = nb()
                    for kc in range(KC):
                        mm(pb[0:n, :], ws[:, kc, 0:n], bufA[:, kc, tq * 512:(tq + 1) * 512], kc == 0, kc == KC - 1, [dw, d_A], [pd])
                    act.op(lambda e: e.activation(out=pbf[0:n, PAD + tq * 512:PAD + (tq + 1) * 512], in_=pb[0:n, :], func=AF.Copy), reads=[pd], writes=[dp])
                return pbf, dp

            def tshift(pbf, dp, n, mu_ap, zout, dz):
                f0, df0 = fbs[0]
                dve.op(lambda e: e.tensor_tensor(out=f0[0:n, 0:T], in0=pbf[0:n, PAD - 1:PAD - 1 + T], in1=pbf[0:n, PAD:PAD + T], op=ALU.subtract),
                       reads=[dp], writes=[df0])
                dve.op(lambda e: e.scalar_tensor_tensor(out=zout, in0=f0[0:n, 0:T], scalar=mu_ap, in1=pbf[0:n, PAD:PAD + T], op0=ALU.mult, op1=ALU.add),
                       reads=[df0, dp, d_const], writes=[dz])

            z1f, dz1 = fbs[1]
            z1 = z1f[:, PAD:PAD + T]
            pbf, dp = proj_block(4096, 128)
            tshift(pbf, dp, 128, pv[:, PV["mu_lo"]:PV["mu_lo"] + 1], z1, dz1)
            act.op(lambda e: e.activation(out=lo1[0:64, :], in_=z1[0:64, :], func=AF.Tanh), reads=[dz1], writes=[d_lo1])
            act.op(lambda e: e.activation(out=lo1[64:128, :], in_=z1[64:128, :], func=AF.Copy), reads=[dz1], writes=[d_lo1])
            pbf, dp = proj_block(4224, 128)
            tshift(pbf, dp, 128, pv[:, PV["mu_lo"] + 1:PV["mu_lo"] + 2], z1, dz1)
            act.op(lambda e: e.activation(out=sg1, in_=z1, func=AF.Sigmoid), reads=[dz1], writes=[d_sg1])
            pbf, dp = proj_block(4352, 32)
            tshift(pbf, dp, 32, pv[0:32, PV["mu_lo"] + 2:PV["mu_lo"] + 3], z1[0:32, :], dz1)
            act.op(lambda e: e.activation(out=sg2[0:32, :], in_=z1[0:32, :], func=AF.Sigmoid), reads=[dz1], writes=[d_sg2])
            for j in range(24):
                pbf, dp = proj_block(1024 + j * 128, 128)
                tshift(pbf, dp, 128, pvc("mu_rkv", j), z1, dz1)
                sp.dma(rkv_s[j], z1, reads=[dz1])
            for cb in range(8):
                gi = cb // 2
                w = (2, 4, 8, 16)[gi]
                pbf, dp = proj_block(cb * 128, 128)
                (fa, dfa), (fb_, dfb) = fbs[1], fbs[2]
                src, dsrc = pbf, dp
                sh = 1
                k = 0
                while sh < w:
                    dst, ddst = (fa, dfa) if k % 2 == 0 else (fb_, dfb)
                    dve.op(lambda e: e.tensor_tensor(out=dst[:, PAD:PAD + T], in0=src[:, PAD:PAD + T], in1=src[:, PAD - sh:PAD - sh + T], op=ALU.add),
                           reads=[dsrc], writes=[ddst])
                    src, dsrc = dst, ddst
                    sh *= 2
                    k += 1
                po, dpo = pooled[cb % 2]
                dve.op(lambda e: e.scalar_tensor_tensor(out=po, in0=src[:, PAD:PAD + T], scalar=1.0 / w, in1=pbf[:, PAD:PAD + T], op0=ALU.mult, op1=ALU.subtract),
                       reads=[dsrc, dp], writes=[dpo])
                f0, df0 = fbs[0]
                dve.op(lambda e: e.tensor_tensor(out=f0[:, 0:16], in0=src[:, PAD:PAD + 16], in1=rcnt[:, gi * 16:(gi + 1) * 16], op=ALU.mult),
                       reads=[dsrc, d_const], writes=[df0])
                dve.op(lambda e: e.tensor_tensor(out=po[:, 0:16], in0=f0[:, 0:16], in1=pbf[:, PAD:PAD + 16], op=ALU.subtract),
                       reads=[df0, dp], writes=[dpo])
                if cb % 2 == 1:
                    for db in range(2):
                        for tq in range(NTQ):
                            pb, pd = nb()
                            for cc in range(2):
                                mm(pb, pwb[:, gi * 2 + cc, db * 128:(db + 1) * 128], pooled[cc][0][:, tq * 512:(tq + 1) * 512], cc == 0, cc == 1,
                                   [d_pw, pooled[cc][1]], [pd])
                            blk = gi * 2 + db
                            act.op(lambda e: e.activation(out=bufB[:, blk, tq * 512:(tq + 1) * 512], in_=pb, func=AF.Copy, scale=pvc("pool_scale", blk)),
                                   reads=[pd, d_const], writes=[d_B])
            fw.barrier()
            if "mixT" in dbg_aps:
                tmp = Alloc(big, B1_OFF, A_OFF).f32(T)
                dtmp = Dep()
                for kc in range(8):
                    dve.op(lambda e: e.tensor_copy(out=tmp, in_=bufB[:, kc, :]), reads=[d_B], writes=[dtmp])
                    sp.dma(dbg_aps["mixT"][kc * 128:(kc + 1) * 128, :], tmp, reads=[dtmp])
                fw.barrier()


        if stop >= 3:
            QTK = 512
            NQ = T // QTK
            al = Alloc(big, M_OFF, WORDS)
            lw = al.bf16(1024)
            g2a = al.bf16(1024)
            g2b = al.bf16(1024)
            S32 = al.f32(64)
            Sbf = al.bf16(64)
            d_lw, d_S32, d_Sbf = Dep(), Dep(), Dep()
            sets = []
            for i in range(2):
                sets.append(dict(AR=r3(al.bf16(8 * 128), 8), BK=r3(al.bf16(8 * 128), 8), BKh=r3(al.bf16(8 * 128), 8), vb=al.bf16(QTK),
                                 GL=al.f32(8), gbuf=al.bf16(QTK), bonus=al.f32(QTK), ybuf=al.f32(QTK),
                                 d_AR=Dep(), d_BK=Dep(), d_BKh=Dep(), d_vb=Dep(), d_GL=Dep(), d_g=Dep(), d_bonus=Dep(), d_y=Dep(),
                                 d_ARr=[Dep() for _ in range(4)]))
            Fq = [al.f32(QTK) for _ in range(8)]
            dFq = [Dep() for _ in range(8)]
            NBall = r3(al.bf16(8 * 128), 8)
            KBall = r3(al.bf16(8 * 128), 8)
            TT = r3(al.bf16(8 * 64), 8)
            TM = r3(al.bf16(8 * 320), 8)
            APU = r3(al.bf16(8 * 128), 8)
            Mc = r3(al.f32(8 * 64), 8)
            CcT = r3(al.f32(8 * 64), 8)
            Wg = [[r3(al.bf16(2 * 128), 2) for _ in range(2)] for _ in range(4)]
            NTg = [[r3(al.bf16(2 * 64), 2) for _ in range(2)] for _ in range(4)]
            d_NB = [Dep() for _ in range(4)]
            d_KB = [Dep() for _ in range(4)]
            d_TT = [Dep() for _ in range(4)]
            d_TM = [Dep() for _ in range(4)]
            d_APU = [Dep() for _ in range(4)]
            d_Mc = [Dep() for _ in range(4)]
            d_Cc = [Dep() for _ in range(4)]
            dWg = [[Dep(), Dep()] for _ in range(4)]
            dNTg = [[Dep(), Dep()] for _ in range(4)]
            yc, sq, rs = al.f32(QTK), al.f32(QTK), al.f32(QTK)
            dyc, dsq, drs = Dep(), Dep(), Dep()
            pool.dma(lw[0:64, :], w2, writes=[d_lw])
            pool.dma(lw[64:128, :], a2, writes=[d_lw])
            pool.dma(g2a, g2[0:128, :], writes=[d_lw])
            pool.dma(g2b[0:32, :], g2[128:160, :], writes=[d_lw])
            HS = [slice(0, 64), slice(64, 128)]
            i64b = i64.unsqueeze(1).to_broadcast([128, 2, 64])
            pyb, pdyb = banks[7]
            nbm = [0]

            def nb7():
                b = banks[nbm[0] % 7]
                nbm[0] += 1
                return b

            def prep_gen(u):
                hp, q = divmod(u, NQ)
                S_ = sets[u % 2]
                cs = slice(hp * 128, (hp + 1) * 128)
                tsl = slice(q * QTK, (q + 1) * QTK)
                k_, sgw, alr, cum, kk, f5, f6, f7 = Fq
                dk, dsgw, dalr, dcum, dkk, df5, df6, df7 = dFq
                AR, BK, BKh, vb, GL, gbuf, bonus = S_["AR"], S_["BK"], S_["BKh"], S_["vb"], S_["GL"], S_["gbuf"], S_["bonus"]
                d_AR, d_BK, d_BKh, d_vb, d_GL, d_g, d_bonus = S_["d_AR"], S_["d_BK"], S_["d_BKh"], S_["d_vb"], S_["d_GL"], S_["d_g"], S_["d_bonus"]
                sp.dma(k_, rkv_s[8 + hp][:, tsl], writes=[dk])
                pb, pd = nb7()
                mm(pb, lw[0:64, cs], lo1[0:64, tsl], True, True, [d_lw, d_lo1], [pd])
                act.op(lambda e: e.activation(out=sgw, in_=pb, func=AF.Sigmoid, bias=pvc("w0", hp)), reads=[pd, d_const], writes=[dsgw])
                pb, pd = nb7()
                mm(pb, lw[64:128, cs], lo1[64:128, tsl], True, True, [d_lw, d_lo1], [pd])
                act.op(lambda e: e.activation(out=alr, in_=pb, func=AF.Sigmoid, bias=pvc("a0", hp)), reads=[pd, d_const], writes=[dalr])
                yield
                pb, pd = nb7()
                mm(pb, g2a[:, cs], sg1[:, tsl], True, False, [d_lw, d_sg1], [pd])
                mm(pb, g2b[0:32, cs], sg2[0:32, tsl], False, True, [d_lw, d_sg2], [pd])
                act.op(lambda e: e.activation(out=gbuf, in_=pb, func=AF.Copy), reads=[pd], writes=[d_g])
                dve.op(lambda e: e.tensor_tensor_scan(out=cum, data0=rmask[:, 0:QTK], data1=sgw, initial=0.0, op0=ALU.mult, op1=ALU.add),
                       reads=[d_const, dsgw], writes=[dcum])
                yield
                act.op(lambda e: e.activation(out=kk, in_=k_, func=AF.Copy, scale=pvc("k_k", hp)), reads=[dk, d_const], writes=[dkk])
                pool.op(lambda e: e.tensor_tensor(out=f5, in0=kk, in1=kk, op=ALU.mult), reads=[dkk], writes=[df5])
                yield
                pb, pd = nb7()
                mm(pb, blk1, f5, True, True, [d_const, df5], [pd])
                dve.op(lambda e: e.tensor_scalar(out=f6, in0=pb, scalar1=1e-24, scalar2=None, op0=ALU.max), reads=[pd], writes=[df6])
                act.op(lambda e: e.activation(out=f6, in_=f6, func=AF.Sqrt), reads=[df6], writes=[df6])
                yield
                dve.op(lambda e: e.reciprocal(out=f6, in_=f6), reads=[df6], writes=[df6])
                yield
                pool.op(lambda e: e.tensor_tensor(out=kk, in0=kk, in1=f6, op=ALU.mult), reads=[dkk, df6], writes=[dkk])
                dve.op(lambda e: e.tensor_scalar(out=f5, in0=alr, scalar1=pvc("k_a", hp), scalar2=pvc("omka", hp), op0=ALU.mult, op1=ALU.add),
                       reads=[dalr, d_const], writes=[df5])
                yield
                pool.op(lambda e: e.tensor_tensor(out=f5, in0=f5, in1=k_, op=ALU.mult), reads=[df5, dk], writes=[df5])
                pool.op(lambda e: e.tensor_tensor(out=alr, in0=alr, in1=kk, op=ALU.mult), reads=[dalr, dkk], writes=[dalr])
                yield
                act.op(lambda e: e.activation(out=f6, in_=cum, func=AF.Exp, scale=CDEC), reads=[dcum], writes=[df6])
                dve.op(lambda e: e.tensor_tensor(out=BK[:, :, 0:64], in0=r3(alr, 8), in1=r3(f6, 8), op=ALU.mult), reads=[dalr, df6], writes=[d_BK])
                pool.op(lambda e: e.tensor_tensor(out=BK[:, :, 64:128], in0=r3(f5, 8), in1=r3(f6, 8), op=ALU.mult), reads=[df5, df6], writes=[d_BK])
                yield
                dve.op(lambda e: e.tensor_tensor(out=r3(f6, 8), in0=r3(cum, 8), in1=r3(cum, 8)[:, :, 63:64].to_broadcast([128, 8, 64]), op=ALU.subtract),
                       reads=[dcum], writes=[df6])
                act.op(lambda e: e.activation(out=f6, in_=f6, func=AF.Exp, scale=CDEC), reads=[df6], writes=[df6])
                yield
                dve.op(lambda e: e.tensor_tensor(out=BKh[:, :, 0:64], in0=r3(alr, 8), in1=r3(f6, 8), op=ALU.mult), reads=[dalr, df6], writes=[d_BKh])
                pool.op(lambda e: e.tensor_tensor(out=BKh[:, :, 64:128], in0=r3(f5, 8), in1=r3(f6, 8), op=ALU.mult), reads=[df5, df6], writes=[d_BKh])
                yield
                pool.op(lambda e: e.tensor_tensor(out=f6, in0=cum, in1=sgw, op=ALU.subtract), reads=[dcum, dsgw], writes=[df6])
                act.op(lambda e: e.activation(out=f6, in_=f6, func=AF.Exp, scale=-CDEC), reads=[df6], writes=[df6])
                yield
                dve.op(lambda e: e.scalar_tensor_tensor(out=AR[:, :, 0:64], in0=r3(kk, 8), scalar=-1.0, in1=r3(f6, 8), op0=ALU.mult, op1=ALU.mult),
                       reads=[dkk, df6], writes=[d_AR])
                act.op(lambda e: e.activation(out=GL, in_=r3(cum, 8)[:, :, 63], func=AF.Exp, scale=-CDEC), reads=[dcum], writes=[d_GL])
                yield
                act.op(lambda e: e.activation(out=f6, in_=cum, func=AF.Exp, scale=-CDEC), reads=[dcum], writes=[df6])
                sp.dma(k_, rkv_s[hp][:, tsl], writes=[dk])
                pool.op(lambda e: e.tensor_tensor(out=AR[:, :, 64:128], in0=r3(k_, 8), in1=r3(f6, 8), op=ALU.mult), reads=[dk, df6], writes=[d_AR] + S_["d_ARr"])
                yield
                dve.op(lambda e: e.scalar_tensor_tensor(out=f6, in0=k_, scalar=pvc("r_k", hp), in1=f5, op0=ALU.mult, op1=ALU.mult),
                       reads=[dk, df5, d_const], writes=[df6])
                sp.dma(sgw, rkv_s[16 + hp][:, tsl], writes=[dsgw])
                yield
                pb, pd = nb7()
                mm(pb, blk1, f6, True, True, [d_const, df6], [pd])
                dve.op(lambda e: e.tensor_tensor(out=bonus, in0=pb, in1=sgw, op=ALU.mult), reads=[pd, dsgw], writes=[d_bonus])
                act.op(lambda e: e.activation(out=vb, in_=sgw, func=AF.Copy), reads=[dsgw], writes=[d_vb])
                yield

            def chunk_gen(u):
                hp, q = divmod(u, NQ)
                S_ = sets[u % 2]
                tsl = slice(q * QTK, (q + 1) * QTK)
                AR, BK, BKh, vb, GL, gbuf, bonus, ybuf = S_["AR"], S_["BK"], S_["BKh"], S_["vb"], S_["GL"], S_["gbuf"], S_["bonus"], S_["ybuf"]
                d_AR, d_BK, d_BKh, d_vb, d_GL, d_g, d_bonus, dy = S_["d_AR"], S_["d_BK"], S_["d_BKh"], S_["d_vb"], S_["d_GL"], S_["d_g"], S_["d_bonus"], S_["d_y"]
                d_ARr = S_["d_ARr"]
                if q == 0:
                    dve.op(lambda e: e.memset(S32, 0.0), writes=[d_S32])
                    dve.op(lambda e: e.memset(Sbf, 0.0), writes=[d_Sbf])
                pool.op(lambda e: e.tensor_tensor(out=Mc, in0=i64.unsqueeze(1).to_broadcast([128, 8, 64]),
                                                  in1=GL.unsqueeze(2).to_broadcast([128, 8, 64]), op=ALU.mult),
                        reads=[d_const, d_GL], writes=d_Mc)
                for g in range(4):
                    l0 = 2 * g
                    pa, pda = nb7()
                    pb_, pdb = nb7()
                    pt, pdt = nb7()
                    pv_, pdv = nb7()
                    for ci in range(2):
                        c = l0 + ci
                        for h in range(2):
                            hs = HS[h]
                            mm(pa[hs, ci * 128:(ci + 1) * 128], BK[hs, c, 0:64], AR[hs, c, :], True, True, [d_BK, d_AR, d_ARr[g]], [pda])
                            mm(pb_[hs, ci * 128:(ci + 1) * 128], BK[hs, c, 64:128], AR[hs, c, :], True, True, [d_BK, d_AR, d_ARr[g]], [pdb])
                            mm(pt[hs, ci * 64:(ci + 1) * 64], AR[hs, c, 0:64], BK[hs, c, 0:64], True, True, [d_BK, d_AR], [pdt])
                            idh = identb[hs, 64 * h:64 * h + 64]
                            mm(pv_[hs, ci * 256:ci * 256 + 64], vb[hs, c * 64:(c + 1) * 64], idh, True, True, [d_vb, d_const], [pdv])
                            mm(pv_[hs, ci * 256 + 64:ci * 256 + 128], BKh[hs, c, 0:64], idh, True, True, [d_BKh, d_const], [pdv])
                            mm(pv_[hs, ci * 256 + 128:ci * 256 + 192], BKh[hs, c, 64:128], idh, True, True, [d_BKh, d_const], [pdv])
                            mm(pv_[hs, ci * 256 + 192:ci * 256 + 256], AR[hs, c, 0:64], idh, True, True, [d_AR, d_const], [pdv])
                    dve.op(lambda e: e.tensor_tensor(out=NBall[:, l0:l0 + 2, :], in0=r3(pa[:, 0:256], 2), in1=r3(mSI, 2), op=ALU.mult),
                           reads=[pda, d_const], writes=[d_NB[g]])
                    dve.op(lambda e: e.tensor_tensor(out=KBall[:, l0:l0 + 2, :], in0=r3(pb_[:, 0:256], 2), in1=r3(mSI, 2), op=ALU.mult),
                           reads=[pdb, d_const], writes=[d_KB[g]])
                    dve.op(lambda e: e.tensor_tensor(out=NTg[g][0], in0=r3(pt[:, 0:128], 2), in1=r3(mL, 2), op=ALU.mult),
                           reads=[pdt, d_const], writes=[dNTg[g][0]])
                    act.op(lambda e: e.activation(out=TM[:, l0:l0 + 2, 0:256], in_=r3(pv_, 2), func=AF.Copy), reads=[pdv], writes=[d_TM[g]])
                    pool.op(lambda e: e.tensor_tensor(out=Wg[g][0][:, :, 64:128], in0=NBall[:, l0:l0 + 2, 0:64], in1=i64b, op=ALU.add),
                            reads=[d_NB[g], d_const], writes=[dWg[g][0]])
                    yield
                for g in range(4):
                    l0 = 2 * g
                    p0, pd0 = nb7()
                    q0, qd0 = nb7()
                    NT, dNT = NTg[g][0], dNTg[g][0]
                    for ci in range(2):
                        for h in range(2):
                            hs = HS[h]
                            mm(p0[hs, ci * 64:(ci + 1) * 64], NT[hs, ci, :], NBall[hs, l0 + ci, 0:64], True, True, [dNT, d_NB[g]], [pd0])
                            mm(q0[hs, ci * 64:(ci + 1) * 64], NBall[hs, l0 + ci, 0:64], NT[hs, ci, :], True, True, [dNT, d_NB[g]], [qd0])
                    act.op(lambda e: e.activation(out=Wg[g][0][:, :, 0:64], in_=r3(p0[:, 0:128], 2), func=AF.Copy), reads=[pd0], writes=[dWg[g][0]])
                    act.op(lambda e: e.activation(out=NTg[g][1], in_=r3(q0[:, 0:128], 2), func=AF.Copy), reads=[qd0], writes=[dNTg[g][1]])
                    yield
                cur, ntc = 0, 1
                for lvl in range(1, 6):
                    last = lvl == 5
                    for g in range(4):
                        l0 = 2 * g
                        Wc, dWc = Wg[g][cur], dWg[g][cur]
                        NTc, dNTc = NTg[g][ntc], dNTg[g][ntc]
                        p1, pd1 = nb7()
                        if not last:
                            q1, qd1 = nb7()
                        for ci in range(2):
                            for h in range(2):
                                hs = HS[h]
                                if not last:
                                    mm(p1[hs, ci * 128:(ci + 1) * 128], NTc[hs, ci, :], Wc[hs, ci, :], True, True, [dNTc, dWc], [pd1])
                                    mm(q1[hs, ci * 64:(ci + 1) * 64], Wc[hs, ci, 0:64], NTc[hs, ci, :], True, True, [dNTc, dWc], [qd1])
                                else:
                                    mm(p1[hs, ci * 64:(ci + 1) * 64], NTc[hs, ci, :], Wc[hs, ci, 64:128], True, True, [dNTc, dWc], [pd1])
                        if not last:
                            Wn, dWn = Wg[g][1 - cur], dWg[g][1 - cur]
                            NTn, dNTn = NTg[g][1 - ntc], dNTg[g][1 - ntc]
                            act.op(lambda e: e.activation(out=Wn[:, :, 0:64], in_=r3(p1[:, 0:256], 2)[:, :, 0:64], func=AF.Copy), reads=[pd1], writes=[dWn])
                            dve.op(lambda e: e.tensor_tensor(out=Wn[:, :, 64:128], in0=r3(p1[:, 0:256], 2)[:, :, 64:128], in1=Wc[:, :, 64:128], op=ALU.add),
                                   reads=[pd1, dWc], writes=[dWn])
                            act.op(lambda e: e.activation(out=NTn, in_=r3(q1[:, 0:128], 2), func=AF.Copy), reads=[qd1], writes=[dNTn])
                        else:
                            dve.op(lambda e: e.tensor_tensor(out=TT[:, l0:l0 + 2, :], in0=r3(p1[:, 0:128], 2), in1=Wc[:, :, 64:128], op=ALU.add),
                                   reads=[pd1, dWc], writes=[d_TT[g]])
                        if g % 2 == 1:
                            yield
                    cur, ntc = 1 - cur, 1 - ntc
                for g in range(4):
                    l0 = 2 * g
                    pw, pdw = nb7()
                    for ci in range(2):
                        for h in range(2):
                            hs = HS[h]
                            mm(pw[hs, ci * 64:(ci + 1) * 64], KBall[hs, l0 + ci, 0:64], TM[hs, l0 + ci, 0:64], True, True, [d_KB[g], d_TM[g]], [pdw])
                    act.op(lambda e: e.activation(out=TM[:, l0:l0 + 2, 256:320], in_=r3(pw[:, 0:128], 2), func=AF.Copy), reads=[pdw], writes=[d_TM[g]])
                yield
                for g in range(4):
                    l0 = 2 * g
                    pq, pdq = nb7()
                    for ci in range(2):
                        for h in range(2):
                            hs = HS[h]
                            mm(pq[hs, ci * 128:(ci + 1) * 128], TT[hs, l0 + ci, :], TM[hs, l0 + ci, 192:320], True, True, [d_TT[g], d_TM[g]], [pdq])
                    dve.op(lambda e: e.tensor_copy(out=APU[:, l0:l0 + 2, :], in_=r3(pq[:, 0:256], 2)), reads=[pdq], writes=[d_APU[g]])
                yield
                for g in range(4):
                    l0 = 2 * g
                    pm, pdm = nb7()
                    pc, pdc = nb7()
                    pr, pdr = nb7()
                    for ci in range(2):
                        l = l0 + ci
                        for h in range(2):
                            hs = HS[h]
                            mm(pm[hs, ci * 64:(ci + 1) * 64], APU[hs, l, 0:64], TM[hs, l, 64:128], True, True, [d_APU[g], d_TM[g]], [pdm])
                            mm(pc[hs, ci * 64:(ci + 1) * 64], TM[hs, l, 64:128], APU[hs, l, 64:128], True, False, [d_APU[g], d_TM[g]], [pdc])
                            mm(pc[hs, ci * 64:(ci + 1) * 64], TM[hs, l, 128:192], TM[hs, l, 0:64], False, True, [d_TM[g]], [pdc])
                            mm(pr[hs, ci * 64:(ci + 1) * 64], APU[hs, l, 0:64], NBall[hs, l, 64:128], True, True, [d_APU[g], d_NB[g]], [pdr])
                    dve.op(lambda e: e.tensor_tensor(out=Mc[:, l0:l0 + 2, :], in0=r3(pm[:, 0:128], 2), in1=Mc[:, l0:l0 + 2, :], op=ALU.add),
                           reads=[pdm, d_Mc[g]], writes=[d_Mc[g]])
                    act.op(lambda e: e.activation(out=CcT[:, l0:l0 + 2, :], in_=r3(pc[:, 0:128], 2), func=AF.Copy), reads=[pdc], writes=[d_Cc[g]])
                    dve.op(lambda e: e.tensor_tensor(out=AR[:, l0:l0 + 2, 64:128], in0=r3(pr[:, 0:128], 2), in1=AR[:, l0:l0 + 2, 64:128], op=ALU.add),
                           reads=[pdr, d_ARr[g], d_AR], writes=[d_ARr[g]])
                    if g % 2 == 1:
                        yield
                for l in range(8):
                    g = l // 2
                    ps_, pds = nb7()
                    for h in range(2):
                        hs = HS[h]
                        mm(ps_[hs, 0:64], Mc[hs, l, :], S32[hs, :], True, True, [d_Mc[g], d_S32], [pds])
                    for h in range(2):
                        hs = HS[h]
                        mm(pyb[hs, l * 64:(l + 1) * 64], Sbf[hs, :], AR[hs, l, 64:128], True, False, [d_Sbf, d_ARr[g]], [pdyb])
                        mm(pyb[hs, l * 64:(l + 1) * 64], APU[hs, l, 64:128], NBall[hs, l, 64:128], False, False, [d_APU[g], d_NB[g]], [pdyb])
                        mm(pyb[hs, l * 64:(l + 1) * 64], TM[hs, l, 0:64], KBall[hs, l, 64:128], False, True, [d_TM[g], d_KB[g]], [pdyb])
                    dve.op(lambda e: e.tensor_tensor(out=S32, in0=ps_[:, 0:64], in1=CcT[:, l, :], op=ALU.add), reads=[pds, d_Cc[g], d_S32], writes=[d_S32])
                    act.op(lambda e: e.activation(out=Sbf, in_=S32, func=AF.Copy), reads=[d_S32], writes=[d_Sbf])
                    yield
                act.op(lambda e: e.activation(out=ybuf, in_=pyb, func=AF.Copy), reads=[pdyb], writes=[dy])
                pb, pd = nb7()
                mm(pb, blk1, ybuf, True, True, [d_const, dy], [pd])
                dve.op(lambda e: e.scalar_tensor_tensor(out=yc, in0=pb, scalar=-1.0 / 64, in1=ybuf, op0=ALU.mult, op1=ALU.add),
                       reads=[pd, dy], writes=[dyc])
                pool.op(lambda e: e.tensor_tensor(out=sq, in0=yc, in1=yc, op=ALU.mult), reads=[dyc], writes=[dsq])
                yield
                pb, pd = nb7()
                mm(pb, blk1, sq, True, True, [d_const, dsq], [pd])
                dve.op(lambda e: e.tensor_scalar(out=rs, in0=pb, scalar1=1.0 / 64, scalar2=64e-5, op0=ALU.mult, op1=ALU.add), reads=[pd], writes=[drs])
                act.op(lambda e: e.activation(out=rs, in_=rs, func=AF.Sqrt), reads=[drs], writes=[drs])
                yield
                dve.op(lambda e: e.reciprocal(out=rs, in_=rs), reads=[drs], writes=[drs])
                pool.op(lambda e: e.tensor_tensor(out=yc, in0=yc, in1=rs, op=ALU.mult), reads=[dyc, drs], writes=[dyc])
                yield
                dve.op(lambda e: e.tensor_scalar(out=yc, in0=yc, scalar1=pvc("ln_w", hp), scalar2=pvc("ln_b", hp), op0=ALU.mult, op1=ALU.add),
                       reads=[dyc, d_const], writes=[dyc])
                pool.op(lambda e: e.tensor_tensor(out=yc, in0=yc, in1=bonus, op=ALU.add), reads=[dyc, d_bonus], writes=[dyc])
                dve.op(lambda e: e.tensor_tensor(out=bufB[:, 8 + hp, tsl], in0=yc, in1=gbuf, op=ALU.mult), reads=[dyc, d_g], writes=[d_B])
                yield

            def run_interleaved(gens):
                gens = [g for g in gens if g is not None]
                while gens:
                    for g in list(gens):
                        try:
                            next(g)
                        except StopIteration:
                            gens.remove(g)

            NU = 8 * NQ
            run_interleaved([prep_gen(0)])
            for u in range(NU):
                run_interleaved([chunk_gen(u), prep_gen(u + 1) if u + 1 < NU else None])
            fw.barrier()
            if "rwT" in dbg_aps:
                tmp = Alloc(big, M_OFF, WORDS).f32(T)
                dtmp = Dep()
                for kc in range(8):
                    dve.op(lambda e: e.tensor_copy(out=tmp, in_=bufB[:, 8 + kc, :]), reads=[d_B], writes=[dtmp])
                    sp.dma(dbg_aps["rwT"][kc * 128:(kc + 1) * 128, :], tmp, reads=[dtmp])
                fw.barrier()

        def out_proj(srcT, d_src, wmat, res_fn, dst_dram, al):
            wbs2 = [(r3(al.bf16(16 * 512), 16), Dep()) for _ in range(2)]
            xts = [(al.f32(512), Dep()) for _ in range(3)]
            hos = [(al.f32(512), Dep()) for _ in range(2)]
            i = 0
            for dblk in range(4):
                ws, dw = wbs2[dblk % 2]
                ds_ = slice(dblk * 512, (dblk + 1) * 512)
                pool.dma(ws, wmat[:, ds_].rearrange("(kc p) n -> p kc n", p=128), writes=[dw])
                for tt in range(NTT):
                    rows = slice(tt * 128, (tt + 1) * 128)
                    xt, dx = xts[i % 3]
                    ho, dh = hos[i % 2]
                    i += 1
                    sp.dma(xt, res_fn(rows, ds_), writes=[dx])
                    pb, pd = nb()
                    for kc in range(KC):
                        mm(pb, srcT[:, kc, rows], ws[:, kc, :], kc == 0, kc == KC - 1, [d_src, dw], [pd])
                    dve.op(lambda e: e.tensor_tensor(out=ho, in0=pb, in1=xt, op=ALU.add), reads=[pd, dx], writes=[dh])
                    act.dma(dst_dram[rows, ds_], ho, reads=[dh])

        if stop >= 4:
            out_proj(bufB, d_B, w_out, lambda rows, cols: x[rows, cols], h1_s, Alloc(big, M_OFF, A_OFF))
            fw.barrier()
            load_gB(1)
            norm_tiles(Alloc(big, M_OFF, A_OFF), NTT, lambda i: h1_s[i * 128:(i + 1) * 128, :], bufA, d_A)
            fw.barrier()
            if "h1" in dbg_aps:
                tmp = Alloc(big, M_OFF, A_OFF).f32(D)
                dtmp = Dep()
                for tt in range(NTT):
                    sp.dma(tmp, h1_s[tt * 128:(tt + 1) * 128, :], writes=[dtmp])
                    sp.dma(dbg_aps["h1"][tt * 128:(tt + 1) * 128, :], tmp, reads=[dtmp])
                fw.barrier()


        if stop >= 5:
            kv_al = Alloc(big, M_OFF, M_OFF + 4096)
            KT = r3(kv_al.bf16(16 * 256), 16)
            Vb = r3(kv_al.bf16(2 * D), 2)
            d_KT, d_Vb, d_memT = Dep(), Dep(), Dep()
            alB = Alloc(big, B0_OFF, M_OFF)
            alM = Alloc(big, M_OFF + 4096, A_OFF)
            memT = r3(alB.bf16(16 * 256), 16)
            wbs5 = [(r3(alB.bf16(16 * 512), 16), Dep()), (r3(alM.bf16(16 * 512), 16), Dep())]
            load_gB(3)
            norm_tiles(alB, 2, lambda i: mem[i * 128:(i + 1) * 128, :], memT, d_memT)
            for g in range(8):
                ws, dw = wbs5[g % 2]
                pool.dma(ws, w_kv[:, g * 512:(g + 1) * 512].rearrange("(kc p) n -> p kc n", p=128), writes=[dw])
                if g < 4:
                    for j in range(4):
                        cb = g * 4 + j
                        pb, pd = nb()
                        for kc in range(KC):
                            mm(pb[:, 0:256], ws[:, kc, j * 128:(j + 1) * 128], memT[:, kc, :], kc == 0, kc == KC - 1, [dw, d_memT], [pd])
                        act.op(lambda e: e.activation(out=KT[:, cb, :], in_=pb[:, 0:256], func=AF.Copy), reads=[pd], writes=[d_KT])
                else:
                    for mc in range(2):
                        pb, pd = nb()
                        for kc in range(KC):
                            mm(pb, memT[:, kc, mc * 128:(mc + 1) * 128], ws[:, kc, :], kc == 0, kc == KC - 1, [dw, d_memT], [pd])
                        dve.op(lambda e: e.tensor_copy(out=Vb[:, mc, (g - 4) * 512:(g - 3) * 512], in_=pb), reads=[pd], writes=[d_Vb])
            fw.barrier()
            alM = Alloc(big, M_OFF + 4096, A_OFF)
            wbs6 = [(r3(alM.bf16(16 * 256), 16), Dep()) for _ in range(2)]
            qscale = float(512 ** -0.5)
            for g in range(8):
                ws, dw = wbs6[g % 2]
                pool.dma(ws, w_q[:, g * 256:(g + 1) * 256].rearrange("(kc p) n -> p kc n", p=128), writes=[dw])
                for j in range(2):
                    cb = g * 2 + j
                    for tq in range(NTQ):
                        ts_ = slice(tq * 512, (tq + 1) * 512)
                        pb, pd = nb()
                        for kc in range(KC):
                            mm(pb, ws[:, kc, j * 128:(j + 1) * 128], bufA[:, kc, ts_], kc == 0, kc == KC - 1, [dw, d_A], [pd])
                        if tq % 2 == 0:
                            act.op(lambda e: e.activation(out=bufB[:, cb, ts_], in_=pb, func=AF.Copy, scale=qscale), reads=[pd], writes=[d_B])
                        else:
                            dve.op(lambda e: e.tensor_scalar(out=bufB[:, cb, ts_], in0=pb, scalar1=qscale, scalar2=None, op0=ALU.mult), reads=[pd], writes=[d_B])
            fw.barrier()
            alM = Alloc(big, M_OFF + 4096, A_OFF)
            Es = [(r3(alM.bf16(2 * 512), 2), Dep()) for _ in range(2)]
            rinvs = [(alM.f32(512), Dep()) for _ in range(2)]
            it = 0
            for h in range(4):
                for tq in range(NTQ):
                    ts_ = slice(tq * 512, (tq + 1) * 512)
                    E, dE = Es[it % 2]
                    rinv, dri = rinvs[it % 2]
                    it += 1
                    for mc in range(2):
                        pb, pd = nb()
                        for c in range(4):
                            mm(pb, KT[:, h * 4 + c, mc * 128:(mc + 1) * 128], bufB[:, h * 4 + c, ts_], c == 0, c == 3, [d_KT, d_B], [pd])
                        act.op(lambda e: e.activation(out=E[:, mc, :], in_=pb, func=AF.Exp), reads=[pd], writes=[dE])
                    pb, pd = nb()
                    for mc in range(2):
                        mm(pb, onesb, E[:, mc, :], mc == 0, mc == 1, [d_const, dE], [pd])
                    dve.op(lambda e: e.reciprocal(out=rinv, in_=pb), reads=[pd], writes=[dri])
                    for c in range(4):
                        pb, pd = nb()
                        for mc in range(2):
                            mm(pb, Vb[:, mc, h * 512 + c * 128:h * 512 + (c + 1) * 128], E[:, mc, :], mc == 0, mc == 1, [d_Vb, dE], [pd])
                        dve.op(lambda e: e.tensor_tensor(out=bufA[:, h * 4 + c, ts_], in0=pb, in1=rinv, op=ALU.mult), reads=[pd, dri], writes=[d_A])
            fw.barrier()
            out_proj(bufA, d_A, w_o, lambda rows, cols: h1_s[rows, cols], h2_s, Alloc(big, B0_OFF, M_OFF))
            fw.barrier()
            if "h2" in dbg_aps:
                tmp = Alloc(big, B0_OFF, M_OFF).f32(D)
                dtmp = Dep()
                for tt in range(NTT):
                    sp.dma(tmp, h2_s[tt * 128:(tt + 1) * 128, :], writes=[dtmp])
                    sp.dma(dbg_aps["h2"][tt * 128:(tt + 1) * 128, :], tmp, reads=[dtmp])
                fw.barrier()

        if stop >= 8:
            IOA = bass.IndirectOffsetOnAxis
            bc_reg = es.enter_context(nc.gpsimd.register("bc"))
            nc.gpsimd.reg_mov(bc_reg, NROW - 1)
            BCV = nc.gpsimd.snap(bc_reg)
            bw_reg = es.enter_context(nc.gpsimd.register("bw"))
            nc.gpsimd.reg_mov(bw_reg, 8191)
            BWV = nc.gpsimd.snap(bw_reg)
            al8 = Alloc(big, B0_OFF, WORDS)
            LT = al8.f32(128)
            iop = al8.f32(1)
            siota = al8.f32(32)
            thr8 = al8.f32(8)
            p1a, p2a = al8.f32(16), al8.f32(16)
            pos1i = al8.f32(16).bitcast(I32)
            pos2i = al8.f32(16).bitcast(I32)
            widx = al8.f32(NSLOT * 4).bitcast(I32)
            d_c8, d_pos, d_widx, d_pp = Dep(), Dep(), Dep(), Dep()
            P8_TOP = al8.top
            sp.dma(LT, cst[:, 768:896], writes=[d_c8])
            sp.dma(iop, cst[:, 896:897], writes=[d_c8], allow_slow_non_contiguous=True)
            sp.dma(siota, cst[:, 897:929], writes=[d_c8])
            sp.dma(thr8, cst[:, 929:937], writes=[d_c8])
            fw.barrier()
            xnb_all = r3(al8.bf16(16 * D), 16)
            d_xnb = [Dep() for _ in range(16)]
            xts = [(al8.f32(D), Dep()) for _ in range(2)]
            xn32s = [(al8.f32(D), Dep()) for _ in range(2)]
            junk = al8.bf16(D)
            d_junk = Dep()
            h32s = [(r3(al8.f32(16 * 128), 16), Dep()) for _ in range(2)]
            wr32 = r3(al8.f32(16 * 20), 16)
            d_wr = Dep()
            logits = r3(al8.f32(16 * 20), 16)
            d_log = Dep()
            sts = [(al8.f32(8), Dep()) for _ in range(4)]
            sp.dma(wr32, w_r.rearrange("(kc p) n -> p kc n", p=128), writes=[d_wr])
            load_gB(2)
            for tt in range(16):
                xt, dx = xts[tt % 2]
                xn32, dxn = xn32s[tt % 2]
                h32, d_h32 = h32s[tt % 2]
                st, d_st = sts[tt % 4]
                sp.dma(xt, h2_s[tt * 128:(tt + 1) * 128, :], writes=[dx])
                act.op(lambda e: e.activation(out=junk, in_=xt, func=AF.Square, accum_out=st[:, 0:1]), reads=[dx], writes=[d_junk, d_st])
                dve.op(lambda e: e.tensor_scalar(out=st[:, 1:2], in0=st[:, 0:1], scalar1=1.0 / D, scalar2=1e-6, op0=ALU.mult, op1=ALU.add),
                       reads=[d_st], writes=[d_st])
                act.op(lambda e: e.activation(out=st[:, 2:3], in_=st[:, 1:2], func=AF.Sqrt), reads=[d_st], writes=[d_st])
                dve.op(lambda e: e.reciprocal(out=st[:, 3:4], in_=st[:, 2:3]), reads=[d_st], writes=[d_st])
                dve.op(lambda e: e.scalar_tensor_tensor(out=xn32, in0=xt, scalar=st[:, 3:4], in1=gBt, op0=ALU.mult, op1=ALU.mult),
                       reads=[dx, d_st, d_gB], writes=[dxn])
                act.op(lambda e: e.activation(out=xnb_all[:, tt, :], in_=xn32, func=AF.Copy), reads=[dxn], writes=[d_xnb[tt]])
                for q in range(4):
                    pf, pdf = nb()
                    for j in range(4):
                        kc = q * 4 + j
                        mm(pf[:, j * 128:(j + 1) * 128], xn32[:, kc * 128:(kc + 1) * 128], identf, True, True, [dxn, d_const], [pdf])
                    if q % 2 == 0:
                        dve.op(lambda e: e.tensor_copy(out=h32[:, q * 4:q * 4 + 4, :], in_=r3(pf, 4)), reads=[pdf], writes=[d_h32])
                    else:
                        act.op(lambda e: e.activation(out=h32[:, q * 4:q * 4 + 4, :], in_=r3(pf, 4), func=AF.Copy), reads=[pdf], writes=[d_h32])
                pb, pd = nb()
                for kc in range(KC):
                    mm(pb[:, 0:20], h32[:, kc, :], wr32[:, kc, :], kc == 0, False, [d_h32, d_wr], [pd])
                mm(pb[:, 0:20], ones1[0:1, 0:128], brow[0:1, 0:20], False, True, [d_const], [pd])
                dve.op(lambda e: e.tensor_copy(out=logits[:, tt, :], in_=pb[:, 0:20]), reads=[pd], writes=[d_log])
            NT_ = 16
            rt = [al8.f32(NT_ * 4) for _ in range(12)]
            rt4 = al8.f32(NT_ * 16)
            sel1 = al8.f32(NT_ * 16)
            sel2 = al8.f32(NT_ * 16)
            ind = al8.f32(NT_ * 16)
            tot = r3(al8.f32(NT_ * 16), NT_)
            tcum = r3(al8.f32(NT_ * 16), NT_)
            posall = al8.f32(NT_ * 16)
            ptmp = al8.f32(NT_ * 16)
            c8 = al8.f32(16 * 8)
            cnt, nsl, bsl, bsl256 = al8.f32(16), al8.f32(16), al8.f32(16), al8.f32(16)
            total = al8.f32(1)
            es32 = al8.f32(NSLOT * 16)
            esf, unused, wbase = al8.f32(NSLOT), al8.f32(NSLOT), al8.f32(NSLOT)
            pos1f, pos2f = al8.f32(16), al8.f32(16)
            widxf = al8.f32(NSLOT * 4).rearrange("p (s q) -> p s q", q=4)
            d_rt = Dep()
            lg = logits[:, :, 0:4]
            le = logits[:, :, 4:20].rearrange("p t (g e) -> p t g e", g=4)
            gmax, gsum, gw, m1, m2 = [rt[i][:, 0:NT_] for i in range(5)]
            goh, gsh, esel, oh1, e2 = [r3(rt[7 + i], NT_) for i in range(5)]
            t4 = rt4.rearrange("p (t g e) -> p t g e", t=NT_, g=4)

            def bc3(v):
                return v.unsqueeze(2).to_broadcast([128, NT_, 4])

            def v4(a):
                return a.rearrange("p (t g e) -> p t g e", t=NT_, g=4)
            R = [d_log, d_rt, d_c8]
            W_ = [d_rt]
            dve.op(lambda e: e.tensor_reduce(out=gmax, in_=lg, axis=AX.X, op=ALU.max), R, W_)
            dve.op(lambda e: e.tensor_tensor(out=goh, in0=lg, in1=bc3(gmax), op=ALU.is_equal), R, W_)
            dve.op(lambda e: e.tensor_tensor(out=gsh, in0=lg, in1=bc3(gmax), op=ALU.subtract), R, W_)
            act.op(lambda e: e.activation(out=gsh, in_=gsh, func=AF.Exp), R, W_)
            dve.op(lambda e: e.tensor_reduce(out=gsum, in_=gsh, axis=AX.X, op=ALU.add), R, W_)
            dve.op(lambda e: e.reciprocal(out=gw, in_=gsum), R, W_)
            dve.op(lambda e: e.tensor_tensor(out=t4, in0=le, in1=goh.unsqueeze(3).to_broadcast([128, NT_, 4, 4]), op=ALU.mult), R, W_)
            dve.op(lambda e: e.tensor_reduce(out=esel, in_=t4.rearrange("p t g e -> p t e g"), axis=AX.X, op=ALU.add), R, W_)
            dve.op(lambda e: e.tensor_reduce(out=m1, in_=esel, axis=AX.X, op=ALU.max), R, W_)
            dve.op(lambda e: e.tensor_tensor(out=oh1, in0=esel, in1=bc3(m1), op=ALU.is_equal), R, W_)
            dve.op(lambda e: e.scalar_tensor_tensor(out=e2, in0=oh1, scalar=-1e30, in1=esel, op0=ALU.mult, op1=ALU.add), R, W_)
            dve.op(lambda e: e.tensor_reduce(out=m2, in_=e2, axis=AX.X, op=ALU.max), R, W_)
            dve.op(lambda e: e.tensor_tensor(out=e2, in0=e2, in1=bc3(m2), op=ALU.is_equal), R, W_)
            dve.op(lambda e: e.tensor_tensor(out=p1a, in0=m1, in1=m2, op=ALU.subtract), R, W_ + [d_pp])
            act.op(lambda e: e.activation(out=p1a, in_=p1a, func=AF.Sigmoid), R + [d_pp], W_ + [d_pp])
            dve.op(lambda e: e.tensor_scalar(out=p2a, in0=p1a, scalar1=-1.0, scalar2=1.0, op0=ALU.mult, op1=ALU.add), R + [d_pp], W_ + [d_pp])
            dve.op(lambda e: e.tensor_tensor(out=p1a, in0=p1a, in1=gw, op=ALU.mult), R + [d_pp], W_ + [d_pp])
            dve.op(lambda e: e.tensor_tensor(out=p2a, in0=p2a, in1=gw, op=ALU.mult), R + [d_pp], W_ + [d_pp])
            dve.op(lambda e: e.tensor_tensor(out=v4(sel1), in0=goh.unsqueeze(3).to_broadcast([128, NT_, 4, 4]),
                                             in1=oh1.unsqueeze(2).to_broadcast([128, NT_, 4, 4]), op=ALU.mult), R, W_)
            dve.op(lambda e: e.tensor_tensor(out=v4(sel2), in0=goh.unsqueeze(3).to_broadcast([128, NT_, 4, 4]),
                                             in1=e2.unsqueeze(2).to_broadcast([128, NT_, 4, 4]), op=ALU.mult), R, W_)
            dve.op(lambda e: e.tensor_tensor(out=ind, in0=sel1, in1=sel2, op=ALU.add), R, W_)
            pw, pdw = nb()
            mm(pw[:, 0:256], LT, ind, True, True, [d_rt, d_c8], [pdw])
            pt_, pdt = nb()
            mm(pt_[:, 0:256], ones1, ind, True, True, [d_rt, d_const], [pdt])
            dve.op(lambda e: e.tensor_copy(out=tot, in_=r3(pt_[:, 0:256], NT_)), R + [pdt], W_)
            dve.op(lambda e: e.memset(tcum[:, 0, :], 0.0), R, W_)
            for tt in range(1, NT_):
                dve.op(lambda e: e.tensor_tensor(out=tcum[:, tt, :], in0=tcum[:, tt - 1, :], in1=tot[:, tt - 1, :], op=ALU.add), R, W_)
            dve.op(lambda e: e.tensor_tensor(out=cnt, in0=tcum[:, NT_ - 1, :], in1=tot[:, NT_ - 1, :], op=ALU.add), R, W_)
            dve.op(lambda e: e.tensor_tensor(out=r3(c8, 16), in0=cnt.unsqueeze(2).to_broadcast([128, 16, 8]),
                                             in1=thr8.unsqueeze(1).to_broadcast([128, 16, 8]), op=ALU.is_gt), R, W_)
            dve.op(lambda e: e.tensor_reduce(out=nsl, in_=r3(c8, 16), axis=AX.X, op=ALU.add), R, W_)
            dve.op(lambda e: e.memset(bsl[:, 0:1], 0.0), R, W_)
            for ex in range(1, 16):
                dve.op(lambda e: e.tensor_tensor(out=bsl[:, ex:ex + 1], in0=bsl[:, ex - 1:ex], in1=nsl[:, ex - 1:ex], op=ALU.add), R, W_)
            dve.op(lambda e: e.tensor_tensor(out=total, in0=bsl[:, 15:16], in1=nsl[:, 15:16], op=ALU.add), R, W_)
            dve.op(lambda e: e.tensor_scalar(out=bsl256, in0=bsl, scalar1=float(SL), scalar2=None, op0=ALU.mult), R, W_)
            dve.op(lambda e: e.tensor_tensor(out=posall, in0=pw[:, 0:256], in1=tcum.rearrange("p t e -> p (t e)"), op=ALU.add), R + [pdw], W_)
            dve.op(lambda e: e.tensor_tensor(out=r3(posall, NT_), in0=r3(posall, NT_), in1=bsl256.unsqueeze(1).to_broadcast([128, NT_, 16]), op=ALU.add), R, W_)
            dve.op(lambda e: e.tensor_tensor(out=ptmp, in0=posall, in1=sel1, op=ALU.mult), R, W_)
            dve.op(lambda e: e.tensor_reduce(out=pos1f, in_=r3(ptmp, NT_), axis=AX.X, op=ALU.add), R, W_)
            dve.op(lambda e: e.tensor_tensor(out=ptmp, in0=posall, in1=sel2, op=ALU.mult), R, W_)
            dve.op(lambda e: e.tensor_reduce(out=pos2f, in_=r3(ptmp, NT_), axis=AX.X, op=ALU.add), R, W_)
            dve.op(lambda e: e.tensor_copy(out=pos1i, in_=pos1f), R, W_ + [d_pos])
            dve.op(lambda e: e.tensor_copy(out=pos2i, in_=pos2f), R, W_ + [d_pos])
            dve.op(lambda e: e.tensor_tensor(out=r3(es32, NSLOT), in0=bsl.unsqueeze(1).to_broadcast([128, NSLOT, 16]),
                                             in1=siota[:, 0:NSLOT].unsqueeze(2).to_broadcast([128, NSLOT, 16]), op=ALU.is_le), R, W_)
            dve.op(lambda e: e.tensor_reduce(out=esf, in_=r3(es32, NSLOT), axis=AX.X, op=ALU.add), R, W_)
            dve.op(lambda e: e.tensor_scalar(out=unused, in0=siota[:, 0:NSLOT], scalar1=total[:, 0:1], scalar2=1.0e6, op0=ALU.is_ge, op1=ALU.mult), R, W_)
            dve.op(lambda e: e.tensor_scalar(out=wbase, in0=esf, scalar1=-1.0, scalar2=512.0, op0=ALU.add, op1=ALU.mult), R, W_)
            dve.op(lambda e: e.tensor_tensor(out=wbase, in0=wbase, in1=unused, op=ALU.add), R, W_)
            dve.op(lambda e: e.tensor_scalar(out=wbase, in0=wbase, scalar1=iop[:, 0:1], scalar2=None, op0=ALU.add), R, W_)
            for q in range(4):
                dve.op(lambda e: e.tensor_scalar(out=widxf[:, :, q], in0=wbase, scalar1=float(128 * q), scalar2=None, op0=ALU.add), R, W_)
            dve.op(lambda e: e.tensor_copy(out=widx, in_=widxf.rearrange("p s q -> p (s q)")), R, W_ + [d_widx])
            if "route" in dbg_aps:
                sp.dma(dbg_aps["route"][:, 0:16], pos1f, reads=[d_rt])
                sp.dma(dbg_aps["route"][:, 16:32], pos2f, reads=[d_rt])
                sp.dma(dbg_aps["route"][:, 32:64], wbase, reads=[d_rt])
                sp.dma(dbg_aps["route"][:, 64:80], p1a, reads=[d_pp])
                sp.dma(dbg_aps["route"][:, 80:96], p2a, reads=[d_pp])
                sp.dma(dbg_aps["route"][:, 96:112], cnt, reads=[d_rt])
            for tt in range(16):
                for posi in (pos1i, pos2i):
                    pool.dma_fn(lambda e: e.indirect_dma_start(out=Xs, out_offset=IOA(ap=posi[:, tt:tt + 1], axis=0), in_=xnb_all[:, tt, :], in_offset=None,
                                                               bounds_check=BCV, oob_is_err=False),
                                reads=[d_xnb[tt], d_pos], writes=[d_Xs])
            fw.barrier()
            ald = Alloc(big, P8_TOP, WORDS)
            wbufs = [(ald.bf16(8192), [Dep() for _ in range(4)]) for _ in range(6)]
            xsls = [(r3(ald.bf16(NA * D), NA), Dep()) for _ in range(2)]
            XTs = [(r3(ald.bf16(16 * SL), 16), Dep()) for _ in range(2)]
            hids = [(r3(ald.bf16(4 * SL), 4), Dep()) for _ in range(2)]
            sbs = [(ald.bf16(SL), Dep()) for _ in range(2)]
            yos = [(ald.f32(D), Dep()) for _ in range(2)]
            cnt8 = dict(yi=0, ei=0)

            def wload(i, s):
                wsl = []
                for m, wl in enumerate((wg_l, wu_l, wd_l)):
                    buf, deps = wbufs[(3 * i + m) % 6]
                    for q in range(4):
                        pool.dma_fn(lambda e: e.indirect_dma_start(out=buf[:, q * 2048:(q + 1) * 2048], out_offset=None, in_=wl,
                                                                   in_offset=IOA(ap=widx[:, s * 4 + q:s * 4 + q + 1], axis=0), bounds_check=BWV, oob_is_err=False),
                                    reads=[d_widx], writes=[deps[q]])
                    wsl.append((buf, deps))
                return wsl

            def xload(i, s):
                xsl, dxs = xsls[i % 2]
                sp.dma(xsl, Xs[s * SL:(s + 1) * SL, :].rearrange("(a p) n -> p a n", p=128), reads=[d_Xs], writes=[dxs])

            def emit_T(i, s):
                xsl, dxs = xsls[i % 2]
                XT, dXT = XTs[i % 2]
                for a in range(NA):
                    for q4 in range(4):
                        pb, pd = nb()
                        for j in range(4):
                            kc = q4 * 4 + j
                            mm(pb[:, j * 128:(j + 1) * 128], xsl[:, a, kc * 128:(kc + 1) * 128], identb, True, True, [dxs, d_const], [pd])
                        cnt8["ei"] += 1
                        if cnt8["ei"] % 2 == 0:
                            act.op(lambda e: e.activation(out=XT[:, q4 * 4:q4 * 4 + 4, a * 128:(a + 1) * 128], in_=r3(pb, 4), func=AF.Copy), reads=[pd], writes=[dXT])
                        else:
                            dve.op(lambda e: e.tensor_copy(out=XT[:, q4 * 4:q4 * 4 + 4, a * 128:(a + 1) * 128], in_=r3(pb, 4)), reads=[pd], writes=[dXT])

            def emit_GU(i, s, wsl):
                wg, dwg = r3(wsl[0][0], 16), wsl[0][1]
                wu, dwu = r3(wsl[1][0], 16), wsl[1][1]
                XT, dXT = XTs[i % 2]
                hid, dhid = hids[i % 2]
                for ffc in range(4):
                    pg, pdg = nb()
                    for kc in range(KC):
                        mm(pg[:, 0:SL], wg[:, kc, ffc * 128:(ffc + 1) * 128], XT[:, kc, :], kc == 0, kc == KC - 1, [dwg[kc // 4], dXT], [pdg])
                    pu, pdu = nb()
                    for kc in range(KC):
                        mm(pu[:, 0:SL], wu[:, kc, ffc * 128:(ffc + 1) * 128], XT[:, kc, :], kc == 0, kc == KC - 1, [dwu[kc // 4], dXT], [pdu])
                    sb_, dsb = sbs[ffc % 2]
                    act.op(lambda e: e.activation(out=sb_, in_=pg[:, 0:SL], func=AF.Silu), reads=[pdg], writes=[dsb])
                    dve.op(lambda e: e.tensor_tensor(out=hid[:, ffc, :], in0=pu[:, 0:SL], in1=sb_, op=ALU.mult), reads=[pdu, dsb], writes=[dhid])

            def emit_D(i, s, wsl):
                wd, dwd = r3(wsl[2][0], 4), wsl[2][1]
                hid, dhid = hids[i % 2]
                for a in range(NA):
                    yo, dyo = yos[cnt8["yi"] % 2]
                    cnt8["yi"] += 1
                    for dblk in range(4):
                        ds_ = slice(dblk * 512, (dblk + 1) * 512)
                        pb, pd = nb()
                        for ffc in range(4):
                            mm(pb, hid[:, ffc, a * 128:(a + 1) * 128], wd[:, ffc, ds_], ffc == 0, ffc == 3, [dhid, dwd[ffc]], [pd])
                        if dblk % 2 == 0:
                            act.op(lambda e: e.activation(out=yo[:, ds_], in_=pb, func=AF.Copy), reads=[pd], writes=[dyo])
                        else:
                            dve.op(lambda e: e.tensor_copy(out=yo[:, ds_], in_=pb), reads=[pd], writes=[dyo])
                    r0 = s * SL + a * 128
                    sp.dma(Ys[r0:r0 + 128, :], yo, reads=[dyo], writes=[d_Ys])

            lo_n = NSLOT - NSLOT // 3
            lo, hi = list(range(lo_n)), list(range(NSLOT - 1, lo_n - 1, -1))
            order = []
            while lo or hi:
                order += lo[:2]
                lo = lo[2:]
                if hi:
                    order.append(hi.pop(0))
            assert sorted(order) == list(range(NSLOT))
            xload(0, order[0])
            emit_T(0, order[0])
            for i, s in enumerate(order):
                wsl = wload(i, s)
                if i + 1 < NSLOT:
                    xload(i + 1, order[i + 1])
                emit_GU(i, s, wsl)
                if i + 1 < NSLOT:
                    emit_T(i + 1, order[i + 1])
                emit_D(i, s, wsl)
            fw.barrier()
            ale = Alloc(big, P8_TOP, WORDS)
            cts = [(ale.f32(D), Dep()) for _ in range(2)]
            y1s = [(ale.f32(D), Dep()) for _ in range(2)]
            y2s = [(ale.f32(D), Dep()) for _ in range(2)]
            junk2 = ale.bf16(D)
            sts2 = [(ale.f32(8), Dep()) for _ in range(4)]
            load_gB(4)
            for tt in range(16):
                xt, dx = cts[tt % 2]
                y1, dy1 = y1s[tt % 2]
                y2, dy2 = y2s[tt % 2]
                st, d_st = sts2[tt % 4]
                pool.dma(xt, h2_s[tt * 128:(tt + 1) * 128, :], writes=[dx])
                pool.dma_fn(lambda e: e.indirect_dma_start(out=y1, out_offset=None, in_=Ys, in_offset=IOA(ap=pos1i[:, tt:tt + 1], axis=0),
                                                           bounds_check=BCV, oob_is_err=False), reads=[d_Ys, d_pos], writes=[dy1])
                pool.dma_fn(lambda e: e.indirect_dma_start(out=y2, out_offset=None, in_=Ys, in_offset=IOA(ap=pos2i[:, tt:tt + 1], axis=0),
                                                           bounds_check=BCV, oob_is_err=False), reads=[d_Ys, d_pos], writes=[dy2])
                dve.op(lambda e: e.scalar_tensor_tensor(out=xt, in0=y1, scalar=p1a[:, tt:tt + 1], in1=xt, op0=ALU.mult, op1=ALU.add),
                       reads=[dy1, dx, d_pp], writes=[dx])
                dve.op(lambda e: e.scalar_tensor_tensor(out=xt, in0=y2, scalar=p2a[:, tt:tt + 1], in1=xt, op0=ALU.mult, op1=ALU.add),
                       reads=[dy2, dx, d_pp], writes=[dx])
                if "h3" in dbg_aps:
                    sp.dma(dbg_aps["h3"][tt * 128:(tt + 1) * 128, :], xt, reads=[dx])
                act.op(lambda e: e.activation(out=junk2, in_=xt, func=AF.Square, accum_out=st[:, 0:1]), reads=[dx], writes=[d_junk, d_st])
                dve.op(lambda e: e.tensor_scalar(out=st[:, 1:2], in0=st[:, 0:1], scalar1=1.0 / D, scalar2=1e-6, op0=ALU.mult, op1=ALU.add),
                       reads=[d_st], writes=[d_st])
                act.op(lambda e: e.activation(out=st[:, 2:3], in_=st[:, 1:2], func=AF.Sqrt), reads=[d_st], writes=[d_st])
                dve.op(lambda e: e.reciprocal(out=st[:, 3:4], in_=st[:, 2:3]), reads=[d_st], writes=[d_st])
                dve.op(lambda e: e.scalar_tensor_tensor(out=xt, in0=xt, scalar=st[:, 3:4], in1=gBt, op0=ALU.mult, op1=ALU.mult),
                       reads=[dx, d_st, d_gB], writes=[dx])
                sp.dma(out[tt * 128:(tt + 1) * 128, :], xt, reads=[dx])
            fw.barrier()

        fw.barrier()
    return nc


def host_consts(inp):
    l = 0
    f = np.float32
    gBh = np.stack([np.broadcast_to(v, (128, D)) for v in (inp["norm_mix_g"][l], inp["norm_xattn_g"][l], inp["norm_ffn_g"][l],
                                                            inp["norm_mem_g"][l], inp["norm_final_g"])]).astype(f)
    pvh = np.zeros((128, NPV), f)

    def col(v, n):
        return np.ascontiguousarray(np.asarray(v, f).reshape(n, 128).T)
    pvh[:, 0:8] = col(inp["pool_scale"][l], 8)
    mu = np.asarray(inp["rwkv_mu"][l], f)
    pvh[:, 8:32] = col(mu[0:3072], 24)
    pvh[:, 32] = mu[3072:3200]
    pvh[:, 33] = mu[3200:3328]
    pvh[0:32, 34] = mu[3328:3360]
    pvh[:, 35:43] = col(inp["rwkv_w0"][l], 8)
    pvh[:, 43:51] = col(inp["rwkv_a0"][l], 8)
    pvh[:, 51:59] = col(inp["rwkv_k_k"][l], 8)
    pvh[:, 59:67] = col(inp["rwkv_k_a"][l], 8)
    pvh[:, 67:75] = col(inp["rwkv_ln_w"][l], 8)
    pvh[:, 75:83] = col(inp["rwkv_ln_b"][l], 8)
    pvh[:, 83:91] = col(np.asarray(inp["rwkv_r_k"][l]).reshape(-1), 8)
    cst = np.zeros((128, 1024), f)
    p = np.arange(128)
    cst[:, 0:128] = np.eye(128, dtype=f)
    cst[:, 128:256] = (p[:, None] // 64 == p[None, :] // 64).astype(f)
    s = p % 64
    tcol = np.arange(64)
    strict = (s[:, None] < tcol[None, :]).astype(f)
    incl = (s[:, None] <= tcol[None, :]).astype(f)
    one = np.concatenate([strict, incl], 1)
    cst[:, 256:512] = np.concatenate([one, one], 1)
    low = (s[:, None] > tcol[None, :]).astype(f)
    cst[:, 512:640] = np.concatenate([low, low], 1)
    cst[:, 640:704] = (s[:, None] == tcol[None, :]).astype(f)
    tt = np.arange(16)
    for gi, w in enumerate((2, 4, 8, 16)):
        cst[:, 704 + gi * 16:704 + (gi + 1) * 16] = (1.0 / np.minimum(tt + 1, w)).astype(f)[None, :]
    cst[:, 768:896] = (p[:, None] < p[None, :]).astype(f)
    cst[:, 896] = p.astype(f)
    cst[:, 897:929] = np.arange(32, dtype=f)[None, :]
    cst[:, 929:937] = (float(SL) * np.arange(8, dtype=f))[None, :]
    rm = np.ones((128, T), f)
    rm[:, ::64] = 0.0
    w_r = np.concatenate([inp["moe_w_group"][l], inp["moe_w_expert"][l]], 1).astype(f)
    b_r = np.concatenate([inp["moe_b_group"][l], inp["moe_b_expert"][l]])[None, :].astype(f)
    return dict(gB=gBh, pv=pvh, cst=cst, rmask=rm, w_r=np.ascontiguousarray(w_r), b_r=b_r)


def make_in_maps(inp, cores):
    l = 0
    c = host_consts(inp)
    shared = dict(
        w_in=inp["w_in"][l], pool_w=inp["pool_w"][l], w2=inp["rwkv_w2"][l], a2=inp["rwkv_a2"][l], g2=inp["rwkv_g2"][l],
        w_out=inp["w_out"][l], w_q=inp["xattn_w_q"][l], w_kv=inp["xattn_w_kv"][l], w_o=inp["xattn_w_o"][l],
        wg_l=np.asarray(inp["moe_w_gate"][l], np.float32).reshape(16, 4, 4, 128, 512).transpose(0, 1, 3, 2, 4).reshape(8192, 2048),
        wu_l=np.asarray(inp["moe_w_up"][l], np.float32).reshape(16, 4, 4, 128, 512).transpose(0, 1, 3, 2, 4).reshape(8192, 2048),
        wd_l=np.asarray(inp["moe_w_down"][l], np.float32).reshape(8192, 2048), **c)
    shared = {k: np.ascontiguousarray(np.asarray(v, np.float32)) for k, v in shared.items()}
    maps = []
    for b in cores:
        m = dict(shared)
        m["x"] = np.ascontiguousarray(inp["x"][b])
        m["mem"] = np.ascontiguousarray(inp["mem"][b])
        maps.append(m)
    return maps


def kernel(**inputs):
    inp = {k: np.asarray(v) for k, v in inputs.items()}
    nc = build()
    maps = make_in_maps(inp, list(range(8)))
    res = run_bass_kernel_spmd(nc, maps, core_ids=list(range(8)))
    return np.stack([np.asarray(r["out"]) for r in res.results], 0).astype(np.float32)
```

```python
import numpy as np
import concourse.bass as bass
import concourse.mybir as mybir
from concourse.bass_utils import run_bass_kernel_spmd
from contextlib import ExitStack

F32 = mybir.dt.float32
BF16 = mybir.dt.bfloat16
I32 = mybir.dt.int32
AF = mybir.ActivationFunctionType
ALU = mybir.AluOpType
AX = mybir.AxisListType

D = 2048
KC = 16
T = 2048
NTT = T // 128
NTQ = T // 512
NCH = T // 64
PAD = 16
CDEC = float(np.exp(-0.5))
WORDS = 51200
NSLOT, SL = 26, 384
NA = SL // 128


class Dep:
    __slots__ = ("w", "r")

    def __init__(self):
        self.w = None
        self.r = {}


class Eng:
    def __init__(self, fw, name, b, is_pe=False):
        self.fw, self.name, self.b, self.is_pe = fw, name, b, is_pe
        self.sem = fw.new_sem(name)
        self.cnt = 0
        self.waited = {}
        self.dma_slots = None
        self.dma_i = 0

    def _wait(self, tok):
        sem, val = tok
        if self.waited.get(id(sem), 0) < val:
            self.b.wait_ge(sem, val)
            self.waited[id(sem)] = val

    def _collect(self, reads, writes):
        for d in reads:
            if d.w is not None and not (self.is_pe and d.w[0] is self.sem):
                self._wait(d.w)
        for d in writes:
            if d.w is not None and not (self.is_pe and d.w[0] is self.sem):
                self._wait(d.w)
            for t in d.r.values():
                if not (self.is_pe and t[0] is self.sem):
                    self._wait(t)

    def op(self, fn, reads=(), writes=()):
        self._collect(reads, writes)
        inst = fn(self.b)
        self.cnt += 1
        inst.then_inc(self.sem, 1)
        tok = (self.sem, self.cnt)
        for d in reads:
            d.r[id(self.sem)] = tok
        for d in writes:
            d.w = tok
            d.r = {}
        return tok

    def dma(self, out, in_, reads=(), writes=(), **kw):
        return self.dma_fn(lambda e: e.dma_start(out=out, in_=in_, **kw), reads, writes)

    def dma_fn(self, fn, reads=(), writes=()):
        if self.dma_slots is None:
            self.dma_slots = [[self.fw.new_sem(f"{self.name}_d{i}"), 0] for i in range(8)]
        self._collect(reads, writes)
        slot = self.dma_slots[self.dma_i % len(self.dma_slots)]
        self.dma_i += 1
        if slot[1] > 0:
            self._wait((slot[0], slot[1]))
        inst = fn(self.b)
        slot[1] += 16
        inst.then_inc(slot[0], 16)
        tok = (slot[0], slot[1])
        for d in reads:
            d.r[id(slot[0])] = tok
        for d in writes:
            d.w = tok
            d.r = {}
        return tok


class FW:
    def __init__(self, nc, es):
        self.nc, self.es = nc, es
        self.pe = Eng(self, "pe", nc.tensor, True)
        self.act = Eng(self, "act", nc.scalar)
        self.dve = Eng(self, "dve", nc.vector)
        self.pool = Eng(self, "pool", nc.gpsimd)
        self.sp = Eng(self, "sp", nc.sync)
        self.engs = [self.pe, self.act, self.dve, self.pool, self.sp]

    def new_sem(self, name):
        return self.es.enter_context(self.nc.semaphore(name))

    def barrier(self):
        toks = []
        for e in self.engs:
            if e.cnt > 0:
                toks.append((e.sem, e.cnt))
            if e.dma_slots:
                for s in e.dma_slots:
                    if s[1] > 0:
                        toks.append((s[0], s[1]))
        for e in self.engs:
            for t in toks:
                if t[0] is not e.sem:
                    e._wait(t)


class Alloc:
    def __init__(self, big, start, end):
        self.big, self.top, self.end = big, start, end

    def f32(self, n):
        a = self.big[:, self.top:self.top + n]
        self.top += n
        assert self.top <= self.end, (self.top, self.end)
        return a

    def bf16(self, n):
        w = (n + 1) // 2
        a = self.big[:, self.top:self.top + w].bitcast(BF16)
        self.top += w
        assert self.top <= self.end, (self.top, self.end)
        return a[:, 0:n]


def r3(ap, a):
    return ap.rearrange("p (a b) -> p a b", a=a)


PV = dict(pool_scale=0, mu_rkv=8, mu_lo=32, w0=35, a0=43, k_k=51, k_a=59, ln_w=67, ln_b=75, r_k=83, omka=91)
NPV = 99


def build(stop=99, dbg=()):
    nc = bass.Bass("TRN2", target_bir_lowering=False)

    def din(name, shape):
        return nc.dram_tensor(name, list(shape), F32, kind="ExternalInput").ap()

    x = din("x", [T, D])
    mem = din("mem", [256, D])
    w_in = din("w_in", [D, 4384])
    pool_w = din("pool_w", [4, 256, 256])
    w2 = din("w2", [64, 1024])
    a2 = din("a2", [64, 1024])
    g2 = din("g2", [160, 1024])
    w_out = din("w_out", [D, D])
    w_q = din("w_q", [D, D])
    w_kv = din("w_kv", [D, 2 * D])
    w_o = din("w_o", [D, D])
    w_r = din("w_r", [D, 20])
    b_r = din("b_r", [1, 20])
    wg_l = din("wg_l", [8192, 2048])
    wu_l = din("wu_l", [8192, 2048])
    wd_l = din("wd_l", [8192, 2048])
    gB = din("gB", [5, 128, D])
    pvd = din("pv", [128, NPV])
    cst = din("cst", [128, 1024])
    rmask_d = din("rmask", [128, T])
    out = nc.dram_tensor("out", [T, D], F32, kind="ExternalOutput").ap()
    dbg_aps = {}
    for name, shape in dbg:
        dbg_aps[name] = nc.dram_tensor(name, list(shape), F32, kind="ExternalOutput").ap()
    rkv_s = nc.dram_tensor("rkv_s", [24, 128, T], F32, kind="Internal").ap()
    h1_s = nc.dram_tensor("h1_s", [T, D], F32, kind="Internal").ap()
    h2_s = nc.dram_tensor("h2_s", [T, D], F32, kind="Internal").ap()
    NROW = NSLOT * SL
    Xs = nc.dram_tensor("Xs", [NROW, D], BF16, kind="Internal").ap()
    Ys = nc.dram_tensor("Ys", [NROW, D], F32, kind="Internal").ap()

    with ExitStack() as es:
        fw = FW(nc, es)
        pe, act, dve, pool, sp = fw.pe, fw.act, fw.dve, fw.pool, fw.sp
        big = es.enter_context(nc.sbuf_tensor("big", [128, WORDS], F32))[:]
        banks = [(es.enter_context(nc.psum_tensor(f"bk{i}", [128, 512], F32))[:], Dep()) for i in range(8)]
        bki = [0]

        def nb():
            b = banks[bki[0] % 8]
            bki[0] += 1
            return b

        def mm(o, lhsT, rhs, start, stop, reads, writes):
            pe.op(lambda e: e.matmul(o, lhsT=lhsT, rhs=rhs, start=start, stop=stop), reads, writes)

        CONST_W = 7424
        ca = Alloc(big, 0, CONST_W)
        identf = ca.f32(128)
        blk1 = ca.f32(128)
        mSI = ca.f32(256)
        mL = ca.f32(128)
        i64 = ca.f32(64)
        rcnt = ca.f32(64)
        pv = ca.f32(NPV + 1)
        identb = ca.bf16(128)
        onesb = ca.bf16(128)
        rmask = ca.bf16(T)
        gBt = ca.f32(D)
        lo1 = ca.bf16(T)
        sg1 = ca.bf16(T)
        sg2 = ca.bf16(T)
        ones1 = ca.f32(128)
        brow = ca.f32(20)
        d_const, d_gB, d_lo1, d_sg1, d_sg2 = Dep(), Dep(), Dep(), Dep(), Dep()
        B0_OFF = CONST_W
        B1_OFF = B0_OFF + 8192
        M_OFF = B1_OFF + 8192
        A_OFF = WORDS - 16384
        bufA = r3(big[:, A_OFF:WORDS].bitcast(BF16), 16)
        bufB = r3(big[:, B0_OFF:M_OFF].bitcast(BF16), 16)
        d_A, d_B = Dep(), Dep()

        sp.dma(identf, cst[:, 0:128], writes=[d_const])
        sp.dma(blk1, cst[:, 128:256], writes=[d_const])
        sp.dma(mSI, cst[:, 256:512], writes=[d_const])
        sp.dma(mL, cst[:, 512:640], writes=[d_const])
        sp.dma(i64, cst[:, 640:704], writes=[d_const])
        sp.dma(rcnt, cst[:, 704:768], writes=[d_const])
        sp.dma(pv[:, 0:NPV], pvd, writes=[d_const])
        sp.dma(brow[0:1, :], b_r, writes=[d_const])
        pool.dma(identb, cst[:, 0:128], writes=[d_const])
        pool.dma(rmask, rmask_d, writes=[d_const])
        pool.op(lambda e: e.memset(onesb, 1.0), writes=[d_const])
        pool.op(lambda e: e.memset(ones1, 1.0), writes=[d_const])
        dve.op(lambda e: e.tensor_scalar(out=pv[:, PV["omka"]:PV["omka"] + 8], in0=pv[:, PV["k_a"]:PV["k_a"] + 8],
                                         scalar1=-1.0, scalar2=1.0, op0=ALU.mult, op1=ALU.add), reads=[d_const], writes=[d_const])
        fw.barrier()

        def pvc(name, j):
            c = PV[name] + j
            return pv[:, c:c + 1]

        d_Xs, d_Ys = Dep(), Dep()
        zf = [0]

        def zero_fill(n, zt, dz):
            while n > 0 and zf[0] < NROW // 128 and stop >= 8:
                c = zf[0]
                pool.dma(Xs[c * 128:(c + 1) * 128, :], zt, reads=[dz])
                zf[0] += 1
                n -= 1

        def load_gB(i):
            sp.dma(gBt, gB[i], writes=[d_gB])

        def norm_tiles(al, ntiles, src_fn, dstT, d_dst, tok_off=0, keep=None):
            xts = [(al.f32(D), Dep()) for _ in range(2)]
            xns = [(al.bf16(D), Dep()) for _ in range(2)]
            junk = al.bf16(D)
            d_junk = Dep()
            sts = [(al.f32(8), Dep()) for _ in range(4)]
            for i in range(ntiles):
                xt, dx = xts[i % 2]
                xn, dn = xns[i % 2]
                st, d_st = sts[i % 4]
                sp.dma(xt, src_fn(i), writes=[dx])
                if keep is not None:
                    keep(i, xt, dx)
                act.op(lambda e: e.activation(out=junk, in_=xt, func=AF.Square, accum_out=st[:, 0:1]), reads=[dx], writes=[d_junk, d_st])
                dve.op(lambda e: e.tensor_scalar(out=st[:, 1:2], in0=st[:, 0:1], scalar1=1.0 / D, scalar2=1e-6, op0=ALU.mult, op1=ALU.add),
                       reads=[d_st], writes=[d_st])
                act.op(lambda e: e.activation(out=st[:, 2:3], in_=st[:, 1:2], func=AF.Sqrt), reads=[d_st], writes=[d_st])
                dve.op(lambda e: e.reciprocal(out=st[:, 3:4], in_=st[:, 2:3]), reads=[d_st], writes=[d_st])
                dve.op(lambda e: e.scalar_tensor_tensor(out=xn, in0=xt, scalar=st[:, 3:4], in1=gBt, op0=ALU.mult, op1=ALU.mult),
                       reads=[dx, d_st, d_gB], writes=[dn])
                for q in range(4):
                    pb, pd = nb()
                    for j in range(4):
                        kc = q * 4 + j
                        mm(pb[:, j * 128:(j + 1) * 128], xn[:, kc * 128:(kc + 1) * 128], identb, True, True, [dn, d_const], [pd])
                    t0 = tok_off + i * 128
                    eng = act if q % 2 == 0 else dve
                    if eng is act:
                        act.op(lambda e: e.activation(out=dstT[:, q * 4:q * 4 + 4, t0:t0 + 128], in_=r3(pb, 4), func=AF.Copy), reads=[pd], writes=[d_dst])
                    else:
                        dve.op(lambda e: e.tensor_copy(out=dstT[:, q * 4:q * 4 + 4, t0:t0 + 128], in_=r3(pb, 4)), reads=[pd], writes=[d_dst])

        def dump(name, ap_sb, dep, dst=None):
            if name in dbg_aps:
                sp.dma(dbg_aps[name] if dst is None else dst, ap_sb, reads=[dep])

        load_gB(0)
        al = Alloc(big, M_OFF, A_OFF)
        norm_tiles(al, NTT, lambda i: x[i * 128:(i + 1) * 128, :], bufA, d_A)
        fw.barrier()
        if "hnT" in dbg_aps:
            tmp = Alloc(big, M_OFF, A_OFF).f32(T)
            dtmp = Dep()
            for kc in range(16):
                dve.op(lambda e: e.tensor_copy(out=tmp, in_=bufA[:, kc, :]), reads=[d_A], writes=[dtmp])
                sp.dma(dbg_aps["hnT"][kc * 128:(kc + 1) * 128, :], tmp, reads=[dtmp])
            fw.barrier()

        if stop >= 2:
            al = Alloc(big, B1_OFF, A_OFF)
            wbs = [(r3(al.bf16(16 * 128), 16), Dep()) for _ in range(4)]
            wbi = [0]
            pbufs = [(al.f32(PAD + T), Dep()) for _ in range(2)]
            fbs = [(al.f32(PAD + T), Dep()) for _ in range(3)]
            pooled = [(al.bf16(T), Dep()) for _ in range(2)]
            pwb = r3(al.bf16(8 * 256), 8)
            d_pw = Dep()
            ztile = al.bf16(D)
            d_zt = Dep()
            dve.op(lambda e: e.memset(ztile, 0.0), writes=[d_zt])
            for pbf, dp in pbufs + fbs:
                dve.op(lambda e: e.memset(pbf[:, 0:PAD], 0.0), writes=[dp])
            pool.dma(pwb, pool_w.rearrange("g (cc p) d -> p (g cc) d", p=128), writes=[d_pw])
            pbi = [0]

            def proj_block(col0, n):
                ws, dw = wbs[wbi[0] % 4]
                wbi[0] += 1
                pool.dma(ws[:, :, 0:n], w_in[:, col0:col0 + n].rearrange("(kc p) n -> p kc n", p=128), writes=[dw])
                if wbi[0] > 4:
                    zero_fill(3, ztile, d_zt)
                pbf, dp = pbufs[pbi[0] % 2]
                pbi[0] += 1
                for tq in range(NTQ):
                    pb, pd = nb()
                    for kc in range(KC):
                        mm(pb[0:n, :], ws[:, kc, 0:n], bufA[:, kc, tq * 512:(tq + 1) * 512], kc == 0, kc == KC - 1, [dw, d_A], [pd])
                    act.op(lambda e: e.activation(out=pbf[0:n, PAD + tq * 512:PAD + (tq + 1) * 512], in_=pb[0:n, :], func=AF.Copy), reads=[pd], writes=[dp])
                return pbf, dp

            def tshift(pbf, dp, n, mu_ap, zout, dz):
                f0, df0 = fbs[0]
                dve.op(lambda e: e.tensor_tensor(out=f0[0:n, 0:T], in0=pbf[0:n, PAD - 1:PAD - 1 + T], in1=pbf[0:n, PAD:PAD + T], op=ALU.subtract),
                       reads=[dp], writes=[df0])
                dve.op(lambda e: e.scalar_tensor_tensor(out=zout, in0=f0[0:n, 0:T], scalar=mu_ap, in1=pbf[0:n, PAD:PAD + T], op0=ALU.mult, op1=ALU.add),
                       reads=[df0, dp, d_const], writes=[dz])

            z1f, dz1 = fbs[1]
            z1 = z1f[:, PAD:PAD + T]
            pbf, dp = proj_block(4096, 128)
            tshift(pbf, dp, 128, pv[:, PV["mu_lo"]:PV["mu_lo"] + 1], z1, dz1)
            act.op(lambda e: e.activation(out=lo1[0:64, :], in_=z1[0:64, :], func=AF.Tanh), reads=[dz1], writes=[d_lo1])
            act.op(lambda e: e.activation(out=lo1[64:128, :], in_=z1[64:128, :], func=AF.Copy), reads=[dz1], writes=[d_lo1])
            pbf, dp = proj_block(4224, 128)
            tshift(pbf, dp, 128, pv[:, PV["mu_lo"] + 1:PV["mu_lo"] + 2], z1, dz1)
            act.op(lambda e: e.activation(out=sg1, in_=z1, func=AF.Sigmoid), reads=[dz1], writes=[d_sg1])
            pbf, dp = proj_block(4352, 32)
            tshift(pbf, dp, 32, pv[0:32, PV["mu_lo"] + 2:PV["mu_lo"] + 3], z1[0:32, :], dz1)
            act.op(lambda e: e.activation(out=sg2[0:32, :], in_=z1[0:32, :], func=AF.Sigmoid), reads=[dz1], writes=[d_sg2])
            for j in range(24):
                pbf, dp = proj_block(1024 + j * 128, 128)
                tshift(pbf, dp, 128, pvc("mu_rkv", j), z1, dz1)
                sp.dma(rkv_s[j], z1, reads=[dz1])
            for cb in range(8):
                gi = cb // 2
                w = (2, 4, 8, 16)[gi]
                pbf, dp = proj_block(cb * 128, 128)
                (fa, dfa), (fb_, dfb) = fbs[1], fbs[2]
                src, dsrc = pbf, dp
                sh = 1
                k = 0
                while sh < w:
                    dst, ddst = (fa, dfa) if k % 2 == 0 else (fb_, dfb)
                    dve.op(lambda e: e.tensor_tensor(out=dst[:, PAD:PAD + T], in0=src[:, PAD:PAD + T], in1=src[:, PAD - sh:PAD - sh + T], op=ALU.add),
                           reads=[dsrc], writes=[ddst])
                    src, dsrc = dst, ddst
                    sh *= 2
                    k += 1
                po, dpo = pooled[cb % 2]
                dve.op(lambda e: e.scalar_tensor_tensor(out=po, in0=src[:, PAD:PAD + T], scalar=1.0 / w, in1=pbf[:, PAD:PAD + T], op0=ALU.mult, op1=ALU.subtract),
                       reads=[dsrc, dp], writes=[dpo])
                f0, df0 = fbs[0]
                dve.op(lambda e: e.tensor_tensor(out=f0[:, 0:16], in0=src[:, PAD:PAD + 16], in1=rcnt[:, gi * 16:(gi + 1) * 16], op=ALU.mult),
                       reads=[dsrc, d_const], writes=[df0])
                dve.op(lambda e: e.tensor_tensor(out=po[:, 0:16], in0=f0[:, 0:16], in1=pbf[:, PAD:PAD + 16], op=ALU.subtract),
                       reads=[df0, dp], writes=[dpo])
                if cb % 2 == 1:
                    for db in range(2):
                        for tq in range(NTQ):
                            pb, pd = nb()
                            for cc in range(2):
                                mm(pb, pwb[:, gi * 2 + cc, db * 128:(db + 1) * 128], pooled[cc][0][:, tq * 512:(tq + 1) * 512], cc == 0, cc == 1,
                                   [d_pw, pooled[cc][1]], [pd])
                            blk = gi * 2 + db
                            act.op(lambda e: e.activation(out=bufB[:, blk, tq * 512:(tq + 1) * 512], in_=pb, func=AF.Copy, scale=pvc("pool_scale", blk)),
                                   reads=[pd, d_const], writes=[d_B])
            zero_fill(NROW, ztile, d_zt)
            fw.barrier()
            if "mixT" in dbg_aps:
                tmp = Alloc(big, B1_OFF, A_OFF).f32(T)
                dtmp = Dep()
                for kc in range(8):
                    dve.op(lambda e: e.tensor_copy(out=tmp, in_=bufB[:, kc, :]), reads=[d_B], writes=[dtmp])
                    sp.dma(dbg_aps["mixT"][kc * 128:(kc + 1) * 128, :], tmp, reads=[dtmp])
                fw.barrier()


        if stop >= 3:
            QTK = 512
            NQ = T // QTK
            al = Alloc(big, M_OFF, WORDS)
            lw = al.bf16(1024)
            g2a = al.bf16(1024)
            g2b = al.bf16(1024)
            S32 = al.f32(64)
            Sbf = al.bf16(64)
            d_lw, d_S32, d_Sbf = Dep(), Dep(), Dep()
            sets = []
            for i in range(2):
                sets.append(dict(AR=r3(al.bf16(8 * 128), 8), BK=r3(al.bf16(8 * 128), 8), BKh=r3(al.bf16(8 * 128), 8), vb=al.bf16(QTK),
                                 GL=al.f32(8), gbuf=al.bf16(QTK), bonus=al.f32(QTK), ybuf=al.f32(QTK),
                                 d_AR=Dep(), d_BK=Dep(), d_BKh=Dep(), d_vb=Dep(), d_GL=Dep(), d_g=Dep(), d_bonus=Dep(), d_y=Dep(),
                                 d_ARr=[Dep() for _ in range(4)]))
            Fq = [al.f32(QTK) for _ in range(8)]
            dFq = [Dep() for _ in range(8)]
            NBall = r3(al.bf16(8 * 128), 8)
            KBall = r3(al.bf16(8 * 128), 8)
            TT = r3(al.bf16(8 * 64), 8)
            TM = r3(al.bf16(8 * 320), 8)
            APU = r3(al.bf16(8 * 128), 8)
            Mc = r3(al.f32(8 * 64), 8)
            CcT = r3(al.f32(8 * 64), 8)
            Wg = [[r3(al.bf16(2 * 128), 2) for _ in range(2)] for _ in range(4)]
            NTg = [[r3(al.bf16(2 * 64), 2) for _ in range(2)] for _ in range(4)]
            d_NB = [Dep() for _ in range(4)]
            d_KB = [Dep() for _ in range(4)]
            d_TT = [Dep() for _ in range(4)]
            d_TM = [Dep() for _ in range(4)]
            d_APU = [Dep() for _ in range(4)]
            d_Mc = [Dep() for _ in range(4)]
            d_Cc = [Dep() for _ in range(4)]
            dWg = [[Dep(), Dep()] for _ in range(4)]
            dNTg = [[Dep(), Dep()] for _ in range(4)]
            yc, sq, rs = al.f32(QTK), al.f32(QTK), al.f32(QTK)
            dyc, dsq, drs = Dep(), Dep(), Dep()
            pool.dma(lw[0:64, :], w2, writes=[d_lw])
            pool.dma(lw[64:128, :], a2, writes=[d_lw])
            pool.dma(g2a, g2[0:128, :], writes=[d_lw])
            pool.dma(g2b[0:32, :], g2[128:160, :], writes=[d_lw])
            HS = [slice(0, 64), slice(64, 128)]
            i64b = i64.unsqueeze(1).to_broadcast([128, 2, 64])
            pyb, pdyb = banks[7]
            nbm = [0]

            def nb7():
                b = banks[nbm[0] % 7]
                nbm[0] += 1
                return b

            def prep_gen(u):
                hp, q = divmod(u, NQ)
                S_ = sets[u % 2]
                cs = slice(hp * 128, (hp + 1) * 128)
                tsl = slice(q * QTK, (q + 1) * QTK)
                k_, sgw, alr, cum, kk, f5, f6, f7 = Fq
                dk, dsgw, dalr, dcum, dkk, df5, df6, df7 = dFq
                AR, BK, BKh, vb, GL, gbuf, bonus = S_["AR"], S_["BK"], S_["BKh"], S_["vb"], S_["GL"], S_["gbuf"], S_["bonus"]
                d_AR, d_BK, d_BKh, d_vb, d_GL, d_g, d_bonus = S_["d_AR"], S_["d_BK"], S_["d_BKh"], S_["d_vb"], S_["d_GL"], S_["d_g"], S_["d_bonus"]
                sp.dma(k_, rkv_s[8 + hp][:, tsl], writes=[dk])
                pb, pd = nb7()
                mm(pb, lw[0:64, cs], lo1[0:64, tsl], True, True, [d_lw, d_lo1], [pd])
                act.op(lambda e: e.activation(out=sgw, in_=pb, func=AF.Sigmoid, bias=pvc("w0", hp)), reads=[pd, d_const], writes=[dsgw])
                pb, pd = nb7()
                mm(pb, lw[64:128, cs], lo1[64:128, tsl], True, True, [d_lw, d_lo1], [pd])
                act.op(lambda e: e.activation(out=alr, in_=pb, func=AF.Sigmoid, bias=pvc("a0", hp)), reads=[pd, d_const], writes=[dalr])
                yield
                pb, pd = nb7()
                mm(pb, g2a[:, cs], sg1[:, tsl], True, False, [d_lw, d_sg1], [pd])
                mm(pb, g2b[0:32, cs], sg2[0:32, tsl], False, True, [d_lw, d_sg2], [pd])
                act.op(lambda e: e.activation(out=gbuf, in_=pb, func=AF.Copy), reads=[pd], writes=[d_g])
                dve.op(lambda e: e.tensor_tensor_scan(out=cum, data0=rmask[:, 0:QTK], data1=sgw, initial=0.0, op0=ALU.mult, op1=ALU.add),
                       reads=[d_const, dsgw], writes=[dcum])
                yield
                act.op(lambda e: e.activation(out=kk, in_=k_, func=AF.Copy, scale=pvc("k_k", hp)), reads=[dk, d_const], writes=[dkk])
                pool.op(lambda e: e.tensor_tensor(out=f5, in0=kk, in1=kk, op=ALU.mult), reads=[dkk], writes=[df5])
                yield
                pb, pd = nb7()
                mm(pb, blk1, f5, True, True, [d_const, df5], [pd])
                dve.op(lambda e: e.tensor_scalar(out=f6, in0=pb, scalar1=1e-24, scalar2=None, op0=ALU.max), reads=[pd], writes=[df6])
                act.op(lambda e: e.activation(out=f6, in_=f6, func=AF.Sqrt), reads=[df6], writes=[df6])
                yield
                dve.op(lambda e: e.reciprocal(out=f6, in_=f6), reads=[df6], writes=[df6])
                yield
                pool.op(lambda e: e.tensor_tensor(out=kk, in0=kk, in1=f6, op=ALU.mult), reads=[dkk, df6], writes=[dkk])
                dve.op(lambda e: e.tensor_scalar(out=f5, in0=alr, scalar1=pvc("k_a", hp), scalar2=pvc("omka", hp), op0=ALU.mult, op1=ALU.add),
                       reads=[dalr, d_const], writes=[df5])
                yield
                pool.op(lambda e: e.tensor_tensor(out=f5, in0=f5, in1=k_, op=ALU.mult), reads=[df5, dk], writes=[df5])
                pool.op(lambda e: e.tensor_tensor(out=alr, in0=alr, in1=kk, op=ALU.mult), reads=[dalr, dkk], writes=[dalr])
                yield
                act.op(lambda e: e.activation(out=f6, in_=cum, func=AF.Exp, scale=CDEC), reads=[dcum], writes=[df6])
                dve.op(lambda e: e.tensor_tensor(out=BK[:, :, 0:64], in0=r3(alr, 8), in1=r3(f6, 8), op=ALU.mult), reads=[dalr, df6], writes=[d_BK])
                pool.op(lambda e: e.tensor_tensor(out=BK[:, :, 64:128], in0=r3(f5, 8), in1=r3(f6, 8), op=ALU.mult), reads=[df5, df6], writes=[d_BK])
                yield
                dve.op(lambda e: e.tensor_tensor(out=r3(f6, 8), in0=r3(cum, 8), in1=r3(cum, 8)[:, :, 63:64].to_broadcast([128, 8, 64]), op=ALU.subtract),
                       reads=[dcum], writes=[df6])
                act.op(lambda e: e.activation(out=f6, in_=f6, func=AF.Exp, scale=CDEC), reads=[df6], writes=[df6])
                yield
                dve.op(lambda e: e.tensor_tensor(out=BKh[:, :, 0:64], in0=r3(alr, 8), in1=r3(f6, 8), op=ALU.mult), reads=[dalr, df6], writes=[d_BKh])
                pool.op(lambda e: e.tensor_tensor(out=BKh[:, :, 64:128], in0=r3(f5, 8), in1=r3(f6, 8), op=ALU.mult), reads=[df5, df6], writes=[d_BKh])
                yield
                pool.op(lambda e: e.tensor_tensor(out=f6, in0=cum, in1=sgw, op=ALU.subtract), reads=[dcum, dsgw], writes=[df6])
                act.op(lambda e: e.activation(out=f6, in_=f6, func=AF.Exp, scale=-CDEC), reads=[df6], writes=[df6])
                yield
                dve.op(lambda e: e.scalar_tensor_tensor(out=AR[:, :, 0:64], in0=r3(kk, 8), scalar=-1.0, in1=r3(f6, 8), op0=ALU.mult, op1=ALU.mult),
                       reads=[dkk, df6], writes=[d_AR])
                act.op(lambda e: e.activation(out=GL, in_=r3(cum, 8)[:, :, 63], func=AF.Exp, scale=-CDEC), reads=[dcum], writes=[d_GL])
                yield
                act.op(lambda e: e.activation(out=f6, in_=cum, func=AF.Exp, scale=-CDEC), reads=[dcum], writes=[df6])
                sp.dma(k_, rkv_s[hp][:, tsl], writes=[dk])
                pool.op(lambda e: e.tensor_tensor(out=AR[:, :, 64:128], in0=r3(k_, 8), in1=r3(f6, 8), op=ALU.mult), reads=[dk, df6], writes=[d_AR] + S_["d_ARr"])
                yield
                dve.op(lambda e: e.scalar_tensor_tensor(out=f6, in0=k_, scalar=pvc("r_k", hp), in1=f5, op0=ALU.mult, op1=ALU.mult),
                       reads=[dk, df5, d_const], writes=[df6])
                sp.dma(sgw, rkv_s[16 + hp][:, tsl], writes=[dsgw])
                yield
                pb, pd = nb7()
                mm(pb, blk1, f6, True, True, [d_const, df6], [pd])
                dve.op(lambda e: e.tensor_tensor(out=bonus, in0=pb, in1=sgw, op=ALU.mult), reads=[pd, dsgw], writes=[d_bonus])
                act.op(lambda e: e.activation(out=vb, in_=sgw, func=AF.Copy), reads=[dsgw], writes=[d_vb])
                yield

            def chunk_gen(u):
                hp, q = divmod(u, NQ)
                S_ = sets[u % 2]
                tsl = slice(q * QTK, (q + 1) * QTK)
                AR, BK, BKh, vb, GL, gbuf, bonus, ybuf = S_["AR"], S_["BK"], S_["BKh"], S_["vb"], S_["GL"], S_["gbuf"], S_["bonus"], S_["ybuf"]
                d_AR, d_BK, d_BKh, d_vb, d_GL, d_g, d_bonus, dy = S_["d_AR"], S_["d_BK"], S_["d_BKh"], S_["d_vb"], S_["d_GL"], S_["d_g"], S_["d_bonus"], S_["d_y"]
                d_ARr = S_["d_ARr"]
                if q == 0:
                    dve.op(lambda e: e.memset(S32, 0.0), writes=[d_S32])
                    dve.op(lambda e: e.memset(Sbf, 0.0), writes=[d_Sbf])
                pool.op(lambda e: e.tensor_tensor(out=Mc, in0=i64.unsqueeze(1).to_broadcast([128, 8, 64]),
                                                  in1=GL.unsqueeze(2).to_broadcast([128, 8, 64]), op=ALU.mult),
                        reads=[d_const, d_GL], writes=d_Mc)
                for g in range(4):
                    l0 = 2 * g
                    pa, pda = nb7()
                    pb_, pdb = nb7()
                    pt, pdt = nb7()
                    pv_, pdv = nb7()
                    for ci in range(2):
                        c = l0 + ci
                        for h in range(2):
                            hs = HS[h]
                            mm(pa[hs, ci * 128:(ci + 1) * 128], BK[hs, c, 0:64], AR[hs, c, :], True, True, [d_BK, d_AR, d_ARr[g]], [pda])
                            mm(pb_[hs, ci * 128:(ci + 1) * 128], BK[hs, c, 64:128], AR[hs, c, :], True, True, [d_BK, d_AR, d_ARr[g]], [pdb])
                            mm(pt[hs, ci * 64:(ci + 1) * 64], AR[hs, c, 0:64], BK[hs, c, 0:64], True, True, [d_BK, d_AR], [pdt])
                            idh = identb[hs, 64 * h:64 * h + 64]
                            mm(pv_[hs, ci * 256:ci * 256 + 64], vb[hs, c * 64:(c + 1) * 64], idh, True, True, [d_vb, d_const], [pdv])
                            mm(pv_[hs, ci * 256 + 64:ci * 256 + 128], BKh[hs, c, 0:64], idh, True, True, [d_BKh, d_const], [pdv])
                            mm(pv_[hs, ci * 256 + 128:ci * 256 + 192], BKh[hs, c, 64:128], idh, True, True, [d_BKh, d_const], [pdv])
                            mm(pv_[hs, ci * 256 + 192:ci * 256 + 256], AR[hs, c, 0:64], idh, True, True, [d_AR, d_const], [pdv])
                    dve.op(lambda e: e.tensor_tensor(out=NBall[:, l0:l0 + 2, :], in0=r3(pa[:, 0:256], 2), in1=r3(mSI, 2), op=ALU.mult),
                           reads=[pda, d_const], writes=[d_NB[g]])
                    dve.op(lambda e: e.tensor_tensor(out=KBall[:, l0:l0 + 2, :], in0=r3(pb_[:, 0:256], 2), in1=r3(mSI, 2), op=ALU.mult),
                           reads=[pdb, d_const], writes=[d_KB[g]])
                    dve.op(lambda e: e.tensor_tensor(out=NTg[g][0], in0=r3(pt[:, 0:128], 2), in1=r3(mL, 2), op=ALU.mult),
                           reads=[pdt, d_const], writes=[dNTg[g][0]])
                    act.op(lambda e: e.activation(out=TM[:, l0:l0 + 2, 0:256], in_=r3(pv_, 2), func=AF.Copy), reads=[pdv], writes=[d_TM[g]])
                    pool.op(lambda e: e.tensor_tensor(out=Wg[g][0][:, :, 64:128], in0=NBall[:, l0:l0 + 2, 0:64], in1=i64b, op=ALU.add),
                            reads=[d_NB[g], d_const], writes=[dWg[g][0]])
                    yield
                for g in range(4):
                    l0 = 2 * g
                    p0, pd0 = nb7()
                    q0, qd0 = nb7()
                    NT, dNT = NTg[g][0], dNTg[g][0]
                    for ci in range(2):
                        for h in range(2):
                            hs = HS[h]
                            mm(p0[hs, ci * 64:(ci + 1) * 64], NT[hs, ci, :], NBall[hs, l0 + ci, 0:64], True, True, [dNT, d_NB[g]], [pd0])
                            mm(q0[hs, ci * 64:(ci + 1) * 64], NBall[hs, l0 + ci, 0:64], NT[hs, ci, :], True, True, [dNT, d_NB[g]], [qd0])
                    act.op(lambda e: e.activation(out=Wg[g][0][:, :, 0:64], in_=r3(p0[:, 0:128], 2), func=AF.Copy), reads=[pd0], writes=[dWg[g][0]])
                    act.op(lambda e: e.activation(out=NTg[g][1], in_=r3(q0[:, 0:128], 2), func=AF.Copy), reads=[qd0], writes=[dNTg[g][1]])
                    yield
                cur, ntc = 0, 1
                for lvl in range(1, 6):
                    last = lvl == 5
                    for g in range(4):
                        l0 = 2 * g
                        Wc, dWc = Wg[g][cur], dWg[g][cur]
                        NTc, dNTc = NTg[g][ntc], dNTg[g][ntc]
                        p1, pd1 = nb7()
                        if not last:
                            q1, qd1 = nb7()
                        for ci in range(2):
                            for h in range(2):
                                hs = HS[h]
                                if not last:
                                    mm(p1[hs, ci * 128:(ci + 1) * 128], NTc[hs, ci, :], Wc[hs, ci, :], True, True, [dNTc, dWc], [pd1])
                                    mm(q1[hs, ci * 64:(ci + 1) * 64], Wc[hs, ci, 0:64], NTc[hs, ci, :], True, True, [dNTc, dWc], [qd1])
                                else:
                                    mm(p1[hs, ci * 64:(ci + 1) * 64], NTc[hs, ci, :], Wc[hs, ci, 64:128], True, True, [dNTc, dWc], [pd1])
                        if not last:
                            Wn, dWn = Wg[g][1 - cur], dWg[g][1 - cur]
                            NTn, dNTn = NTg[g][1 - ntc], dNTg[g][1 - ntc]
                            act.op(lambda e: e.activation(out=Wn[:, :, 0:64], in_=r3(p1[:, 0:256], 2)[:, :, 0:64], func=AF.Copy), reads=[pd1], writes=[dWn])
                            dve.op(lambda e: e.tensor_tensor(out=Wn[:, :, 64:128], in0=r3(p1[:, 0:256], 2)[:, :, 64:128], in1=Wc[:, :, 64:128], op=ALU.add),
                                   reads=[pd1, dWc], writes=[dWn])
                            act.op(lambda e: e.activation(out=NTn, in_=r3(q1[:, 0:128], 2), func=AF.Copy), reads=[qd1], writes=[dNTn])
                        else:
                            dve.op(lambda e: e.tensor_tensor(out=TT[:, l0:l0 + 2, :], in0=r3(p1[:, 0:128], 2), in1=Wc[:, :, 64:128], op=ALU.add),
                                   reads=[pd1, dWc], writes=[d_TT[g]])
                        if g % 2 == 1:
                            yield
                    cur, ntc = 1 - cur, 1 - ntc
                for g in range(4):
                    l0 = 2 * g
                    pw, pdw = nb7()
                    for ci in range(2):
                        for h in range(2):
                            hs = HS[h]
                            mm(pw[hs, ci * 64:(ci + 1) * 64], KBall[hs, l0 + ci, 0:64], TM[hs, l0 + ci, 0:64], True, True, [d_KB[g], d_TM[g]], [pdw])
                    act.op(lambda e: e.activation(out=TM[:, l0:l0 + 2, 256:320], in_=r3(pw[:, 0:128], 2), func=AF.Copy), reads=[pdw], writes=[d_TM[g]])
                yield
                for g in range(4):
                    l0 = 2 * g
                    pq, pdq = nb7()
                    for ci in range(2):
                        for h in range(2):
                            hs = HS[h]
                            mm(pq[hs, ci * 128:(ci + 1) * 128], TT[hs, l0 + ci, :], TM[hs, l0 + ci, 192:320], True, True, [d_TT[g], d_TM[g]], [pdq])
                    dve.op(lambda e: e.tensor_copy(out=APU[:, l0:l0 + 2, :], in_=r3(pq[:, 0:256], 2)), reads=[pdq], writes=[d_APU[g]])
                yield
                for g in range(4):
                    l0 = 2 * g
                    pm, pdm = nb7()
                    pc, pdc = nb7()
                    pr, pdr = nb7()
                    for ci in range(2):
                        l = l0 + ci
                        for h in range(2):
                            hs = HS[h]
                            mm(pm[hs, ci * 64:(ci + 1) * 64], APU[hs, l, 0:64], TM[hs, l, 64:128], True, True, [d_APU[g], d_TM[g]], [pdm])
                            mm(pc[hs, ci * 64:(ci + 1) * 64], TM[hs, l, 64:128], APU[hs, l, 64:128], True, False, [d_APU[g], d_TM[g]], [pdc])
                            mm(pc[hs, ci * 64:(ci + 1) * 64], TM[hs, l, 128:192], TM[hs, l, 0:64], False, True, [d_TM[g]], [pdc])
                            mm(pr[hs, ci * 64:(ci + 1) * 64], APU[hs, l, 0:64], NBall[hs, l, 64:128], True, True, [d_APU[g], d_NB[g]], [pdr])
                    dve.op(lambda e: e.tensor_tensor(out=Mc[:, l0:l0 + 2, :], in0=r3(pm[:, 0:128], 2), in1=Mc[:, l0:l0 + 2, :], op=ALU.add),
                           reads=[pdm, d_Mc[g]], writes=[d_Mc[g]])
                    act.op(lambda e: e.activation(out=CcT[:, l0:l0 + 2, :], in_=r3(pc[:, 0:128], 2), func=AF.Copy), reads=[pdc], writes=[d_Cc[g]])
                    dve.op(lambda e: e.tensor_tensor(out=AR[:, l0:l0 + 2, 64:128], in0=r3(pr[:, 0:128], 2), in1=AR[:, l0:l0 + 2, 64:128], op=ALU.add),
                           reads=[pdr, d_ARr[g], d_AR], writes=[d_ARr[g]])
                    if g % 2 == 1:
                        yield
                for l in range(8):
                    g = l // 2
                    ps_, pds = nb7()
                    for h in range(2):
                        hs = HS[h]
                        mm(ps_[hs, 0:64], Mc[hs, l, :], S32[hs, :], True, True, [d_Mc[g], d_S32], [pds])
                    for h in range(2):
                        hs = HS[h]
                        mm(pyb[hs, l * 64:(l + 1) * 64], Sbf[hs, :], AR[hs, l, 64:128], True, False, [d_Sbf, d_ARr[g]], [pdyb])
                        mm(pyb[hs, l * 64:(l + 1) * 64], APU[hs, l, 64:128], NBall[hs, l, 64:128], False, False, [d_APU[g], d_NB[g]], [pdyb])
                        mm(pyb[hs, l * 64:(l + 1) * 64], TM[hs, l, 0:64], KBall[hs, l, 64:128], False, True, [d_TM[g], d_KB[g]], [pdyb])
                    dve.op(lambda e: e.tensor_tensor(out=S32, in0=ps_[:, 0:64], in1=CcT[:, l, :], op=ALU.add), reads=[pds, d_Cc[g], d_S32], writes=[d_S32])
                    act.op(lambda e: e.activation(out=Sbf, in_=S32, func=AF.Copy), reads=[d_S32], writes=[d_Sbf])
                    yield
                act.op(lambda e: e.activation(out=ybuf, in_=pyb, func=AF.Copy), reads=[pdyb], writes=[dy])
                pb, pd = nb7()
                mm(pb, blk1, ybuf, True, True, [d_const, dy], [pd])
                dve.op(lambda e: e.scalar_tensor_tensor(out=yc, in0=pb, scalar=-1.0 / 64, in1=ybuf, op0=ALU.mult, op1=ALU.add),
                       reads=[pd, dy], writes=[dyc])
                pool.op(lambda e: e.tensor_tensor(out=sq, in0=yc, in1=yc, op=ALU.mult), reads=[dyc], writes=[dsq])
                yield
                pb, pd = nb7()
                mm(pb, blk1, sq, True, True, [d_const, dsq], [pd])
                dve.op(lambda e: e.tensor_scalar(out=rs, in0=pb, scalar1=1.0 / 64, scalar2=64e-5, op0=ALU.mult, op1=ALU.add), reads=[pd], writes=[drs])
                act.op(lambda e: e.activation(out=rs, in_=rs, func=AF.Sqrt), reads=[drs], writes=[drs])
                yield
                dve.op(lambda e: e.reciprocal(out=rs, in_=rs), reads=[drs], writes=[drs])
                pool.op(lambda e: e.tensor_tensor(out=yc, in0=yc, in1=rs, op=ALU.mult), reads=[dyc, drs], writes=[dyc])
                yield
                dve.op(lambda e: e.tensor_scalar(out=yc, in0=yc, scalar1=pvc("ln_w", hp), scalar2=pvc("ln_b", hp), op0=ALU.mult, op1=ALU.add),
                       reads=[dyc, d_const], writes=[dyc])
                pool.op(lambda e: e.tensor_tensor(out=yc, in0=yc, in1=bonus, op=ALU.add), reads=[dyc, d_bonus], writes=[dyc])
                dve.op(lambda e: e.tensor_tensor(out=bufB[:, 8 + hp, tsl], in0=yc, in1=gbuf, op=ALU.mult), reads=[dyc, d_g], writes=[d_B])
                yield

            def run_interleaved(gens):
                gens = [g for g in gens if g is not None]
                while gens:
                    for g in list(gens):
                        try:
                            next(g)
                        except StopIteration:
                            gens.remove(g)

            NU = 8 * NQ
            run_interleaved([prep_gen(0)])
            for u in range(NU):
                run_interleaved([chunk_gen(u), prep_gen(u + 1) if u + 1 < NU else None])
            fw.barrier()
            if "rwT" in dbg_aps:
                tmp = Alloc(big, M_OFF, WORDS).f32(T)
                dtmp = Dep()
                for kc in range(8):
                    dve.op(lambda e: e.tensor_copy(out=tmp, in_=bufB[:, 8 + kc, :]), reads=[d_B], writes=[dtmp])
                    sp.dma(dbg_aps["rwT"][kc * 128:(kc + 1) * 128, :], tmp, reads=[dtmp])
                fw.barrier()

        def out_proj(srcT, d_src, wmat, res_fn, dst_dram, al):
            wbs2 = [(r3(al.bf16(16 * 512), 16), Dep()) for _ in range(2)]
            xts = [(al.f32(512), Dep()) for _ in range(3)]
            hos = [(al.f32(512), Dep()) for _ in range(2)]
            i = 0
            for dblk in range(4):
                ws, dw = wbs2[dblk % 2]
                ds_ = slice(dblk * 512, (dblk + 1) * 512)
                pool.dma(ws, wmat[:, ds_].rearrange("(kc p) n -> p kc n", p=128), writes=[dw])
                for tt in range(NTT):
                    rows = slice(tt * 128, (tt + 1) * 128)
                    xt, dx = xts[i % 3]
                    ho, dh = hos[i % 2]
                    i += 1
                    sp.dma(xt, res_fn(rows, ds_), writes=[dx])
                    pb, pd = nb()
                    for kc in range(KC):
                        mm(pb, srcT[:, kc, rows], ws[:, kc, :], kc == 0, kc == KC - 1, [d_src, dw], [pd])
                    dve.op(lambda e: e.tensor_tensor(out=ho, in0=pb, in1=xt, op=ALU.add), reads=[pd, dx], writes=[dh])
                    act.dma(dst_dram[rows, ds_], ho, reads=[dh])

        if stop >= 4:
            out_proj(bufB, d_B, w_out, lambda rows, cols: x[rows, cols], h1_s, Alloc(big, M_OFF, A_OFF))
            fw.barrier()
            load_gB(1)
            norm_tiles(Alloc(big, M_OFF, A_OFF), NTT, lambda i: h1_s[i * 128:(i + 1) * 128, :], bufA, d_A)
            fw.barrier()
            if "h1" in dbg_aps:
                tmp = Alloc(big, M_OFF, A_OFF).f32(D)
                dtmp = Dep()
                for tt in range(NTT):
                    sp.dma(tmp, h1_s[tt * 128:(tt + 1) * 128, :], writes=[dtmp])
                    sp.dma(dbg_aps["h1"][tt * 128:(tt + 1) * 128, :], tmp, reads=[dtmp])
                fw.barrier()


        if stop >= 5:
            kv_al = Alloc(big, M_OFF, M_OFF + 4096)
            KT = r3(kv_al.bf16(16 * 256), 16)
            Vb = r3(kv_al.bf16(2 * D), 2)
            d_KT, d_Vb, d_memT = Dep(), Dep(), Dep()
            alB = Alloc(big, B0_OFF, M_OFF)
            alM = Alloc(big, M_OFF + 4096, A_OFF)
            memT = r3(alB.bf16(16 * 256), 16)
            wbs5 = [(r3(alB.bf16(16 * 512), 16), Dep()), (r3(alM.bf16(16 * 512), 16), Dep())]
            load_gB(3)
            norm_tiles(alB, 2, lambda i: mem[i * 128:(i + 1) * 128, :], memT, d_memT)
            for g in range(8):
                ws, dw = wbs5[g % 2]
                pool.dma(ws, w_kv[:, g * 512:(g + 1) * 512].rearrange("(kc p) n -> p kc n", p=128), writes=[dw])
                if g < 4:
                    for j in range(4):
                        cb = g * 4 + j
                        pb, pd = nb()
                        for kc in range(KC):
                            mm(pb[:, 0:256], ws[:, kc, j * 128:(j + 1) * 128], memT[:, kc, :], kc == 0, kc == KC - 1, [dw, d_memT], [pd])
                        act.op(lambda e: e.activation(out=KT[:, cb, :], in_=pb[:, 0:256], func=AF.Copy), reads=[pd], writes=[d_KT])
                else:
                    for mc in range(2):
                        pb, pd = nb()
                        for kc in range(KC):
                            mm(pb, memT[:, kc, mc * 128:(mc + 1) * 128], ws[:, kc, :], kc == 0, kc == KC - 1, [dw, d_memT], [pd])
                        dve.op(lambda e: e.tensor_copy(out=Vb[:, mc, (g - 4) * 512:(g - 3) * 512], in_=pb), reads=[pd], writes=[d_Vb])
            fw.barrier()
            alM = Alloc(big, M_OFF + 4096, A_OFF)
            wbs6 = [(r3(alM.bf16(16 * 256), 16), Dep()) for _ in range(2)]
            qscale = float(512 ** -0.5)
            for g in range(8):
                ws, dw = wbs6[g % 2]
                pool.dma(ws, w_q[:, g * 256:(g + 1) * 256].rearrange("(kc p) n -> p kc n", p=128), writes=[dw])
                for j in range(2):
                    cb = g * 2 + j
                    for tq in range(NTQ):
                        ts_ = slice(tq * 512, (tq + 1) * 512)
                        pb, pd = nb()
                        for kc in range(KC):
                            mm(pb, ws[:, kc, j * 128:(j + 1) * 128], bufA[:, kc, ts_], kc == 0, kc == KC - 1, [dw, d_A], [pd])
                        if tq % 2 == 0:
                            act.op(lambda e: e.activation(out=bufB[:, cb, ts_], in_=pb, func=AF.Copy, scale=qscale), reads=[pd], writes=[d_B])
                        else:
                            dve.op(lambda e: e.tensor_scalar(out=bufB[:, cb, ts_], in0=pb, scalar1=qscale, scalar2=None, op0=ALU.mult), reads=[pd], writes=[d_B])
            fw.barrier()
            alM = Alloc(big, M_OFF + 4096, A_OFF)
            Es = [(r3(alM.bf16(2 * 512), 2), Dep()) for _ in range(2)]
            rinvs = [(alM.f32(512), Dep()) for _ in range(2)]
            it = 0
            for h in range(4):
                for tq in range(NTQ):
                    ts_ = slice(tq * 512, (tq + 1) * 512)
                    E, dE = Es[it % 2]
                    rinv, dri = rinvs[it % 2]
                    it += 1
                    for mc in range(2):
                        pb, pd = nb()
                        for c in range(4):
                            mm(pb, KT[:, h * 4 + c, mc * 128:(mc + 1) * 128], bufB[:, h * 4 + c, ts_], c == 0, c == 3, [d_KT, d_B], [pd])
                        act.op(lambda e: e.activation(out=E[:, mc, :], in_=pb, func=AF.Exp), reads=[pd], writes=[dE])
                    pb, pd = nb()
                    for mc in range(2):
                        mm(pb, onesb, E[:, mc, :], mc == 0, mc == 1, [d_const, dE], [pd])
                    dve.op(lambda e: e.reciprocal(out=rinv, in_=pb), reads=[pd], writes=[dri])
                    for c in range(4):
                        pb, pd = nb()
                        for mc in range(2):
                            mm(pb, Vb[:, mc, h * 512 + c * 128:h * 512 + (c + 1) * 128], E[:, mc, :], mc == 0, mc == 1, [d_Vb, dE], [pd])
                        dve.op(lambda e: e.tensor_tensor(out=bufA[:, h * 4 + c, ts_], in0=pb, in1=rinv, op=ALU.mult), reads=[pd, dri], writes=[d_A])
            fw.barrier()
            out_proj(bufA, d_A, w_o, lambda rows, cols: h1_s[rows, cols], h2_s, Alloc(big, B0_OFF, M_OFF))
            fw.barrier()
            if "h2" in dbg_aps:
                tmp = Alloc(big, B0_OFF, M_OFF).f32(D)
                dtmp = Dep()
                for tt in range(NTT):
                    sp.dma(tmp, h2_s[tt * 128:(tt + 1) * 128, :], writes=[dtmp])
                    sp.dma(dbg_aps["h2"][tt * 128:(tt + 1) * 128, :], tmp, reads=[dtmp])
                fw.barrier()

        if stop >= 8:
            IOA = bass.IndirectOffsetOnAxis
            bc_reg = es.enter_context(nc.gpsimd.register("bc"))
            nc.gpsimd.reg_mov(bc_reg, NROW - 1)
            BCV = nc.gpsimd.snap(bc_reg)
            bw_reg = es.enter_context(nc.gpsimd.register("bw"))
            nc.gpsimd.reg_mov(bw_reg, 8191)
            BWV = nc.gpsimd.snap(bw_reg)
            al8 = Alloc(big, B0_OFF, WORDS)
            LT = al8.f32(128)
            iop = al8.f32(1)
            siota = al8.f32(32)
            thr8 = al8.f32(8)
            p1a, p2a = al8.f32(16), al8.f32(16)
            pos1i = al8.f32(16).bitcast(I32)
            pos2i = al8.f32(16).bitcast(I32)
            widx = al8.f32(NSLOT * 4).bitcast(I32)
            d_c8, d_pos, d_widx, d_pp = Dep(), Dep(), Dep(), Dep()
            P8_TOP = al8.top
            sp.dma(LT, cst[:, 768:896], writes=[d_c8])
            sp.dma(iop, cst[:, 896:897], writes=[d_c8], allow_slow_non_contiguous=True)
            sp.dma(siota, cst[:, 897:929], writes=[d_c8])
            sp.dma(thr8, cst[:, 929:937], writes=[d_c8])
            fw.barrier()
            xnb_all = r3(al8.bf16(16 * D), 16)
            d_xnb = [Dep() for _ in range(16)]
            xts = [(al8.f32(D), Dep()) for _ in range(2)]
            xn32s = [(al8.f32(D), Dep()) for _ in range(2)]
            junk = al8.bf16(D)
            d_junk = Dep()
            h32s = [(r3(al8.f32(16 * 128), 16), Dep()) for _ in range(2)]
            wr32 = r3(al8.f32(16 * 20), 16)
            d_wr = Dep()
            logits = r3(al8.f32(16 * 20), 16)
            d_log = Dep()
            sts = [(al8.f32(8), Dep()) for _ in range(4)]
            sp.dma(wr32, w_r.rearrange("(kc p) n -> p kc n", p=128), writes=[d_wr])
            load_gB(2)
            for tt in range(16):
                xt, dx = xts[tt % 2]
                xn32, dxn = xn32s[tt % 2]
                h32, d_h32 = h32s[tt % 2]
                st, d_st = sts[tt % 4]
                sp.dma(xt, h2_s[tt * 128:(tt + 1) * 128, :], writes=[dx])
                act.op(lambda e: e.activation(out=junk, in_=xt, func=AF.Square, accum_out=st[:, 0:1]), reads=[dx], writes=[d_junk, d_st])
                dve.op(lambda e: e.tensor_scalar(out=st[:, 1:2], in0=st[:, 0:1], scalar1=1.0 / D, scalar2=1e-6, op0=ALU.mult, op1=ALU.add),
                       reads=[d_st], writes=[d_st])
                act.op(lambda e: e.activation(out=st[:, 2:3], in_=st[:, 1:2], func=AF.Sqrt), reads=[d_st], writes=[d_st])
                dve.op(lambda e: e.reciprocal(out=st[:, 3:4], in_=st[:, 2:3]), reads=[d_st], writes=[d_st])
                dve.op(lambda e: e.scalar_tensor_tensor(out=xn32, in0=xt, scalar=st[:, 3:4], in1=gBt, op0=ALU.mult, op1=ALU.mult),
                       reads=[dx, d_st, d_gB], writes=[dxn])
                act.op(lambda e: e.activation(out=xnb_all[:, tt, :], in_=xn32, func=AF.Copy), reads=[dxn], writes=[d_xnb[tt]])
                for q in range(4):
                    pf, pdf = nb()
                    for j in range(4):
                        kc = q * 4 + j
                        mm(pf[:, j * 128:(j + 1) * 128], xn32[:, kc * 128:(kc + 1) * 128], identf, True, True, [dxn, d_const], [pdf])
                    if q % 2 == 0:
                        dve.op(lambda e: e.tensor_copy(out=h32[:, q * 4:q * 4 + 4, :], in_=r3(pf, 4)), reads=[pdf], writes=[d_h32])
                    else:
                        act.op(lambda e: e.activation(out=h32[:, q * 4:q * 4 + 4, :], in_=r3(pf, 4), func=AF.Copy), reads=[pdf], writes=[d_h32])
                pb, pd = nb()
                for kc in range(KC):
                    mm(pb[:, 0:20], h32[:, kc, :], wr32[:, kc, :], kc == 0, False, [d_h32, d_wr], [pd])
                mm(pb[:, 0:20], ones1[0:1, 0:128], brow[0:1, 0:20], False, True, [d_const], [pd])
                dve.op(lambda e: e.tensor_copy(out=logits[:, tt, :], in_=pb[:, 0:20]), reads=[pd], writes=[d_log])
            NT_ = 16
            rt = [al8.f32(NT_ * 4) for _ in range(12)]
            rt4 = al8.f32(NT_ * 16)
            sel1 = al8.f32(NT_ * 16)
            sel2 = al8.f32(NT_ * 16)
            ind = al8.f32(NT_ * 16)
            tot = r3(al8.f32(NT_ * 16), NT_)
            tcum = r3(al8.f32(NT_ * 16), NT_)
            posall = al8.f32(NT_ * 16)
            ptmp = al8.f32(NT_ * 16)
            c8 = al8.f32(16 * 8)
            cnt, nsl, bsl, bsl256 = al8.f32(16), al8.f32(16), al8.f32(16), al8.f32(16)
            total = al8.f32(1)
            es32 = al8.f32(NSLOT * 16)
            esf, unused, wbase = al8.f32(NSLOT), al8.f32(NSLOT), al8.f32(NSLOT)
            pos1f, pos2f = al8.f32(16), al8.f32(16)
            widxf = al8.f32(NSLOT * 4).rearrange("p (s q) -> p s q", q=4)
            d_rt = Dep()
            lg = logits[:, :, 0:4]
            le = logits[:, :, 4:20].rearrange("p t (g e) -> p t g e", g=4)
            gmax, gsum, gw, m1, m2 = [rt[i][:, 0:NT_] for i in range(5)]
            goh, gsh, esel, oh1, e2 = [r3(rt[7 + i], NT_) for i in range(5)]
            t4 = rt4.rearrange("p (t g e) -> p t g e", t=NT_, g=4)

            def bc3(v):
                return v.unsqueeze(2).to_broadcast([128, NT_, 4])

            def v4(a):
                return a.rearrange("p (t g e) -> p t g e", t=NT_, g=4)
            R = [d_log, d_rt, d_c8]
            W_ = [d_rt]
            dve.op(lambda e: e.tensor_reduce(out=gmax, in_=lg, axis=AX.X, op=ALU.max), R, W_)
            dve.op(lambda e: e.tensor_tensor(out=goh, in0=lg, in1=bc3(gmax), op=ALU.is_equal), R, W_)
            dve.op(lambda e: e.tensor_tensor(out=gsh, in0=lg, in1=bc3(gmax), op=ALU.subtract), R, W_)
            act.op(lambda e: e.activation(out=gsh, in_=gsh, func=AF.Exp), R, W_)
            dve.op(lambda e: e.tensor_reduce(out=gsum, in_=gsh, axis=AX.X, op=ALU.add), R, W_)
            dve.op(lambda e: e.reciprocal(out=gw, in_=gsum), R, W_)
            dve.op(lambda e: e.tensor_tensor(out=t4, in0=le, in1=goh.unsqueeze(3).to_broadcast([128, NT_, 4, 4]), op=ALU.mult), R, W_)
            dve.op(lambda e: e.tensor_reduce(out=esel, in_=t4.rearrange("p t g e -> p t e g"), axis=AX.X, op=ALU.add), R, W_)
            dve.op(lambda e: e.tensor_reduce(out=m1, in_=esel, axis=AX.X, op=ALU.max), R, W_)
            dve.op(lambda e: e.tensor_tensor(out=oh1, in0=esel, in1=bc3(m1), op=ALU.is_equal), R, W_)
            dve.op(lambda e: e.scalar_tensor_tensor(out=e2, in0=oh1, scalar=-1e30, in1=esel, op0=ALU.mult, op1=ALU.add), R, W_)
            dve.op(lambda e: e.tensor_reduce(out=m2, in_=e2, axis=AX.X, op=ALU.max), R, W_)
            dve.op(lambda e: e.tensor_tensor(out=e2, in0=e2, in1=bc3(m2), op=ALU.is_equal), R, W_)
            dve.op(lambda e: e.tensor_tensor(out=p1a, in0=m1, in1=m2, op=ALU.subtract), R, W_ + [d_pp])
            act.op(lambda e: e.activation(out=p1a, in_=p1a, func=AF.Sigmoid), R + [d_pp], W_ + [d_pp])
            dve.op(lambda e: e.tensor_scalar(out=p2a, in0=p1a, scalar1=-1.0, scalar2=1.0, op0=ALU.mult, op1=ALU.add), R + [d_pp], W_ + [d_pp])
            dve.op(lambda e: e.tensor_tensor(out=p1a, in0=p1a, in1=gw, op=ALU.mult), R + [d_pp], W_ + [d_pp])
            dve.op(lambda e: e.tensor_tensor(out=p2a, in0=p2a, in1=gw, op=ALU.mult), R + [d_pp], W_ + [d_pp])
            dve.op(lambda e: e.tensor_tensor(out=v4(sel1), in0=goh.unsqueeze(3).to_broadcast([128, NT_, 4, 4]),
                                             in1=oh1.unsqueeze(2).to_broadcast([128, NT_, 4, 4]), op=ALU.mult), R, W_)
            dve.op(lambda e: e.tensor_tensor(out=v4(sel2), in0=goh.unsqueeze(3).to_broadcast([128, NT_, 4, 4]),
                                             in1=e2.unsqueeze(2).to_broadcast([128, NT_, 4, 4]), op=ALU.mult), R, W_)
            dve.op(lambda e: e.tensor_tensor(out=ind, in0=sel1, in1=sel2, op=ALU.add), R, W_)
            pw, pdw = nb()
            mm(pw[:, 0:256], LT, ind, True, True, [d_rt, d_c8], [pdw])
            pt_, pdt = nb()
            mm(pt_[:, 0:256], ones1, ind, True, True, [d_rt, d_const], [pdt])
            dve.op(lambda e: e.tensor_copy(out=tot, in_=r3(pt_[:, 0:256], NT_)), R + [pdt], W_)
            dve.op(lambda e: e.memset(tcum[:, 0, :], 0.0), R, W_)
            for tt in range(1, NT_):
                dve.op(lambda e: e.tensor_tensor(out=tcum[:, tt, :], in0=tcum[:, tt - 1, :], in1=tot[:, tt - 1, :], op=ALU.add), R, W_)
            dve.op(lambda e: e.tensor_tensor(out=cnt, in0=tcum[:, NT_ - 1, :], in1=tot[:, NT_ - 1, :], op=ALU.add), R, W_)
            dve.op(lambda e: e.tensor_tensor(out=r3(c8, 16), in0=cnt.unsqueeze(2).to_broadcast([128, 16, 8]),
                                             in1=thr8.unsqueeze(1).to_broadcast([128, 16, 8]), op=ALU.is_gt), R, W_)
            dve.op(lambda e: e.tensor_reduce(out=nsl, in_=r3(c8, 16), axis=AX.X, op=ALU.add), R, W_)
            dve.op(lambda e: e.memset(bsl[:, 0:1], 0.0), R, W_)
            for ex in range(1, 16):
                dve.op(lambda e: e.tensor_tensor(out=bsl[:, ex:ex + 1], in0=bsl[:, ex - 1:ex], in1=nsl[:, ex - 1:ex], op=ALU.add), R, W_)
            dve.op(lambda e: e.tensor_tensor(out=total, in0=bsl[:, 15:16], in1=nsl[:, 15:16], op=ALU.add), R, W_)
            dve.op(lambda e: e.tensor_scalar(out=bsl256, in0=bsl, scalar1=float(SL), scalar2=None, op0=ALU.mult), R, W_)
            dve.op(lambda e: e.tensor_tensor(out=posall, in0=pw[:, 0:256], in1=tcum.rearrange("p t e -> p (t e)"), op=ALU.add), R + [pdw], W_)
            dve.op(lambda e: e.tensor_tensor(out=r3(posall, NT_), in0=r3(posall, NT_), in1=bsl256.unsqueeze(1).to_broadcast([128, NT_, 16]), op=ALU.add), R, W_)
            dve.op(lambda e: e.tensor_tensor(out=ptmp, in0=posall, in1=sel1, op=ALU.mult), R, W_)
            dve.op(lambda e: e.tensor_reduce(out=pos1f, in_=r3(ptmp, NT_), axis=AX.X, op=ALU.add), R, W_)
            dve.op(lambda e: e.tensor_tensor(out=ptmp, in0=posall, in1=sel2, op=ALU.mult), R, W_)
            dve.op(lambda e: e.tensor_reduce(out=pos2f, in_=r3(ptmp, NT_), axis=AX.X, op=ALU.add), R, W_)
            dve.op(lambda e: e.tensor_copy(out=pos1i, in_=pos1f), R, W_ + [d_pos])
            dve.op(lambda e: e.tensor_copy(out=pos2i, in_=pos2f), R, W_ + [d_pos])
            dve.op(lambda e: e.tensor_tensor(out=r3(es32, NSLOT), in0=bsl.unsqueeze(1).to_broadcast([128, NSLOT, 16]),
                                             in1=siota[:, 0:NSLOT].unsqueeze(2).to_broadcast([128, NSLOT, 16]), op=ALU.is_le), R, W_)
            dve.op(lambda e: e.tensor_reduce(out=esf, in_=r3(es32, NSLOT), axis=AX.X, op=ALU.add), R, W_)
            dve.op(lambda e: e.tensor_scalar(out=unused, in0=siota[:, 0:NSLOT], scalar1=total[:, 0:1], scalar2=1.0e6, op0=ALU.is_ge, op1=ALU.mult), R, W_)
            dve.op(lambda e: e.tensor_scalar(out=wbase, in0=esf, scalar1=-1.0, scalar2=512.0, op0=ALU.add, op1=ALU.mult), R, W_)
            dve.op(lambda e: e.tensor_tensor(out=wbase, in0=wbase, in1=unused, op=ALU.add), R, W_)
            dve.op(lambda e: e.tensor_scalar(out=wbase, in0=wbase, scalar1=iop[:, 0:1], scalar2=None, op0=ALU.add), R, W_)
            for q in range(4):
                dve.op(lambda e: e.tensor_scalar(out=widxf[:, :, q], in0=wbase, scalar1=float(128 * q), scalar2=None, op0=ALU.add), R, W_)
            dve.op(lambda e: e.tensor_copy(out=widx, in_=widxf.rearrange("p s q -> p (s q)")), R, W_ + [d_widx])
            if "route" in dbg_aps:
                sp.dma(dbg_aps["route"][:, 0:16], pos1f, reads=[d_rt])
                sp.dma(dbg_aps["route"][:, 16:32], pos2f, reads=[d_rt])
                sp.dma(dbg_aps["route"][:, 32:64], wbase, reads=[d_rt])
                sp.dma(dbg_aps["route"][:, 64:80], p1a, reads=[d_pp])
                sp.dma(dbg_aps["route"][:, 80:96], p2a, reads=[d_pp])
                sp.dma(dbg_aps["route"][:, 96:112], cnt, reads=[d_rt])
            for tt in range(16):
                for posi in (pos1i, pos2i):
                    pool.dma_fn(lambda e: e.indirect_dma_start(out=Xs, out_offset=IOA(ap=posi[:, tt:tt + 1], axis=0), in_=xnb_all[:, tt, :], in_offset=None,
                                                               bounds_check=BCV, oob_is_err=False),
                                reads=[d_xnb[tt], d_pos], writes=[d_Xs])
            fw.barrier()
            ald = Alloc(big, P8_TOP, WORDS)
            wbufs = [(ald.bf16(8192), [Dep() for _ in range(4)]) for _ in range(6)]
            xsls = [(r3(ald.bf16(NA * D), NA), Dep()) for _ in range(2)]
            XTs = [(r3(ald.bf16(16 * SL), 16), Dep()) for _ in range(2)]
            hids = [(r3(ald.bf16(4 * SL), 4), Dep()) for _ in range(2)]
            sbs = [(ald.bf16(SL), Dep()) for _ in range(2)]
            yos = [(ald.f32(D), Dep()) for _ in range(2)]
            cnt8 = dict(yi=0, ei=0)

            def wload(i, s):
                wsl = []
                for m, wl in enumerate((wg_l, wu_l, wd_l)):
                    buf, deps = wbufs[(3 * i + m) % 6]
                    for q in range(4):
                        pool.dma_fn(lambda e: e.indirect_dma_start(out=buf[:, q * 2048:(q + 1) * 2048], out_offset=None, in_=wl,
                                                                   in_offset=IOA(ap=widx[:, s * 4 + q:s * 4 + q + 1], axis=0), bounds_check=BWV, oob_is_err=False),
                                    reads=[d_widx], writes=[deps[q]])
                    wsl.append((buf, deps))
                return wsl

            def xload(i, s):
                xsl, dxs = xsls[i % 2]
                sp.dma(xsl, Xs[s * SL:(s + 1) * SL, :].rearrange("(a p) n -> p a n", p=128), reads=[d_Xs], writes=[dxs])

            def emit_T(i, s):
                xsl, dxs = xsls[i % 2]
                XT, dXT = XTs[i % 2]
                for a in range(NA):
                    for q4 in range(4):
                        pb, pd = nb()
                        for j in range(4):
                            kc = q4 * 4 + j
                            mm(pb[:, j * 128:(j + 1) * 128], xsl[:, a, kc * 128:(kc + 1) * 128], identb, True, True, [dxs, d_const], [pd])
                        cnt8["ei"] += 1
                        if cnt8["ei"] % 2 == 0:
                            act.op(lambda e: e.activation(out=XT[:, q4 * 4:q4 * 4 + 4, a * 128:(a + 1) * 128], in_=r3(pb, 4), func=AF.Copy), reads=[pd], writes=[dXT])
                        else:
                            dve.op(lambda e: e.tensor_copy(out=XT[:, q4 * 4:q4 * 4 + 4, a * 128:(a + 1) * 128], in_=r3(pb, 4)), reads=[pd], writes=[dXT])

            def emit_GU(i, s, wsl):
                wg, dwg = r3(wsl[0][0], 16), wsl[0][1]
                wu, dwu = r3(wsl[1][0], 16), wsl[1][1]
                XT, dXT = XTs[i % 2]
                hid, dhid = hids[i % 2]
                for ffc in range(4):
                    pg, pdg = nb()
                    for kc in range(KC):
                        mm(pg[:, 0:SL], wg[:, kc, ffc * 128:(ffc + 1) * 128], XT[:, kc, :], kc == 0, kc == KC - 1, [dwg[kc // 4], dXT], [pdg])
                    pu, pdu = nb()
                    for kc in range(KC):
                        mm(pu[:, 0:SL], wu[:, kc, ffc * 128:(ffc + 1) * 128], XT[:, kc, :], kc == 0, kc == KC - 1, [dwu[kc // 4], dXT], [pdu])
                    sb_, dsb = sbs[ffc % 2]
                    act.op(lambda e: e.activation(out=sb_, in_=pg[:, 0:SL], func=AF.Silu), reads=[pdg], writes=[dsb])
                    dve.op(lambda e: e.tensor_tensor(out=hid[:, ffc, :], in0=pu[:, 0:SL], in1=sb_, op=ALU.mult), reads=[pdu, dsb], writes=[dhid])

            def emit_D(i, s, wsl):
                wd, dwd = r3(wsl[2][0], 4), wsl[2][1]
                hid, dhid = hids[i % 2]
                for a in range(NA):
                    yo, dyo = yos[cnt8["yi"] % 2]
                    cnt8["yi"] += 1
                    for dblk in range(4):
                        ds_ = slice(dblk * 512, (dblk + 1) * 512)
                        pb, pd = nb()
                        for ffc in range(4):
                            mm(pb, hid[:, ffc, a * 128:(a + 1) * 128], wd[:, ffc, ds_], ffc == 0, ffc == 3, [dhid, dwd[ffc]], [pd])
                        if dblk % 2 == 0:
                            act.op(lambda e: e.activation(out=yo[:, ds_], in_=pb, func=AF.Copy), reads=[pd], writes=[dyo])
                        else:
                            dve.op(lambda e: e.tensor_copy(out=yo[:, ds_], in_=pb), reads=[pd], writes=[dyo])
                    r0 = s * SL + a * 128
                    sp.dma(Ys[r0:r0 + 128, :], yo, reads=[dyo], writes=[d_Ys])

            lo_n = NSLOT - NSLOT // 3
            lo, hi = list(range(lo_n)), list(range(NSLOT - 1, lo_n - 1, -1))
            order = []
            while lo or hi:
                order += lo[:2]
                lo = lo[2:]
                if hi:
                    order.append(hi.pop(0))
            assert sorted(order) == list(range(NSLOT))
            xload(0, order[0])
            emit_T(0, order[0])
            for i, s in enumerate(order):
                wsl = wload(i, s)
                if i + 1 < NSLOT:
                    xload(i + 1, order[i + 1])
                emit_GU(i, s, wsl)
                if i + 1 < NSLOT:
                    emit_T(i + 1, order[i + 1])
                emit_D(i, s, wsl)
            fw.barrier()
            ale = Alloc(big, P8_TOP, WORDS)
            cts = [(ale.f32(D), Dep()) for _ in range(2)]
            y1s = [(ale.f32(D), Dep()) for _ in range(2)]
            y2s = [(ale.f32(D), Dep()) for _ in range(2)]
            junk2 = ale.bf16(D)
            sts2 = [(ale.f32(8), Dep()) for _ in range(4)]
            load_gB(4)
            for tt in range(16):
                xt, dx = cts[tt % 2]
                y1, dy1 = y1s[tt % 2]
                y2, dy2 = y2s[tt % 2]
                st, d_st = sts2[tt % 4]
                pool.dma(xt, h2_s[tt * 128:(tt + 1) * 128, :], writes=[dx])
                pool.dma_fn(lambda e: e.indirect_dma_start(out=y1, out_offset=None, in_=Ys, in_offset=IOA(ap=pos1i[:, tt:tt + 1], axis=0),
                                                           bounds_check=BCV, oob_is_err=False), reads=[d_Ys, d_pos], writes=[dy1])
                pool.dma_fn(lambda e: e.indirect_dma_start(out=y2, out_offset=None, in_=Ys, in_offset=IOA(ap=pos2i[:, tt:tt + 1], axis=0),
                                                           bounds_check=BCV, oob_is_err=False), reads=[d_Ys, d_pos], writes=[dy2])
                dve.op(lambda e: e.scalar_tensor_tensor(out=xt, in0=y1, scalar=p1a[:, tt:tt + 1], in1=xt, op0=ALU.mult, op1=ALU.add),
                       reads=[dy1, dx, d_pp], writes=[dx])
                dve.op(lambda e: e.scalar_tensor_tensor(out=xt, in0=y2, scalar=p2a[:, tt:tt + 1], in1=xt, op0=ALU.mult, op1=ALU.add),
                       reads=[dy2, dx, d_pp], writes=[dx])
                if "h3" in dbg_aps:
                    sp.dma(dbg_aps["h3"][tt * 128:(tt + 1) * 128, :], xt, reads=[dx])
                act.op(lambda e: e.activation(out=junk2, in_=xt, func=AF.Square, accum_out=st[:, 0:1]), reads=[dx], writes=[d_junk, d_st])
                dve.op(lambda e: e.tensor_scalar(out=st[:, 1:2], in0=st[:, 0:1], scalar1=1.0 / D, scalar2=1e-6, op0=ALU.mult, op1=ALU.add),
                       reads=[d_st], writes=[d_st])
                act.op(lambda e: e.activation(out=st[:, 2:3], in_=st[:, 1:2], func=AF.Sqrt), reads=[d_st], writes=[d_st])
                dve.op(lambda e: e.reciprocal(out=st[:, 3:4], in_=st[:, 2:3]), reads=[d_st], writes=[d_st])
                dve.op(lambda e: e.scalar_tensor_tensor(out=xt, in0=xt, scalar=st[:, 3:4], in1=gBt, op0=ALU.mult, op1=ALU.mult),
                       reads=[dx, d_st, d_gB], writes=[dx])
                sp.dma(out[tt * 128:(tt + 1) * 128, :], xt, reads=[dx])
            fw.barrier()

        fw.barrier()
    return nc


def host_consts(inp):
    l = 0
    f = np.float32
    gBh = np.stack([np.broadcast_to(v, (128, D)) for v in (inp["norm_mix_g"][l], inp["norm_xattn_g"][l], inp["norm_ffn_g"][l],
                                                            inp["norm_mem_g"][l], inp["norm_final_g"])]).astype(f)
    pvh = np.zeros((128, NPV), f)

    def col(v, n):
        return np.ascontiguousarray(np.asarray(v, f).reshape(n, 128).T)
    pvh[:, 0:8] = col(inp["pool_scale"][l], 8)
    mu = np.asarray(inp["rwkv_mu"][l], f)
    pvh[:, 8:32] = col(mu[0:3072], 24)
    pvh[:, 32] = mu[3072:3200]
    pvh[:, 33] = mu[3200:3328]
    pvh[0:32, 34] = mu[3328:3360]
    pvh[:, 35:43] = col(inp["rwkv_w0"][l], 8)
    pvh[:, 43:51] = col(inp["rwkv_a0"][l], 8)
    pvh[:, 51:59] = col(inp["rwkv_k_k"][l], 8)
    pvh[:, 59:67] = col(inp["rwkv_k_a"][l], 8)
    pvh[:, 67:75] = col(inp["rwkv_ln_w"][l], 8)
    pvh[:, 75:83] = col(inp["rwkv_ln_b"][l], 8)
    pvh[:, 83:91] = col(np.asarray(inp["rwkv_r_k"][l]).reshape(-1), 8)
    cst = np.zeros((128, 1024), f)
    p = np.arange(128)
    cst[:, 0:128] = np.eye(128, dtype=f)
    cst[:, 128:256] = (p[:, None] // 64 == p[None, :] // 64).astype(f)
    s = p % 64
    tcol = np.arange(64)
    strict = (s[:, None] < tcol[None, :]).astype(f)
    incl = (s[:, None] <= tcol[None, :]).astype(f)
    one = np.concatenate([strict, incl], 1)
    cst[:, 256:512] = np.concatenate([one, one], 1)
    low = (s[:, None] > tcol[None, :]).astype(f)
    cst[:, 512:640] = np.concatenate([low, low], 1)
    cst[:, 640:704] = (s[:, None] == tcol[None, :]).astype(f)
    tt = np.arange(16)
    for gi, w in enumerate((2, 4, 8, 16)):
        cst[:, 704 + gi * 16:704 + (gi + 1) * 16] = (1.0 / np.minimum(tt + 1, w)).astype(f)[None, :]
    cst[:, 768:896] = (p[:, None] < p[None, :]).astype(f)
    cst[:, 896] = p.astype(f)
    cst[:, 897:929] = np.arange(32, dtype=f)[None, :]
    cst[:, 929:937] = (float(SL) * np.arange(8, dtype=f))[None, :]
    rm = np.ones((128, T), f)
    rm[:, ::64] = 0.0
    w_r = np.concatenate([inp["moe_w_group"][l], inp["moe_w_expert"][l]], 1).astype(f)
    b_r = np.concatenate([inp["moe_b_group"][l], inp["moe_b_expert"][l]])[None, :].astype(f)
    return dict(gB=gBh, pv=pvh, cst=cst, rmask=rm, w_r=np.ascontiguousarray(w_r), b_r=b_r)


def make_in_maps(inp, cores):
    l = 0
    c = host_consts(inp)
    shared = dict(
        w_in=inp["w_in"][l], pool_w=inp["pool_w"][l], w2=inp["rwkv_w2"][l], a2=inp["rwkv_a2"][l], g2=inp["rwkv_g2"][l],
        w_out=inp["w_out"][l], w_q=inp["xattn_w_q"][l], w_kv=inp["xattn_w_kv"][l], w_o=inp["xattn_w_o"][l],
        wg_l=np.asarray(inp["moe_w_gate"][l], np.float32).reshape(16, 4, 4, 128, 512).transpose(0, 1, 3, 2, 4).reshape(8192, 2048),
        wu_l=np.asarray(inp["moe_w_up"][l], np.float32).reshape(16, 4, 4, 128, 512).transpose(0, 1, 3, 2, 4).reshape(8192, 2048),
        wd_l=np.asarray(inp["moe_w_down"][l], np.float32).reshape(8192, 2048), **c)
    shared = {k: np.ascontiguousarray(np.asarray(v, np.float32)) for k, v in shared.items()}
    maps = []
    for b in cores:
        m = dict(shared)
        m["x"] = np.ascontiguousarray(inp["x"][b])
        m["mem"] = np.ascontiguousarray(inp["mem"][b])
        maps.append(m)
    return maps


def kernel(**inputs):
    inp = {k: np.asarray(v) for k, v in inputs.items()}
    nc = build()
    maps = make_in_maps(inp, list(range(8)))
    res = run_bass_kernel_spmd(nc, maps, core_ids=list(range(8)))
    return np.stack([np.asarray(r["out"]) for r in res.results], 0).astype(np.float32)
```

```python
import numpy as np
import concourse.bass as bass
import concourse.mybir as mybir
from concourse.bass_utils import run_bass_kernel_spmd
from contextlib import ExitStack

F32 = mybir.dt.float32
BF16 = mybir.dt.bfloat16
I32 = mybir.dt.int32
AF = mybir.ActivationFunctionType
ALU = mybir.AluOpType
AX = mybir.AxisListType

D = 2048
KC = 16
T = 2048
NTT = T // 128
NTQ = T // 512
NCH = T // 64
PAD = 16
CDEC = float(np.exp(-0.5))
WORDS = 51200
NSLOT, SL = 26, 384
NA = SL // 128


class Dep:
    __slots__ = ("w", "r")

    def __init__(self):
        self.w = None
        self.r = {}


class Eng:
    def __init__(self, fw, name, b, is_pe=False):
        self.fw, self.name, self.b, self.is_pe = fw, name, b, is_pe
        self.sem = fw.new_sem(name)
        self.cnt = 0
        self.waited = {}
        self.dma_slots = None
        self.dma_i = 0

    def _wait(self, tok):
        sem, val = tok
        if self.waited.get(id(sem), 0) < val:
            self.b.wait_ge(sem, val)
            self.waited[id(sem)] = val

    def _collect(self, reads, writes):
        for d in reads:
            if d.w is not None and not (self.is_pe and d.w[0] is self.sem):
                self._wait(d.w)
        for d in writes:
            if d.w is not None and not (self.is_pe and d.w[0] is self.sem):
                self._wait(d.w)
            for t in d.r.values():
                if not (self.is_pe and t[0] is self.sem):
                    self._wait(t)

    def op(self, fn, reads=(), writes=()):
        self._collect(reads, writes)
        inst = fn(self.b)
        self.cnt += 1
        inst.then_inc(self.sem, 1)
        tok = (self.sem, self.cnt)
        for d in reads:
            d.r[id(self.sem)] = tok
        for d in writes:
            d.w = tok
            d.r = {}
        return tok

    def dma(self, out, in_, reads=(), writes=(), **kw):
        return self.dma_fn(lambda e: e.dma_start(out=out, in_=in_, **kw), reads, writes)

    def dma_fn(self, fn, reads=(), writes=()):
        if self.dma_slots is None:
            self.dma_slots = [[self.fw.new_sem(f"{self.name}_d{i}"), 0] for i in range(8)]
        self._collect(reads, writes)
        slot = self.dma_slots[self.dma_i % len(self.dma_slots)]
        self.dma_i += 1
        if slot[1] > 0:
            self._wait((slot[0], slot[1]))
        inst = fn(self.b)
        slot[1] += 16
        inst.then_inc(slot[0], 16)
        tok = (slot[0], slot[1])
        for d in reads:
            d.r[id(slot[0])] = tok
        for d in writes:
            d.w = tok
            d.r = {}
        return tok


class FW:
    def __init__(self, nc, es):
        self.nc, self.es = nc, es
        self.pe = Eng(self, "pe", nc.tensor, True)
        self.act = Eng(self, "act", nc.scalar)
        self.dve = Eng(self, "dve", nc.vector)
        self.pool = Eng(self, "pool", nc.gpsimd)
        self.sp = Eng(self, "sp", nc.sync)
        self.engs = [self.pe, self.act, self.dve, self.pool, self.sp]

    def new_sem(self, name):
        return self.es.enter_context(self.nc.semaphore(name))

    def barrier(self):
        toks = []
        for e in self.engs:
            if e.cnt > 0:
                toks.append((e.sem, e.cnt))
            if e.dma_slots:
                for s in e.dma_slots:
                    if s[1] > 0:
                        toks.append((s[0], s[1]))
        for e in self.engs:
            for t in toks:
                if t[0] is not e.sem:
                    e._wait(t)


class Alloc:
    def __init__(self, big, start, end):
        self.big, self.top, self.end = big, start, end

    def f32(self, n):
        a = self.big[:, self.top:self.top + n]
        self.top += n
        assert self.top <= self.end, (self.top, self.end)
        return a

    def bf16(self, n):
        w = (n + 1) // 2
        a = self.big[:, self.top:self.top + w].bitcast(BF16)
        self.top += w
        assert self.top <= self.end, (self.top, self.end)
        return a[:, 0:n]


def r3(ap, a):
    return ap.rearrange("p (a b) -> p a b", a=a)


PV = dict(pool_scale=0, mu_rkv=8, mu_lo=32, w0=35, a0=43, k_k=51, k_a=59, ln_w=67, ln_b=75, r_k=83, omka=91)
NPV = 99


def build(stop=99, dbg=()):
    nc = bass.Bass("TRN2", target_bir_lowering=False)

    def din(name, shape):
        return nc.dram_tensor(name, list(shape), F32, kind="ExternalInput").ap()

    x = din("x", [T, D])
    mem = din("mem", [256, D])
    w_in = din("w_in", [D, 4384])
    pool_w = din("pool_w", [4, 256, 256])
    w2 = din("w2", [64, 1024])
    a2 = din("a2", [64, 1024])
    g2 = din("g2", [160, 1024])
    w_out = din("w_out", [D, D])
    w_q = din("w_q", [D, D])
    w_kv = din("w_kv", [D, 2 * D])
    w_o = din("w_o", [D, D])
    w_r = din("w_r", [D, 20])
    b_r = din("b_r", [1, 20])
    wg_l = din("wg_l", [8192, 2048])
    wu_l = din("wu_l", [8192, 2048])
    wd_l = din("wd_l", [8192, 2048])
    gB = din("gB", [5, 128, D])
    pvd = din("pv", [128, NPV])
    cst = din("cst", [128, 1024])
    rmask_d = din("rmask", [128, T])
    out = nc.dram_tensor("out", [T, D], F32, kind="ExternalOutput").ap()
    dbg_aps = {}
    for name, shape in dbg:
        dbg_aps[name] = nc.dram_tensor(name, list(shape), F32, kind="ExternalOutput").ap()
    rkv_s = nc.dram_tensor("rkv_s", [24, 128, T], F32, kind="Internal").ap()
    h1_s = nc.dram_tensor("h1_s", [T, D], F32, kind="Internal").ap()
    h2_s = nc.dram_tensor("h2_s", [T, D], F32, kind="Internal").ap()
    NROW = NSLOT * SL
    Xs = nc.dram_tensor("Xs", [NROW, D], BF16, kind="Internal").ap()
    Ys = nc.dram_tensor("Ys", [NROW, D], F32, kind="Internal").ap()

    with ExitStack() as es:
        fw = FW(nc, es)
        pe, act, dve, pool, sp = fw.pe, fw.act, fw.dve, fw.pool, fw.sp
        big = es.enter_context(nc.sbuf_tensor("big", [128, WORDS], F32))[:]
        banks = [(es.enter_context(nc.psum_tensor(f"bk{i}", [128, 512], F32))[:], Dep()) for i in range(8)]
        bki = [0]

        def nb():
            b = banks[bki[0] % 8]
            bki[0] += 1
            return b

        def mm(o, lhsT, rhs, start, stop, reads, writes):
            pe.op(lambda e: e.matmul(o, lhsT=lhsT, rhs=rhs, start=start, stop=stop), reads, writes)

        CONST_W = 7424
        ca = Alloc(big, 0, CONST_W)
        identf = ca.f32(128)
        blk1 = ca.f32(128)
        mSI = ca.f32(256)
        mL = ca.f32(128)
        i64 = ca.f32(64)
        rcnt = ca.f32(64)
        pv = ca.f32(NPV + 1)
        identb = ca.bf16(128)
        onesb = ca.bf16(128)
        rmask = ca.bf16(T)
        gBt = ca.f32(D)
        lo1 = ca.bf16(T)
        sg1 = ca.bf16(T)
        sg2 = ca.bf16(T)
        ones1 = ca.f32(128)
        brow = ca.f32(20)
        d_const, d_gB, d_lo1, d_sg1, d_sg2 = Dep(), Dep(), Dep(), Dep(), Dep()
        B0_OFF = CONST_W
        B1_OFF = B0_OFF + 8192
        M_OFF = B1_OFF + 8192
        A_OFF = WORDS - 16384
        bufA = r3(big[:, A_OFF:WORDS].bitcast(BF16), 16)
        bufB = r3(big[:, B0_OFF:M_OFF].bitcast(BF16), 16)
        d_A, d_B = Dep(), Dep()

        sp.dma(identf, cst[:, 0:128], writes=[d_const])
        sp.dma(blk1, cst[:, 128:256], writes=[d_const])
        sp.dma(mSI, cst[:, 256:512], writes=[d_const])
        sp.dma(mL, cst[:, 512:640], writes=[d_const])
        sp.dma(i64, cst[:, 640:704], writes=[d_const])
        sp.dma(rcnt, cst[:, 704:768], writes=[d_const])
        sp.dma(pv[:, 0:NPV], pvd, writes=[d_const])
        sp.dma(brow[0:1, :], b_r, writes=[d_const])
        pool.dma(identb, cst[:, 0:128], writes=[d_const])
        pool.dma(rmask, rmask_d, writes=[d_const])
        pool.op(lambda e: e.memset(onesb, 1.0), writes=[d_const])
        pool.op(lambda e: e.memset(ones1, 1.0), writes=[d_const])
        dve.op(lambda e: e.tensor_scalar(out=pv[:, PV["omka"]:PV["omka"] + 8], in0=pv[:, PV["k_a"]:PV["k_a"] + 8],
                                         scalar1=-1.0, scalar2=1.0, op0=ALU.mult, op1=ALU.add), reads=[d_const], writes=[d_const])
        fw.barrier()

        def pvc(name, j):
            c = PV[name] + j
            return pv[:, c:c + 1]

        d_Xs, d_Ys = Dep(), Dep()
        zf = [0]

        def zero_fill(n, zt, dz):
            while n > 0 and zf[0] < NROW // 128 and stop >= 8:
                c = zf[0]
                pool.dma(Xs[c * 128:(c + 1) * 128, :], zt, reads=[dz])
                zf[0] += 1
                n -= 1

        def load_gB(i):
            sp.dma(gBt, gB[i], writes=[d_gB])

        def norm_tiles(al, ntiles, src_fn, dstT, d_dst, tok_off=0, keep=None):
            xts = [(al.f32(D), Dep()) for _ in range(2)]
            xns = [(al.bf16(D), Dep()) for _ in range(2)]
            junk = al.bf16(D)
            d_junk = Dep()
            sts = [(al.f32(8), Dep()) for _ in range(4)]
            for i in range(ntiles):
                xt, dx = xts[i % 2]
                xn, dn = xns[i % 2]
                st, d_st = sts[i % 4]
                sp.dma(xt, src_fn(i), writes=[dx])
                if keep is not None:
                    keep(i, xt, dx)
                act.op(lambda e: e.activation(out=junk, in_=xt, func=AF.Square, accum_out=st[:, 0:1]), reads=[dx], writes=[d_junk, d_st])
                dve.op(lambda e: e.tensor_scalar(out=st[:, 1:2], in0=st[:, 0:1], scalar1=1.0 / D, scalar2=1e-6, op0=ALU.mult, op1=ALU.add),
                       reads=[d_st], writes=[d_st])
                act.op(lambda e: e.activation(out=st[:, 2:3], in_=st[:, 1:2], func=AF.Sqrt), reads=[d_st], writes=[d_st])
                dve.op(lambda e: e.reciprocal(out=st[:, 3:4], in_=st[:, 2:3]), reads=[d_st], writes=[d_st])
                dve.op(lambda e: e.scalar_tensor_tensor(out=xn, in0=xt, scalar=st[:, 3:4], in1=gBt, op0=ALU.mult, op1=ALU.mult),
                       reads=[dx, d_st, d_gB], writes=[dn])
                for q in range(4):
                    pb, pd = nb()
                    for j in range(4):
                        kc = q * 4 + j
                        mm(pb[:, j * 128:(j + 1) * 128], xn[:, kc * 128:(kc + 1) * 128], identb, True, True, [dn, d_const], [pd])
                    t0 = tok_off + i * 128
                    eng = act if q % 2 == 0 else dve
                    if eng is act:
                        act.op(lambda e: e.activation(out=dstT[:, q * 4:q * 4 + 4, t0:t0 + 128], in_=r3(pb, 4), func=AF.Copy), reads=[pd], writes=[d_dst])
                    else:
                        dve.op(lambda e: e.tensor_copy(out=dstT[:, q * 4:q * 4 + 4, t0:t0 + 128], in_=r3(pb, 4)), reads=[pd], writes=[d_dst])

        def dump(name, ap_sb, dep, dst=None):
            if name in dbg_aps:
                sp.dma(dbg_aps[name] if dst is None else dst, ap_sb, reads=[dep])

        load_gB(0)
        al = Alloc(big, M_OFF, A_OFF)
        norm_tiles(al, NTT, lambda i: x[i * 128:(i + 1) * 128, :], bufA, d_A)
        fw.barrier()
        if "hnT" in dbg_aps:
            tmp = Alloc(big, M_OFF, A_OFF).f32(T)
            dtmp = Dep()
            for kc in range(16):
                dve.op(lambda e: e.tensor_copy(out=tmp, in_=bufA[:, kc, :]), reads=[d_A], writes=[dtmp])
                sp.dma(dbg_aps["hnT"][kc * 128:(kc + 1) * 128, :], tmp, reads=[dtmp])
            fw.barrier()

        if stop >= 2:
            al = Alloc(big, B1_OFF, A_OFF)
            wbs = [(r3(al.bf16(16 * 128), 16), Dep()) for _ in range(4)]
            wbi = [0]
            pbufs = [(al.f32(PAD + T), Dep()) for _ in range(2)]
            fbs = [(al.f32(PAD + T), Dep()) for _ in range(3)]
            pooled = [(al.bf16(T), Dep()) for _ in range(2)]
            pwb = r3(al.bf16(8 * 256), 8)
            d_pw = Dep()
            ztile = al.bf16(D)
            d_zt = Dep()
            dve.op(lambda e: e.memset(ztile, 0.0), writes=[d_zt])
            for pbf, dp in pbufs + fbs:
                dve.op(lambda e: e.memset(pbf[:, 0:PAD], 0.0), writes=[dp])
            pool.dma(pwb, pool_w.rearrange("g (cc p) d -> p (g cc) d", p=128), writes=[d_pw])
            pbi = [0]

            def proj_block(col0, n):
                ws, dw = wbs[wbi[0] % 4]
                wbi[0] += 1
                pool.dma(ws[:, :, 0:n], w_in[:, col0:col0 + n].rearrange("(kc p) n -> p kc n", p=128), writes=[dw])
                if wbi[0] > 4:
                    zero_fill(3, ztile, d_zt)
                pbf, dp = pbufs[pbi[0] % 2]
                pbi[0] += 1
                for tq in range(NTQ):
                    pb, pd = nb()
                    for kc in range(KC):
                        mm(pb[0:n, :], ws[:, kc, 0:n], bufA[:, kc, tq * 512:(tq + 1) * 512], kc == 0, kc == KC - 1, [dw, d_A], [pd])
                    act.op(lambda e: e.activation(out=pbf[0:n, PAD + tq * 512:PAD + (tq + 1) * 512], in_=pb[0:n, :], func=AF.Copy), reads=[pd], writes=[dp])
                return pbf, dp

            def tshift(pbf, dp, n, mu_ap, zout, dz):
                f0, df0 = fbs[0]
                dve.op(lambda e: e.tensor_tensor(out=f0[0:n, 0:T], in0=pbf[0:n, PAD - 1:PAD - 1 + T], in1=pbf[0:n, PAD:PAD + T], op=ALU.subtract),
                       reads=[dp], writes=[df0])
                dve.op(lambda e: e.scalar_tensor_tensor(out=zout, in0=f0[0:n, 0:T], scalar=mu_ap, in1=pbf[0:n, PAD:PAD + T], op0=ALU.mult, op1=ALU.add),
                       reads=[df0, dp, d_const], writes=[dz])

            z1f, dz1 = fbs[1]
            z1 = z1f[:, PAD:PAD + T]
            pbf, dp = proj_block(4096, 128)
            tshift(pbf, dp, 128, pv[:, PV["mu_lo"]:PV["mu_lo"] + 1], z1, dz1)
            act.op(lambda e: e.activation(out=lo1[0:64, :], in_=z1[0:64, :], func=AF.Tanh), reads=[dz1], writes=[d_lo1])
            act.op(lambda e: e.activation(out=lo1[64:128, :], in_=z1[64:128, :], func=AF.Copy), reads=[dz1], writes=[d_lo1])
            pbf, dp = proj_block(4224, 128)
            tshift(pbf, dp, 128, pv[:, PV["mu_lo"] + 1:PV["mu_lo"] + 2], z1, dz1)
            act.op(lambda e: e.activation(out=sg1, in_=z1, func=AF.Sigmoid), reads=[dz1], writes=[d_sg1])
            pbf, dp = proj_block(4352, 32)
            tshift(pbf, dp, 32, pv[0:32, PV["mu_lo"] + 2:PV["mu_lo"] + 3], z1[0:32, :], dz1)
            act.op(lambda e: e.activation(out=sg2[0:32, :], in_=z1[0:32, :], func=AF.Sigmoid), reads=[dz1], writes=[d_sg2])
            for j in range(24):
                pbf, dp = proj_block(1024 + j * 128, 128)
                tshift(pbf, dp, 128, pvc("mu_rkv", j), z1, dz1)
                sp.dma(rkv_s[j], z1, reads=[dz1])
            for cb in range(8):
                gi = cb // 2
                w = (2, 4, 8, 16)[gi]
                pbf, dp = proj_block(cb * 128, 128)
                (fa, dfa), (fb_, dfb) = fbs[1], fbs[2]
                src, dsrc = pbf, dp
                sh = 1
                k = 0
                while sh < w:
                    dst, ddst = (fa, dfa) if k % 2 == 0 else (fb_, dfb)
                    dve.op(lambda e: e.tensor_tensor(out=dst[:, PAD:PAD + T], in0=src[:, PAD:PAD + T], in1=src[:, PAD - sh:PAD - sh + T], op=ALU.add),
                           reads=[dsrc], writes=[ddst])
                    src, dsrc = dst, ddst
                    sh *= 2
                    k += 1
                po, dpo = pooled[cb % 2]
                dve.op(lambda e: e.scalar_tensor_tensor(out=po, in0=src[:, PAD:PAD + T], scalar=1.0 / w, in1=pbf[:, PAD:PAD + T], op0=ALU.mult, op1=ALU.subtract),
                       reads=[dsrc, dp], writes=[dpo])
                f0, df0 = fbs[0]
                dve.op(lambda e: e.tensor_tensor(out=f0[:, 0:16], in0=src[:, PAD:PAD + 16], in1=rcnt[:, gi * 16:(gi + 1) * 16], op=ALU.mult),
                       reads=[dsrc, d_const], writes=[df0])
                dve.op(lambda e: e.tensor_tensor(out=po[:, 0:16], in0=f0[:, 0:16], in1=pbf[:, PAD:PAD + 16], op=ALU.subtract),
                       reads=[df0, dp], writes=[dpo])
                if cb % 2 == 1:
                    for db in range(2):
                        for tq in range(NTQ):
                            pb, pd = nb()
                            for cc in range(2):
                                mm(pb, pwb[:, gi * 2 + cc, db * 128:(db + 1) * 128], pooled[cc][0][:, tq * 512:(tq + 1) * 512], cc == 0, cc == 1,
                                   [d_pw, pooled[cc][1]], [pd])
                            blk = gi * 2 + db
                            act.op(lambda e: e.activation(out=bufB[:, blk, tq * 512:(tq + 1) * 512], in_=pb, func=AF.Copy, scale=pvc("pool_scale", blk)),
                                   reads=[pd, d_const], writes=[d_B])
            zero_fill(NROW, ztile, d_zt)
            fw.barrier()
            if "mixT" in dbg_aps:
                tmp = Alloc(big, B1_OFF, A_OFF).f32(T)
                dtmp = Dep()
                for kc in range(8):
                    dve.op(lambda e: e.tensor_copy(out=tmp, in_=bufB[:, kc, :]), reads=[d_B], writes=[dtmp])
                    sp.dma(dbg_aps["mixT"][kc * 128:(kc + 1) * 128, :], tmp, reads=[dtmp])
                fw.barrier()


        if stop >= 3:
            QTK = 512
            NQ = T // QTK
            al = Alloc(big, M_OFF, WORDS)
            lw = al.bf16(1024)
            g2a = al.bf16(1024)
            g2b = al.bf16(1024)
            S32 = al.f32(64)
            Sbf = al.bf16(64)
            d_lw, d_S32, d_Sbf = Dep(), Dep(), Dep()
            sets = []
            for i in range(3):
                sets.append(dict(AR=r3(al.bf16(8 * 128), 8), BK=r3(al.bf16(8 * 128), 8), BKh=r3(al.bf16(8 * 128), 8), vb=al.bf16(QTK),
                                 GL=al.f32(8), gbuf=al.bf16(QTK), bonus=al.f32(QTK), ybuf=al.f32(QTK),
                                 d_AR=Dep(), d_BK=Dep(), d_BKh=Dep(), d_vb=Dep(), d_GL=Dep(), d_g=Dep(), d_bonus=Dep(), d_y=Dep(),
                                 d_ARr=[Dep() for _ in range(4)]))
            Fq = [al.f32(QTK) for _ in range(8)]
            dFq = [Dep() for _ in range(8)]
            cbs = []
            for i in range(2):
                cbs.append(dict(NBall=r3(al.bf16(8 * 128), 8), KBall=r3(al.bf16(8 * 128), 8), TM=r3(al.bf16(8 * 320), 8), APU=r3(al.bf16(8 * 128), 8),
                                Mc=r3(al.f32(8 * 64), 8), CcT=r3(al.f32(8 * 64), 8),
                                d_NB=[Dep() for _ in range(4)], d_KB=[Dep() for _ in range(4)], d_TM=[Dep() for _ in range(4)],
                                d_APU=[Dep() for _ in range(4)], d_Mc=[Dep() for _ in range(4)], d_Cc=[Dep() for _ in range(4)]))
            TT = r3(al.bf16(8 * 64), 8)
            Wg = [[r3(al.bf16(2 * 128), 2) for _ in range(2)] for _ in range(4)]
            NTg = [[r3(al.bf16(2 * 64), 2) for _ in range(2)] for _ in range(4)]
            d_TT = [Dep() for _ in range(4)]
            dWg = [[Dep(), Dep()] for _ in range(4)]
            dNTg = [[Dep(), Dep()] for _ in range(4)]
            yc, sq, rs = al.f32(QTK), al.f32(QTK), al.f32(QTK)
            dyc, dsq, drs = Dep(), Dep(), Dep()
            pool.dma(lw[0:64, :], w2, writes=[d_lw])
            pool.dma(lw[64:128, :], a2, writes=[d_lw])
            pool.dma(g2a, g2[0:128, :], writes=[d_lw])
            pool.dma(g2b[0:32, :], g2[128:160, :], writes=[d_lw])
            HS = [slice(0, 64), slice(64, 128)]
            i64b = i64.unsqueeze(1).to_broadcast([128, 2, 64])
            pyb, pdyb = banks[7]
            nbm = [0]

            def nb7():
                b = banks[nbm[0] % 7]
                nbm[0] += 1
                return b

            def prep_gen(u):
                hp, q = divmod(u, NQ)
                S_ = sets[u % 3]
                cs = slice(hp * 128, (hp + 1) * 128)
                tsl = slice(q * QTK, (q + 1) * QTK)
                k_, sgw, alr, cum, kk, f5, f6, f7 = Fq
                dk, dsgw, dalr, dcum, dkk, df5, df6, df7 = dFq
                AR, BK, BKh, vb, GL, gbuf, bonus = S_["AR"], S_["BK"], S_["BKh"], S_["vb"], S_["GL"], S_["gbuf"], S_["bonus"]
                d_AR, d_BK, d_BKh, d_vb, d_GL, d_g, d_bonus = S_["d_AR"], S_["d_BK"], S_["d_BKh"], S_["d_vb"], S_["d_GL"], S_["d_g"], S_["d_bonus"]
                sp.dma(k_, rkv_s[8 + hp][:, tsl], writes=[dk])
                pb, pd = nb7()
                mm(pb, lw[0:64, cs], lo1[0:64, tsl], True, True, [d_lw, d_lo1], [pd])
                act.op(lambda e: e.activation(out=sgw, in_=pb, func=AF.Sigmoid, bias=pvc("w0", hp)), reads=[pd, d_const], writes=[dsgw])
                pb, pd = nb7()
                mm(pb, lw[64:128, cs], lo1[64:128, tsl], True, True, [d_lw, d_lo1], [pd])
                act.op(lambda e: e.activation(out=alr, in_=pb, func=AF.Sigmoid, bias=pvc("a0", hp)), reads=[pd, d_const], writes=[dalr])
                yield
                pb, pd = nb7()
                mm(pb, g2a[:, cs], sg1[:, tsl], True, False, [d_lw, d_sg1], [pd])
                mm(pb, g2b[0:32, cs], sg2[0:32, tsl], False, True, [d_lw, d_sg2], [pd])
                act.op(lambda e: e.activation(out=gbuf, in_=pb, func=AF.Copy), reads=[pd], writes=[d_g])
                dve.op(lambda e: e.tensor_tensor_scan(out=cum, data0=rmask[:, 0:QTK], data1=sgw, initial=0.0, op0=ALU.mult, op1=ALU.add),
                       reads=[d_const, dsgw], writes=[dcum])
                yield
                act.op(lambda e: e.activation(out=kk, in_=k_, func=AF.Copy, scale=pvc("k_k", hp)), reads=[dk, d_const], writes=[dkk])
                pool.op(lambda e: e.tensor_tensor(out=f5, in0=kk, in1=kk, op=ALU.mult), reads=[dkk], writes=[df5])
                yield
                pb, pd = nb7()
                mm(pb, blk1, f5, True, True, [d_const, df5], [pd])
                dve.op(lambda e: e.tensor_scalar(out=f6, in0=pb, scalar1=1e-24, scalar2=None, op0=ALU.max), reads=[pd], writes=[df6])
                act.op(lambda e: e.activation(out=f6, in_=f6, func=AF.Sqrt), reads=[df6], writes=[df6])
                yield
                dve.op(lambda e: e.reciprocal(out=f6, in_=f6), reads=[df6], writes=[df6])
                yield
                pool.op(lambda e: e.tensor_tensor(out=kk, in0=kk, in1=f6, op=ALU.mult), reads=[dkk, df6], writes=[dkk])
                dve.op(lambda e: e.tensor_scalar(out=f5, in0=alr, scalar1=pvc("k_a", hp), scalar2=pvc("omka", hp), op0=ALU.mult, op1=ALU.add),
                       reads=[dalr, d_const], writes=[df5])
                yield
                pool.op(lambda e: e.tensor_tensor(out=f5, in0=f5, in1=k_, op=ALU.mult), reads=[df5, dk], writes=[df5])
                pool.op(lambda e: e.tensor_tensor(out=alr, in0=alr, in1=kk, op=ALU.mult), reads=[dalr, dkk], writes=[dalr])
                yield
                act.op(lambda e: e.activation(out=f6, in_=cum, func=AF.Exp, scale=CDEC), reads=[dcum], writes=[df6])
                dve.op(lambda e: e.tensor_tensor(out=BK[:, :, 0:64], in0=r3(alr, 8), in1=r3(f6, 8), op=ALU.mult), reads=[dalr, df6], writes=[d_BK])
                pool.op(lambda e: e.tensor_tensor(out=BK[:, :, 64:128], in0=r3(f5, 8), in1=r3(f6, 8), op=ALU.mult), reads=[df5, df6], writes=[d_BK])
                yield
                dve.op(lambda e: e.tensor_tensor(out=r3(f6, 8), in0=r3(cum, 8), in1=r3(cum, 8)[:, :, 63:64].to_broadcast([128, 8, 64]), op=ALU.subtract),
                       reads=[dcum], writes=[df6])
                act.op(lambda e: e.activation(out=f6, in_=f6, func=AF.Exp, scale=CDEC), reads=[df6], writes=[df6])
                yield
                dve.op(lambda e: e.tensor_tensor(out=BKh[:, :, 0:64], in0=r3(alr, 8), in1=r3(f6, 8), op=ALU.mult), reads=[dalr, df6], writes=[d_BKh])
                pool.op(lambda e: e.tensor_tensor(out=BKh[:, :, 64:128], in0=r3(f5, 8), in1=r3(f6, 8), op=ALU.mult), reads=[df5, df6], writes=[d_BKh])
                yield
                pool.op(lambda e: e.tensor_tensor(out=f6, in0=cum, in1=sgw, op=ALU.subtract), reads=[dcum, dsgw], writes=[df6])
                act.op(lambda e: e.activation(out=f6, in_=f6, func=AF.Exp, scale=-CDEC), reads=[df6], writes=[df6])
                yield
                dve.op(lambda e: e.scalar_tensor_tensor(out=AR[:, :, 0:64], in0=r3(kk, 8), scalar=-1.0, in1=r3(f6, 8), op0=ALU.mult, op1=ALU.mult),
                       reads=[dkk, df6], writes=[d_AR])
                act.op(lambda e: e.activation(out=GL, in_=r3(cum, 8)[:, :, 63], func=AF.Exp, scale=-CDEC), reads=[dcum], writes=[d_GL])
                yield
                act.op(lambda e: e.activation(out=f6, in_=cum, func=AF.Exp, scale=-CDEC), reads=[dcum], writes=[df6])
                sp.dma(k_, rkv_s[hp][:, tsl], writes=[dk])
                pool.op(lambda e: e.tensor_tensor(out=AR[:, :, 64:128], in0=r3(k_, 8), in1=r3(f6, 8), op=ALU.mult), reads=[dk, df6], writes=[d_AR] + S_["d_ARr"])
                yield
                dve.op(lambda e: e.scalar_tensor_tensor(out=f6, in0=k_, scalar=pvc("r_k", hp), in1=f5, op0=ALU.mult, op1=ALU.mult),
                       reads=[dk, df5, d_const], writes=[df6])
                sp.dma(sgw, rkv_s[16 + hp][:, tsl], writes=[dsgw])
                yield
                pb, pd = nb7()
                mm(pb, blk1, f6, True, True, [d_const, df6], [pd])
                dve.op(lambda e: e.tensor_tensor(out=bonus, in0=pb, in1=sgw, op=ALU.mult), reads=[pd, dsgw], writes=[d_bonus])
                act.op(lambda e: e.activation(out=vb, in_=sgw, func=AF.Copy), reads=[dsgw], writes=[d_vb])
                yield

            def front_gen(u):
                hp, q = divmod(u, NQ)
                S_ = sets[u % 3]
                C_ = cbs[u % 2]
                tsl = slice(q * QTK, (q + 1) * QTK)
                AR, BK, BKh, vb, GL, gbuf, bonus, ybuf = S_["AR"], S_["BK"], S_["BKh"], S_["vb"], S_["GL"], S_["gbuf"], S_["bonus"], S_["ybuf"]
                d_AR, d_BK, d_BKh, d_vb, d_GL, d_g, d_bonus, dy = S_["d_AR"], S_["d_BK"], S_["d_BKh"], S_["d_vb"], S_["d_GL"], S_["d_g"], S_["d_bonus"], S_["d_y"]
                d_ARr = S_["d_ARr"]
                NBall, KBall, TM, APU, Mc, CcT = C_["NBall"], C_["KBall"], C_["TM"], C_["APU"], C_["Mc"], C_["CcT"]
                d_NB, d_KB, d_TM, d_APU, d_Mc, d_Cc = C_["d_NB"], C_["d_KB"], C_["d_TM"], C_["d_APU"], C_["d_Mc"], C_["d_Cc"]
                pool.op(lambda e: e.tensor_tensor(out=Mc, in0=i64.unsqueeze(1).to_broadcast([128, 8, 64]),
                                                  in1=GL.unsqueeze(2).to_broadcast([128, 8, 64]), op=ALU.mult),
                        reads=[d_const, d_GL], writes=d_Mc)
                for g in range(4):
                    l0 = 2 * g
                    pa, pda = nb7()
                    pb_, pdb = nb7()
                    pt, pdt = nb7()
                    pv_, pdv = nb7()
                    for ci in range(2):
                        c = l0 + ci
                        for h in range(2):
                            hs = HS[h]
                            mm(pa[hs, ci * 128:(ci + 1) * 128], BK[hs, c, 0:64], AR[hs, c, :], True, True, [d_BK, d_AR, d_ARr[g]], [pda])
                            mm(pb_[hs, ci * 128:(ci + 1) * 128], BK[hs, c, 64:128], AR[hs, c, :], True, True, [d_BK, d_AR, d_ARr[g]], [pdb])
                            mm(pt[hs, ci * 64:(ci + 1) * 64], AR[hs, c, 0:64], BK[hs, c, 0:64], True, True, [d_BK, d_AR], [pdt])
                            idh = identb[hs, 64 * h:64 * h + 64]
                            mm(pv_[hs, ci * 256:ci * 256 + 64], vb[hs, c * 64:(c + 1) * 64], idh, True, True, [d_vb, d_const], [pdv])
                            mm(pv_[hs, ci * 256 + 64:ci * 256 + 128], BKh[hs, c, 0:64], idh, True, True, [d_BKh, d_const], [pdv])
                            mm(pv_[hs, ci * 256 + 128:ci * 256 + 192], BKh[hs, c, 64:128], idh, True, True, [d_BKh, d_const], [pdv])
                            mm(pv_[hs, ci * 256 + 192:ci * 256 + 256], AR[hs, c, 0:64], idh, True, True, [d_AR, d_const], [pdv])
                    dve.op(lambda e: e.tensor_tensor(out=NBall[:, l0:l0 + 2, :], in0=r3(pa[:, 0:256], 2), in1=r3(mSI, 2), op=ALU.mult),
                           reads=[pda, d_const], writes=[d_NB[g]])
                    dve.op(lambda e: e.tensor_tensor(out=KBall[:, l0:l0 + 2, :], in0=r3(pb_[:, 0:256], 2), in1=r3(mSI, 2), op=ALU.mult),
                           reads=[pdb, d_const], writes=[d_KB[g]])
                    dve.op(lambda e: e.tensor_tensor(out=NTg[g][0], in0=r3(pt[:, 0:128], 2), in1=r3(mL, 2), op=ALU.mult),
                           reads=[pdt, d_const], writes=[dNTg[g][0]])
                    act.op(lambda e: e.activation(out=TM[:, l0:l0 + 2, 0:256], in_=r3(pv_, 2), func=AF.Copy), reads=[pdv], writes=[d_TM[g]])
                    pool.op(lambda e: e.tensor_tensor(out=Wg[g][0][:, :, 64:128], in0=NBall[:, l0:l0 + 2, 0:64], in1=i64b, op=ALU.add),
                            reads=[d_NB[g], d_const], writes=[dWg[g][0]])
                    yield
                for g in range(4):
                    l0 = 2 * g
                    p0, pd0 = nb7()
                    q0, qd0 = nb7()
                    NT, dNT = NTg[g][0], dNTg[g][0]
                    for ci in range(2):
                        for h in range(2):
                            hs = HS[h]
                            mm(p0[hs, ci * 64:(ci + 1) * 64], NT[hs, ci, :], NBall[hs, l0 + ci, 0:64], True, True, [dNT, d_NB[g]], [pd0])
                            mm(q0[hs, ci * 64:(ci + 1) * 64], NBall[hs, l0 + ci, 0:64], NT[hs, ci, :], True, True, [dNT, d_NB[g]], [qd0])
                    act.op(lambda e: e.activation(out=Wg[g][0][:, :, 0:64], in_=r3(p0[:, 0:128], 2), func=AF.Copy), reads=[pd0], writes=[dWg[g][0]])
                    act.op(lambda e: e.activation(out=NTg[g][1], in_=r3(q0[:, 0:128], 2), func=AF.Copy), reads=[qd0], writes=[dNTg[g][1]])
                    yield
                cur, ntc = 0, 1
                for lvl in range(1, 6):
                    last = lvl == 5
                    for g in range(4):
                        l0 = 2 * g
                        Wc, dWc = Wg[g][cur], dWg[g][cur]
                        NTc, dNTc = NTg[g][ntc], dNTg[g][ntc]
                        p1, pd1 = nb7()
                        if not last:
                            q1, qd1 = nb7()
                        for ci in range(2):
                            for h in range(2):
                                hs = HS[h]
                                if not last:
                                    mm(p1[hs, ci * 128:(ci + 1) * 128], NTc[hs, ci, :], Wc[hs, ci, :], True, True, [dNTc, dWc], [pd1])
                                    mm(q1[hs, ci * 64:(ci + 1) * 64], Wc[hs, ci, 0:64], NTc[hs, ci, :], True, True, [dNTc, dWc], [qd1])
                                else:
                                    mm(p1[hs, ci * 64:(ci + 1) * 64], NTc[hs, ci, :], Wc[hs, ci, 64:128], True, True, [dNTc, dWc], [pd1])
                        if not last:
                            Wn, dWn = Wg[g][1 - cur], dWg[g][1 - cur]
                            NTn, dNTn = NTg[g][1 - ntc], dNTg[g][1 - ntc]
                            act.op(lambda e: e.activation(out=Wn[:, :, 0:64], in_=r3(p1[:, 0:256], 2)[:, :, 0:64], func=AF.Copy), reads=[pd1], writes=[dWn])
                            dve.op(lambda e: e.tensor_tensor(out=Wn[:, :, 64:128], in0=r3(p1[:, 0:256], 2)[:, :, 64:128], in1=Wc[:, :, 64:128], op=ALU.add),
                                   reads=[pd1, dWc], writes=[dWn])
                            act.op(lambda e: e.activation(out=NTn, in_=r3(q1[:, 0:128], 2), func=AF.Copy), reads=[qd1], writes=[dNTn])
                        else:
                            dve.op(lambda e: e.tensor_tensor(out=TT[:, l0:l0 + 2, :], in0=r3(p1[:, 0:128], 2), in1=Wc[:, :, 64:128], op=ALU.add),
                                   reads=[pd1, dWc], writes=[d_TT[g]])
                        if g % 2 == 1:
                            yield
                    cur, ntc = 1 - cur, 1 - ntc
                for g in range(4):
                    l0 = 2 * g
                    pw, pdw = nb7()
                    for ci in range(2):
                        for h in range(2):
                            hs = HS[h]
                            mm(pw[hs, ci * 64:(ci + 1) * 64], KBall[hs, l0 + ci, 0:64], TM[hs, l0 + ci, 0:64], True, True, [d_KB[g], d_TM[g]], [pdw])
                    act.op(lambda e: e.activation(out=TM[:, l0:l0 + 2, 256:320], in_=r3(pw[:, 0:128], 2), func=AF.Copy), reads=[pdw], writes=[d_TM[g]])
                yield
                for g in range(4):
                    l0 = 2 * g
                    pq, pdq = nb7()
                    for ci in range(2):
                        for h in range(2):
                            hs = HS[h]
                            mm(pq[hs, ci * 128:(ci + 1) * 128], TT[hs, l0 + ci, :], TM[hs, l0 + ci, 192:320], True, True, [d_TT[g], d_TM[g]], [pdq])
                    dve.op(lambda e: e.tensor_copy(out=APU[:, l0:l0 + 2, :], in_=r3(pq[:, 0:256], 2)), reads=[pdq], writes=[d_APU[g]])
                yield
                for g in range(4):
                    l0 = 2 * g
                    pm, pdm = nb7()
                    pc, pdc = nb7()
                    pr, pdr = nb7()
                    for ci in range(2):
                        l = l0 + ci
                        for h in range(2):
                            hs = HS[h]
                            mm(pm[hs, ci * 64:(ci + 1) * 64], APU[hs, l, 0:64], TM[hs, l, 64:128], True, True, [d_APU[g], d_TM[g]], [pdm])
                            mm(pc[hs, ci * 64:(ci + 1) * 64], TM[hs, l, 64:128], APU[hs, l, 64:128], True, False, [d_APU[g], d_TM[g]], [pdc])
                            mm(pc[hs, ci * 64:(ci + 1) * 64], TM[hs, l, 128:192], TM[hs, l, 0:64], False, True, [d_TM[g]], [pdc])
                            mm(pr[hs, ci * 64:(ci + 1) * 64], APU[hs, l, 0:64], NBall[hs, l, 64:128], True, True, [d_APU[g], d_NB[g]], [pdr])
                    dve.op(lambda e: e.tensor_tensor(out=Mc[:, l0:l0 + 2, :], in0=r3(pm[:, 0:128], 2), in1=Mc[:, l0:l0 + 2, :], op=ALU.add),
                           reads=[pdm, d_Mc[g]], writes=[d_Mc[g]])
                    act.op(lambda e: e.activation(out=CcT[:, l0:l0 + 2, :], in_=r3(pc[:, 0:128], 2), func=AF.Copy), reads=[pdc], writes=[d_Cc[g]])
                    dve.op(lambda e: e.tensor_tensor(out=AR[:, l0:l0 + 2, 64:128], in0=r3(pr[:, 0:128], 2), in1=AR[:, l0:l0 + 2, 64:128], op=ALU.add),
                           reads=[pdr, d_ARr[g], d_AR], writes=[d_ARr[g]])
                    if g % 2 == 1:
                        yield
            def back_gen(u):
                hp, q = divmod(u, NQ)
                S_ = sets[u % 3]
                C_ = cbs[u % 2]
                tsl = slice(q * QTK, (q + 1) * QTK)
                AR, BK, BKh, vb, GL, gbuf, bonus, ybuf = S_["AR"], S_["BK"], S_["BKh"], S_["vb"], S_["GL"], S_["gbuf"], S_["bonus"], S_["ybuf"]
                d_AR, d_BK, d_BKh, d_vb, d_GL, d_g, d_bonus, dy = S_["d_AR"], S_["d_BK"], S_["d_BKh"], S_["d_vb"], S_["d_GL"], S_["d_g"], S_["d_bonus"], S_["d_y"]
                d_ARr = S_["d_ARr"]
                NBall, KBall, TM, APU, Mc, CcT = C_["NBall"], C_["KBall"], C_["TM"], C_["APU"], C_["Mc"], C_["CcT"]
                d_NB, d_KB, d_TM, d_APU, d_Mc, d_Cc = C_["d_NB"], C_["d_KB"], C_["d_TM"], C_["d_APU"], C_["d_Mc"], C_["d_Cc"]
                if q == 0:
                    dve.op(lambda e: e.memset(S32, 0.0), writes=[d_S32])
                    dve.op(lambda e: e.memset(Sbf, 0.0), writes=[d_Sbf])
                for l in range(8):
                    g = l // 2
                    ps_, pds = nb7()
                    for h in range(2):
                        hs = HS[h]
                        mm(ps_[hs, 0:64], Mc[hs, l, :], S32[hs, :], True, True, [d_Mc[g], d_S32], [pds])
                    for h in range(2):
                        hs = HS[h]
                        mm(pyb[hs, l * 64:(l + 1) * 64], Sbf[hs, :], AR[hs, l, 64:128], True, False, [d_Sbf, d_ARr[g]], [pdyb])
                        mm(pyb[hs, l * 64:(l + 1) * 64], APU[hs, l, 64:128], NBall[hs, l, 64:128], False, False, [d_APU[g], d_NB[g]], [pdyb])
                        mm(pyb[hs, l * 64:(l + 1) * 64], TM[hs, l, 0:64], KBall[hs, l, 64:128], False, True, [d_TM[g], d_KB[g]], [pdyb])
                    dve.op(lambda e: e.tensor_tensor(out=S32, in0=ps_[:, 0:64], in1=CcT[:, l, :], op=ALU.add), reads=[pds, d_Cc[g], d_S32], writes=[d_S32])
                    act.op(lambda e: e.activation(out=Sbf, in_=S32, func=AF.Copy), reads=[d_S32], writes=[d_Sbf])
                    yield
                act.op(lambda e: e.activation(out=ybuf, in_=pyb, func=AF.Copy), reads=[pdyb], writes=[dy])
                pb, pd = nb7()
                mm(pb, blk1, ybuf, True, True, [d_const, dy], [pd])
                dve.op(lambda e: e.scalar_tensor_tensor(out=yc, in0=pb, scalar=-1.0 / 64, in1=ybuf, op0=ALU.mult, op1=ALU.add),
                       reads=[pd, dy], writes=[dyc])
                pool.op(lambda e: e.tensor_tensor(out=sq, in0=yc, in1=yc, op=ALU.mult), reads=[dyc], writes=[dsq])
                yield
                pb, pd = nb7()
                mm(pb, blk1, sq, True, True, [d_const, dsq], [pd])
                dve.op(lambda e: e.tensor_scalar(out=rs, in0=pb, scalar1=1.0 / 64, scalar2=64e-5, op0=ALU.mult, op1=ALU.add), reads=[pd], writes=[drs])
                act.op(lambda e: e.activation(out=rs, in_=rs, func=AF.Sqrt), reads=[drs], writes=[drs])
                yield
                dve.op(lambda e: e.reciprocal(out=rs, in_=rs), reads=[drs], writes=[drs])
                pool.op(lambda e: e.tensor_tensor(out=yc, in0=yc, in1=rs, op=ALU.mult), reads=[dyc, drs], writes=[dyc])
                yield
                dve.op(lambda e: e.tensor_scalar(out=yc, in0=yc, scalar1=pvc("ln_w", hp), scalar2=pvc("ln_b", hp), op0=ALU.mult, op1=ALU.add),
                       reads=[dyc, d_const], writes=[dyc])
                pool.op(lambda e: e.tensor_tensor(out=yc, in0=yc, in1=bonus, op=ALU.add), reads=[dyc, d_bonus], writes=[dyc])
                dve.op(lambda e: e.tensor_tensor(out=bufB[:, 8 + hp, tsl], in0=yc, in1=gbuf, op=ALU.mult), reads=[dyc, d_g], writes=[d_B])
                yield

            def run_interleaved(gens):
                gens = [g for g in gens if g is not None]
                while gens:
                    for g in list(gens):
                        try:
                            next(g)
                        except StopIteration:
                            gens.remove(g)

            NU = 8 * NQ
            run_interleaved([prep_gen(0)])
            run_interleaved([front_gen(0), prep_gen(1)])
            for u in range(NU):
                run_interleaved([back_gen(u), front_gen(u + 1) if u + 1 < NU else None, prep_gen(u + 2) if u + 2 < NU else None])
            fw.barrier()
            if "rwT" in dbg_aps:
                tmp = Alloc(big, M_OFF, WORDS).f32(T)
                dtmp = Dep()
                for kc in range(8):
                    dve.op(lambda e: e.tensor_copy(out=tmp, in_=bufB[:, 8 + kc, :]), reads=[d_B], writes=[dtmp])
                    sp.dma(dbg_aps["rwT"][kc * 128:(kc + 1) * 128, :], tmp, reads=[dtmp])
                fw.barrier()

        def out_proj(srcT, d_src, wmat, res_fn, dst_dram, al):
            wbs2 = [(r3(al.bf16(16 * 512), 16), Dep()) for _ in range(2)]
            xts = [(al.f32(512), Dep()) for _ in range(3)]
            hos = [(al.f32(512), Dep()) for _ in range(2)]
            i = 0
            for dblk in range(4):
                ws, dw = wbs2[dblk % 2]
                ds_ = slice(dblk * 512, (dblk + 1) * 512)
                pool.dma(ws, wmat[:, ds_].rearrange("(kc p) n -> p kc n", p=128), writes=[dw])
                for tt in range(NTT):
                    rows = slice(tt * 128, (tt + 1) * 128)
                    xt, dx = xts[i % 3]
                    ho, dh = hos[i % 2]
                    i += 1
                    sp.dma(xt, res_fn(rows, ds_), writes=[dx])
                    pb, pd = nb()
                    for kc in range(KC):
                        mm(pb, srcT[:, kc, rows], ws[:, kc, :], kc == 0, kc == KC - 1, [d_src, dw], [pd])
                    dve.op(lambda e: e.tensor_tensor(out=ho, in0=pb, in1=xt, op=ALU.add), reads=[pd, dx], writes=[dh])
                    act.dma(dst_dram[rows, ds_], ho, reads=[dh])

        if stop >= 4:
            out_proj(bufB, d_B, w_out, lambda rows, cols: x[rows, cols], h1_s, Alloc(big, M_OFF, A_OFF))
            fw.barrier()
            load_gB(1)
            norm_tiles(Alloc(big, M_OFF, A_OFF), NTT, lambda i: h1_s[i * 128:(i + 1) * 128, :], bufA, d_A)
            fw.barrier()
            if "h1" in dbg_aps:
                tmp = Alloc(big, M_OFF, A_OFF).f32(D)
                dtmp = Dep()
                for tt in range(NTT):
                    sp.dma(tmp, h1_s[tt * 128:(tt + 1) * 128, :], writes=[dtmp])
                    sp.dma(dbg_aps["h1"][tt * 128:(tt + 1) * 128, :], tmp, reads=[dtmp])
                fw.barrier()


        if stop >= 5:
            kv_al = Alloc(big, M_OFF, M_OFF + 4096)
            KT = r3(kv_al.bf16(16 * 256), 16)
            Vb = r3(kv_al.bf16(2 * D), 2)
            d_KT, d_Vb, d_memT = Dep(), Dep(), Dep()
            alB = Alloc(big, B0_OFF, M_OFF)
            alM = Alloc(big, M_OFF + 4096, A_OFF)
            memT = r3(alB.bf16(16 * 256), 16)
            wbs5 = [(r3(alB.bf16(16 * 512), 16), Dep()), (r3(alM.bf16(16 * 512), 16), Dep())]
            load_gB(3)
            norm_tiles(alB, 2, lambda i: mem[i * 128:(i + 1) * 128, :], memT, d_memT)
            for g in range(8):
                ws, dw = wbs5[g % 2]
                pool.dma(ws, w_kv[:, g * 512:(g + 1) * 512].rearrange("(kc p) n -> p kc n", p=128), writes=[dw])
                if g < 4:
                    for j in range(4):
                        cb = g * 4 + j
                        pb, pd = nb()
                        for kc in range(KC):
                            mm(pb[:, 0:256], ws[:, kc, j * 128:(j + 1) * 128], memT[:, kc, :], kc == 0, kc == KC - 1, [dw, d_memT], [pd])
                        act.op(lambda e: e.activation(out=KT[:, cb, :], in_=pb[:, 0:256], func=AF.Copy), reads=[pd], writes=[d_KT])
                else:
                    for mc in range(2):
                        pb, pd = nb()
                        for kc in range(KC):
                            mm(pb, memT[:, kc, mc * 128:(mc + 1) * 128], ws[:, kc, :], kc == 0, kc == KC - 1, [dw, d_memT], [pd])
                        dve.op(lambda e: e.tensor_copy(out=Vb[:, mc, (g - 4) * 512:(g - 3) * 512], in_=pb), reads=[pd], writes=[d_Vb])
            fw.barrier()
            alM = Alloc(big, M_OFF + 4096, A_OFF)
            wbs6 = [(r3(alM.bf16(16 * 256), 16), Dep()) for _ in range(2)]
            qscale = float(512 ** -0.5)
            for g in range(8):
                ws, dw = wbs6[g % 2]
                pool.dma(ws, w_q[:, g * 256:(g + 1) * 256].rearrange("(kc p) n -> p kc n", p=128), writes=[dw])
                for j in range(2):
                    cb = g * 2 + j
                    for tq in range(NTQ):
                        ts_ = slice(tq * 512, (tq + 1) * 512)
                        pb, pd = nb()
                        for kc in range(KC):
                            mm(pb, ws[:, kc, j * 128:(j + 1) * 128], bufA[:, kc, ts_], kc == 0, kc == KC - 1, [dw, d_A], [pd])
                        if tq % 2 == 0:
                            act.op(lambda e: e.activation(out=bufB[:, cb, ts_], in_=pb, func=AF.Copy, scale=qscale), reads=[pd], writes=[d_B])
                        else:
                            dve.op(lambda e: e.tensor_scalar(out=bufB[:, cb, ts_], in0=pb, scalar1=qscale, scalar2=None, op0=ALU.mult), reads=[pd], writes=[d_B])
            fw.barrier()
            alM = Alloc(big, M_OFF + 4096, A_OFF)
            Es = [(r3(alM.bf16(2 * 512), 2), Dep()) for _ in range(2)]
            rinvs = [(alM.f32(512), Dep()) for _ in range(2)]
            it = 0
            for h in range(4):
                for tq in range(NTQ):
                    ts_ = slice(tq * 512, (tq + 1) * 512)
                    E, dE = Es[it % 2]
                    rinv, dri = rinvs[it % 2]
                    it += 1
                    for mc in range(2):
                        pb, pd = nb()
                        for c in range(4):
                            mm(pb, KT[:, h * 4 + c, mc * 128:(mc + 1) * 128], bufB[:, h * 4 + c, ts_], c == 0, c == 3, [d_KT, d_B], [pd])
                        act.op(lambda e: e.activation(out=E[:, mc, :], in_=pb, func=AF.Exp), reads=[pd], writes=[dE])
                    pb, pd = nb()
                    for mc in range(2):
                        mm(pb, onesb, E[:, mc, :], mc == 0, mc == 1, [d_const, dE], [pd])
                    dve.op(lambda e: e.reciprocal(out=rinv, in_=pb), reads=[pd], writes=[dri])
                    for c in range(4):
                        pb, pd = nb()
                        for mc in range(2):
                            mm(pb, Vb[:, mc, h * 512 + c * 128:h * 512 + (c + 1) * 128], E[:, mc, :], mc == 0, mc == 1, [d_Vb, dE], [pd])
                        dve.op(lambda e: e.tensor_tensor(out=bufA[:, h * 4 + c, ts_], in0=pb, in1=rinv, op=ALU.mult), reads=[pd, dri], writes=[d_A])
            fw.barrier()
            out_proj(bufA, d_A, w_o, lambda rows, cols: h1_s[rows, cols], h2_s, Alloc(big, B0_OFF, M_OFF))
            fw.barrier()
            if "h2" in dbg_aps:
                tmp = Alloc(big, B0_OFF, M_OFF).f32(D)
                dtmp = Dep()
                for tt in range(NTT):
                    sp.dma(tmp, h2_s[tt * 128:(tt + 1) * 128, :], writes=[dtmp])
                    sp.dma(dbg_aps["h2"][tt * 128:(tt + 1) * 128, :], tmp, reads=[dtmp])
                fw.barrier()

        if stop >= 8:
            IOA = bass.IndirectOffsetOnAxis
            bc_reg = es.enter_context(nc.gpsimd.register("bc"))
            nc.gpsimd.reg_mov(bc_reg, NROW - 1)
            BCV = nc.gpsimd.snap(bc_reg)
            bw_reg = es.enter_context(nc.gpsimd.register("bw"))
            nc.gpsimd.reg_mov(bw_reg, 8191)
            BWV = nc.gpsimd.snap(bw_reg)
            al8 = Alloc(big, B0_OFF, WORDS)
            LT = al8.f32(128)
            iop = al8.f32(1)
            siota = al8.f32(32)
            thr8 = al8.f32(8)
            p1a, p2a = al8.f32(16), al8.f32(16)
            pos1i = al8.f32(16).bitcast(I32)
            pos2i = al8.f32(16).bitcast(I32)
            widx = al8.f32(NSLOT * 4).bitcast(I32)
            d_c8, d_pos, d_widx, d_pp = Dep(), Dep(), Dep(), Dep()
            P8_TOP = al8.top
            sp.dma(LT, cst[:, 768:896], writes=[d_c8])
            sp.dma(iop, cst[:, 896:897], writes=[d_c8], allow_slow_non_contiguous=True)
            sp.dma(siota, cst[:, 897:929], writes=[d_c8])
            sp.dma(thr8, cst[:, 929:937], writes=[d_c8])
            fw.barrier()
            xnb_all = r3(al8.bf16(16 * D), 16)
            d_xnb = [Dep() for _ in range(16)]
            xts = [(al8.f32(D), Dep()) for _ in range(2)]
            xn32s = [(al8.f32(D), Dep()) for _ in range(2)]
            junk = al8.bf16(D)
            d_junk = Dep()
            h32s = [(r3(al8.f32(16 * 128), 16), Dep()) for _ in range(2)]
            wr32 = r3(al8.f32(16 * 20), 16)
            d_wr = Dep()
            logits = r3(al8.f32(16 * 20), 16)
            d_log = Dep()
            sts = [(al8.f32(8), Dep()) for _ in range(4)]
            sp.dma(wr32, w_r.rearrange("(kc p) n -> p kc n", p=128), writes=[d_wr])
            load_gB(2)
            for tt in range(16):
                xt, dx = xts[tt % 2]
                xn32, dxn = xn32s[tt % 2]
                h32, d_h32 = h32s[tt % 2]
                st, d_st = sts[tt % 4]
                sp.dma(xt, h2_s[tt * 128:(tt + 1) * 128, :], writes=[dx])
                act.op(lambda e: e.activation(out=junk, in_=xt, func=AF.Square, accum_out=st[:, 0:1]), reads=[dx], writes=[d_junk, d_st])
                dve.op(lambda e: e.tensor_scalar(out=st[:, 1:2], in0=st[:, 0:1], scalar1=1.0 / D, scalar2=1e-6, op0=ALU.mult, op1=ALU.add),
                       reads=[d_st], writes=[d_st])
                act.op(lambda e: e.activation(out=st[:, 2:3], in_=st[:, 1:2], func=AF.Sqrt), reads=[d_st], writes=[d_st])
                dve.op(lambda e: e.reciprocal(out=st[:, 3:4], in_=st[:, 2:3]), reads=[d_st], writes=[d_st])
                dve.op(lambda e: e.scalar_tensor_tensor(out=xn32, in0=xt, scalar=st[:, 3:4], in1=gBt, op0=ALU.mult, op1=ALU.mult),
                       reads=[dx, d_st, d_gB], writes=[dxn])
                act.op(lambda e: e.activation(out=xnb_all[:, tt, :], in_=xn32, func=AF.Copy), reads=[dxn], writes=[d_xnb[tt]])
                for q in range(4):
                    pf, pdf = nb()
                    for j in range(4):
                        kc = q * 4 + j
                        mm(pf[:, j * 128:(j + 1) * 128], xn32[:, kc * 128:(kc + 1) * 128], identf, True, True, [dxn, d_const], [pdf])
                    if q % 2 == 0:
                        dve.op(lambda e: e.tensor_copy(out=h32[:, q * 4:q * 4 + 4, :], in_=r3(pf, 4)), reads=[pdf], writes=[d_h32])
                    else:
                        act.op(lambda e: e.activation(out=h32[:, q * 4:q * 4 + 4, :], in_=r3(pf, 4), func=AF.Copy), reads=[pdf], writes=[d_h32])
                pb, pd = nb()
                for kc in range(KC):
                    mm(pb[:, 0:20], h32[:, kc, :], wr32[:, kc, :], kc == 0, False, [d_h32, d_wr], [pd])
                mm(pb[:, 0:20], ones1[0:1, 0:128], brow[0:1, 0:20], False, True, [d_const], [pd])
                dve.op(lambda e: e.tensor_copy(out=logits[:, tt, :], in_=pb[:, 0:20]), reads=[pd], writes=[d_log])
            NT_ = 16
            rt = [al8.f32(NT_ * 4) for _ in range(12)]
            rt4 = al8.f32(NT_ * 16)
            sel1 = al8.f32(NT_ * 16)
            sel2 = al8.f32(NT_ * 16)
            ind = al8.f32(NT_ * 16)
            tot = r3(al8.f32(NT_ * 16), NT_)
            tcum = r3(al8.f32(NT_ * 16), NT_)
            posall = al8.f32(NT_ * 16)
            ptmp = al8.f32(NT_ * 16)
            c8 = al8.f32(16 * 8)
            cnt, nsl, bsl, bsl256 = al8.f32(16), al8.f32(16), al8.f32(16), al8.f32(16)
            total = al8.f32(1)
            es32 = al8.f32(NSLOT * 16)
            esf, unused, wbase = al8.f32(NSLOT), al8.f32(NSLOT), al8.f32(NSLOT)
            pos1f, pos2f = al8.f32(16), al8.f32(16)
            widxf = al8.f32(NSLOT * 4).rearrange("p (s q) -> p s q", q=4)
            d_rt = Dep()
            lg = logits[:, :, 0:4]
            le = logits[:, :, 4:20].rearrange("p t (g e) -> p t g e", g=4)
            gmax, gsum, gw, m1, m2 = [rt[i][:, 0:NT_] for i in range(5)]
            goh, gsh, esel, oh1, e2 = [r3(rt[7 + i], NT_) for i in range(5)]
            t4 = rt4.rearrange("p (t g e) -> p t g e", t=NT_, g=4)

            def bc3(v):
                return v.unsqueeze(2).to_broadcast([128, NT_, 4])

            def v4(a):
                return a.rearrange("p (t g e) -> p t g e", t=NT_, g=4)
            R = [d_log, d_rt, d_c8]
            W_ = [d_rt]
            dve.op(lambda e: e.tensor_reduce(out=gmax, in_=lg, axis=AX.X, op=ALU.max), R, W_)
            dve.op(lambda e: e.tensor_tensor(out=goh, in0=lg, in1=bc3(gmax), op=ALU.is_equal), R, W_)
            dve.op(lambda e: e.tensor_tensor(out=gsh, in0=lg, in1=bc3(gmax), op=ALU.subtract), R, W_)
            act.op(lambda e: e.activation(out=gsh, in_=gsh, func=AF.Exp), R, W_)
            dve.op(lambda e: e.tensor_reduce(out=gsum, in_=gsh, axis=AX.X, op=ALU.add), R, W_)
            dve.op(lambda e: e.reciprocal(out=gw, in_=gsum), R, W_)
            dve.op(lambda e: e.tensor_tensor(out=t4, in0=le, in1=goh.unsqueeze(3).to_broadcast([128, NT_, 4, 4]), op=ALU.mult), R, W_)
            dve.op(lambda e: e.tensor_reduce(out=esel, in_=t4.rearrange("p t g e -> p t e g"), axis=AX.X, op=ALU.add), R, W_)
            dve.op(lambda e: e.tensor_reduce(out=m1, in_=esel, axis=AX.X, op=ALU.max), R, W_)
            dve.op(lambda e: e.tensor_tensor(out=oh1, in0=esel, in1=bc3(m1), op=ALU.is_equal), R, W_)
            dve.op(lambda e: e.scalar_tensor_tensor(out=e2, in0=oh1, scalar=-1e30, in1=esel, op0=ALU.mult, op1=ALU.add), R, W_)
            dve.op(lambda e: e.tensor_reduce(out=m2, in_=e2, axis=AX.X, op=ALU.max), R, W_)
            dve.op(lambda e: e.tensor_tensor(out=e2, in0=e2, in1=bc3(m2), op=ALU.is_equal), R, W_)
            dve.op(lambda e: e.tensor_tensor(out=p1a, in0=m1, in1=m2, op=ALU.subtract), R, W_ + [d_pp])
            act.op(lambda e: e.activation(out=p1a, in_=p1a, func=AF.Sigmoid), R + [d_pp], W_ + [d_pp])
            dve.op(lambda e: e.tensor_scalar(out=p2a, in0=p1a, scalar1=-1.0, scalar2=1.0, op0=ALU.mult, op1=ALU.add), R + [d_pp], W_ + [d_pp])
            dve.op(lambda e: e.tensor_tensor(out=p1a, in0=p1a, in1=gw, op=ALU.mult), R + [d_pp], W_ + [d_pp])
            dve.op(lambda e: e.tensor_tensor(out=p2a, in0=p2a, in1=gw, op=ALU.mult), R + [d_pp], W_ + [d_pp])
            dve.op(lambda e: e.tensor_tensor(out=v4(sel1), in0=goh.unsqueeze(3).to_broadcast([128, NT_, 4, 4]),
                                             in1=oh1.unsqueeze(2).to_broadcast([128, NT_, 4, 4]), op=ALU.mult), R, W_)
            dve.op(lambda e: e.tensor_tensor(out=v4(sel2), in0=goh.unsqueeze(3).to_broadcast([128, NT_, 4, 4]),
                                             in1=e2.unsqueeze(2).to_broadcast([128, NT_, 4, 4]), op=ALU.mult), R, W_)
            dve.op(lambda e: e.tensor_tensor(out=ind, in0=sel1, in1=sel2, op=ALU.add), R, W_)
            pw, pdw = nb()
            mm(pw[:, 0:256], LT, ind, True, True, [d_rt, d_c8], [pdw])
            pt_, pdt = nb()
            mm(pt_[:, 0:256], ones1, ind, True, True, [d_rt, d_const], [pdt])
            dve.op(lambda e: e.tensor_copy(out=tot, in_=r3(pt_[:, 0:256], NT_)), R + [pdt], W_)
            dve.op(lambda e: e.memset(tcum[:, 0, :], 0.0), R, W_)
            for tt in range(1, NT_):
                dve.op(lambda e: e.tensor_tensor(out=tcum[:, tt, :], in0=tcum[:, tt - 1, :], in1=tot[:, tt - 1, :], op=ALU.add), R, W_)
            dve.op(lambda e: e.tensor_tensor(out=cnt, in0=tcum[:, NT_ - 1, :], in1=tot[:, NT_ - 1, :], op=ALU.add), R, W_)
            dve.op(lambda e: e.tensor_tensor(out=r3(c8, 16), in0=cnt.unsqueeze(2).to_broadcast([128, 16, 8]),
                                             in1=thr8.unsqueeze(1).to_broadcast([128, 16, 8]), op=ALU.is_gt), R, W_)
            dve.op(lambda e: e.tensor_reduce(out=nsl, in_=r3(c8, 16), axis=AX.X, op=ALU.add), R, W_)
            dve.op(lambda e: e.memset(bsl[:, 0:1], 0.0), R, W_)
            for ex in range(1, 16):
                dve.op(lambda e: e.tensor_tensor(out=bsl[:, ex:ex + 1], in0=bsl[:, ex - 1:ex], in1=nsl[:, ex - 1:ex], op=ALU.add), R, W_)
            dve.op(lambda e: e.tensor_tensor(out=total, in0=bsl[:, 15:16], in1=nsl[:, 15:16], op=ALU.add), R, W_)
            dve.op(lambda e: e.tensor_scalar(out=bsl256, in0=bsl, scalar1=float(SL), scalar2=None, op0=ALU.mult), R, W_)
            dve.op(lambda e: e.tensor_tensor(out=posall, in0=pw[:, 0:256], in1=tcum.rearrange("p t e -> p (t e)"), op=ALU.add), R + [pdw], W_)
            dve.op(lambda e: e.tensor_tensor(out=r3(posall, NT_), in0=r3(posall, NT_), in1=bsl256.unsqueeze(1).to_broadcast([128, NT_, 16]), op=ALU.add), R, W_)
            dve.op(lambda e: e.tensor_tensor(out=ptmp, in0=posall, in1=sel1, op=ALU.mult), R, W_)
            dve.op(lambda e: e.tensor_reduce(out=pos1f, in_=r3(ptmp, NT_), axis=AX.X, op=ALU.add), R, W_)
            dve.op(lambda e: e.tensor_tensor(out=ptmp, in0=posall, in1=sel2, op=ALU.mult), R, W_)
            dve.op(lambda e: e.tensor_reduce(out=pos2f, in_=r3(ptmp, NT_), axis=AX.X, op=ALU.add), R, W_)
            dve.op(lambda e: e.tensor_copy(out=pos1i, in_=pos1f), R, W_ + [d_pos])
            dve.op(lambda e: e.tensor_copy(out=pos2i, in_=pos2f), R, W_ + [d_pos])
            dve.op(lambda e: e.tensor_tensor(out=r3(es32, NSLOT), in0=bsl.unsqueeze(1).to_broadcast([128, NSLOT, 16]),
                                             in1=siota[:, 0:NSLOT].unsqueeze(2).to_broadcast([128, NSLOT, 16]), op=ALU.is_le), R, W_)
            dve.op(lambda e: e.tensor_reduce(out=esf, in_=r3(es32, NSLOT), axis=AX.X, op=ALU.add), R, W_)
            dve.op(lambda e: e.tensor_scalar(out=unused, in0=siota[:, 0:NSLOT], scalar1=total[:, 0:1], scalar2=1.0e6, op0=ALU.is_ge, op1=ALU.mult), R, W_)
            dve.op(lambda e: e.tensor_scalar(out=wbase, in0=esf, scalar1=-1.0, scalar2=512.0, op0=ALU.add, op1=ALU.mult), R, W_)
            dve.op(lambda e: e.tensor_tensor(out=wbase, in0=wbase, in1=unused, op=ALU.add), R, W_)
            dve.op(lambda e: e.tensor_scalar(out=wbase, in0=wbase, scalar1=iop[:, 0:1], scalar2=None, op0=ALU.add), R, W_)
            for q in range(4):
                dve.op(lambda e: e.tensor_scalar(out=widxf[:, :, q], in0=wbase, scalar1=float(128 * q), scalar2=None, op0=ALU.add), R, W_)
            dve.op(lambda e: e.tensor_copy(out=widx, in_=widxf.rearrange("p s q -> p (s q)")), R, W_ + [d_widx])
            if "route" in dbg_aps:
                sp.dma(dbg_aps["route"][:, 0:16], pos1f, reads=[d_rt])
                sp.dma(dbg_aps["route"][:, 16:32], pos2f, reads=[d_rt])
                sp.dma(dbg_aps["route"][:, 32:64], wbase, reads=[d_rt])
                sp.dma(dbg_aps["route"][:, 64:80], p1a, reads=[d_pp])
                sp.dma(dbg_aps["route"][:, 80:96], p2a, reads=[d_pp])
                sp.dma(dbg_aps["route"][:, 96:112], cnt, reads=[d_rt])
            for tt in range(16):
                for posi in (pos1i, pos2i):
                    pool.dma_fn(lambda e: e.indirect_dma_start(out=Xs, out_offset=IOA(ap=posi[:, tt:tt + 1], axis=0), in_=xnb_all[:, tt, :], in_offset=None,
                                                               bounds_check=BCV, oob_is_err=False),
                                reads=[d_xnb[tt], d_pos], writes=[d_Xs])
            fw.barrier()
            ald = Alloc(big, P8_TOP, WORDS)
            wbufs = [(ald.bf16(8192), [Dep() for _ in range(4)]) for _ in range(6)]
            xsls = [(r3(ald.bf16(NA * D), NA), Dep()) for _ in range(2)]
            XTs = [(r3(ald.bf16(16 * SL), 16), Dep()) for _ in range(2)]
            hids = [(r3(ald.bf16(4 * SL), 4), Dep()) for _ in range(2)]
            sbs = [(ald.bf16(SL), Dep()) for _ in range(2)]
            yos = [(ald.f32(D), Dep()) for _ in range(2)]
            cnt8 = dict(yi=0, ei=0)

            def wload(i, s):
                wsl = []
                for m, wl in enumerate((wg_l, wu_l, wd_l)):
                    buf, deps = wbufs[(3 * i + m) % 6]
                    for q in range(4):
                        pool.dma_fn(lambda e: e.indirect_dma_start(out=buf[:, q * 2048:(q + 1) * 2048], out_offset=None, in_=wl,
                                                                   in_offset=IOA(ap=widx[:, s * 4 + q:s * 4 + q + 1], axis=0), bounds_check=BWV, oob_is_err=False),
                                    reads=[d_widx], writes=[deps[q]])
                    wsl.append((buf, deps))
                return wsl

            def xload(i, s):
                xsl, dxs = xsls[i % 2]
                sp.dma(xsl, Xs[s * SL:(s + 1) * SL, :].rearrange("(a p) n -> p a n", p=128), reads=[d_Xs], writes=[dxs])

            def emit_T(i, s):
                xsl, dxs = xsls[i % 2]
                XT, dXT = XTs[i % 2]
                for a in range(NA):
                    for q4 in range(4):
                        pb, pd = nb()
                        for j in range(4):
                            kc = q4 * 4 + j
                            mm(pb[:, j * 128:(j + 1) * 128], xsl[:, a, kc * 128:(kc + 1) * 128], identb, True, True, [dxs, d_const], [pd])
                        cnt8["ei"] += 1
                        if cnt8["ei"] % 2 == 0:
                            act.op(lambda e: e.activation(out=XT[:, q4 * 4:q4 * 4 + 4, a * 128:(a + 1) * 128], in_=r3(pb, 4), func=AF.Copy), reads=[pd], writes=[dXT])
                        else:
                            dve.op(lambda e: e.tensor_copy(out=XT[:, q4 * 4:q4 * 4 + 4, a * 128:(a + 1) * 128], in_=r3(pb, 4)), reads=[pd], writes=[dXT])

            def emit_GU(i, s, wsl):
                wg, dwg = r3(wsl[0][0], 16), wsl[0][1]
                wu, dwu = r3(wsl[1][0], 16), wsl[1][1]
                XT, dXT = XTs[i % 2]
                hid, dhid = hids[i % 2]
                for ffc in range(4):
                    pg, pdg = nb()
                    for kc in range(KC):
                        mm(pg[:, 0:SL], wg[:, kc, ffc * 128:(ffc + 1) * 128], XT[:, kc, :], kc == 0, kc == KC - 1, [dwg[kc // 4], dXT], [pdg])
                    pu, pdu = nb()
                    for kc in range(KC):
                        mm(pu[:, 0:SL], wu[:, kc, ffc * 128:(ffc + 1) * 128], XT[:, kc, :], kc == 0, kc == KC - 1, [dwu[kc // 4], dXT], [pdu])
                    sb_, dsb = sbs[ffc % 2]
                    act.op(lambda e: e.activation(out=sb_, in_=pg[:, 0:SL], func=AF.Silu), reads=[pdg], writes=[dsb])
                    dve.op(lambda e: e.tensor_tensor(out=hid[:, ffc, :], in0=pu[:, 0:SL], in1=sb_, op=ALU.mult), reads=[pdu, dsb], writes=[dhid])

            def emit_D(i, s, wsl):
                wd, dwd = r3(wsl[2][0], 4), wsl[2][1]
                hid, dhid = hids[i % 2]
                for a in range(NA):
                    yo, dyo = yos[cnt8["yi"] % 2]
                    cnt8["yi"] += 1
                    for dblk in range(4):
                        ds_ = slice(dblk * 512, (dblk + 1) * 512)
                        pb, pd = nb()
                        for ffc in range(4):
                            mm(pb, hid[:, ffc, a * 128:(a + 1) * 128], wd[:, ffc, ds_], ffc == 0, ffc == 3, [dhid, dwd[ffc]], [pd])
                        if dblk % 2 == 0:
                            act.op(lambda e: e.activation(out=yo[:, ds_], in_=pb, func=AF.Copy), reads=[pd], writes=[dyo])
                        else:
                            dve.op(lambda e: e.tensor_copy(out=yo[:, ds_], in_=pb), reads=[pd], writes=[dyo])
                    r0 = s * SL + a * 128
                    sp.dma(Ys[r0:r0 + 128, :], yo, reads=[dyo], writes=[d_Ys])

            lo_n = NSLOT - NSLOT // 3
            lo, hi = list(range(lo_n)), list(range(NSLOT - 1, lo_n - 1, -1))
            order = []
            while lo or hi:
                order += lo[:2]
                lo = lo[2:]
                if hi:
                    order.append(hi.pop(0))
            assert sorted(order) == list(range(NSLOT))
            xload(0, order[0])
            emit_T(0, order[0])
            for i, s in enumerate(order):
                wsl = wload(i, s)
                if i + 1 < NSLOT:
                    xload(i + 1, order[i + 1])
                emit_GU(i, s, wsl)
                if i + 1 < NSLOT:
                    emit_T(i + 1, order[i + 1])
                emit_D(i, s, wsl)
            fw.barrier()
            ale = Alloc(big, P8_TOP, WORDS)
            cts = [(ale.f32(D), Dep()) for _ in range(2)]
            y1s = [(ale.f32(D), Dep()) for _ in range(2)]
            y2s = [(ale.f32(D), Dep()) for _ in range(2)]
            junk2 = ale.bf16(D)
            sts2 = [(ale.f32(8), Dep()) for _ in range(4)]
            load_gB(4)
            for tt in range(16):
                xt, dx = cts[tt % 2]
                y1, dy1 = y1s[tt % 2]
                y2, dy2 = y2s[tt % 2]
                st, d_st = sts2[tt % 4]
                pool.dma(xt, h2_s[tt * 128:(tt + 1) * 128, :], writes=[dx])
                pool.dma_fn(lambda e: e.indirect_dma_start(out=y1, out_offset=None, in_=Ys, in_offset=IOA(ap=pos1i[:, tt:tt + 1], axis=0),
                                                           bounds_check=BCV, oob_is_err=False), reads=[d_Ys, d_pos], writes=[dy1])
                pool.dma_fn(lambda e: e.indirect_dma_start(out=y2, out_offset=None, in_=Ys, in_offset=IOA(ap=pos2i[:, tt:tt + 1], axis=0),
                                                           bounds_check=BCV, oob_is_err=False), reads=[d_Ys, d_pos], writes=[dy2])
                dve.op(lambda e: e.scalar_tensor_tensor(out=xt, in0=y1, scalar=p1a[:, tt:tt + 1], in1=xt, op0=ALU.mult, op1=ALU.add),
                       reads=[dy1, dx, d_pp], writes=[dx])
                dve.op(lambda e: e.scalar_tensor_tensor(out=xt, in0=y2, scalar=p2a[:, tt:tt + 1], in1=xt, op0=ALU.mult, op1=ALU.add),
                       reads=[dy2, dx, d_pp], writes=[dx])
                if "h3" in dbg_aps:
                    sp.dma(dbg_aps["h3"][tt * 128:(tt + 1) * 128, :], xt, reads=[dx])
                act.op(lambda e: e.activation(out=junk2, in_=xt, func=AF.Square, accum_out=st[:, 0:1]), reads=[dx], writes=[d_junk, d_st])
                dve.op(lambda e: e.tensor_scalar(out=st[:, 1:2], in0=st[:, 0:1], scalar1=1.0 / D, scalar2=1e-6, op0=ALU.mult, op1=ALU.add),
                       reads=[d_st], writes=[d_st])
                act.op(lambda e: e.activation(out=st[:, 2:3], in_=st[:, 1:2], func=AF.Sqrt), reads=[d_st], writes=[d_st])
                dve.op(lambda e: e.reciprocal(out=st[:, 3:4], in_=st[:, 2:3]), reads=[d_st], writes=[d_st])
                dve.op(lambda e: e.scalar_tensor_tensor(out=xt, in0=xt, scalar=st[:, 3:4], in1=gBt, op0=ALU.mult, op1=ALU.mult),
                       reads=[dx, d_st, d_gB], writes=[dx])
                sp.dma(out[tt * 128:(tt + 1) * 128, :], xt, reads=[dx])
            fw.barrier()

        fw.barrier()
    return nc


def host_consts(inp):
    l = 0
    f = np.float32
    gBh = np.stack([np.broadcast_to(v, (128, D)) for v in (inp["norm_mix_g"][l], inp["norm_xattn_g"][l], inp["norm_ffn_g"][l],
                                                            inp["norm_mem_g"][l], inp["norm_final_g"])]).astype(f)
    pvh = np.zeros((128, NPV), f)

    def col(v, n):
        return np.ascontiguousarray(np.asarray(v, f).reshape(n, 128).T)
    pvh[:, 0:8] = col(inp["pool_scale"][l], 8)
    mu = np.asarray(inp["rwkv_mu"][l], f)
    pvh[:, 8:32] = col(mu[0:3072], 24)
    pvh[:, 32] = mu[3072:3200]
    pvh[:, 33] = mu[3200:3328]
    pvh[0:32, 34] = mu[3328:3360]
    pvh[:, 35:43] = col(inp["rwkv_w0"][l], 8)
    pvh[:, 43:51] = col(inp["rwkv_a0"][l], 8)
    pvh[:, 51:59] = col(inp["rwkv_k_k"][l], 8)
    pvh[:, 59:67] = col(inp["rwkv_k_a"][l], 8)
    pvh[:, 67:75] = col(inp["rwkv_ln_w"][l], 8)
    pvh[:, 75:83] = col(inp["rwkv_ln_b"][l], 8)
    pvh[:, 83:91] = col(np.asarray(inp["rwkv_r_k"][l]).reshape(-1), 8)
    cst = np.zeros((128, 1024), f)
    p = np.arange(128)
    cst[:, 0:128] = np.eye(128, dtype=f)
    cst[:, 128:256] = (p[:, None] // 64 == p[None, :] // 64).astype(f)
    s = p % 64
    tcol = np.arange(64)
    strict = (s[:, None] < tcol[None, :]).astype(f)
    incl = (s[:, None] <= tcol[None, :]).astype(f)
    one = np.concatenate([strict, incl], 1)
    cst[:, 256:512] = np.concatenate([one, one], 1)
    low = (s[:, None] > tcol[None, :]).astype(f)
    cst[:, 512:640] = np.concatenate([low, low], 1)
    cst[:, 640:704] = (s[:, None] == tcol[None, :]).astype(f)
    tt = np.arange(16)
    for gi, w in enumerate((2, 4, 8, 16)):
        cst[:, 704 + gi * 16:704 + (gi + 1) * 16] = (1.0 / np.minimum(tt + 1, w)).astype(f)[None, :]
    cst[:, 768:896] = (p[:, None] < p[None, :]).astype(f)
    cst[:, 896] = p.astype(f)
    cst[:, 897:929] = np.arange(32, dtype=f)[None, :]
    cst[:, 929:937] = (float(SL) * np.arange(8, dtype=f))[None, :]
    rm = np.ones((128, T), f)
    rm[:, ::64] = 0.0
    w_r = np.concatenate([inp["moe_w_group"][l], inp["moe_w_expert"][l]], 1).astype(f)
    b_r = np.concatenate([inp["moe_b_group"][l], inp["moe_b_expert"][l]])[None, :].astype(f)
    return dict(gB=gBh, pv=pvh, cst=cst, rmask=rm, w_r=np.ascontiguousarray(w_r), b_r=b_r)


def make_in_maps(inp, cores):
    l = 0
    c = host_consts(inp)
    shared = dict(
        w_in=inp["w_in"][l], pool_w=inp["pool_w"][l], w2=inp["rwkv_w2"][l], a2=inp["rwkv_a2"][l], g2=inp["rwkv_g2"][l],
        w_out=inp["w_out"][l], w_q=inp["xattn_w_q"][l], w_kv=inp["xattn_w_kv"][l], w_o=inp["xattn_w_o"][l],
        wg_l=np.asarray(inp["moe_w_gate"][l], np.float32).reshape(16, 4, 4, 128, 512).transpose(0, 1, 3, 2, 4).reshape(8192, 2048),
        wu_l=np.asarray(inp["moe_w_up"][l], np.float32).reshape(16, 4, 4, 128, 512).transpose(0, 1, 3, 2, 4).reshape(8192, 2048),
        wd_l=np.asarray(inp["moe_w_down"][l], np.float32).reshape(8192, 2048), **c)
    shared = {k: np.ascontiguousarray(np.asarray(v, np.float32)) for k, v in shared.items()}
    maps = []
    for b in cores:
        m = dict(shared)
        m["x"] = np.ascontiguousarray(inp["x"][b])
        m["mem"] = np.ascontiguousarray(inp["mem"][b])
        maps.append(m)
    return maps


def kernel(**inputs):
    inp = {k: np.asarray(v) for k, v in inputs.items()}
    nc = build()
    maps = make_in_maps(inp, list(range(8)))
    res = run_bass_kernel_spmd(nc, maps, core_ids=list(range(8)))
    return np.stack([np.asarray(r["out"]) for r in res.results], 0).astype(np.float32)
```

```python
import numpy as np
import concourse.bass as bass
import concourse.mybir as mybir
from concourse.bass_utils import run_bass_kernel_spmd
from contextlib import ExitStack

F32 = mybir.dt.float32
BF16 = mybir.dt.bfloat16
I32 = mybir.dt.int32
AF = mybir.ActivationFunctionType
ALU = mybir.AluOpType
AX = mybir.AxisListType

D = 2048
KC = 16
T = 2048
NTT = T // 128
NTQ = T // 512
NCH = T // 64
PAD = 16
CDEC = float(np.exp(-0.5))
WORDS = 51200
NSLOT, SL = 26, 384
NA = SL // 128


class Dep:
    __slots__ = ("w", "r", "p")

    def __init__(self):
        self.w = None
        self.r = {}
        self.p = {}


class Eng:
    def __init__(self, fw, name, b, is_pe=False):
        self.fw, self.name, self.b, self.is_pe = fw, name, b, is_pe
        self.sem = fw.new_sem(name)
        self.cnt = 0
        self.waited = {}
        self.dma_slots = None
        self.dma_i = 0

    def _wait(self, tok):
        sem, val = tok
        if self.waited.get(id(sem), 0) < val:
            self.b.wait_ge(sem, val)
            self.waited[id(sem)] = val

    def _collect(self, reads, writes, pws=()):
        def w_(t):
            if t is not None and not (self.is_pe and t[0] is self.sem):
                self._wait(t)
        for d in reads:
            w_(d.w)
            for t in d.p.values():
                w_(t)
        for d in writes:
            w_(d.w)
            for t in d.p.values():
                w_(t)
            for t in d.r.values():
                w_(t)
        for d in pws:
            w_(d.w)
            for t in d.r.values():
                w_(t)

    def op(self, fn, reads=(), writes=(), pws=()):
        self._collect(reads, writes, pws)
        inst = fn(self.b)
        self.cnt += 1
        inst.then_inc(self.sem, 1)
        tok = (self.sem, self.cnt)
        for d in reads:
            d.r[id(self.sem)] = tok
        for d in writes:
            d.w = tok
            d.r = {}
            d.p = {}
        for d in pws:
            d.p[id(self.sem)] = tok
        return tok

    def dma(self, out, in_, reads=(), writes=(), **kw):
        return self.dma_fn(lambda e: e.dma_start(out=out, in_=in_, **kw), reads, writes)

    def dma_fn(self, fn, reads=(), writes=()):
        if self.dma_slots is None:
            self.dma_slots = [[self.fw.new_sem(f"{self.name}_d{i}"), 0] for i in range(8)]
        self._collect(reads, writes)
        slot = self.dma_slots[self.dma_i % len(self.dma_slots)]
        self.dma_i += 1
        if slot[1] > 0:
            self._wait((slot[0], slot[1]))
        inst = fn(self.b)
        slot[1] += 16
        inst.then_inc(slot[0], 16)
        tok = (slot[0], slot[1])
        for d in reads:
            d.r[id(slot[0])] = tok
        for d in writes:
            d.w = tok
            d.r = {}
            d.p = {}
        return tok


class FW:
    def __init__(self, nc, es):
        self.nc, self.es = nc, es
        self.pe = Eng(self, "pe", nc.tensor, True)
        self.act = Eng(self, "act", nc.scalar)
        self.dve = Eng(self, "dve", nc.vector)
        self.pool = Eng(self, "pool", nc.gpsimd)
        self.sp = Eng(self, "sp", nc.sync)
        self.engs = [self.pe, self.act, self.dve, self.pool, self.sp]

    def new_sem(self, name):
        return self.es.enter_context(self.nc.semaphore(name))

    def barrier(self):
        toks = []
        for e in self.engs:
            if e.cnt > 0:
                toks.append((e.sem, e.cnt))
            if e.dma_slots:
                for s in e.dma_slots:
                    if s[1] > 0:
                        toks.append((s[0], s[1]))
        for e in self.engs:
            for t in toks:
                if t[0] is not e.sem:
                    e._wait(t)


class Alloc:
    def __init__(self, big, start, end):
        self.big, self.top, self.end = big, start, end

    def f32(self, n):
        a = self.big[:, self.top:self.top + n]
        self.top += n
        assert self.top <= self.end, (self.top, self.end)
        return a

    def bf16(self, n):
        w = (n + 1) // 2
        a = self.big[:, self.top:self.top + w].bitcast(BF16)
        self.top += w
        assert self.top <= self.end, (self.top, self.end)
        return a[:, 0:n]


def r3(ap, a):
    return ap.rearrange("p (a b) -> p a b", a=a)


PV = dict(pool_scale=0, mu_rkv=8, mu_lo=32, w0=35, a0=43, k_k=51, k_a=59, ln_w=67, ln_b=75, r_k=83, omka=91)
NPV = 99


def build(stop=99, dbg=()):
    nc = bass.Bass("TRN2", target_bir_lowering=False)

    def din(name, shape):
        return nc.dram_tensor(name, list(shape), F32, kind="ExternalInput").ap()

    x = din("x", [T, D])
    mem = din("mem", [256, D])
    w_in = din("w_in", [D, 4384])
    pool_w = din("pool_w", [4, 256, 256])
    w2 = din("w2", [64, 1024])
    a2 = din("a2", [64, 1024])
    g2 = din("g2", [160, 1024])
    w_out = din("w_out", [D, D])
    w_q = din("w_q", [D, D])
    w_kv = din("w_kv", [D, 2 * D])
    w_o = din("w_o", [D, D])
    w_r = din("w_r", [D, 20])
    b_r = din("b_r", [1, 20])
    wg_l = din("wg_l", [8192, 2048])
    wu_l = din("wu_l", [8192, 2048])
    wd_l = din("wd_l", [8192, 2048])
    gB = din("gB", [5, 128, D])
    pvd = din("pv", [128, NPV])
    cst = din("cst", [128, 1024])
    rmask_d = din("rmask", [128, T])
    out = nc.dram_tensor("out", [T, D], F32, kind="ExternalOutput").ap()
    dbg_aps = {}
    for name, shape in dbg:
        dbg_aps[name] = nc.dram_tensor(name, list(shape), F32, kind="ExternalOutput").ap()
    rkv_s = nc.dram_tensor("rkv_s", [24, 128, T], F32, kind="Internal").ap()
    h1_s = nc.dram_tensor("h1_s", [T, D], F32, kind="Internal").ap()
    h2_s = nc.dram_tensor("h2_s", [T, D], F32, kind="Internal").ap()
    NROW = NSLOT * SL
    Xs = nc.dram_tensor("Xs", [NROW, D], BF16, kind="Internal").ap()
    Ys = nc.dram_tensor("Ys", [NROW, D], F32, kind="Internal").ap()

    with ExitStack() as es:
        fw = FW(nc, es)
        pe, act, dve, pool, sp = fw.pe, fw.act, fw.dve, fw.pool, fw.sp
        big = es.enter_context(nc.sbuf_tensor("big", [128, WORDS], F32))[:]
        banks = [(es.enter_context(nc.psum_tensor(f"bk{i}", [128, 512], F32))[:], Dep()) for i in range(8)]
        bki = [0]

        def nb():
            b = banks[bki[0] % 8]
            bki[0] += 1
            return b

        def mm(o, lhsT, rhs, start, stop, reads, writes):
            pe.op(lambda e: e.matmul(o, lhsT=lhsT, rhs=rhs, start=start, stop=stop), reads, writes)

        CONST_W = 7424
        ca = Alloc(big, 0, CONST_W)
        identf = ca.f32(128)
        blk1 = ca.f32(128)
        mSI = ca.f32(256)
        mL = ca.f32(128)
        i64 = ca.f32(64)
        rcnt = ca.f32(64)
        pv = ca.f32(NPV + 1)
        identb = ca.bf16(128)
        onesb = ca.bf16(128)
        rmask = ca.bf16(T)
        gBt = ca.f32(D)
        lo1 = ca.bf16(T)
        sg1 = ca.bf16(T)
        sg2 = ca.bf16(T)
        ones1 = ca.f32(128)
        brow = ca.f32(20)
        d_const, d_gB, d_lo1, d_sg1, d_sg2 = Dep(), Dep(), Dep(), Dep(), Dep()
        B0_OFF = CONST_W
        B1_OFF = B0_OFF + 8192
        M_OFF = B1_OFF + 8192
        A_OFF = WORDS - 16384
        bufA = r3(big[:, A_OFF:WORDS].bitcast(BF16), 16)
        bufB = r3(big[:, B0_OFF:M_OFF].bitcast(BF16), 16)
        d_A, d_B = Dep(), Dep()

        sp.dma(identf, cst[:, 0:128], writes=[d_const])
        sp.dma(blk1, cst[:, 128:256], writes=[d_const])
        sp.dma(mSI, cst[:, 256:512], writes=[d_const])
        sp.dma(mL, cst[:, 512:640], writes=[d_const])
        sp.dma(i64, cst[:, 640:704], writes=[d_const])
        sp.dma(rcnt, cst[:, 704:768], writes=[d_const])
        sp.dma(pv[:, 0:NPV], pvd, writes=[d_const])
        sp.dma(brow[0:1, :], b_r, writes=[d_const])
        pool.dma(identb, cst[:, 0:128], writes=[d_const])
        pool.dma(rmask, rmask_d, writes=[d_const])
        pool.op(lambda e: e.memset(onesb, 1.0), writes=[d_const])
        pool.op(lambda e: e.memset(ones1, 1.0), writes=[d_const])
        dve.op(lambda e: e.tensor_scalar(out=pv[:, PV["omka"]:PV["omka"] + 8], in0=pv[:, PV["k_a"]:PV["k_a"] + 8],
                                         scalar1=-1.0, scalar2=1.0, op0=ALU.mult, op1=ALU.add), reads=[d_const], writes=[d_const])
        fw.barrier()

        def pvc(name, j):
            c = PV[name] + j
            return pv[:, c:c + 1]

        d_Xs, d_Ys = Dep(), Dep()
        zf = [0]

        def zero_fill(n, zt, dz):
            while n > 0 and zf[0] < NROW // 128 and stop >= 8:
                c = zf[0]
                pool.dma(Xs[c * 128:(c + 1) * 128, :], zt, reads=[dz])
                zf[0] += 1
                n -= 1

        def load_gB(i):
            sp.dma(gBt, gB[i], writes=[d_gB])

        def norm_tiles(al, ntiles, src_fn, dstT, d_dst, tok_off=0, keep=None, nbuf=3):
            xts = [(al.f32(D), Dep()) for _ in range(nbuf)]
            xns = [(al.bf16(D), Dep()) for _ in range(nbuf)]
            junk = al.bf16(D)
            d_junk = Dep()
            sts = [(al.f32(8), Dep()) for _ in range(4)]

            def stage_a(i):
                xt, dx = xts[i % nbuf]
                xn, dn = xns[i % nbuf]
                st, d_st = sts[i % 4]
                sp.dma(xt, src_fn(i), writes=[dx])
                if keep is not None:
                    keep(i, xt, dx)
                act.op(lambda e: e.activation(out=junk, in_=xt, func=AF.Square, accum_out=st[:, 0:1]), reads=[dx], writes=[d_junk, d_st])
                dve.op(lambda e: e.tensor_scalar(out=st[:, 1:2], in0=st[:, 0:1], scalar1=1.0 / D, scalar2=1e-6, op0=ALU.mult, op1=ALU.add),
                       reads=[d_st], writes=[d_st])
                act.op(lambda e: e.activation(out=st[:, 2:3], in_=st[:, 1:2], func=AF.Sqrt), reads=[d_st], writes=[d_st])
                dve.op(lambda e: e.reciprocal(out=st[:, 3:4], in_=st[:, 2:3]), reads=[d_st], writes=[d_st])
                dve.op(lambda e: e.scalar_tensor_tensor(out=xn, in0=xt, scalar=st[:, 3:4], in1=gBt, op0=ALU.mult, op1=ALU.mult),
                       reads=[dx, d_st, d_gB], writes=[dn])

            def stage_b(i):
                xn, dn = xns[i % nbuf]
                for q in range(4):
                    pb, pd = nb()
                    for j in range(4):
                        kc = q * 4 + j
                        mm(pb[:, j * 128:(j + 1) * 128], xn[:, kc * 128:(kc + 1) * 128], identb, True, True, [dn, d_const], [pd])
                    t0 = tok_off + i * 128
                    if q % 2 == 0:
                        act.op(lambda e: e.activation(out=dstT[:, q * 4:q * 4 + 4, t0:t0 + 128], in_=r3(pb, 4), func=AF.Copy), reads=[pd], pws=[d_dst])
                    else:
                        dve.op(lambda e: e.tensor_copy(out=dstT[:, q * 4:q * 4 + 4, t0:t0 + 128], in_=r3(pb, 4)), reads=[pd], pws=[d_dst])

            for i in range(ntiles + 1):
                if i < ntiles:
                    stage_a(i)
                if i >= 1:
                    stage_b(i - 1)

        def dump(name, ap_sb, dep, dst=None):
            if name in dbg_aps:
                sp.dma(dbg_aps[name] if dst is None else dst, ap_sb, reads=[dep])

        load_gB(0)
        al = Alloc(big, M_OFF, A_OFF)
        norm_tiles(al, NTT, lambda i: x[i * 128:(i + 1) * 128, :], bufA, d_A)
        fw.barrier()
        if "hnT" in dbg_aps:
            tmp = Alloc(big, M_OFF, A_OFF).f32(T)
            dtmp = Dep()
            for kc in range(16):
                dve.op(lambda e: e.tensor_copy(out=tmp, in_=bufA[:, kc, :]), reads=[d_A], writes=[dtmp])
                sp.dma(dbg_aps["hnT"][kc * 128:(kc + 1) * 128, :], tmp, reads=[dtmp])
            fw.barrier()

        if stop >= 2:
            al = Alloc(big, B1_OFF, A_OFF)
            wbs = [(r3(al.bf16(16 * 128), 16), Dep()) for _ in range(4)]
            wbi = [0]
            pbufs = [(al.f32(PAD + T), Dep()) for _ in range(2)]
            fbs = [(al.f32(PAD + T), Dep()) for _ in range(3)]
            pooled = [(al.bf16(T), Dep()) for _ in range(2)]
            pwb = r3(al.bf16(8 * 256), 8)
            d_pw = Dep()
            ztile = al.bf16(D)
            d_zt = Dep()
            dve.op(lambda e: e.memset(ztile, 0.0), writes=[d_zt])
            for pbf, dp in pbufs + fbs:
                dve.op(lambda e: e.memset(pbf[:, 0:PAD], 0.0), writes=[dp])
            pool.dma(pwb, pool_w.rearrange("g (cc p) d -> p (g cc) d", p=128), writes=[d_pw])
            pbi = [0]

            def proj_block(col0, n):
                ws, dw = wbs[wbi[0] % 4]
                wbi[0] += 1
                pool.dma(ws[:, :, 0:n], w_in[:, col0:col0 + n].rearrange("(kc p) n -> p kc n", p=128), writes=[dw])
                if wbi[0] > 4:
                    zero_fill(3, ztile, d_zt)
                pbf, dp = pbufs[pbi[0] % 2]
                pbi[0] += 1
                for tq in range(NTQ):
                    pb, pd = nb()
                    for kc in range(KC):
                        mm(pb[0:n, :], ws[:, kc, 0:n], bufA[:, kc, tq * 512:(tq + 1) * 512], kc == 0, kc == KC - 1, [dw, d_A], [pd])
                    act.op(lambda e: e.activation(out=pbf[0:n, PAD + tq * 512:PAD + (tq + 1) * 512], in_=pb[0:n, :], func=AF.Copy), reads=[pd], writes=[dp])
                return pbf, dp

            def tshift(pbf, dp, n, mu_ap, zout, dz):
                f0, df0 = fbs[0]
                dve.op(lambda e: e.tensor_tensor(out=f0[0:n, 0:T], in0=pbf[0:n, PAD - 1:PAD - 1 + T], in1=pbf[0:n, PAD:PAD + T], op=ALU.subtract),
                       reads=[dp], writes=[df0])
                dve.op(lambda e: e.scalar_tensor_tensor(out=zout, in0=f0[0:n, 0:T], scalar=mu_ap, in1=pbf[0:n, PAD:PAD + T], op0=ALU.mult, op1=ALU.add),
                       reads=[df0, dp, d_const], writes=[dz])

            z1f, dz1 = fbs[1]
            z1 = z1f[:, PAD:PAD + T]
            pbf, dp = proj_block(4096, 128)
            tshift(pbf, dp, 128, pv[:, PV["mu_lo"]:PV["mu_lo"] + 1], z1, dz1)
            act.op(lambda e: e.activation(out=lo1[0:64, :], in_=z1[0:64, :], func=AF.Tanh), reads=[dz1], writes=[d_lo1])
            act.op(lambda e: e.activation(out=lo1[64:128, :], in_=z1[64:128, :], func=AF.Copy), reads=[dz1], writes=[d_lo1])
            pbf, dp = proj_block(4224, 128)
            tshift(pbf, dp, 128, pv[:, PV["mu_lo"] + 1:PV["mu_lo"] + 2], z1, dz1)
            act.op(lambda e: e.activation(out=sg1, in_=z1, func=AF.Sigmoid), reads=[dz1], writes=[d_sg1])
            pbf, dp = proj_block(4352, 32)
            tshift(pbf, dp, 32, pv[0:32, PV["mu_lo"] + 2:PV["mu_lo"] + 3], z1[0:32, :], dz1)
            act.op(lambda e: e.activation(out=sg2[0:32, :], in_=z1[0:32, :], func=AF.Sigmoid), reads=[dz1], writes=[d_sg2])
            for j in range(24):
                pbf, dp = proj_block(1024 + j * 128, 128)
                tshift(pbf, dp, 128, pvc("mu_rkv", j), z1, dz1)
                sp.dma(rkv_s[j], z1, reads=[dz1])
            for cb in range(8):
                gi = cb // 2
                w = (2, 4, 8, 16)[gi]
                pbf, dp = proj_block(cb * 128, 128)
                (fa, dfa), (fb_, dfb) = fbs[1], fbs[2]
                src, dsrc = pbf, dp
                sh = 1
                k = 0
                while sh < w:
                    dst, ddst = (fa, dfa) if k % 2 == 0 else (fb_, dfb)
                    dve.op(lambda e: e.tensor_tensor(out=dst[:, PAD:PAD + T], in0=src[:, PAD:PAD + T], in1=src[:, PAD - sh:PAD - sh + T], op=ALU.add),
                           reads=[dsrc], writes=[ddst])
                    src, dsrc = dst, ddst
                    sh *= 2
                    k += 1
                po, dpo = pooled[cb % 2]
                dve.op(lambda e: e.scalar_tensor_tensor(out=po, in0=src[:, PAD:PAD + T], scalar=1.0 / w, in1=pbf[:, PAD:PAD + T], op0=ALU.mult, op1=ALU.subtract),
                       reads=[dsrc, dp], writes=[dpo])
                f0, df0 = fbs[0]
                dve.op(lambda e: e.tensor_tensor(out=f0[:, 0:16], in0=src[:, PAD:PAD + 16], in1=rcnt[:, gi * 16:(gi + 1) * 16], op=ALU.mult),
                       reads=[dsrc, d_const], writes=[df0])
                dve.op(lambda e: e.tensor_tensor(out=po[:, 0:16], in0=f0[:, 0:16], in1=pbf[:, PAD:PAD + 16], op=ALU.subtract),
                       reads=[df0, dp], writes=[dpo])
                if cb % 2 == 1:
                    for db in range(2):
                        for tq in range(NTQ):
                            pb, pd = nb()
                            for cc in range(2):
                                mm(pb, pwb[:, gi * 2 + cc, db * 128:(db + 1) * 128], pooled[cc][0][:, tq * 512:(tq + 1) * 512], cc == 0, cc == 1,
                                   [d_pw, pooled[cc][1]], [pd])
                            blk = gi * 2 + db
                            act.op(lambda e: e.activation(out=bufB[:, blk, tq * 512:(tq + 1) * 512], in_=pb, func=AF.Copy, scale=pvc("pool_scale", blk)),
                                   reads=[pd, d_const], writes=[d_B])
            zero_fill(NROW, ztile, d_zt)
            fw.barrier()
            if "mixT" in dbg_aps:
                tmp = Alloc(big, B1_OFF, A_OFF).f32(T)
                dtmp = Dep()
                for kc in range(8):
                    dve.op(lambda e: e.tensor_copy(out=tmp, in_=bufB[:, kc, :]), reads=[d_B], writes=[dtmp])
                    sp.dma(dbg_aps["mixT"][kc * 128:(kc + 1) * 128, :], tmp, reads=[dtmp])
                fw.barrier()


        if stop >= 3:
            QTK = 512
            NQ = T // QTK
            al = Alloc(big, M_OFF, WORDS)
            lw = al.bf16(1024)
            g2a = al.bf16(1024)
            g2b = al.bf16(1024)
            S32 = al.f32(64)
            Sbf = al.bf16(64)
            d_lw, d_S32, d_Sbf = Dep(), Dep(), Dep()
            sets = []
            for i in range(3):
                sets.append(dict(AR=r3(al.bf16(8 * 128), 8), BK=r3(al.bf16(8 * 128), 8), BKh=r3(al.bf16(8 * 128), 8), vb=al.bf16(QTK),
                                 GL=al.f32(8), gbuf=al.bf16(QTK), bonus=al.f32(QTK), ybuf=al.f32(QTK),
                                 d_AR=Dep(), d_BK=Dep(), d_BKh=Dep(), d_vb=Dep(), d_GL=Dep(), d_g=Dep(), d_bonus=Dep(), d_y=Dep(),
                                 d_ARr=[Dep() for _ in range(4)]))
            Fq = [al.f32(QTK) for _ in range(8)]
            dFq = [Dep() for _ in range(8)]
            cbs = []
            for i in range(2):
                cbs.append(dict(NBall=r3(al.bf16(8 * 128), 8), KBall=r3(al.bf16(8 * 128), 8), TM=r3(al.bf16(8 * 320), 8), APU=r3(al.bf16(8 * 128), 8),
                                Mc=r3(al.f32(8 * 64), 8), CcT=r3(al.f32(8 * 64), 8),
                                d_NB=[Dep() for _ in range(4)], d_KB=[Dep() for _ in range(4)], d_TM=[Dep() for _ in range(4)],
                                d_APU=[Dep() for _ in range(4)], d_Mc=[Dep() for _ in range(4)], d_Cc=[Dep() for _ in range(4)]))
            TT = r3(al.bf16(8 * 64), 8)
            Wg = [[r3(al.bf16(2 * 128), 2) for _ in range(2)] for _ in range(4)]
            NTg = [[r3(al.bf16(2 * 64), 2) for _ in range(2)] for _ in range(4)]
            d_TT = [Dep() for _ in range(4)]
            dWg = [[Dep(), Dep()] for _ in range(4)]
            dNTg = [[Dep(), Dep()] for _ in range(4)]
            yc, sq, rs = al.f32(QTK), al.f32(QTK), al.f32(QTK)
            dyc, dsq, drs = Dep(), Dep(), Dep()
            pool.dma(lw[0:64, :], w2, writes=[d_lw])
            pool.dma(lw[64:128, :], a2, writes=[d_lw])
            pool.dma(g2a, g2[0:128, :], writes=[d_lw])
            pool.dma(g2b[0:32, :], g2[128:160, :], writes=[d_lw])
            HS = [slice(0, 64), slice(64, 128)]
            i64b = i64.unsqueeze(1).to_broadcast([128, 2, 64])
            pyb, pdyb = banks[7]
            nbm = [0]

            def nb7():
                b = banks[nbm[0] % 7]
                nbm[0] += 1
                return b

            def prep_gen(u):
                hp, q = divmod(u, NQ)
                S_ = sets[u % 3]
                cs = slice(hp * 128, (hp + 1) * 128)
                tsl = slice(q * QTK, (q + 1) * QTK)
                k_, sgw, alr, cum, kk, f5, f6, f7 = Fq
                dk, dsgw, dalr, dcum, dkk, df5, df6, df7 = dFq
                AR, BK, BKh, vb, GL, gbuf, bonus = S_["AR"], S_["BK"], S_["BKh"], S_["vb"], S_["GL"], S_["gbuf"], S_["bonus"]
                d_AR, d_BK, d_BKh, d_vb, d_GL, d_g, d_bonus = S_["d_AR"], S_["d_BK"], S_["d_BKh"], S_["d_vb"], S_["d_GL"], S_["d_g"], S_["d_bonus"]
                sp.dma(k_, rkv_s[8 + hp][:, tsl], writes=[dk])
                pb, pd = nb7()
                mm(pb, lw[0:64, cs], lo1[0:64, tsl], True, True, [d_lw, d_lo1], [pd])
                act.op(lambda e: e.activation(out=sgw, in_=pb, func=AF.Sigmoid, bias=pvc("w0", hp)), reads=[pd, d_const], writes=[dsgw])
                pb, pd = nb7()
                mm(pb, lw[64:128, cs], lo1[64:128, tsl], True, True, [d_lw, d_lo1], [pd])
                act.op(lambda e: e.activation(out=alr, in_=pb, func=AF.Sigmoid, bias=pvc("a0", hp)), reads=[pd, d_const], writes=[dalr])
                yield
                pb, pd = nb7()
                mm(pb, g2a[:, cs], sg1[:, tsl], True, False, [d_lw, d_sg1], [pd])
                mm(pb, g2b[0:32, cs], sg2[0:32, tsl], False, True, [d_lw, d_sg2], [pd])
                act.op(lambda e: e.activation(out=gbuf, in_=pb, func=AF.Copy), reads=[pd], writes=[d_g])
                dve.op(lambda e: e.tensor_tensor_scan(out=cum, data0=rmask[:, 0:QTK], data1=sgw, initial=0.0, op0=ALU.mult, op1=ALU.add),
                       reads=[d_const, dsgw], writes=[dcum])
                yield
                act.op(lambda e: e.activation(out=kk, in_=k_, func=AF.Copy, scale=pvc("k_k", hp)), reads=[dk, d_const], writes=[dkk])
                pool.op(lambda e: e.tensor_tensor(out=f5, in0=kk, in1=kk, op=ALU.mult), reads=[dkk], writes=[df5])
                yield
                pb, pd = nb7()
                mm(pb, blk1, f5, True, True, [d_const, df5], [pd])
                dve.op(lambda e: e.tensor_scalar(out=f6, in0=pb, scalar1=1e-24, scalar2=None, op0=ALU.max), reads=[pd], writes=[df6])
                act.op(lambda e: e.activation(out=f6, in_=f6, func=AF.Sqrt), reads=[df6], writes=[df6])
                yield
                dve.op(lambda e: e.reciprocal(out=f6, in_=f6), reads=[df6], writes=[df6])
                yield
                pool.op(lambda e: e.tensor_tensor(out=kk, in0=kk, in1=f6, op=ALU.mult), reads=[dkk, df6], writes=[dkk])
                dve.op(lambda e: e.tensor_scalar(out=f5, in0=alr, scalar1=pvc("k_a", hp), scalar2=pvc("omka", hp), op0=ALU.mult, op1=ALU.add),
                       reads=[dalr, d_const], writes=[df5])
                yield
                pool.op(lambda e: e.tensor_tensor(out=f5, in0=f5, in1=k_, op=ALU.mult), reads=[df5, dk], writes=[df5])
                pool.op(lambda e: e.tensor_tensor(out=alr, in0=alr, in1=kk, op=ALU.mult), reads=[dalr, dkk], writes=[dalr])
                yield
                act.op(lambda e: e.activation(out=f6, in_=cum, func=AF.Exp, scale=CDEC), reads=[dcum], writes=[df6])
                dve.op(lambda e: e.tensor_tensor(out=BK[:, :, 0:64], in0=r3(alr, 8), in1=r3(f6, 8), op=ALU.mult), reads=[dalr, df6], writes=[d_BK])
                pool.op(lambda e: e.tensor_tensor(out=BK[:, :, 64:128], in0=r3(f5, 8), in1=r3(f6, 8), op=ALU.mult), reads=[df5, df6], writes=[d_BK])
                yield
                dve.op(lambda e: e.tensor_tensor(out=r3(f6, 8), in0=r3(cum, 8), in1=r3(cum, 8)[:, :, 63:64].to_broadcast([128, 8, 64]), op=ALU.subtract),
                       reads=[dcum], writes=[df6])
                act.op(lambda e: e.activation(out=f6, in_=f6, func=AF.Exp, scale=CDEC), reads=[df6], writes=[df6])
                yield
                dve.op(lambda e: e.tensor_tensor(out=BKh[:, :, 0:64], in0=r3(alr, 8), in1=r3(f6, 8), op=ALU.mult), reads=[dalr, df6], writes=[d_BKh])
                pool.op(lambda e: e.tensor_tensor(out=BKh[:, :, 64:128], in0=r3(f5, 8), in1=r3(f6, 8), op=ALU.mult), reads=[df5, df6], writes=[d_BKh])
                yield
                pool.op(lambda e: e.tensor_tensor(out=f6, in0=cum, in1=sgw, op=ALU.subtract), reads=[dcum, dsgw], writes=[df6])
                act.op(lambda e: e.activation(out=f6, in_=f6, func=AF.Exp, scale=-CDEC), reads=[df6], writes=[df6])
                yield
                dve.op(lambda e: e.scalar_tensor_tensor(out=AR[:, :, 0:64], in0=r3(kk, 8), scalar=-1.0, in1=r3(f6, 8), op0=ALU.mult, op1=ALU.mult),
                       reads=[dkk, df6], writes=[d_AR])
                act.op(lambda e: e.activation(out=GL, in_=r3(cum, 8)[:, :, 63], func=AF.Exp, scale=-CDEC), reads=[dcum], writes=[d_GL])
                yield
                act.op(lambda e: e.activation(out=f6, in_=cum, func=AF.Exp, scale=-CDEC), reads=[dcum], writes=[df6])
                sp.dma(k_, rkv_s[hp][:, tsl], writes=[dk])
                pool.op(lambda e: e.tensor_tensor(out=AR[:, :, 64:128], in0=r3(k_, 8), in1=r3(f6, 8), op=ALU.mult), reads=[dk, df6], writes=[d_AR] + S_["d_ARr"])
                yield
                dve.op(lambda e: e.scalar_tensor_tensor(out=f6, in0=k_, scalar=pvc("r_k", hp), in1=f5, op0=ALU.mult, op1=ALU.mult),
                       reads=[dk, df5, d_const], writes=[df6])
                sp.dma(sgw, rkv_s[16 + hp][:, tsl], writes=[dsgw])
                yield
                pb, pd = nb7()
                mm(pb, blk1, f6, True, True, [d_const, df6], [pd])
                dve.op(lambda e: e.tensor_tensor(out=bonus, in0=pb, in1=sgw, op=ALU.mult), reads=[pd, dsgw], writes=[d_bonus])
                act.op(lambda e: e.activation(out=vb, in_=sgw, func=AF.Copy), reads=[dsgw], writes=[d_vb])
                yield

            def front_gen(u):
                hp, q = divmod(u, NQ)
                S_ = sets[u % 3]
                C_ = cbs[u % 2]
                tsl = slice(q * QTK, (q + 1) * QTK)
                AR, BK, BKh, vb, GL, gbuf, bonus, ybuf = S_["AR"], S_["BK"], S_["BKh"], S_["vb"], S_["GL"], S_["gbuf"], S_["bonus"], S_["ybuf"]
                d_AR, d_BK, d_BKh, d_vb, d_GL, d_g, d_bonus, dy = S_["d_AR"], S_["d_BK"], S_["d_BKh"], S_["d_vb"], S_["d_GL"], S_["d_g"], S_["d_bonus"], S_["d_y"]
                d_ARr = S_["d_ARr"]
                NBall, KBall, TM, APU, Mc, CcT = C_["NBall"], C_["KBall"], C_["TM"], C_["APU"], C_["Mc"], C_["CcT"]
                d_NB, d_KB, d_TM, d_APU, d_Mc, d_Cc = C_["d_NB"], C_["d_KB"], C_["d_TM"], C_["d_APU"], C_["d_Mc"], C_["d_Cc"]
                pool.op(lambda e: e.tensor_tensor(out=Mc, in0=i64.unsqueeze(1).to_broadcast([128, 8, 64]),
                                                  in1=GL.unsqueeze(2).to_broadcast([128, 8, 64]), op=ALU.mult),
                        reads=[d_const, d_GL], writes=d_Mc)
                for g in range(4):
                    l0 = 2 * g
                    pa, pda = nb7()
                    pb_, pdb = nb7()
                    pt, pdt = nb7()
                    pv_, pdv = nb7()
                    for ci in range(2):
                        c = l0 + ci
                        for h in range(2):
                            hs = HS[h]
                            mm(pa[hs, ci * 128:(ci + 1) * 128], BK[hs, c, 0:64], AR[hs, c, :], True, True, [d_BK, d_AR, d_ARr[g]], [pda])
                            mm(pb_[hs, ci * 128:(ci + 1) * 128], BK[hs, c, 64:128], AR[hs, c, :], True, True, [d_BK, d_AR, d_ARr[g]], [pdb])
                            mm(pt[hs, ci * 64:(ci + 1) * 64], AR[hs, c, 0:64], BK[hs, c, 0:64], True, True, [d_BK, d_AR], [pdt])
                            idh = identb[hs, 64 * h:64 * h + 64]
                            mm(pv_[hs, ci * 256:ci * 256 + 64], vb[hs, c * 64:(c + 1) * 64], idh, True, True, [d_vb, d_const], [pdv])
                            mm(pv_[hs, ci * 256 + 64:ci * 256 + 128], BKh[hs, c, 0:64], idh, True, True, [d_BKh, d_const], [pdv])
                            mm(pv_[hs, ci * 256 + 128:ci * 256 + 192], BKh[hs, c, 64:128], idh, True, True, [d_BKh, d_const], [pdv])
                            mm(pv_[hs, ci * 256 + 192:ci * 256 + 256], AR[hs, c, 0:64], idh, True, True, [d_AR, d_const], [pdv])
                    dve.op(lambda e: e.tensor_tensor(out=NBall[:, l0:l0 + 2, :], in0=r3(pa[:, 0:256], 2), in1=r3(mSI, 2), op=ALU.mult),
                           reads=[pda, d_const], writes=[d_NB[g]])
                    dve.op(lambda e: e.tensor_tensor(out=KBall[:, l0:l0 + 2, :], in0=r3(pb_[:, 0:256], 2), in1=r3(mSI, 2), op=ALU.mult),
                           reads=[pdb, d_const], writes=[d_KB[g]])
                    dve.op(lambda e: e.tensor_tensor(out=NTg[g][0], in0=r3(pt[:, 0:128], 2), in1=r3(mL, 2), op=ALU.mult),
                           reads=[pdt, d_const], writes=[dNTg[g][0]])
                    act.op(lambda e: e.activation(out=TM[:, l0:l0 + 2, 0:256], in_=r3(pv_, 2), func=AF.Copy), reads=[pdv], writes=[d_TM[g]])
                    pool.op(lambda e: e.tensor_tensor(out=Wg[g][0][:, :, 64:128], in0=NBall[:, l0:l0 + 2, 0:64], in1=i64b, op=ALU.add),
                            reads=[d_NB[g], d_const], writes=[dWg[g][0]])
                    yield
                for g in range(4):
                    l0 = 2 * g
                    p0, pd0 = nb7()
                    q0, qd0 = nb7()
                    NT, dNT = NTg[g][0], dNTg[g][0]
                    for ci in range(2):
                        for h in range(2):
                            hs = HS[h]
                            mm(p0[hs, ci * 64:(ci + 1) * 64], NT[hs, ci, :], NBall[hs, l0 + ci, 0:64], True, True, [dNT, d_NB[g]], [pd0])
                            mm(q0[hs, ci * 64:(ci + 1) * 64], NBall[hs, l0 + ci, 0:64], NT[hs, ci, :], True, True, [dNT, d_NB[g]], [qd0])
                    act.op(lambda e: e.activation(out=Wg[g][0][:, :, 0:64], in_=r3(p0[:, 0:128], 2), func=AF.Copy), reads=[pd0], writes=[dWg[g][0]])
                    act.op(lambda e: e.activation(out=NTg[g][1], in_=r3(q0[:, 0:128], 2), func=AF.Copy), reads=[qd0], writes=[dNTg[g][1]])
                    yield
                cur, ntc = 0, 1
                for lvl in range(1, 6):
                    last = lvl == 5
                    for g in range(4):
                        l0 = 2 * g
                        Wc, dWc = Wg[g][cur], dWg[g][cur]
                        NTc, dNTc = NTg[g][ntc], dNTg[g][ntc]
                        p1, pd1 = nb7()
                        if not last:
                            q1, qd1 = nb7()
                        for ci in range(2):
                            for h in range(2):
                                hs = HS[h]
                                if not last:
                                    mm(p1[hs, ci * 128:(ci + 1) * 128], NTc[hs, ci, :], Wc[hs, ci, :], True, True, [dNTc, dWc], [pd1])
                                    mm(q1[hs, ci * 64:(ci + 1) * 64], Wc[hs, ci, 0:64], NTc[hs, ci, :], True, True, [dNTc, dWc], [qd1])
                                else:
                                    mm(p1[hs, ci * 64:(ci + 1) * 64], NTc[hs, ci, :], Wc[hs, ci, 64:128], True, True, [dNTc, dWc], [pd1])
                        if not last:
                            Wn, dWn = Wg[g][1 - cur], dWg[g][1 - cur]
                            NTn, dNTn = NTg[g][1 - ntc], dNTg[g][1 - ntc]
                            act.op(lambda e: e.activation(out=Wn[:, :, 0:64], in_=r3(p1[:, 0:256], 2)[:, :, 0:64], func=AF.Copy), reads=[pd1], writes=[dWn])
                            dve.op(lambda e: e.tensor_tensor(out=Wn[:, :, 64:128], in0=r3(p1[:, 0:256], 2)[:, :, 64:128], in1=Wc[:, :, 64:128], op=ALU.add),
                                   reads=[pd1, dWc], writes=[dWn])
                            act.op(lambda e: e.activation(out=NTn, in_=r3(q1[:, 0:128], 2), func=AF.Copy), reads=[qd1], writes=[dNTn])
                        else:
                            dve.op(lambda e: e.tensor_tensor(out=TT[:, l0:l0 + 2, :], in0=r3(p1[:, 0:128], 2), in1=Wc[:, :, 64:128], op=ALU.add),
                                   reads=[pd1, dWc], writes=[d_TT[g]])
                        if g % 2 == 1:
                            yield
                    cur, ntc = 1 - cur, 1 - ntc
                for g in range(4):
                    l0 = 2 * g
                    pw, pdw = nb7()
                    for ci in range(2):
                        for h in range(2):
                            hs = HS[h]
                            mm(pw[hs, ci * 64:(ci + 1) * 64], KBall[hs, l0 + ci, 0:64], TM[hs, l0 + ci, 0:64], True, True, [d_KB[g], d_TM[g]], [pdw])
                    act.op(lambda e: e.activation(out=TM[:, l0:l0 + 2, 256:320], in_=r3(pw[:, 0:128], 2), func=AF.Copy), reads=[pdw], writes=[d_TM[g]])
                yield
                for g in range(4):
                    l0 = 2 * g
                    pq, pdq = nb7()
                    for ci in range(2):
                        for h in range(2):
                            hs = HS[h]
                            mm(pq[hs, ci * 128:(ci + 1) * 128], TT[hs, l0 + ci, :], TM[hs, l0 + ci, 192:320], True, True, [d_TT[g], d_TM[g]], [pdq])
                    dve.op(lambda e: e.tensor_copy(out=APU[:, l0:l0 + 2, :], in_=r3(pq[:, 0:256], 2)), reads=[pdq], writes=[d_APU[g]])
                yield
                for g in range(4):
                    l0 = 2 * g
                    pm, pdm = nb7()
                    pc, pdc = nb7()
                    pr, pdr = nb7()
                    for ci in range(2):
                        l = l0 + ci
                        for h in range(2):
                            hs = HS[h]
                            mm(pm[hs, ci * 64:(ci + 1) * 64], APU[hs, l, 0:64], TM[hs, l, 64:128], True, True, [d_APU[g], d_TM[g]], [pdm])
                            mm(pc[hs, ci * 64:(ci + 1) * 64], TM[hs, l, 64:128], APU[hs, l, 64:128], True, False, [d_APU[g], d_TM[g]], [pdc])
                            mm(pc[hs, ci * 64:(ci + 1) * 64], TM[hs, l, 128:192], TM[hs, l, 0:64], False, True, [d_TM[g]], [pdc])
                            mm(pr[hs, ci * 64:(ci + 1) * 64], APU[hs, l, 0:64], NBall[hs, l, 64:128], True, True, [d_APU[g], d_NB[g]], [pdr])
                    dve.op(lambda e: e.tensor_tensor(out=Mc[:, l0:l0 + 2, :], in0=r3(pm[:, 0:128], 2), in1=Mc[:, l0:l0 + 2, :], op=ALU.add),
                           reads=[pdm, d_Mc[g]], writes=[d_Mc[g]])
                    act.op(lambda e: e.activation(out=CcT[:, l0:l0 + 2, :], in_=r3(pc[:, 0:128], 2), func=AF.Copy), reads=[pdc], writes=[d_Cc[g]])
                    dve.op(lambda e: e.tensor_tensor(out=AR[:, l0:l0 + 2, 64:128], in0=r3(pr[:, 0:128], 2), in1=AR[:, l0:l0 + 2, 64:128], op=ALU.add),
                           reads=[pdr, d_ARr[g], d_AR], writes=[d_ARr[g]])
                    if g % 2 == 1:
                        yield
            def back_gen(u):
                hp, q = divmod(u, NQ)
                S_ = sets[u % 3]
                C_ = cbs[u % 2]
                tsl = slice(q * QTK, (q + 1) * QTK)
                AR, BK, BKh, vb, GL, gbuf, bonus, ybuf = S_["AR"], S_["BK"], S_["BKh"], S_["vb"], S_["GL"], S_["gbuf"], S_["bonus"], S_["ybuf"]
                d_AR, d_BK, d_BKh, d_vb, d_GL, d_g, d_bonus, dy = S_["d_AR"], S_["d_BK"], S_["d_BKh"], S_["d_vb"], S_["d_GL"], S_["d_g"], S_["d_bonus"], S_["d_y"]
                d_ARr = S_["d_ARr"]
                NBall, KBall, TM, APU, Mc, CcT = C_["NBall"], C_["KBall"], C_["TM"], C_["APU"], C_["Mc"], C_["CcT"]
                d_NB, d_KB, d_TM, d_APU, d_Mc, d_Cc = C_["d_NB"], C_["d_KB"], C_["d_TM"], C_["d_APU"], C_["d_Mc"], C_["d_Cc"]
                if q == 0:
                    dve.op(lambda e: e.memset(S32, 0.0), writes=[d_S32])
                    dve.op(lambda e: e.memset(Sbf, 0.0), writes=[d_Sbf])
                for l in range(8):
                    g = l // 2
                    ps_, pds = nb7()
                    for h in range(2):
                        hs = HS[h]
                        mm(ps_[hs, 0:64], Mc[hs, l, :], S32[hs, :], True, True, [d_Mc[g], d_S32], [pds])
                    for h in range(2):
                        hs = HS[h]
                        mm(pyb[hs, l * 64:(l + 1) * 64], Sbf[hs, :], AR[hs, l, 64:128], True, False, [d_Sbf, d_ARr[g]], [pdyb])
                        mm(pyb[hs, l * 64:(l + 1) * 64], APU[hs, l, 64:128], NBall[hs, l, 64:128], False, False, [d_APU[g], d_NB[g]], [pdyb])
                        mm(pyb[hs, l * 64:(l + 1) * 64], TM[hs, l, 0:64], KBall[hs, l, 64:128], False, True, [d_TM[g], d_KB[g]], [pdyb])
                    dve.op(lambda e: e.tensor_tensor(out=S32, in0=ps_[:, 0:64], in1=CcT[:, l, :], op=ALU.add), reads=[pds, d_Cc[g], d_S32], writes=[d_S32])
                    act.op(lambda e: e.activation(out=Sbf, in_=S32, func=AF.Copy), reads=[d_S32], writes=[d_Sbf])
                    yield
                act.op(lambda e: e.activation(out=ybuf, in_=pyb, func=AF.Copy), reads=[pdyb], writes=[dy])
                pb, pd = nb7()
                mm(pb, blk1, ybuf, True, True, [d_const, dy], [pd])
                dve.op(lambda e: e.scalar_tensor_tensor(out=yc, in0=pb, scalar=-1.0 / 64, in1=ybuf, op0=ALU.mult, op1=ALU.add),
                       reads=[pd, dy], writes=[dyc])
                pool.op(lambda e: e.tensor_tensor(out=sq, in0=yc, in1=yc, op=ALU.mult), reads=[dyc], writes=[dsq])
                yield
                pb, pd = nb7()
                mm(pb, blk1, sq, True, True, [d_const, dsq], [pd])
                dve.op(lambda e: e.tensor_scalar(out=rs, in0=pb, scalar1=1.0 / 64, scalar2=64e-5, op0=ALU.mult, op1=ALU.add), reads=[pd], writes=[drs])
                act.op(lambda e: e.activation(out=rs, in_=rs, func=AF.Sqrt), reads=[drs], writes=[drs])
                yield
                dve.op(lambda e: e.reciprocal(out=rs, in_=rs), reads=[drs], writes=[drs])
                pool.op(lambda e: e.tensor_tensor(out=yc, in0=yc, in1=rs, op=ALU.mult), reads=[dyc, drs], writes=[dyc])
                yield
                dve.op(lambda e: e.tensor_scalar(out=yc, in0=yc, scalar1=pvc("ln_w", hp), scalar2=pvc("ln_b", hp), op0=ALU.mult, op1=ALU.add),
                       reads=[dyc, d_const], writes=[dyc])
                pool.op(lambda e: e.tensor_tensor(out=yc, in0=yc, in1=bonus, op=ALU.add), reads=[dyc, d_bonus], writes=[dyc])
                dve.op(lambda e: e.tensor_tensor(out=bufB[:, 8 + hp, tsl], in0=yc, in1=gbuf, op=ALU.mult), reads=[dyc, d_g], writes=[d_B])
                yield

            def run_interleaved(gens):
                gens = [g for g in gens if g is not None]
                while gens:
                    for g in list(gens):
                        try:
                            next(g)
                        except StopIteration:
                            gens.remove(g)

            NU = 8 * NQ
            run_interleaved([prep_gen(0)])
            run_interleaved([front_gen(0), prep_gen(1)])
            for u in range(NU):
                run_interleaved([back_gen(u), front_gen(u + 1) if u + 1 < NU else None, prep_gen(u + 2) if u + 2 < NU else None])
            fw.barrier()
            if "rwT" in dbg_aps:
                tmp = Alloc(big, M_OFF, WORDS).f32(T)
                dtmp = Dep()
                for kc in range(8):
                    dve.op(lambda e: e.tensor_copy(out=tmp, in_=bufB[:, 8 + kc, :]), reads=[d_B], writes=[dtmp])
                    sp.dma(dbg_aps["rwT"][kc * 128:(kc + 1) * 128, :], tmp, reads=[dtmp])
                fw.barrier()

        def out_proj(srcT, d_src, wmat, res_fn, dst_dram, al):
            wbs2 = [(r3(al.bf16(16 * 512), 16), Dep()) for _ in range(2)]
            xts = [(al.f32(512), Dep()) for _ in range(3)]
            hos = [(al.f32(512), Dep()) for _ in range(2)]
            i = 0
            for dblk in range(4):
                ws, dw = wbs2[dblk % 2]
                ds_ = slice(dblk * 512, (dblk + 1) * 512)
                pool.dma(ws, wmat[:, ds_].rearrange("(kc p) n -> p kc n", p=128), writes=[dw])
                for tt in range(NTT):
                    rows = slice(tt * 128, (tt + 1) * 128)
                    xt, dx = xts[i % 3]
                    ho, dh = hos[i % 2]
                    i += 1
                    sp.dma(xt, res_fn(rows, ds_), writes=[dx])
                    pb, pd = nb()
                    for kc in range(KC):
                        mm(pb, srcT[:, kc, rows], ws[:, kc, :], kc == 0, kc == KC - 1, [d_src, dw], [pd])
                    dve.op(lambda e: e.tensor_tensor(out=ho, in0=pb, in1=xt, op=ALU.add), reads=[pd, dx], writes=[dh])
                    act.dma(dst_dram[rows, ds_], ho, reads=[dh])

        if stop >= 4:
            out_proj(bufB, d_B, w_out, lambda rows, cols: x[rows, cols], h1_s, Alloc(big, M_OFF, A_OFF))
            fw.barrier()
            load_gB(1)
            norm_tiles(Alloc(big, M_OFF, A_OFF), NTT, lambda i: h1_s[i * 128:(i + 1) * 128, :], bufA, d_A)
            fw.barrier()
            if "h1" in dbg_aps:
                tmp = Alloc(big, M_OFF, A_OFF).f32(D)
                dtmp = Dep()
                for tt in range(NTT):
                    sp.dma(tmp, h1_s[tt * 128:(tt + 1) * 128, :], writes=[dtmp])
                    sp.dma(dbg_aps["h1"][tt * 128:(tt + 1) * 128, :], tmp, reads=[dtmp])
                fw.barrier()


        if stop >= 5:
            kv_al = Alloc(big, M_OFF, M_OFF + 4096)
            KT = r3(kv_al.bf16(16 * 256), 16)
            Vb = r3(kv_al.bf16(2 * D), 2)
            d_KT, d_Vb, d_memT = Dep(), Dep(), Dep()
            alB = Alloc(big, B0_OFF, M_OFF)
            alM = Alloc(big, M_OFF + 4096, A_OFF)
            memT = r3(alB.bf16(16 * 256), 16)
            wbs5 = [(r3(alB.bf16(16 * 512), 16), Dep()), (r3(alM.bf16(16 * 512), 16), Dep())]
            load_gB(3)
            norm_tiles(alB, 2, lambda i: mem[i * 128:(i + 1) * 128, :], memT, d_memT, nbuf=2)
            for g in range(8):
                ws, dw = wbs5[g % 2]
                pool.dma(ws, w_kv[:, g * 512:(g + 1) * 512].rearrange("(kc p) n -> p kc n", p=128), writes=[dw])
                if g < 4:
                    for j in range(4):
                        cb = g * 4 + j
                        pb, pd = nb()
                        for kc in range(KC):
                            mm(pb[:, 0:256], ws[:, kc, j * 128:(j + 1) * 128], memT[:, kc, :], kc == 0, kc == KC - 1, [dw, d_memT], [pd])
                        act.op(lambda e: e.activation(out=KT[:, cb, :], in_=pb[:, 0:256], func=AF.Copy), reads=[pd], writes=[d_KT])
                else:
                    for mc in range(2):
                        pb, pd = nb()
                        for kc in range(KC):
                            mm(pb, memT[:, kc, mc * 128:(mc + 1) * 128], ws[:, kc, :], kc == 0, kc == KC - 1, [dw, d_memT], [pd])
                        dve.op(lambda e: e.tensor_copy(out=Vb[:, mc, (g - 4) * 512:(g - 3) * 512], in_=pb), reads=[pd], writes=[d_Vb])
            fw.barrier()
            alM = Alloc(big, M_OFF + 4096, A_OFF)
            wbs6 = [(r3(alM.bf16(16 * 256), 16), Dep()) for _ in range(2)]
            qscale = float(512 ** -0.5)
            for g in range(8):
                ws, dw = wbs6[g % 2]
                pool.dma(ws, w_q[:, g * 256:(g + 1) * 256].rearrange("(kc p) n -> p kc n", p=128), writes=[dw])
                for j in range(2):
                    cb = g * 2 + j
                    for tq in range(NTQ):
                        ts_ = slice(tq * 512, (tq + 1) * 512)
                        pb, pd = nb()
                        for kc in range(KC):
                            mm(pb, ws[:, kc, j * 128:(j + 1) * 128], bufA[:, kc, ts_], kc == 0, kc == KC - 1, [dw, d_A], [pd])
                        if tq % 2 == 0:
                            act.op(lambda e: e.activation(out=bufB[:, cb, ts_], in_=pb, func=AF.Copy, scale=qscale), reads=[pd], writes=[d_B])
                        else:
                            dve.op(lambda e: e.tensor_scalar(out=bufB[:, cb, ts_], in0=pb, scalar1=qscale, scalar2=None, op0=ALU.mult), reads=[pd], writes=[d_B])
            fw.barrier()
            alM = Alloc(big, M_OFF + 4096, A_OFF)
            Es = [(r3(alM.bf16(2 * 512), 2), Dep()) for _ in range(2)]
            rinvs = [(alM.f32(512), Dep()) for _ in range(2)]
            it = 0
            for h in range(4):
                for tq in range(NTQ):
                    ts_ = slice(tq * 512, (tq + 1) * 512)
                    E, dE = Es[it % 2]
                    rinv, dri = rinvs[it % 2]
                    it += 1
                    for mc in range(2):
                        pb, pd = nb()
                        for c in range(4):
                            mm(pb, KT[:, h * 4 + c, mc * 128:(mc + 1) * 128], bufB[:, h * 4 + c, ts_], c == 0, c == 3, [d_KT, d_B], [pd])
                        act.op(lambda e: e.activation(out=E[:, mc, :], in_=pb, func=AF.Exp), reads=[pd], writes=[dE])
                    pb, pd = nb()
                    for mc in range(2):
                        mm(pb, onesb, E[:, mc, :], mc == 0, mc == 1, [d_const, dE], [pd])
                    dve.op(lambda e: e.reciprocal(out=rinv, in_=pb), reads=[pd], writes=[dri])
                    for c in range(4):
                        pb, pd = nb()
                        for mc in range(2):
                            mm(pb, Vb[:, mc, h * 512 + c * 128:h * 512 + (c + 1) * 128], E[:, mc, :], mc == 0, mc == 1, [d_Vb, dE], [pd])
                        dve.op(lambda e: e.tensor_tensor(out=bufA[:, h * 4 + c, ts_], in0=pb, in1=rinv, op=ALU.mult), reads=[pd, dri], writes=[d_A])
            fw.barrier()
            out_proj(bufA, d_A, w_o, lambda rows, cols: h1_s[rows, cols], h2_s, Alloc(big, B0_OFF, M_OFF))
            fw.barrier()
            if "h2" in dbg_aps:
                tmp = Alloc(big, B0_OFF, M_OFF).f32(D)
                dtmp = Dep()
                for tt in range(NTT):
                    sp.dma(tmp, h2_s[tt * 128:(tt + 1) * 128, :], writes=[dtmp])
                    sp.dma(dbg_aps["h2"][tt * 128:(tt + 1) * 128, :], tmp, reads=[dtmp])
                fw.barrier()

        if stop >= 8:
            IOA = bass.IndirectOffsetOnAxis
            bc_reg = es.enter_context(nc.gpsimd.register("bc"))
            nc.gpsimd.reg_mov(bc_reg, NROW - 1)
            BCV = nc.gpsimd.snap(bc_reg)
            bw_reg = es.enter_context(nc.gpsimd.register("bw"))
            nc.gpsimd.reg_mov(bw_reg, 8191)
            BWV = nc.gpsimd.snap(bw_reg)
            al8 = Alloc(big, B0_OFF, WORDS)
            LT = al8.f32(128)
            iop = al8.f32(1)
            siota = al8.f32(32)
            thr8 = al8.f32(8)
            p1a, p2a = al8.f32(16), al8.f32(16)
            pos1i = al8.f32(16).bitcast(I32)
            pos2i = al8.f32(16).bitcast(I32)
            widx = al8.f32(NSLOT * 4).bitcast(I32)
            d_c8, d_pos, d_widx, d_pp = Dep(), Dep(), Dep(), Dep()
            P8_TOP = al8.top
            sp.dma(LT, cst[:, 768:896], writes=[d_c8])
            sp.dma(iop, cst[:, 896:897], writes=[d_c8], allow_slow_non_contiguous=True)
            sp.dma(siota, cst[:, 897:929], writes=[d_c8])
            sp.dma(thr8, cst[:, 929:937], writes=[d_c8])
            fw.barrier()
            xnb_all = r3(al8.bf16(16 * D), 16)
            d_xnb = [Dep() for _ in range(16)]
            xts = [(al8.f32(D), Dep()) for _ in range(3)]
            xn32s = [(al8.f32(D), Dep()) for _ in range(2)]
            junk = al8.bf16(D)
            d_junk = Dep()
            h32s = [(r3(al8.f32(16 * 128), 16), Dep()) for _ in range(2)]
            wr32 = r3(al8.f32(16 * 20), 16)
            d_wr = Dep()
            logits = r3(al8.f32(16 * 20), 16)
            d_log = Dep()
            sts = [(al8.f32(8), Dep()) for _ in range(4)]
            sp.dma(wr32, w_r.rearrange("(kc p) n -> p kc n", p=128), writes=[d_wr])
            load_gB(2)

            def a8_stage_a(tt):
                xt, dx = xts[tt % 3]
                xn32, dxn = xn32s[tt % 2]
                st, d_st = sts[tt % 4]
                sp.dma(xt, h2_s[tt * 128:(tt + 1) * 128, :], writes=[dx])
                act.op(lambda e: e.activation(out=junk, in_=xt, func=AF.Square, accum_out=st[:, 0:1]), reads=[dx], writes=[d_junk, d_st])
                dve.op(lambda e: e.tensor_scalar(out=st[:, 1:2], in0=st[:, 0:1], scalar1=1.0 / D, scalar2=1e-6, op0=ALU.mult, op1=ALU.add),
                       reads=[d_st], writes=[d_st])
                act.op(lambda e: e.activation(out=st[:, 2:3], in_=st[:, 1:2], func=AF.Sqrt), reads=[d_st], writes=[d_st])
                dve.op(lambda e: e.reciprocal(out=st[:, 3:4], in_=st[:, 2:3]), reads=[d_st], writes=[d_st])
                dve.op(lambda e: e.scalar_tensor_tensor(out=xn32, in0=xt, scalar=st[:, 3:4], in1=gBt, op0=ALU.mult, op1=ALU.mult),
                       reads=[dx, d_st, d_gB], writes=[dxn])
                act.op(lambda e: e.activation(out=xnb_all[:, tt, :], in_=xn32, func=AF.Copy), reads=[dxn], writes=[d_xnb[tt]])

            def a8_stage_b(tt):
                xn32, dxn = xn32s[tt % 2]
                h32, d_h32 = h32s[tt % 2]
                for q in range(4):
                    pf, pdf = nb()
                    for j in range(4):
                        kc = q * 4 + j
                        mm(pf[:, j * 128:(j + 1) * 128], xn32[:, kc * 128:(kc + 1) * 128], identf, True, True, [dxn, d_const], [pdf])
                    if q % 2 == 0:
                        dve.op(lambda e: e.tensor_copy(out=h32[:, q * 4:q * 4 + 4, :], in_=r3(pf, 4)), reads=[pdf], pws=[d_h32])
                    else:
                        act.op(lambda e: e.activation(out=h32[:, q * 4:q * 4 + 4, :], in_=r3(pf, 4), func=AF.Copy), reads=[pdf], pws=[d_h32])
                pb, pd = nb()
                for kc in range(KC):
                    mm(pb[:, 0:20], h32[:, kc, :], wr32[:, kc, :], kc == 0, False, [d_h32, d_wr], [pd])
                mm(pb[:, 0:20], ones1[0:1, 0:128], brow[0:1, 0:20], False, True, [d_const], [pd])
                dve.op(lambda e: e.tensor_copy(out=logits[:, tt, :], in_=pb[:, 0:20]), reads=[pd], writes=[d_log])

            for tt in range(17):
                if tt < 16:
                    a8_stage_a(tt)
                if tt >= 1:
                    a8_stage_b(tt - 1)
            NT_ = 16
            rt = [al8.f32(NT_ * 4) for _ in range(12)]
            rt4 = al8.f32(NT_ * 16)
            sel1 = al8.f32(NT_ * 16)
            sel2 = al8.f32(NT_ * 16)
            ind = al8.f32(NT_ * 16)
            tot = r3(al8.f32(NT_ * 16), NT_)
            tcum = r3(al8.f32(NT_ * 16), NT_)
            posall = al8.f32(NT_ * 16)
            ptmp = al8.f32(NT_ * 16)
            c8 = al8.f32(16 * 8)
            cnt, nsl, bsl, bsl256 = al8.f32(16), al8.f32(16), al8.f32(16), al8.f32(16)
            total = al8.f32(1)
            es32 = al8.f32(NSLOT * 16)
            esf, unused, wbase = al8.f32(NSLOT), al8.f32(NSLOT), al8.f32(NSLOT)
            pos1f, pos2f = al8.f32(16), al8.f32(16)
            widxf = al8.f32(NSLOT * 4).rearrange("p (s q) -> p s q", q=4)
            d_rt = Dep()
            lg = logits[:, :, 0:4]
            le = logits[:, :, 4:20].rearrange("p t (g e) -> p t g e", g=4)
            gmax, gsum, gw, m1, m2 = [rt[i][:, 0:NT_] for i in range(5)]
            goh, gsh, esel, oh1, e2 = [r3(rt[7 + i], NT_) for i in range(5)]
            t4 = rt4.rearrange("p (t g e) -> p t g e", t=NT_, g=4)

            def bc3(v):
                return v.unsqueeze(2).to_broadcast([128, NT_, 4])

            def v4(a):
                return a.rearrange("p (t g e) -> p t g e", t=NT_, g=4)
            R = [d_log, d_rt, d_c8]
            W_ = [d_rt]
            dve.op(lambda e: e.tensor_reduce(out=gmax, in_=lg, axis=AX.X, op=ALU.max), R, W_)
            dve.op(lambda e: e.tensor_tensor(out=goh, in0=lg, in1=bc3(gmax), op=ALU.is_equal), R, W_)
            dve.op(lambda e: e.tensor_tensor(out=gsh, in0=lg, in1=bc3(gmax), op=ALU.subtract), R, W_)
            act.op(lambda e: e.activation(out=gsh, in_=gsh, func=AF.Exp), R, W_)
            dve.op(lambda e: e.tensor_reduce(out=gsum, in_=gsh, axis=AX.X, op=ALU.add), R, W_)
            dve.op(lambda e: e.reciprocal(out=gw, in_=gsum), R, W_)
            dve.op(lambda e: e.tensor_tensor(out=t4, in0=le, in1=goh.unsqueeze(3).to_broadcast([128, NT_, 4, 4]), op=ALU.mult), R, W_)
            dve.op(lambda e: e.tensor_reduce(out=esel, in_=t4.rearrange("p t g e -> p t e g"), axis=AX.X, op=ALU.add), R, W_)
            dve.op(lambda e: e.tensor_reduce(out=m1, in_=esel, axis=AX.X, op=ALU.max), R, W_)
            dve.op(lambda e: e.tensor_tensor(out=oh1, in0=esel, in1=bc3(m1), op=ALU.is_equal), R, W_)
            dve.op(lambda e: e.scalar_tensor_tensor(out=e2, in0=oh1, scalar=-1e30, in1=esel, op0=ALU.mult, op1=ALU.add), R, W_)
            dve.op(lambda e: e.tensor_reduce(out=m2, in_=e2, axis=AX.X, op=ALU.max), R, W_)
            dve.op(lambda e: e.tensor_tensor(out=e2, in0=e2, in1=bc3(m2), op=ALU.is_equal), R, W_)
            dve.op(lambda e: e.tensor_tensor(out=p1a, in0=m1, in1=m2, op=ALU.subtract), R, W_ + [d_pp])
            act.op(lambda e: e.activation(out=p1a, in_=p1a, func=AF.Sigmoid), R + [d_pp], W_ + [d_pp])
            dve.op(lambda e: e.tensor_scalar(out=p2a, in0=p1a, scalar1=-1.0, scalar2=1.0, op0=ALU.mult, op1=ALU.add), R + [d_pp], W_ + [d_pp])
            dve.op(lambda e: e.tensor_tensor(out=p1a, in0=p1a, in1=gw, op=ALU.mult), R + [d_pp], W_ + [d_pp])
            dve.op(lambda e: e.tensor_tensor(out=p2a, in0=p2a, in1=gw, op=ALU.mult), R + [d_pp], W_ + [d_pp])
            dve.op(lambda e: e.tensor_tensor(out=v4(sel1), in0=goh.unsqueeze(3).to_broadcast([128, NT_, 4, 4]),
                                             in1=oh1.unsqueeze(2).to_broadcast([128, NT_, 4, 4]), op=ALU.mult), R, W_)
            dve.op(lambda e: e.tensor_tensor(out=v4(sel2), in0=goh.unsqueeze(3).to_broadcast([128, NT_, 4, 4]),
                                             in1=e2.unsqueeze(2).to_broadcast([128, NT_, 4, 4]), op=ALU.mult), R, W_)
            dve.op(lambda e: e.tensor_tensor(out=ind, in0=sel1, in1=sel2, op=ALU.add), R, W_)
            pw, pdw = nb()
            mm(pw[:, 0:256], LT, ind, True, True, [d_rt, d_c8], [pdw])
            pt_, pdt = nb()
            mm(pt_[:, 0:256], ones1, ind, True, True, [d_rt, d_const], [pdt])
            dve.op(lambda e: e.tensor_copy(out=tot, in_=r3(pt_[:, 0:256], NT_)), R + [pdt], W_)
            dve.op(lambda e: e.memset(tcum[:, 0, :], 0.0), R, W_)
            for tt in range(1, NT_):
                dve.op(lambda e: e.tensor_tensor(out=tcum[:, tt, :], in0=tcum[:, tt - 1, :], in1=tot[:, tt - 1, :], op=ALU.add), R, W_)
            dve.op(lambda e: e.tensor_tensor(out=cnt, in0=tcum[:, NT_ - 1, :], in1=tot[:, NT_ - 1, :], op=ALU.add), R, W_)
            dve.op(lambda e: e.tensor_tensor(out=r3(c8, 16), in0=cnt.unsqueeze(2).to_broadcast([128, 16, 8]),
                                             in1=thr8.unsqueeze(1).to_broadcast([128, 16, 8]), op=ALU.is_gt), R, W_)
            dve.op(lambda e: e.tensor_reduce(out=nsl, in_=r3(c8, 16), axis=AX.X, op=ALU.add), R, W_)
            dve.op(lambda e: e.memset(bsl[:, 0:1], 0.0), R, W_)
            for ex in range(1, 16):
                dve.op(lambda e: e.tensor_tensor(out=bsl[:, ex:ex + 1], in0=bsl[:, ex - 1:ex], in1=nsl[:, ex - 1:ex], op=ALU.add), R, W_)
            dve.op(lambda e: e.tensor_tensor(out=total, in0=bsl[:, 15:16], in1=nsl[:, 15:16], op=ALU.add), R, W_)
            dve.op(lambda e: e.tensor_scalar(out=bsl256, in0=bsl, scalar1=float(SL), scalar2=None, op0=ALU.mult), R, W_)
            dve.op(lambda e: e.tensor_tensor(out=posall, in0=pw[:, 0:256], in1=tcum.rearrange("p t e -> p (t e)"), op=ALU.add), R + [pdw], W_)
            dve.op(lambda e: e.tensor_tensor(out=r3(posall, NT_), in0=r3(posall, NT_), in1=bsl256.unsqueeze(1).to_broadcast([128, NT_, 16]), op=ALU.add), R, W_)
            dve.op(lambda e: e.tensor_tensor(out=ptmp, in0=posall, in1=sel1, op=ALU.mult), R, W_)
            dve.op(lambda e: e.tensor_reduce(out=pos1f, in_=r3(ptmp, NT_), axis=AX.X, op=ALU.add), R, W_)
            dve.op(lambda e: e.tensor_tensor(out=ptmp, in0=posall, in1=sel2, op=ALU.mult), R, W_)
            dve.op(lambda e: e.tensor_reduce(out=pos2f, in_=r3(ptmp, NT_), axis=AX.X, op=ALU.add), R, W_)
            dve.op(lambda e: e.tensor_copy(out=pos1i, in_=pos1f), R, W_ + [d_pos])
            dve.op(lambda e: e.tensor_copy(out=pos2i, in_=pos2f), R, W_ + [d_pos])
            dve.op(lambda e: e.tensor_tensor(out=r3(es32, NSLOT), in0=bsl.unsqueeze(1).to_broadcast([128, NSLOT, 16]),
                                             in1=siota[:, 0:NSLOT].unsqueeze(2).to_broadcast([128, NSLOT, 16]), op=ALU.is_le), R, W_)
            dve.op(lambda e: e.tensor_reduce(out=esf, in_=r3(es32, NSLOT), axis=AX.X, op=ALU.add), R, W_)
            dve.op(lambda e: e.tensor_scalar(out=unused, in0=siota[:, 0:NSLOT], scalar1=total[:, 0:1], scalar2=1.0e6, op0=ALU.is_ge, op1=ALU.mult), R, W_)
            dve.op(lambda e: e.tensor_scalar(out=wbase, in0=esf, scalar1=-1.0, scalar2=512.0, op0=ALU.add, op1=ALU.mult), R, W_)
            dve.op(lambda e: e.tensor_tensor(out=wbase, in0=wbase, in1=unused, op=ALU.add), R, W_)
            dve.op(lambda e: e.tensor_scalar(out=wbase, in0=wbase, scalar1=iop[:, 0:1], scalar2=None, op0=ALU.add), R, W_)
            for q in range(4):
                dve.op(lambda e: e.tensor_scalar(out=widxf[:, :, q], in0=wbase, scalar1=float(128 * q), scalar2=None, op0=ALU.add), R, W_)
            dve.op(lambda e: e.tensor_copy(out=widx, in_=widxf.rearrange("p s q -> p (s q)")), R, W_ + [d_widx])
            if "route" in dbg_aps:
                sp.dma(dbg_aps["route"][:, 0:16], pos1f, reads=[d_rt])
                sp.dma(dbg_aps["route"][:, 16:32], pos2f, reads=[d_rt])
                sp.dma(dbg_aps["route"][:, 32:64], wbase, reads=[d_rt])
                sp.dma(dbg_aps["route"][:, 64:80], p1a, reads=[d_pp])
                sp.dma(dbg_aps["route"][:, 80:96], p2a, reads=[d_pp])
                sp.dma(dbg_aps["route"][:, 96:112], cnt, reads=[d_rt])
            for tt in range(16):
                for posi in (pos1i, pos2i):
                    pool.dma_fn(lambda e: e.indirect_dma_start(out=Xs, out_offset=IOA(ap=posi[:, tt:tt + 1], axis=0), in_=xnb_all[:, tt, :], in_offset=None,
                                                               bounds_check=BCV, oob_is_err=False),
                                reads=[d_xnb[tt], d_pos], writes=[d_Xs])
            fw.barrier()
            ald = Alloc(big, P8_TOP, WORDS)
            wbufs = [(ald.bf16(8192), [Dep() for _ in range(4)]) for _ in range(6)]
            xsls = [(r3(ald.bf16(NA * D), NA), Dep()) for _ in range(2)]
            XTs = [(r3(ald.bf16(16 * SL), 16), Dep()) for _ in range(2)]
            hids = [(r3(ald.bf16(4 * SL), 4), Dep()) for _ in range(2)]
            sbs = [(ald.bf16(SL), Dep()) for _ in range(2)]
            yos = [(ald.f32(D), Dep()) for _ in range(2)]
            cnt8 = dict(yi=0, ei=0)

            def wload(i, s):
                wsl = []
                for m, wl in enumerate((wg_l, wu_l, wd_l)):
                    buf, deps = wbufs[(3 * i + m) % 6]
                    for q in range(4):
                        pool.dma_fn(lambda e: e.indirect_dma_start(out=buf[:, q * 2048:(q + 1) * 2048], out_offset=None, in_=wl,
                                                                   in_offset=IOA(ap=widx[:, s * 4 + q:s * 4 + q + 1], axis=0), bounds_check=BWV, oob_is_err=False),
                                    reads=[d_widx], writes=[deps[q]])
                    wsl.append((buf, deps))
                return wsl

            def xload(i, s):
                xsl, dxs = xsls[i % 2]
                sp.dma(xsl, Xs[s * SL:(s + 1) * SL, :].rearrange("(a p) n -> p a n", p=128), reads=[d_Xs], writes=[dxs])

            def emit_T(i, s):
                xsl, dxs = xsls[i % 2]
                XT, dXT = XTs[i % 2]
                for a in range(NA):
                    for q4 in range(4):
                        pb, pd = nb()
                        for j in range(4):
                            kc = q4 * 4 + j
                            mm(pb[:, j * 128:(j + 1) * 128], xsl[:, a, kc * 128:(kc + 1) * 128], identb, True, True, [dxs, d_const], [pd])
                        cnt8["ei"] += 1
                        if cnt8["ei"] % 2 == 0:
                            act.op(lambda e: e.activation(out=XT[:, q4 * 4:q4 * 4 + 4, a * 128:(a + 1) * 128], in_=r3(pb, 4), func=AF.Copy), reads=[pd], pws=[dXT])
                        else:
                            dve.op(lambda e: e.tensor_copy(out=XT[:, q4 * 4:q4 * 4 + 4, a * 128:(a + 1) * 128], in_=r3(pb, 4)), reads=[pd], pws=[dXT])

            def emit_GU(i, s, wsl):
                wg, dwg = r3(wsl[0][0], 16), wsl[0][1]
                wu, dwu = r3(wsl[1][0], 16), wsl[1][1]
                XT, dXT = XTs[i % 2]
                hid, dhid = hids[i % 2]
                for ffc in range(4):
                    pg, pdg = nb()
                    for kc in range(KC):
                        mm(pg[:, 0:SL], wg[:, kc, ffc * 128:(ffc + 1) * 128], XT[:, kc, :], kc == 0, kc == KC - 1, [dwg[kc // 4], dXT], [pdg])
                    pu, pdu = nb()
                    for kc in range(KC):
                        mm(pu[:, 0:SL], wu[:, kc, ffc * 128:(ffc + 1) * 128], XT[:, kc, :], kc == 0, kc == KC - 1, [dwu[kc // 4], dXT], [pdu])
                    sb_, dsb = sbs[ffc % 2]
                    act.op(lambda e: e.activation(out=sb_, in_=pg[:, 0:SL], func=AF.Silu), reads=[pdg], writes=[dsb])
                    dve.op(lambda e: e.tensor_tensor(out=hid[:, ffc, :], in0=pu[:, 0:SL], in1=sb_, op=ALU.mult), reads=[pdu, dsb], writes=[dhid])

            def emit_D(i, s, wsl):
                wd, dwd = r3(wsl[2][0], 4), wsl[2][1]
                hid, dhid = hids[i % 2]
                for a in range(NA):
                    yo, dyo = yos[cnt8["yi"] % 2]
                    cnt8["yi"] += 1
                    for dblk in range(4):
                        ds_ = slice(dblk * 512, (dblk + 1) * 512)
                        pb, pd = nb()
                        for ffc in range(4):
                            mm(pb, hid[:, ffc, a * 128:(a + 1) * 128], wd[:, ffc, ds_], ffc == 0, ffc == 3, [dhid, dwd[ffc]], [pd])
                        if dblk % 2 == 0:
                            act.op(lambda e: e.activation(out=yo[:, ds_], in_=pb, func=AF.Copy), reads=[pd], pws=[dyo])
                        else:
                            dve.op(lambda e: e.tensor_copy(out=yo[:, ds_], in_=pb), reads=[pd], pws=[dyo])
                    r0 = s * SL + a * 128
                    sp.dma(Ys[r0:r0 + 128, :], yo, reads=[dyo], writes=[d_Ys])

            lo_n = NSLOT - NSLOT // 3
            lo, hi = list(range(lo_n)), list(range(NSLOT - 1, lo_n - 1, -1))
            order = []
            while lo or hi:
                order += lo[:2]
                lo = lo[2:]
                if hi:
                    order.append(hi.pop(0))
            assert sorted(order) == list(range(NSLOT))
            xload(0, order[0])
            emit_T(0, order[0])
            for i, s in enumerate(order):
                wsl = wload(i, s)
                if i + 1 < NSLOT:
                    xload(i + 1, order[i + 1])
                emit_GU(i, s, wsl)
                if i + 1 < NSLOT:
                    emit_T(i + 1, order[i + 1])
                emit_D(i, s, wsl)
            fw.barrier()
            ale = Alloc(big, P8_TOP, WORDS)
            cts = [(ale.f32(D), Dep()) for _ in range(2)]
            y1s = [(ale.f32(D), Dep()) for _ in range(2)]
            y2s = [(ale.f32(D), Dep()) for _ in range(2)]
            junk2 = ale.bf16(D)
            sts2 = [(ale.f32(8), Dep()) for _ in range(4)]
            load_gB(4)
            for tt in range(16):
                xt, dx = cts[tt % 2]
                y1, dy1 = y1s[tt % 2]
                y2, dy2 = y2s[tt % 2]
                st, d_st = sts2[tt % 4]
                pool.dma(xt, h2_s[tt * 128:(tt + 1) * 128, :], writes=[dx])
                pool.dma_fn(lambda e: e.indirect_dma_start(out=y1, out_offset=None, in_=Ys, in_offset=IOA(ap=pos1i[:, tt:tt + 1], axis=0),
                                                           bounds_check=BCV, oob_is_err=False), reads=[d_Ys, d_pos], writes=[dy1])
                pool.dma_fn(lambda e: e.indirect_dma_start(out=y2, out_offset=None, in_=Ys, in_offset=IOA(ap=pos2i[:, tt:tt + 1], axis=0),
                                                           bounds_check=BCV, oob_is_err=False), reads=[d_Ys, d_pos], writes=[dy2])
                dve.op(lambda e: e.scalar_tensor_tensor(out=xt, in0=y1, scalar=p1a[:, tt:tt + 1], in1=xt, op0=ALU.mult, op1=ALU.add),
                       reads=[dy1, dx, d_pp], writes=[dx])
                dve.op(lambda e: e.scalar_tensor_tensor(out=xt, in0=y2, scalar=p2a[:, tt:tt + 1], in1=xt, op0=ALU.mult, op1=ALU.add),
                       reads=[dy2, dx, d_pp], writes=[dx])
                if "h3" in dbg_aps:
                    sp.dma(dbg_aps["h3"][tt * 128:(tt + 1) * 128, :], xt, reads=[dx])
                act.op(lambda e: e.activation(out=junk2, in_=xt, func=AF.Square, accum_out=st[:, 0:1]), reads=[dx], writes=[d_junk, d_st])
                dve.op(lambda e: e.tensor_scalar(out=st[:, 1:2], in0=st[:, 0:1], scalar1=1.0 / D, scalar2=1e-6, op0=ALU.mult, op1=ALU.add),
                       reads=[d_st], writes=[d_st])
                act.op(lambda e: e.activation(out=st[:, 2:3], in_=st[:, 1:2], func=AF.Sqrt), reads=[d_st], writes=[d_st])
                dve.op(lambda e: e.reciprocal(out=st[:, 3:4], in_=st[:, 2:3]), reads=[d_st], writes=[d_st])
                dve.op(lambda e: e.scalar_tensor_tensor(out=xt, in0=xt, scalar=st[:, 3:4], in1=gBt, op0=ALU.mult, op1=ALU.mult),
                       reads=[dx, d_st, d_gB], writes=[dx])
                sp.dma(out[tt * 128:(tt + 1) * 128, :], xt, reads=[dx])
            fw.barrier()

        fw.barrier()
    return nc


def host_consts(inp):
    l = 0
    f = np.float32
    gBh = np.stack([np.broadcast_to(v, (128, D)) for v in (inp["norm_mix_g"][l], inp["norm_xattn_g"][l], inp["norm_ffn_g"][l],
                                                            inp["norm_mem_g"][l], inp["norm_final_g"])]).astype(f)
    pvh = np.zeros((128, NPV), f)

    def col(v, n):
        return np.ascontiguousarray(np.asarray(v, f).reshape(n, 128).T)
    pvh[:, 0:8] = col(inp["pool_scale"][l], 8)
    mu = np.asarray(inp["rwkv_mu"][l], f)
    pvh[:, 8:32] = col(mu[0:3072], 24)
    pvh[:, 32] = mu[3072:3200]
    pvh[:, 33] = mu[3200:3328]
    pvh[0:32, 34] = mu[3328:3360]
    pvh[:, 35:43] = col(inp["rwkv_w0"][l], 8)
    pvh[:, 43:51] = col(inp["rwkv_a0"][l], 8)
    pvh[:, 51:59] = col(inp["rwkv_k_k"][l], 8)
    pvh[:, 59:67] = col(inp["rwkv_k_a"][l], 8)
    pvh[:, 67:75] = col(inp["rwkv_ln_w"][l], 8)
    pvh[:, 75:83] = col(inp["rwkv_ln_b"][l], 8)
    pvh[:, 83:91] = col(np.asarray(inp["rwkv_r_k"][l]).reshape(-1), 8)
    cst = np.zeros((128, 1024), f)
    p = np.arange(128)
    cst[:, 0:128] = np.eye(128, dtype=f)
    cst[:, 128:256] = (p[:, None] // 64 == p[None, :] // 64).astype(f)
    s = p % 64
    tcol = np.arange(64)
    strict = (s[:, None] < tcol[None, :]).astype(f)
    incl = (s[:, None] <= tcol[None, :]).astype(f)
    one = np.concatenate([strict, incl], 1)
    cst[:, 256:512] = np.concatenate([one, one], 1)
    low = (s[:, None] > tcol[None, :]).astype(f)
    cst[:, 512:640] = np.concatenate([low, low], 1)
    cst[:, 640:704] = (s[:, None] == tcol[None, :]).astype(f)
    tt = np.arange(16)
    for gi, w in enumerate((2, 4, 8, 16)):
        cst[:, 704 + gi * 16:704 + (gi + 1) * 16] = (1.0 / np.minimum(tt + 1, w)).astype(f)[None, :]
    cst[:, 768:896] = (p[:, None] < p[None, :]).astype(f)
    cst[:, 896] = p.astype(f)
    cst[:, 897:929] = np.arange(32, dtype=f)[None, :]
    cst[:, 929:937] = (float(SL) * np.arange(8, dtype=f))[None, :]
    rm = np.ones((128, T), f)
    rm[:, ::64] = 0.0
    w_r = np.concatenate([inp["moe_w_group"][l], inp["moe_w_expert"][l]], 1).astype(f)
    b_r = np.concatenate([inp["moe_b_group"][l], inp["moe_b_expert"][l]])[None, :].astype(f)
    return dict(gB=gBh, pv=pvh, cst=cst, rmask=rm, w_r=np.ascontiguousarray(w_r), b_r=b_r)


def make_in_maps(inp, cores):
    l = 0
    c = host_consts(inp)
    shared = dict(
        w_in=inp["w_in"][l], pool_w=inp["pool_w"][l], w2=inp["rwkv_w2"][l], a2=inp["rwkv_a2"][l], g2=inp["rwkv_g2"][l],
        w_out=inp["w_out"][l], w_q=inp["xattn_w_q"][l], w_kv=inp["xattn_w_kv"][l], w_o=inp["xattn_w_o"][l],
        wg_l=np.asarray(inp["moe_w_gate"][l], np.float32).reshape(16, 4, 4, 128, 512).transpose(0, 1, 3, 2, 4).reshape(8192, 2048),
        wu_l=np.asarray(inp["moe_w_up"][l], np.float32).reshape(16, 4, 4, 128, 512).transpose(0, 1, 3, 2, 4).reshape(8192, 2048),
        wd_l=np.asarray(inp["moe_w_down"][l], np.float32).reshape(8192, 2048), **c)
    shared = {k: np.ascontiguousarray(np.asarray(v, np.float32)) for k, v in shared.items()}
    maps = []
    for b in cores:
        m = dict(shared)
        m["x"] = np.ascontiguousarray(inp["x"][b])
        m["mem"] = np.ascontiguousarray(inp["mem"][b])
        maps.append(m)
    return maps


def kernel(**inputs):
    inp = {k: np.asarray(v) for k, v in inputs.items()}
    nc = build()
    maps = make_in_maps(inp, list(range(8)))
    res = run_bass_kernel_spmd(nc, maps, core_ids=list(range(8)))
    return np.stack([np.asarray(r["out"]) for r in res.results], 0).astype(np.float32)
```

```python
import numpy as np
import concourse.bass as bass
import concourse.mybir as mybir
from concourse.bass_utils import run_bass_kernel_spmd
from contextlib import ExitStack

F32 = mybir.dt.float32
BF16 = mybir.dt.bfloat16
I32 = mybir.dt.int32
AF = mybir.ActivationFunctionType
ALU = mybir.AluOpType
AX = mybir.AxisListType

D = 2048
KC = 16
T = 2048
NTT = T // 128
NTQ = T // 512
NCH = T // 64
PAD = 16
CDEC = float(np.exp(-0.5))
WORDS = 51200
NSLOT, SL = 26, 384
NA = SL // 128


class Dep:
    __slots__ = ("w", "r", "p")

    def __init__(self):
        self.w = None
        self.r = {}
        self.p = {}


class Eng:
    def __init__(self, fw, name, b, is_pe=False):
        self.fw, self.name, self.b, self.is_pe = fw, name, b, is_pe
        self.sem = fw.new_sem(name)
        self.cnt = 0
        self.waited = {}
        self.dma_slots = None
        self.dma_i = 0

    def _wait(self, tok):
        sem, val = tok
        if self.waited.get(id(sem), 0) < val:
            self.b.wait_ge(sem, val)
            self.waited[id(sem)] = val

    def _collect(self, reads, writes, pws=()):
        def w_(t):
            if t is not None and not (self.is_pe and t[0] is self.sem):
                self._wait(t)
        for d in reads:
            w_(d.w)
            for t in d.p.values():
                w_(t)
        for d in writes:
            w_(d.w)
            for t in d.p.values():
                w_(t)
            for t in d.r.values():
                w_(t)
        for d in pws:
            w_(d.w)
            for t in d.r.values():
                w_(t)

    def op(self, fn, reads=(), writes=(), pws=()):
        self._collect(reads, writes, pws)
        inst = fn(self.b)
        self.cnt += 1
        inst.then_inc(self.sem, 1)
        tok = (self.sem, self.cnt)
        for d in reads:
            d.r[id(self.sem)] = tok
        for d in writes:
            d.w = tok
            d.r = {}
            d.p = {}
        for d in pws:
            d.p[id(self.sem)] = tok
        return tok

    def dma(self, out, in_, reads=(), writes=(), **kw):
        return self.dma_fn(lambda e: e.dma_start(out=out, in_=in_, **kw), reads, writes)

    def dma_fn(self, fn, reads=(), writes=()):
        if self.dma_slots is None:
            self.dma_slots = [[self.fw.new_sem(f"{self.name}_d{i}"), 0] for i in range(8)]
        self._collect(reads, writes)
        slot = self.dma_slots[self.dma_i % len(self.dma_slots)]
        self.dma_i += 1
        if slot[1] > 0:
            self._wait((slot[0], slot[1]))
        inst = fn(self.b)
        slot[1] += 16
        inst.then_inc(slot[0], 16)
        tok = (slot[0], slot[1])
        for d in reads:
            d.r[id(slot[0])] = tok
        for d in writes:
            d.w = tok
            d.r = {}
            d.p = {}
        return tok


class FW:
    def __init__(self, nc, es):
        self.nc, self.es = nc, es
        self.pe = Eng(self, "pe", nc.tensor, True)
        self.act = Eng(self, "act", nc.scalar)
        self.dve = Eng(self, "dve", nc.vector)
        self.pool = Eng(self, "pool", nc.gpsimd)
        self.sp = Eng(self, "sp", nc.sync)
        self.engs = [self.pe, self.act, self.dve, self.pool, self.sp]

    def new_sem(self, name):
        return self.es.enter_context(self.nc.semaphore(name))

    def barrier(self):
        toks = []
        for e in self.engs:
            if e.cnt > 0:
                toks.append((e.sem, e.cnt))
            if e.dma_slots:
                for s in e.dma_slots:
                    if s[1] > 0:
                        toks.append((s[0], s[1]))
        for e in self.engs:
            for t in toks:
                if t[0] is not e.sem:
                    e._wait(t)


class Alloc:
    def __init__(self, big, start, end):
        self.big, self.top, self.end = big, start, end

    def f32(self, n):
        a = self.big[:, self.top:self.top + n]
        self.top += n
        assert self.top <= self.end, (self.top, self.end)
        return a

    def bf16(self, n):
        w = (n + 1) // 2
        a = self.big[:, self.top:self.top + w].bitcast(BF16)
        self.top += w
        assert self.top <= self.end, (self.top, self.end)
        return a[:, 0:n]


def r3(ap, a):
    return ap.rearrange("p (a b) -> p a b", a=a)


PV = dict(pool_scale=0, mu_rkv=8, mu_lo=32, w0=35, a0=43, k_k=51, k_a=59, ln_w=67, ln_b=75, r_k=83, omka=91)
NPV = 99


def build(stop=99, dbg=()):
    nc = bass.Bass("TRN2", target_bir_lowering=False)

    def din(name, shape):
        return nc.dram_tensor(name, list(shape), F32, kind="ExternalInput").ap()

    x = din("x", [T, D])
    mem = din("mem", [256, D])
    w_in = din("w_in", [D, 4384])
    pool_w = din("pool_w", [4, 256, 256])
    w2 = din("w2", [64, 1024])
    a2 = din("a2", [64, 1024])
    g2 = din("g2", [160, 1024])
    w_out = din("w_out", [D, D])
    w_q = din("w_q", [D, D])
    w_kv = din("w_kv", [D, 2 * D])
    w_o = din("w_o", [D, D])
    w_r = din("w_r", [D, 20])
    b_r = din("b_r", [1, 20])
    wg_l = din("wg_l", [8192, 2048])
    wu_l = din("wu_l", [8192, 2048])
    wd_l = din("wd_l", [8192, 2048])
    gB = din("gB", [5, 128, D])
    pvd = din("pv", [128, NPV])
    cst = din("cst", [128, 1024])
    rmask_d = din("rmask", [128, T])
    out = nc.dram_tensor("out", [T, D], F32, kind="ExternalOutput").ap()
    dbg_aps = {}
    for name, shape in dbg:
        dbg_aps[name] = nc.dram_tensor(name, list(shape), F32, kind="ExternalOutput").ap()
    rkv_s = nc.dram_tensor("rkv_s", [24, 128, T], F32, kind="Internal").ap()
    h1_s = nc.dram_tensor("h1_s", [T, D], F32, kind="Internal").ap()
    h2_s = nc.dram_tensor("h2_s", [T, D], F32, kind="Internal").ap()
    NROW = NSLOT * SL
    Xs = nc.dram_tensor("Xs", [NROW, D], BF16, kind="Internal").ap()
    Ys = nc.dram_tensor("Ys", [NROW, D], F32, kind="Internal").ap()

    with ExitStack() as es:
        fw = FW(nc, es)
        pe, act, dve, pool, sp = fw.pe, fw.act, fw.dve, fw.pool, fw.sp
        big = es.enter_context(nc.sbuf_tensor("big", [128, WORDS], F32))[:]
        banks = [(es.enter_context(nc.psum_tensor(f"bk{i}", [128, 512], F32))[:], Dep()) for i in range(8)]
        bki = [0]

        def nb():
            b = banks[bki[0] % 8]
            bki[0] += 1
            return b

        def mm(o, lhsT, rhs, start, stop, reads, writes):
            pe.op(lambda e: e.matmul(o, lhsT=lhsT, rhs=rhs, start=start, stop=stop), reads, writes)

        CONST_W = 7424
        ca = Alloc(big, 0, CONST_W)
        identf = ca.f32(128)
        blk1 = ca.f32(128)
        mSI = ca.f32(256)
        mL = ca.f32(128)
        i64 = ca.f32(64)
        rcnt = ca.f32(64)
        pv = ca.f32(NPV + 1)
        identb = ca.bf16(128)
        onesb = ca.bf16(128)
        rmask = ca.bf16(T)
        gBt = ca.f32(D)
        lo1 = ca.bf16(T)
        sg1 = ca.bf16(T)
        sg2 = ca.bf16(T)
        ones1 = ca.f32(128)
        brow = ca.f32(20)
        d_const, d_gB, d_lo1, d_sg1, d_sg2 = Dep(), Dep(), Dep(), Dep(), Dep()
        B0_OFF = CONST_W
        B1_OFF = B0_OFF + 8192
        M_OFF = B1_OFF + 8192
        A_OFF = WORDS - 16384
        bufA = r3(big[:, A_OFF:WORDS].bitcast(BF16), 16)
        bufB = r3(big[:, B0_OFF:M_OFF].bitcast(BF16), 16)
        d_A, d_B = Dep(), Dep()

        sp.dma(identf, cst[:, 0:128], writes=[d_const])
        sp.dma(blk1, cst[:, 128:256], writes=[d_const])
        sp.dma(mSI, cst[:, 256:512], writes=[d_const])
        sp.dma(mL, cst[:, 512:640], writes=[d_const])
        sp.dma(i64, cst[:, 640:704], writes=[d_const])
        sp.dma(rcnt, cst[:, 704:768], writes=[d_const])
        sp.dma(pv[:, 0:NPV], pvd, writes=[d_const])
        sp.dma(brow[0:1, :], b_r, writes=[d_const])
        pool.dma(identb, cst[:, 0:128], writes=[d_const])
        pool.dma(rmask, rmask_d, writes=[d_const])
        pool.op(lambda e: e.memset(onesb, 1.0), writes=[d_const])
        pool.op(lambda e: e.memset(ones1, 1.0), writes=[d_const])
        dve.op(lambda e: e.tensor_scalar(out=pv[:, PV["omka"]:PV["omka"] + 8], in0=pv[:, PV["k_a"]:PV["k_a"] + 8],
                                         scalar1=-1.0, scalar2=1.0, op0=ALU.mult, op1=ALU.add), reads=[d_const], writes=[d_const])
        fw.barrier()

        def pvc(name, j):
            c = PV[name] + j
            return pv[:, c:c + 1]

        d_Xs, d_Ys = Dep(), Dep()
        zf = [0]

        def zero_fill(n, zt, dz):
            while n > 0 and zf[0] < NROW // 128 and stop >= 8:
                c = zf[0]
                pool.dma(Xs[c * 128:(c + 1) * 128, :], zt, reads=[dz])
                zf[0] += 1
                n -= 1

        def load_gB(i):
            sp.dma(gBt, gB[i], writes=[d_gB])

        def norm_tiles(al, ntiles, src_fn, dstT, d_dst, tok_off=0, keep=None, nbuf=3):
            xts = [(al.f32(D), Dep()) for _ in range(nbuf)]
            xns = [(al.bf16(D), Dep()) for _ in range(nbuf)]
            junk = al.bf16(D)
            d_junk = Dep()
            sts = [(al.f32(8), Dep()) for _ in range(4)]

            def stage_a(i):
                xt, dx = xts[i % nbuf]
                xn, dn = xns[i % nbuf]
                st, d_st = sts[i % 4]
                sp.dma(xt, src_fn(i), writes=[dx])
                if keep is not None:
                    keep(i, xt, dx)
                act.op(lambda e: e.activation(out=junk, in_=xt, func=AF.Square, accum_out=st[:, 0:1]), reads=[dx], writes=[d_junk, d_st])
                dve.op(lambda e: e.tensor_scalar(out=st[:, 1:2], in0=st[:, 0:1], scalar1=1.0 / D, scalar2=1e-6, op0=ALU.mult, op1=ALU.add),
                       reads=[d_st], writes=[d_st])
                act.op(lambda e: e.activation(out=st[:, 2:3], in_=st[:, 1:2], func=AF.Sqrt), reads=[d_st], writes=[d_st])
                dve.op(lambda e: e.reciprocal(out=st[:, 3:4], in_=st[:, 2:3]), reads=[d_st], writes=[d_st])
                dve.op(lambda e: e.scalar_tensor_tensor(out=xn, in0=xt, scalar=st[:, 3:4], in1=gBt, op0=ALU.mult, op1=ALU.mult),
                       reads=[dx, d_st, d_gB], writes=[dn])

            def stage_b(i):
                xn, dn = xns[i % nbuf]
                for q in range(4):
                    pb, pd = nb()
                    for j in range(4):
                        kc = q * 4 + j
                        mm(pb[:, j * 128:(j + 1) * 128], xn[:, kc * 128:(kc + 1) * 128], identb, True, True, [dn, d_const], [pd])
                    t0 = tok_off + i * 128
                    if q % 2 == 0:
                        act.op(lambda e: e.activation(out=dstT[:, q * 4:q * 4 + 4, t0:t0 + 128], in_=r3(pb, 4), func=AF.Copy), reads=[pd], pws=[d_dst])
                    else:
                        dve.op(lambda e: e.tensor_copy(out=dstT[:, q * 4:q * 4 + 4, t0:t0 + 128], in_=r3(pb, 4)), reads=[pd], pws=[d_dst])

            for i in range(ntiles + 1):
                if i < ntiles:
                    stage_a(i)
                if i >= 1:
                    stage_b(i - 1)

        def dump(name, ap_sb, dep, dst=None):
            if name in dbg_aps:
                sp.dma(dbg_aps[name] if dst is None else dst, ap_sb, reads=[dep])

        load_gB(0)
        al = Alloc(big, M_OFF, A_OFF)
        norm_tiles(al, NTT, lambda i: x[i * 128:(i + 1) * 128, :], bufA, d_A)
        fw.barrier()
        if "hnT" in dbg_aps:
            tmp = Alloc(big, M_OFF, A_OFF).f32(T)
            dtmp = Dep()
            for kc in range(16):
                dve.op(lambda e: e.tensor_copy(out=tmp, in_=bufA[:, kc, :]), reads=[d_A], writes=[dtmp])
                sp.dma(dbg_aps["hnT"][kc * 128:(kc + 1) * 128, :], tmp, reads=[dtmp])
            fw.barrier()

        if stop >= 2:
            al = Alloc(big, B1_OFF, A_OFF)
            wbs = [(r3(al.bf16(16 * 128), 16), Dep()) for _ in range(4)]
            wbi = [0]
            pbufs = [(al.f32(PAD + T), Dep()) for _ in range(2)]
            fbs = [(al.f32(PAD + T), Dep()) for _ in range(3)]
            pooled = [(al.bf16(T), Dep()) for _ in range(2)]
            pwb = r3(al.bf16(8 * 256), 8)
            d_pw = Dep()
            ztile = al.bf16(D)
            d_zt = Dep()
            dve.op(lambda e: e.memset(ztile, 0.0), writes=[d_zt])
            for pbf, dp in pbufs + fbs:
                dve.op(lambda e: e.memset(pbf[:, 0:PAD], 0.0), writes=[dp])
            pool.dma(pwb, pool_w.rearrange("g (cc p) d -> p (g cc) d", p=128), writes=[d_pw])
            pbi = [0]

            def proj_block(col0, n):
                ws, dw = wbs[wbi[0] % 4]
                wbi[0] += 1
                pool.dma(ws[:, :, 0:n], w_in[:, col0:col0 + n].rearrange("(kc p) n -> p kc n", p=128), writes=[dw])
                if wbi[0] > 4:
                    zero_fill(3, ztile, d_zt)
                pbf, dp = pbufs[pbi[0] % 2]
                pbi[0] += 1
                for tq in range(NTQ):
                    pb, pd = nb()
                    for kc in range(KC):
                        mm(pb[0:n, :], ws[:, kc, 0:n], bufA[:, kc, tq * 512:(tq + 1) * 512], kc == 0, kc == KC - 1, [dw, d_A], [pd])
                    act.op(lambda e: e.activation(out=pbf[0:n, PAD + tq * 512:PAD + (tq + 1) * 512], in_=pb[0:n, :], func=AF.Copy), reads=[pd], writes=[dp])
                return pbf, dp

            def tshift(pbf, dp, n, mu_ap, zout, dz):
                f0, df0 = fbs[0]
                dve.op(lambda e: e.tensor_tensor(out=f0[0:n, 0:T], in0=pbf[0:n, PAD - 1:PAD - 1 + T], in1=pbf[0:n, PAD:PAD + T], op=ALU.subtract),
                       reads=[dp], writes=[df0])
                dve.op(lambda e: e.scalar_tensor_tensor(out=zout, in0=f0[0:n, 0:T], scalar=mu_ap, in1=pbf[0:n, PAD:PAD + T], op0=ALU.mult, op1=ALU.add),
                       reads=[df0, dp, d_const], writes=[dz])

            z1f, dz1 = fbs[1]
            z1 = z1f[:, PAD:PAD + T]
            pbf, dp = proj_block(4096, 128)
            tshift(pbf, dp, 128, pv[:, PV["mu_lo"]:PV["mu_lo"] + 1], z1, dz1)
            act.op(lambda e: e.activation(out=lo1[0:64, :], in_=z1[0:64, :], func=AF.Tanh), reads=[dz1], writes=[d_lo1])
            act.op(lambda e: e.activation(out=lo1[64:128, :], in_=z1[64:128, :], func=AF.Copy), reads=[dz1], writes=[d_lo1])
            pbf, dp = proj_block(4224, 128)
            tshift(pbf, dp, 128, pv[:, PV["mu_lo"] + 1:PV["mu_lo"] + 2], z1, dz1)
            act.op(lambda e: e.activation(out=sg1, in_=z1, func=AF.Sigmoid), reads=[dz1], writes=[d_sg1])
            pbf, dp = proj_block(4352, 32)
            tshift(pbf, dp, 32, pv[0:32, PV["mu_lo"] + 2:PV["mu_lo"] + 3], z1[0:32, :], dz1)
            act.op(lambda e: e.activation(out=sg2[0:32, :], in_=z1[0:32, :], func=AF.Sigmoid), reads=[dz1], writes=[d_sg2])
            for j in range(24):
                pbf, dp = proj_block(1024 + j * 128, 128)
                tshift(pbf, dp, 128, pvc("mu_rkv", j), z1, dz1)
                sp.dma(rkv_s[j], z1, reads=[dz1])
            for cb in range(8):
                gi = cb // 2
                w = (2, 4, 8, 16)[gi]
                pbf, dp = proj_block(cb * 128, 128)
                (fa, dfa), (fb_, dfb) = fbs[1], fbs[2]
                src, dsrc = pbf, dp
                sh = 1
                k = 0
                while sh < w:
                    dst, ddst = (fa, dfa) if k % 2 == 0 else (fb_, dfb)
                    dve.op(lambda e: e.tensor_tensor(out=dst[:, PAD:PAD + T], in0=src[:, PAD:PAD + T], in1=src[:, PAD - sh:PAD - sh + T], op=ALU.add),
                           reads=[dsrc], writes=[ddst])
                    src, dsrc = dst, ddst
                    sh *= 2
                    k += 1
                po, dpo = pooled[cb % 2]
                dve.op(lambda e: e.scalar_tensor_tensor(out=po, in0=src[:, PAD:PAD + T], scalar=1.0 / w, in1=pbf[:, PAD:PAD + T], op0=ALU.mult, op1=ALU.subtract),
                       reads=[dsrc, dp], writes=[dpo])
                f0, df0 = fbs[0]
                dve.op(lambda e: e.tensor_tensor(out=f0[:, 0:16], in0=src[:, PAD:PAD + 16], in1=rcnt[:, gi * 16:(gi + 1) * 16], op=ALU.mult),
                       reads=[dsrc, d_const], writes=[df0])
                dve.op(lambda e: e.tensor_tensor(out=po[:, 0:16], in0=f0[:, 0:16], in1=pbf[:, PAD:PAD + 16], op=ALU.subtract),
                       reads=[df0, dp], writes=[dpo])
                if cb % 2 == 1:
                    for db in range(2):
                        for tq in range(NTQ):
                            pb, pd = nb()
                            for cc in range(2):
                                mm(pb, pwb[:, gi * 2 + cc, db * 128:(db + 1) * 128], pooled[cc][0][:, tq * 512:(tq + 1) * 512], cc == 0, cc == 1,
                                   [d_pw, pooled[cc][1]], [pd])
                            blk = gi * 2 + db
                            act.op(lambda e: e.activation(out=bufB[:, blk, tq * 512:(tq + 1) * 512], in_=pb, func=AF.Copy, scale=pvc("pool_scale", blk)),
                                   reads=[pd, d_const], writes=[d_B])
            zero_fill(NROW, ztile, d_zt)
            fw.barrier()
            if "mixT" in dbg_aps:
                tmp = Alloc(big, B1_OFF, A_OFF).f32(T)
                dtmp = Dep()
                for kc in range(8):
                    dve.op(lambda e: e.tensor_copy(out=tmp, in_=bufB[:, kc, :]), reads=[d_B], writes=[dtmp])
                    sp.dma(dbg_aps["mixT"][kc * 128:(kc + 1) * 128, :], tmp, reads=[dtmp])
                fw.barrier()


        if stop >= 3:
            QTK = 512
            NQ = T // QTK
            al = Alloc(big, M_OFF, WORDS)
            lw = al.bf16(1024)
            g2a = al.bf16(1024)
            g2b = al.bf16(1024)
            S32 = al.f32(64)
            Sbf = al.bf16(64)
            d_lw, d_S32, d_Sbf = Dep(), Dep(), Dep()
            sets = []
            for i in range(3):
                sets.append(dict(AR=r3(al.bf16(8 * 128), 8), BK=r3(al.bf16(8 * 128), 8), BKh=r3(al.bf16(8 * 128), 8), vb=al.bf16(QTK),
                                 GL=al.f32(8), gbuf=al.bf16(QTK), bonus=al.f32(QTK), ybuf=al.f32(QTK),
                                 d_AR=Dep(), d_BK=Dep(), d_BKh=Dep(), d_vb=Dep(), d_GL=Dep(), d_g=Dep(), d_bonus=Dep(), d_y=Dep(),
                                 d_ARr=[Dep() for _ in range(4)]))
            Fq = [al.f32(QTK) for _ in range(8)]
            dFq = [Dep() for _ in range(8)]
            cbs = []
            for i in range(2):
                cbs.append(dict(NBall=r3(al.bf16(8 * 128), 8), KBall=r3(al.bf16(8 * 128), 8), TM=r3(al.bf16(8 * 320), 8), APU=r3(al.bf16(8 * 128), 8),
                                Mc=r3(al.f32(8 * 64), 8), CcT=r3(al.f32(8 * 64), 8),
                                d_NB=[Dep() for _ in range(4)], d_KB=[Dep() for _ in range(4)], d_TM=[Dep() for _ in range(4)],
                                d_APU=[Dep() for _ in range(4)], d_Mc=[Dep() for _ in range(4)], d_Cc=[Dep() for _ in range(4)]))
            TT = r3(al.bf16(8 * 64), 8)
            Wg = [[r3(al.bf16(2 * 128), 2) for _ in range(2)] for _ in range(4)]
            NTg = [[r3(al.bf16(2 * 64), 2) for _ in range(2)] for _ in range(4)]
            d_TT = [Dep() for _ in range(4)]
            dWg = [[Dep(), Dep()] for _ in range(4)]
            dNTg = [[Dep(), Dep()] for _ in range(4)]
            yc, sq, rs = al.f32(QTK), al.f32(QTK), al.f32(QTK)
            dyc, dsq, drs = Dep(), Dep(), Dep()
            pool.dma(lw[0:64, :], w2, writes=[d_lw])
            pool.dma(lw[64:128, :], a2, writes=[d_lw])
            pool.dma(g2a, g2[0:128, :], writes=[d_lw])
            pool.dma(g2b[0:32, :], g2[128:160, :], writes=[d_lw])
            HS = [slice(0, 64), slice(64, 128)]
            i64b = i64.unsqueeze(1).to_broadcast([128, 2, 64])
            pyb, pdyb = banks[7]
            nbm = [0]

            def nb7():
                b = banks[nbm[0] % 7]
                nbm[0] += 1
                return b

            def prep_gen(u):
                hp, q = divmod(u, NQ)
                S_ = sets[u % 3]
                cs = slice(hp * 128, (hp + 1) * 128)
                tsl = slice(q * QTK, (q + 1) * QTK)
                k_, sgw, alr, cum, kk, f5, f6, f7 = Fq
                dk, dsgw, dalr, dcum, dkk, df5, df6, df7 = dFq
                AR, BK, BKh, vb, GL, gbuf, bonus = S_["AR"], S_["BK"], S_["BKh"], S_["vb"], S_["GL"], S_["gbuf"], S_["bonus"]
                d_AR, d_BK, d_BKh, d_vb, d_GL, d_g, d_bonus = S_["d_AR"], S_["d_BK"], S_["d_BKh"], S_["d_vb"], S_["d_GL"], S_["d_g"], S_["d_bonus"]
                sp.dma(k_, rkv_s[8 + hp][:, tsl], writes=[dk])
                pb, pd = nb7()
                mm(pb, lw[0:64, cs], lo1[0:64, tsl], True, True, [d_lw, d_lo1], [pd])
                act.op(lambda e: e.activation(out=sgw, in_=pb, func=AF.Sigmoid, bias=pvc("w0", hp)), reads=[pd, d_const], writes=[dsgw])
                pb, pd = nb7()
                mm(pb, lw[64:128, cs], lo1[64:128, tsl], True, True, [d_lw, d_lo1], [pd])
                act.op(lambda e: e.activation(out=alr, in_=pb, func=AF.Sigmoid, bias=pvc("a0", hp)), reads=[pd, d_const], writes=[dalr])
                yield
                pb, pd = nb7()
                mm(pb, g2a[:, cs], sg1[:, tsl], True, False, [d_lw, d_sg1], [pd])
                mm(pb, g2b[0:32, cs], sg2[0:32, tsl], False, True, [d_lw, d_sg2], [pd])
                act.op(lambda e: e.activation(out=gbuf, in_=pb, func=AF.Copy), reads=[pd], writes=[d_g])
                dve.op(lambda e: e.tensor_tensor_scan(out=cum, data0=rmask[:, 0:QTK], data1=sgw, initial=0.0, op0=ALU.mult, op1=ALU.add),
                       reads=[d_const, dsgw], writes=[dcum])
                yield
                act.op(lambda e: e.activation(out=kk, in_=k_, func=AF.Copy, scale=pvc("k_k", hp)), reads=[dk, d_const], writes=[dkk])
                pool.op(lambda e: e.tensor_tensor(out=f5, in0=kk, in1=kk, op=ALU.mult), reads=[dkk], writes=[df5])
                yield
                pb, pd = nb7()
                mm(pb, blk1, f5, True, True, [d_const, df5], [pd])
                dve.op(lambda e: e.tensor_scalar(out=f6, in0=pb, scalar1=1e-24, scalar2=None, op0=ALU.max), reads=[pd], writes=[df6])
                act.op(lambda e: e.activation(out=f6, in_=f6, func=AF.Sqrt), reads=[df6], writes=[df6])
                yield
                dve.op(lambda e: e.reciprocal(out=f6, in_=f6), reads=[df6], writes=[df6])
                yield
                pool.op(lambda e: e.tensor_tensor(out=kk, in0=kk, in1=f6, op=ALU.mult), reads=[dkk, df6], writes=[dkk])
                dve.op(lambda e: e.tensor_scalar(out=f5, in0=alr, scalar1=pvc("k_a", hp), scalar2=pvc("omka", hp), op0=ALU.mult, op1=ALU.add),
                       reads=[dalr, d_const], writes=[df5])
                yield
                pool.op(lambda e: e.tensor_tensor(out=f5, in0=f5, in1=k_, op=ALU.mult), reads=[df5, dk], writes=[df5])
                pool.op(lambda e: e.tensor_tensor(out=alr, in0=alr, in1=kk, op=ALU.mult), reads=[dalr, dkk], writes=[dalr])
                yield
                act.op(lambda e: e.activation(out=f6, in_=cum, func=AF.Exp, scale=CDEC), reads=[dcum], writes=[df6])
                dve.op(lambda e: e.tensor_tensor(out=BK[:, :, 0:64], in0=r3(alr, 8), in1=r3(f6, 8), op=ALU.mult), reads=[dalr, df6], writes=[d_BK])
                pool.op(lambda e: e.tensor_tensor(out=BK[:, :, 64:128], in0=r3(f5, 8), in1=r3(f6, 8), op=ALU.mult), reads=[df5, df6], writes=[d_BK])
                yield
                dve.op(lambda e: e.tensor_tensor(out=r3(f6, 8), in0=r3(cum, 8), in1=r3(cum, 8)[:, :, 63:64].to_broadcast([128, 8, 64]), op=ALU.subtract),
                       reads=[dcum], writes=[df6])
                act.op(lambda e: e.activation(out=f6, in_=f6, func=AF.Exp, scale=CDEC), reads=[df6], writes=[df6])
                yield
                dve.op(lambda e: e.tensor_tensor(out=BKh[:, :, 0:64], in0=r3(alr, 8), in1=r3(f6, 8), op=ALU.mult), reads=[dalr, df6], writes=[d_BKh])
                pool.op(lambda e: e.tensor_tensor(out=BKh[:, :, 64:128], in0=r3(f5, 8), in1=r3(f6, 8), op=ALU.mult), reads=[df5, df6], writes=[d_BKh])
                yield
                pool.op(lambda e: e.tensor_tensor(out=f6, in0=cum, in1=sgw, op=ALU.subtract), reads=[dcum, dsgw], writes=[df6])
                act.op(lambda e: e.activation(out=f6, in_=f6, func=AF.Exp, scale=-CDEC), reads=[df6], writes=[df6])
                yield
                dve.op(lambda e: e.scalar_tensor_tensor(out=AR[:, :, 0:64], in0=r3(kk, 8), scalar=-1.0, in1=r3(f6, 8), op0=ALU.mult, op1=ALU.mult),
                       reads=[dkk, df6], writes=[d_AR])
                act.op(lambda e: e.activation(out=GL, in_=r3(cum, 8)[:, :, 63], func=AF.Exp, scale=-CDEC), reads=[dcum], writes=[d_GL])
                yield
                act.op(lambda e: e.activation(out=f6, in_=cum, func=AF.Exp, scale=-CDEC), reads=[dcum], writes=[df6])
                sp.dma(k_, rkv_s[hp][:, tsl], writes=[dk])
                pool.op(lambda e: e.tensor_tensor(out=AR[:, :, 64:128], in0=r3(k_, 8), in1=r3(f6, 8), op=ALU.mult), reads=[dk, df6], writes=[d_AR] + S_["d_ARr"])
                yield
                dve.op(lambda e: e.scalar_tensor_tensor(out=f6, in0=k_, scalar=pvc("r_k", hp), in1=f5, op0=ALU.mult, op1=ALU.mult),
                       reads=[dk, df5, d_const], writes=[df6])
                sp.dma(sgw, rkv_s[16 + hp][:, tsl], writes=[dsgw])
                yield
                pb, pd = nb7()
                mm(pb, blk1, f6, True, True, [d_const, df6], [pd])
                dve.op(lambda e: e.tensor_tensor(out=bonus, in0=pb, in1=sgw, op=ALU.mult), reads=[pd, dsgw], writes=[d_bonus])
                act.op(lambda e: e.activation(out=vb, in_=sgw, func=AF.Copy), reads=[dsgw], writes=[d_vb])
                yield

            def front_gen(u):
                hp, q = divmod(u, NQ)
                S_ = sets[u % 3]
                C_ = cbs[u % 2]
                tsl = slice(q * QTK, (q + 1) * QTK)
                AR, BK, BKh, vb, GL, gbuf, bonus, ybuf = S_["AR"], S_["BK"], S_["BKh"], S_["vb"], S_["GL"], S_["gbuf"], S_["bonus"], S_["ybuf"]
                d_AR, d_BK, d_BKh, d_vb, d_GL, d_g, d_bonus, dy = S_["d_AR"], S_["d_BK"], S_["d_BKh"], S_["d_vb"], S_["d_GL"], S_["d_g"], S_["d_bonus"], S_["d_y"]
                d_ARr = S_["d_ARr"]
                NBall, KBall, TM, APU, Mc, CcT = C_["NBall"], C_["KBall"], C_["TM"], C_["APU"], C_["Mc"], C_["CcT"]
                d_NB, d_KB, d_TM, d_APU, d_Mc, d_Cc = C_["d_NB"], C_["d_KB"], C_["d_TM"], C_["d_APU"], C_["d_Mc"], C_["d_Cc"]
                pool.op(lambda e: e.tensor_tensor(out=Mc, in0=i64.unsqueeze(1).to_broadcast([128, 8, 64]),
                                                  in1=GL.unsqueeze(2).to_broadcast([128, 8, 64]), op=ALU.mult),
                        reads=[d_const, d_GL], writes=d_Mc)
                for g in range(4):
                    l0 = 2 * g
                    pa, pda = nb7()
                    pb_, pdb = nb7()
                    pt, pdt = nb7()
                    pv_, pdv = nb7()
                    for ci in range(2):
                        c = l0 + ci
                        for h in range(2):
                            hs = HS[h]
                            mm(pa[hs, ci * 128:(ci + 1) * 128], BK[hs, c, 0:64], AR[hs, c, :], True, True, [d_BK, d_AR, d_ARr[g]], [pda])
                            mm(pb_[hs, ci * 128:(ci + 1) * 128], BK[hs, c, 64:128], AR[hs, c, :], True, True, [d_BK, d_AR, d_ARr[g]], [pdb])
                            mm(pt[hs, ci * 64:(ci + 1) * 64], AR[hs, c, 0:64], BK[hs, c, 0:64], True, True, [d_BK, d_AR], [pdt])
                            idh = identb[hs, 64 * h:64 * h + 64]
                            mm(pv_[hs, ci * 256:ci * 256 + 64], vb[hs, c * 64:(c + 1) * 64], idh, True, True, [d_vb, d_const], [pdv])
                            mm(pv_[hs, ci * 256 + 64:ci * 256 + 128], BKh[hs, c, 0:64], idh, True, True, [d_BKh, d_const], [pdv])
                            mm(pv_[hs, ci * 256 + 128:ci * 256 + 192], BKh[hs, c, 64:128], idh, True, True, [d_BKh, d_const], [pdv])
                            mm(pv_[hs, ci * 256 + 192:ci * 256 + 256], AR[hs, c, 0:64], idh, True, True, [d_AR, d_const], [pdv])
                    dve.op(lambda e: e.tensor_tensor(out=NBall[:, l0:l0 + 2, :], in0=r3(pa[:, 0:256], 2), in1=r3(mSI, 2), op=ALU.mult),
                           reads=[pda, d_const], writes=[d_NB[g]])
                    dve.op(lambda e: e.tensor_tensor(out=KBall[:, l0:l0 + 2, :], in0=r3(pb_[:, 0:256], 2), in1=r3(mSI, 2), op=ALU.mult),
                           reads=[pdb, d_const], writes=[d_KB[g]])
                    dve.op(lambda e: e.tensor_tensor(out=NTg[g][0], in0=r3(pt[:, 0:128], 2), in1=r3(mL, 2), op=ALU.mult),
                           reads=[pdt, d_const], writes=[dNTg[g][0]])
                    act.op(lambda e: e.activation(out=TM[:, l0:l0 + 2, 0:256], in_=r3(pv_, 2), func=AF.Copy), reads=[pdv], writes=[d_TM[g]])
                    pool.op(lambda e: e.tensor_tensor(out=Wg[g][0][:, :, 64:128], in0=NBall[:, l0:l0 + 2, 0:64], in1=i64b, op=ALU.add),
                            reads=[d_NB[g], d_const], writes=[dWg[g][0]])
                    yield
                for g in range(4):
                    l0 = 2 * g
                    p0, pd0 = nb7()
                    q0, qd0 = nb7()
                    NT, dNT = NTg[g][0], dNTg[g][0]
                    for ci in range(2):
                        for h in range(2):
                            hs = HS[h]
                            mm(p0[hs, ci * 64:(ci + 1) * 64], NT[hs, ci, :], NBall[hs, l0 + ci, 0:64], True, True, [dNT, d_NB[g]], [pd0])
                            mm(q0[hs, ci * 64:(ci + 1) * 64], NBall[hs, l0 + ci, 0:64], NT[hs, ci, :], True, True, [dNT, d_NB[g]], [qd0])
                    act.op(lambda e: e.activation(out=Wg[g][0][:, :, 0:64], in_=r3(p0[:, 0:128], 2), func=AF.Copy), reads=[pd0], writes=[dWg[g][0]])
                    act.op(lambda e: e.activation(out=NTg[g][1], in_=r3(q0[:, 0:128], 2), func=AF.Copy), reads=[qd0], writes=[dNTg[g][1]])
                    yield
                cur, ntc = 0, 1
                for lvl in range(1, 6):
                    last = lvl == 5
                    for g in range(4):
                        l0 = 2 * g
                        Wc, dWc = Wg[g][cur], dWg[g][cur]
                        NTc, dNTc = NTg[g][ntc], dNTg[g][ntc]
                        p1, pd1 = nb7()
                        if not last:
                            q1, qd1 = nb7()
                        for ci in range(2):
                            for h in range(2):
                                hs = HS[h]
                                if not last:
                                    mm(p1[hs, ci * 128:(ci + 1) * 128], NTc[hs, ci, :], Wc[hs, ci, :], True, True, [dNTc, dWc], [pd1])
                                    mm(q1[hs, ci * 64:(ci + 1) * 64], Wc[hs, ci, 0:64], NTc[hs, ci, :], True, True, [dNTc, dWc], [qd1])
                                else:
                                    mm(p1[hs, ci * 64:(ci + 1) * 64], NTc[hs, ci, :], Wc[hs, ci, 64:128], True, True, [dNTc, dWc], [pd1])
                        if not last:
                            Wn, dWn = Wg[g][1 - cur], dWg[g][1 - cur]
                            NTn, dNTn = NTg[g][1 - ntc], dNTg[g][1 - ntc]
                            act.op(lambda e: e.activation(out=Wn[:, :, 0:64], in_=r3(p1[:, 0:256], 2)[:, :, 0:64], func=AF.Copy), reads=[pd1], writes=[dWn])
                            dve.op(lambda e: e.tensor_tensor(out=Wn[:, :, 64:128], in0=r3(p1[:, 0:256], 2)[:, :, 64:128], in1=Wc[:, :, 64:128], op=ALU.add),
                                   reads=[pd1, dWc], writes=[dWn])
                            act.op(lambda e: e.activation(out=NTn, in_=r3(q1[:, 0:128], 2), func=AF.Copy), reads=[qd1], writes=[dNTn])
                        else:
                            dve.op(lambda e: e.tensor_tensor(out=TT[:, l0:l0 + 2, :], in0=r3(p1[:, 0:128], 2), in1=Wc[:, :, 64:128], op=ALU.add),
                                   reads=[pd1, dWc], writes=[d_TT[g]])
                        if g % 2 == 1:
                            yield
                    cur, ntc = 1 - cur, 1 - ntc
                for g in range(4):
                    l0 = 2 * g
                    pw, pdw = nb7()
                    for ci in range(2):
                        for h in range(2):
                            hs = HS[h]
                            mm(pw[hs, ci * 64:(ci + 1) * 64], KBall[hs, l0 + ci, 0:64], TM[hs, l0 + ci, 0:64], True, True, [d_KB[g], d_TM[g]], [pdw])
                    act.op(lambda e: e.activation(out=TM[:, l0:l0 + 2, 256:320], in_=r3(pw[:, 0:128], 2), func=AF.Copy), reads=[pdw], writes=[d_TM[g]])
                yield
                for g in range(4):
                    l0 = 2 * g
                    pq, pdq = nb7()
                    for ci in range(2):
                        for h in range(2):
                            hs = HS[h]
                            mm(pq[hs, ci * 128:(ci + 1) * 128], TT[hs, l0 + ci, :], TM[hs, l0 + ci, 192:320], True, True, [d_TT[g], d_TM[g]], [pdq])
                    dve.op(lambda e: e.tensor_copy(out=APU[:, l0:l0 + 2, :], in_=r3(pq[:, 0:256], 2)), reads=[pdq], writes=[d_APU[g]])
                yield
                for g in range(4):
                    l0 = 2 * g
                    pm, pdm = nb7()
                    pc, pdc = nb7()
                    pr, pdr = nb7()
                    for ci in range(2):
                        l = l0 + ci
                        for h in range(2):
                            hs = HS[h]
                            mm(pm[hs, ci * 64:(ci + 1) * 64], APU[hs, l, 0:64], TM[hs, l, 64:128], True, True, [d_APU[g], d_TM[g]], [pdm])
                            mm(pc[hs, ci * 64:(ci + 1) * 64], TM[hs, l, 64:128], APU[hs, l, 64:128], True, False, [d_APU[g], d_TM[g]], [pdc])
                            mm(pc[hs, ci * 64:(ci + 1) * 64], TM[hs, l, 128:192], TM[hs, l, 0:64], False, True, [d_TM[g]], [pdc])
                            mm(pr[hs, ci * 64:(ci + 1) * 64], APU[hs, l, 0:64], NBall[hs, l, 64:128], True, True, [d_APU[g], d_NB[g]], [pdr])
                    dve.op(lambda e: e.tensor_tensor(out=Mc[:, l0:l0 + 2, :], in0=r3(pm[:, 0:128], 2), in1=Mc[:, l0:l0 + 2, :], op=ALU.add),
                           reads=[pdm, d_Mc[g]], writes=[d_Mc[g]])
                    act.op(lambda e: e.activation(out=CcT[:, l0:l0 + 2, :], in_=r3(pc[:, 0:128], 2), func=AF.Copy), reads=[pdc], writes=[d_Cc[g]])
                    dve.op(lambda e: e.tensor_tensor(out=AR[:, l0:l0 + 2, 64:128], in0=r3(pr[:, 0:128], 2), in1=AR[:, l0:l0 + 2, 64:128], op=ALU.add),
                           reads=[pdr, d_ARr[g], d_AR], writes=[d_ARr[g]])
                    if g % 2 == 1:
                        yield
            def back_gen(u):
                hp, q = divmod(u, NQ)
                S_ = sets[u % 3]
                C_ = cbs[u % 2]
                tsl = slice(q * QTK, (q + 1) * QTK)
                AR, BK, BKh, vb, GL, gbuf, bonus, ybuf = S_["AR"], S_["BK"], S_["BKh"], S_["vb"], S_["GL"], S_["gbuf"], S_["bonus"], S_["ybuf"]
                d_AR, d_BK, d_BKh, d_vb, d_GL, d_g, d_bonus, dy = S_["d_AR"], S_["d_BK"], S_["d_BKh"], S_["d_vb"], S_["d_GL"], S_["d_g"], S_["d_bonus"], S_["d_y"]
                d_ARr = S_["d_ARr"]
                NBall, KBall, TM, APU, Mc, CcT = C_["NBall"], C_["KBall"], C_["TM"], C_["APU"], C_["Mc"], C_["CcT"]
                d_NB, d_KB, d_TM, d_APU, d_Mc, d_Cc = C_["d_NB"], C_["d_KB"], C_["d_TM"], C_["d_APU"], C_["d_Mc"], C_["d_Cc"]
                if q == 0:
                    dve.op(lambda e: e.memset(S32, 0.0), writes=[d_S32])
                    dve.op(lambda e: e.memset(Sbf, 0.0), writes=[d_Sbf])
                for l in range(8):
                    g = l // 2
                    ps_, pds = nb7()
                    for h in range(2):
                        hs = HS[h]
                        mm(ps_[hs, 0:64], Mc[hs, l, :], S32[hs, :], True, True, [d_Mc[g], d_S32], [pds])
                    for h in range(2):
                        hs = HS[h]
                        mm(pyb[hs, l * 64:(l + 1) * 64], Sbf[hs, :], AR[hs, l, 64:128], True, False, [d_Sbf, d_ARr[g]], [pdyb])
                        mm(pyb[hs, l * 64:(l + 1) * 64], APU[hs, l, 64:128], NBall[hs, l, 64:128], False, False, [d_APU[g], d_NB[g]], [pdyb])
                        mm(pyb[hs, l * 64:(l + 1) * 64], TM[hs, l, 0:64], KBall[hs, l, 64:128], False, True, [d_TM[g], d_KB[g]], [pdyb])
                    dve.op(lambda e: e.tensor_tensor(out=S32, in0=ps_[:, 0:64], in1=CcT[:, l, :], op=ALU.add), reads=[pds, d_Cc[g], d_S32], writes=[d_S32])
                    act.op(lambda e: e.activation(out=Sbf, in_=S32, func=AF.Copy), reads=[d_S32], writes=[d_Sbf])
                    yield
                act.op(lambda e: e.activation(out=ybuf, in_=pyb, func=AF.Copy), reads=[pdyb], writes=[dy])
                pb, pd = nb7()
                mm(pb, blk1, ybuf, True, True, [d_const, dy], [pd])
                dve.op(lambda e: e.scalar_tensor_tensor(out=yc, in0=pb, scalar=-1.0 / 64, in1=ybuf, op0=ALU.mult, op1=ALU.add),
                       reads=[pd, dy], writes=[dyc])
                pool.op(lambda e: e.tensor_tensor(out=sq, in0=yc, in1=yc, op=ALU.mult), reads=[dyc], writes=[dsq])
                yield
                pb, pd = nb7()
                mm(pb, blk1, sq, True, True, [d_const, dsq], [pd])
                dve.op(lambda e: e.tensor_scalar(out=rs, in0=pb, scalar1=1.0 / 64, scalar2=64e-5, op0=ALU.mult, op1=ALU.add), reads=[pd], writes=[drs])
                act.op(lambda e: e.activation(out=rs, in_=rs, func=AF.Sqrt), reads=[drs], writes=[drs])
                yield
                dve.op(lambda e: e.reciprocal(out=rs, in_=rs), reads=[drs], writes=[drs])
                pool.op(lambda e: e.tensor_tensor(out=yc, in0=yc, in1=rs, op=ALU.mult), reads=[dyc, drs], writes=[dyc])
                yield
                dve.op(lambda e: e.tensor_scalar(out=yc, in0=yc, scalar1=pvc("ln_w", hp), scalar2=pvc("ln_b", hp), op0=ALU.mult, op1=ALU.add),
                       reads=[dyc, d_const], writes=[dyc])
                pool.op(lambda e: e.tensor_tensor(out=yc, in0=yc, in1=bonus, op=ALU.add), reads=[dyc, d_bonus], writes=[dyc])
                dve.op(lambda e: e.tensor_tensor(out=bufB[:, 8 + hp, tsl], in0=yc, in1=gbuf, op=ALU.mult), reads=[dyc, d_g], writes=[d_B])
                yield

            def run_interleaved(gens, steps=None):
                steps = steps or [1] * len(gens)
                gens = [[g, k] for g, k in zip(gens, steps) if g is not None]
                while gens:
                    for gk in list(gens):
                        for _ in range(gk[1]):
                            try:
                                next(gk[0])
                            except StopIteration:
                                gens.remove(gk)
                                break

            NU = 8 * NQ
            PIPE_STEPS = [1, 1, 1]
            run_interleaved([prep_gen(0)])
            run_interleaved([front_gen(0), prep_gen(1)])
            for u in range(NU):
                run_interleaved([back_gen(u), front_gen(u + 1) if u + 1 < NU else None, prep_gen(u + 2) if u + 2 < NU else None], PIPE_STEPS)
            fw.barrier()
            if "rwT" in dbg_aps:
                tmp = Alloc(big, M_OFF, WORDS).f32(T)
                dtmp = Dep()
                for kc in range(8):
                    dve.op(lambda e: e.tensor_copy(out=tmp, in_=bufB[:, 8 + kc, :]), reads=[d_B], writes=[dtmp])
                    sp.dma(dbg_aps["rwT"][kc * 128:(kc + 1) * 128, :], tmp, reads=[dtmp])
                fw.barrier()

        def out_proj(srcT, d_src, wmat, res_fn, dst_dram, al):
            wbs2 = [(r3(al.bf16(16 * 512), 16), Dep()) for _ in range(2)]
            xts = [(al.f32(512), Dep()) for _ in range(3)]
            hos = [(al.f32(512), Dep()) for _ in range(2)]
            i = 0
            for dblk in range(4):
                ws, dw = wbs2[dblk % 2]
                ds_ = slice(dblk * 512, (dblk + 1) * 512)
                pool.dma(ws, wmat[:, ds_].rearrange("(kc p) n -> p kc n", p=128), writes=[dw])
                for tt in range(NTT):
                    rows = slice(tt * 128, (tt + 1) * 128)
                    xt, dx = xts[i % 3]
                    ho, dh = hos[i % 2]
                    i += 1
                    sp.dma(xt, res_fn(rows, ds_), writes=[dx])
                    pb, pd = nb()
                    for kc in range(KC):
                        mm(pb, srcT[:, kc, rows], ws[:, kc, :], kc == 0, kc == KC - 1, [d_src, dw], [pd])
                    dve.op(lambda e: e.tensor_tensor(out=ho, in0=pb, in1=xt, op=ALU.add), reads=[pd, dx], writes=[dh])
                    act.dma(dst_dram[rows, ds_], ho, reads=[dh])

        if stop >= 4:
            out_proj(bufB, d_B, w_out, lambda rows, cols: x[rows, cols], h1_s, Alloc(big, M_OFF, A_OFF))
            fw.barrier()
            if stop >= 5:
                d_memT = Dep()
                alB = Alloc(big, B0_OFF, M_OFF)
                memT = r3(alB.bf16(16 * 256), 16)
                wbs5_0 = (r3(alB.bf16(16 * 512), 16), Dep())
                load_gB(3)
                norm_tiles(alB, 2, lambda i: mem[i * 128:(i + 1) * 128, :], memT, d_memT, nbuf=2)
                pool.dma(wbs5_0[0], w_kv[:, 0:512].rearrange("(kc p) n -> p kc n", p=128), writes=[wbs5_0[1]])
            load_gB(1)
            norm_tiles(Alloc(big, M_OFF, A_OFF), NTT, lambda i: h1_s[i * 128:(i + 1) * 128, :], bufA, d_A)
            fw.barrier()
            if "h1" in dbg_aps:
                tmp = Alloc(big, M_OFF, A_OFF).f32(D)
                dtmp = Dep()
                for tt in range(NTT):
                    sp.dma(tmp, h1_s[tt * 128:(tt + 1) * 128, :], writes=[dtmp])
                    sp.dma(dbg_aps["h1"][tt * 128:(tt + 1) * 128, :], tmp, reads=[dtmp])
                fw.barrier()


        if stop >= 5:
            kv_al = Alloc(big, M_OFF, M_OFF + 4096)
            KT = r3(kv_al.bf16(16 * 256), 16)
            Vb = r3(kv_al.bf16(2 * D), 2)
            d_KT, d_Vb = Dep(), Dep()
            alM = Alloc(big, M_OFF + 4096, A_OFF)
            wbs5 = [wbs5_0, (r3(alM.bf16(16 * 512), 16), Dep())]
            for g in range(8):
                ws, dw = wbs5[g % 2]
                if g > 0:
                    pool.dma(ws, w_kv[:, g * 512:(g + 1) * 512].rearrange("(kc p) n -> p kc n", p=128), writes=[dw])
                if g < 4:
                    for j in range(4):
                        cb = g * 4 + j
                        pb, pd = nb()
                        for kc in range(KC):
                            mm(pb[:, 0:256], ws[:, kc, j * 128:(j + 1) * 128], memT[:, kc, :], kc == 0, kc == KC - 1, [dw, d_memT], [pd])
                        act.op(lambda e: e.activation(out=KT[:, cb, :], in_=pb[:, 0:256], func=AF.Copy), reads=[pd], writes=[d_KT])
                else:
                    for mc in range(2):
                        pb, pd = nb()
                        for kc in range(KC):
                            mm(pb, memT[:, kc, mc * 128:(mc + 1) * 128], ws[:, kc, :], kc == 0, kc == KC - 1, [dw, d_memT], [pd])
                        dve.op(lambda e: e.tensor_copy(out=Vb[:, mc, (g - 4) * 512:(g - 3) * 512], in_=pb), reads=[pd], writes=[d_Vb])
            fw.barrier()
            alM = Alloc(big, M_OFF + 4096, A_OFF)
            wbs6 = [(r3(alM.bf16(16 * 256), 16), Dep()) for _ in range(2)]
            qscale = float(512 ** -0.5)
            for g in range(8):
                ws, dw = wbs6[g % 2]
                pool.dma(ws, w_q[:, g * 256:(g + 1) * 256].rearrange("(kc p) n -> p kc n", p=128), writes=[dw])
                for j in range(2):
                    cb = g * 2 + j
                    for tq in range(NTQ):
                        ts_ = slice(tq * 512, (tq + 1) * 512)
                        pb, pd = nb()
                        for kc in range(KC):
                            mm(pb, ws[:, kc, j * 128:(j + 1) * 128], bufA[:, kc, ts_], kc == 0, kc == KC - 1, [dw, d_A], [pd])
                        if tq % 2 == 0:
                            act.op(lambda e: e.activation(out=bufB[:, cb, ts_], in_=pb, func=AF.Copy, scale=qscale), reads=[pd], writes=[d_B])
                        else:
                            dve.op(lambda e: e.tensor_scalar(out=bufB[:, cb, ts_], in0=pb, scalar1=qscale, scalar2=None, op0=ALU.mult), reads=[pd], writes=[d_B])
            fw.barrier()
            alM = Alloc(big, M_OFF + 4096, A_OFF)
            Es = [(r3(alM.bf16(2 * 512), 2), Dep()) for _ in range(2)]
            rinvs = [(alM.f32(512), Dep()) for _ in range(2)]
            it = 0
            for h in range(4):
                for tq in range(NTQ):
                    ts_ = slice(tq * 512, (tq + 1) * 512)
                    E, dE = Es[it % 2]
                    rinv, dri = rinvs[it % 2]
                    it += 1
                    for mc in range(2):
                        pb, pd = nb()
                        for c in range(4):
                            mm(pb, KT[:, h * 4 + c, mc * 128:(mc + 1) * 128], bufB[:, h * 4 + c, ts_], c == 0, c == 3, [d_KT, d_B], [pd])
                        act.op(lambda e: e.activation(out=E[:, mc, :], in_=pb, func=AF.Exp), reads=[pd], writes=[dE])
                    pb, pd = nb()
                    for mc in range(2):
                        mm(pb, onesb, E[:, mc, :], mc == 0, mc == 1, [d_const, dE], [pd])
                    act.op(lambda e: e.activation(out=rinv, in_=pb, func=AF.Ln), reads=[pd], writes=[dri])
                    act.op(lambda e: e.activation(out=rinv, in_=rinv, func=AF.Exp, scale=-1.0), reads=[dri], writes=[dri])
                    for c in range(4):
                        pb, pd = nb()
                        for mc in range(2):
                            mm(pb, Vb[:, mc, h * 512 + c * 128:h * 512 + (c + 1) * 128], E[:, mc, :], mc == 0, mc == 1, [d_Vb, dE], [pd])
                        dve.op(lambda e: e.tensor_tensor(out=bufA[:, h * 4 + c, ts_], in0=pb, in1=rinv, op=ALU.mult), reads=[pd, dri], writes=[d_A])
            fw.barrier()
            out_proj(bufA, d_A, w_o, lambda rows, cols: h1_s[rows, cols], h2_s, Alloc(big, B0_OFF, M_OFF))
            fw.barrier()
            if "h2" in dbg_aps:
                tmp = Alloc(big, B0_OFF, M_OFF).f32(D)
                dtmp = Dep()
                for tt in range(NTT):
                    sp.dma(tmp, h2_s[tt * 128:(tt + 1) * 128, :], writes=[dtmp])
                    sp.dma(dbg_aps["h2"][tt * 128:(tt + 1) * 128, :], tmp, reads=[dtmp])
                fw.barrier()

        if stop >= 8:
            IOA = bass.IndirectOffsetOnAxis
            bc_reg = es.enter_context(nc.gpsimd.register("bc"))
            nc.gpsimd.reg_mov(bc_reg, NROW - 1)
            BCV = nc.gpsimd.snap(bc_reg)
            bw_reg = es.enter_context(nc.gpsimd.register("bw"))
            nc.gpsimd.reg_mov(bw_reg, 8191)
            BWV = nc.gpsimd.snap(bw_reg)
            al8 = Alloc(big, B0_OFF, WORDS)
            LT = al8.f32(128)
            iop = al8.f32(1)
            siota = al8.f32(32)
            thr8 = al8.f32(8)
            p1a, p2a = al8.f32(16), al8.f32(16)
            pos1i = al8.f32(16).bitcast(I32)
            pos2i = al8.f32(16).bitcast(I32)
            widx = al8.f32(NSLOT * 4).bitcast(I32)
            d_c8, d_pos, d_widx, d_pp = Dep(), Dep(), Dep(), Dep()
            P8_TOP = al8.top
            sp.dma(LT, cst[:, 768:896], writes=[d_c8])
            sp.dma(iop, cst[:, 896:897], writes=[d_c8], allow_slow_non_contiguous=True)
            sp.dma(siota, cst[:, 897:929], writes=[d_c8])
            sp.dma(thr8, cst[:, 929:937], writes=[d_c8])
            fw.barrier()
            xnb_all = r3(al8.bf16(16 * D), 16)
            d_xnb = [Dep() for _ in range(16)]
            xts = [(al8.f32(D), Dep()) for _ in range(3)]
            xn32s = [(al8.f32(D), Dep()) for _ in range(2)]
            junk = al8.bf16(D)
            d_junk = Dep()
            h32s = [(r3(al8.f32(16 * 128), 16), Dep()) for _ in range(2)]
            wr32 = r3(al8.f32(16 * 20), 16)
            d_wr = Dep()
            logits = r3(al8.f32(16 * 20), 16)
            d_log = Dep()
            sts = [(al8.f32(8), Dep()) for _ in range(4)]
            sp.dma(wr32, w_r.rearrange("(kc p) n -> p kc n", p=128), writes=[d_wr])
            load_gB(2)

            def a8_stage_a(tt):
                xt, dx = xts[tt % 3]
                xn32, dxn = xn32s[tt % 2]
                st, d_st = sts[tt % 4]
                sp.dma(xt, h2_s[tt * 128:(tt + 1) * 128, :], writes=[dx])
                act.op(lambda e: e.activation(out=junk, in_=xt, func=AF.Square, accum_out=st[:, 0:1]), reads=[dx], writes=[d_junk, d_st])
                dve.op(lambda e: e.tensor_scalar(out=st[:, 1:2], in0=st[:, 0:1], scalar1=1.0 / D, scalar2=1e-6, op0=ALU.mult, op1=ALU.add),
                       reads=[d_st], writes=[d_st])
                act.op(lambda e: e.activation(out=st[:, 2:3], in_=st[:, 1:2], func=AF.Sqrt), reads=[d_st], writes=[d_st])
                dve.op(lambda e: e.reciprocal(out=st[:, 3:4], in_=st[:, 2:3]), reads=[d_st], writes=[d_st])
                dve.op(lambda e: e.scalar_tensor_tensor(out=xn32, in0=xt, scalar=st[:, 3:4], in1=gBt, op0=ALU.mult, op1=ALU.mult),
                       reads=[dx, d_st, d_gB], writes=[dxn])
                act.op(lambda e: e.activation(out=xnb_all[:, tt, :], in_=xn32, func=AF.Copy), reads=[dxn], writes=[d_xnb[tt]])

            def a8_stage_b(tt):
                xn32, dxn = xn32s[tt % 2]
                h32, d_h32 = h32s[tt % 2]
                for q in range(4):
                    pf, pdf = nb()
                    for j in range(4):
                        kc = q * 4 + j
                        mm(pf[:, j * 128:(j + 1) * 128], xn32[:, kc * 128:(kc + 1) * 128], identf, True, True, [dxn, d_const], [pdf])
                    if q % 2 == 0:
                        dve.op(lambda e: e.tensor_copy(out=h32[:, q * 4:q * 4 + 4, :], in_=r3(pf, 4)), reads=[pdf], pws=[d_h32])
                    else:
                        act.op(lambda e: e.activation(out=h32[:, q * 4:q * 4 + 4, :], in_=r3(pf, 4), func=AF.Copy), reads=[pdf], pws=[d_h32])
                pb, pd = nb()
                for kc in range(KC):
                    mm(pb[:, 0:20], h32[:, kc, :], wr32[:, kc, :], kc == 0, False, [d_h32, d_wr], [pd])
                mm(pb[:, 0:20], ones1[0:1, 0:128], brow[0:1, 0:20], False, True, [d_const], [pd])
                dve.op(lambda e: e.tensor_copy(out=logits[:, tt, :], in_=pb[:, 0:20]), reads=[pd], writes=[d_log])

            for tt in range(17):
                if tt < 16:
                    a8_stage_a(tt)
                if tt >= 1:
                    a8_stage_b(tt - 1)
            NT_ = 16
            rt = [al8.f32(NT_ * 4) for _ in range(12)]
            rt4 = al8.f32(NT_ * 16)
            sel1 = al8.f32(NT_ * 16)
            sel2 = al8.f32(NT_ * 16)
            ind = al8.f32(NT_ * 16)
            tot = r3(al8.f32(NT_ * 16), NT_)
            tcum = r3(al8.f32(NT_ * 16), NT_)
            posall = al8.f32(NT_ * 16)
            ptmp = al8.f32(NT_ * 16)
            c8 = al8.f32(16 * 8)
            cnt, nsl, bsl, bsl256 = al8.f32(16), al8.f32(16), al8.f32(16), al8.f32(16)
            total = al8.f32(1)
            es32 = al8.f32(NSLOT * 16)
            esf, unused, wbase = al8.f32(NSLOT), al8.f32(NSLOT), al8.f32(NSLOT)
            pos1f, pos2f = al8.f32(16), al8.f32(16)
            widxf = al8.f32(NSLOT * 4).rearrange("p (s q) -> p s q", q=4)
            d_rt = Dep()
            lg = logits[:, :, 0:4]
            le = logits[:, :, 4:20].rearrange("p t (g e) -> p t g e", g=4)
            gmax, gsum, gw, m1, m2 = [rt[i][:, 0:NT_] for i in range(5)]
            goh, gsh, esel, oh1, e2 = [r3(rt[7 + i], NT_) for i in range(5)]
            t4 = rt4.rearrange("p (t g e) -> p t g e", t=NT_, g=4)

            def bc3(v):
                return v.unsqueeze(2).to_broadcast([128, NT_, 4])

            def v4(a):
                return a.rearrange("p (t g e) -> p t g e", t=NT_, g=4)
            R = [d_log, d_rt, d_c8]
            W_ = [d_rt]
            dve.op(lambda e: e.tensor_reduce(out=gmax, in_=lg, axis=AX.X, op=ALU.max), R, W_)
            dve.op(lambda e: e.tensor_tensor(out=goh, in0=lg, in1=bc3(gmax), op=ALU.is_equal), R, W_)
            dve.op(lambda e: e.tensor_tensor(out=gsh, in0=lg, in1=bc3(gmax), op=ALU.subtract), R, W_)
            act.op(lambda e: e.activation(out=gsh, in_=gsh, func=AF.Exp), R, W_)
            dve.op(lambda e: e.tensor_reduce(out=gsum, in_=gsh, axis=AX.X, op=ALU.add), R, W_)
            dve.op(lambda e: e.reciprocal(out=gw, in_=gsum), R, W_)
            dve.op(lambda e: e.tensor_tensor(out=t4, in0=le, in1=goh.unsqueeze(3).to_broadcast([128, NT_, 4, 4]), op=ALU.mult), R, W_)
            dve.op(lambda e: e.tensor_reduce(out=esel, in_=t4.rearrange("p t g e -> p t e g"), axis=AX.X, op=ALU.add), R, W_)
            dve.op(lambda e: e.tensor_reduce(out=m1, in_=esel, axis=AX.X, op=ALU.max), R, W_)
            dve.op(lambda e: e.tensor_tensor(out=oh1, in0=esel, in1=bc3(m1), op=ALU.is_equal), R, W_)
            dve.op(lambda e: e.scalar_tensor_tensor(out=e2, in0=oh1, scalar=-1e30, in1=esel, op0=ALU.mult, op1=ALU.add), R, W_)
            dve.op(lambda e: e.tensor_reduce(out=m2, in_=e2, axis=AX.X, op=ALU.max), R, W_)
            dve.op(lambda e: e.tensor_tensor(out=e2, in0=e2, in1=bc3(m2), op=ALU.is_equal), R, W_)
            dve.op(lambda e: e.tensor_tensor(out=p1a, in0=m1, in1=m2, op=ALU.subtract), R, W_ + [d_pp])
            act.op(lambda e: e.activation(out=p1a, in_=p1a, func=AF.Sigmoid), R + [d_pp], W_ + [d_pp])
            dve.op(lambda e: e.tensor_scalar(out=p2a, in0=p1a, scalar1=-1.0, scalar2=1.0, op0=ALU.mult, op1=ALU.add), R + [d_pp], W_ + [d_pp])
            dve.op(lambda e: e.tensor_tensor(out=p1a, in0=p1a, in1=gw, op=ALU.mult), R + [d_pp], W_ + [d_pp])
            dve.op(lambda e: e.tensor_tensor(out=p2a, in0=p2a, in1=gw, op=ALU.mult), R + [d_pp], W_ + [d_pp])
            dve.op(lambda e: e.tensor_tensor(out=v4(sel1), in0=goh.unsqueeze(3).to_broadcast([128, NT_, 4, 4]),
                                             in1=oh1.unsqueeze(2).to_broadcast([128, NT_, 4, 4]), op=ALU.mult), R, W_)
            dve.op(lambda e: e.tensor_tensor(out=v4(sel2), in0=goh.unsqueeze(3).to_broadcast([128, NT_, 4, 4]),
                                             in1=e2.unsqueeze(2).to_broadcast([128, NT_, 4, 4]), op=ALU.mult), R, W_)
            dve.op(lambda e: e.tensor_tensor(out=ind, in0=sel1, in1=sel2, op=ALU.add), R, W_)
            pw, pdw = nb()
            mm(pw[:, 0:256], LT, ind, True, True, [d_rt, d_c8], [pdw])
            pt_, pdt = nb()
            mm(pt_[:, 0:256], ones1, ind, True, True, [d_rt, d_const], [pdt])
            dve.op(lambda e: e.tensor_copy(out=tot, in_=r3(pt_[:, 0:256], NT_)), R + [pdt], W_)
            dve.op(lambda e: e.memset(tcum[:, 0, :], 0.0), R, W_)
            for tt in range(1, NT_):
                dve.op(lambda e: e.tensor_tensor(out=tcum[:, tt, :], in0=tcum[:, tt - 1, :], in1=tot[:, tt - 1, :], op=ALU.add), R, W_)
            dve.op(lambda e: e.tensor_tensor(out=cnt, in0=tcum[:, NT_ - 1, :], in1=tot[:, NT_ - 1, :], op=ALU.add), R, W_)
            dve.op(lambda e: e.tensor_tensor(out=r3(c8, 16), in0=cnt.unsqueeze(2).to_broadcast([128, 16, 8]),
                                             in1=thr8.unsqueeze(1).to_broadcast([128, 16, 8]), op=ALU.is_gt), R, W_)
            dve.op(lambda e: e.tensor_reduce(out=nsl, in_=r3(c8, 16), axis=AX.X, op=ALU.add), R, W_)
            dve.op(lambda e: e.memset(bsl[:, 0:1], 0.0), R, W_)
            for ex in range(1, 16):
                dve.op(lambda e: e.tensor_tensor(out=bsl[:, ex:ex + 1], in0=bsl[:, ex - 1:ex], in1=nsl[:, ex - 1:ex], op=ALU.add), R, W_)
            dve.op(lambda e: e.tensor_tensor(out=total, in0=bsl[:, 15:16], in1=nsl[:, 15:16], op=ALU.add), R, W_)
            dve.op(lambda e: e.tensor_scalar(out=bsl256, in0=bsl, scalar1=float(SL), scalar2=None, op0=ALU.mult), R, W_)
            dve.op(lambda e: e.tensor_tensor(out=posall, in0=pw[:, 0:256], in1=tcum.rearrange("p t e -> p (t e)"), op=ALU.add), R + [pdw], W_)
            dve.op(lambda e: e.tensor_tensor(out=r3(posall, NT_), in0=r3(posall, NT_), in1=bsl256.unsqueeze(1).to_broadcast([128, NT_, 16]), op=ALU.add), R, W_)
            dve.op(lambda e: e.tensor_tensor(out=ptmp, in0=posall, in1=sel1, op=ALU.mult), R, W_)
            dve.op(lambda e: e.tensor_reduce(out=pos1f, in_=r3(ptmp, NT_), axis=AX.X, op=ALU.add), R, W_)
            dve.op(lambda e: e.tensor_tensor(out=ptmp, in0=posall, in1=sel2, op=ALU.mult), R, W_)
            dve.op(lambda e: e.tensor_reduce(out=pos2f, in_=r3(ptmp, NT_), axis=AX.X, op=ALU.add), R, W_)
            dve.op(lambda e: e.tensor_copy(out=pos1i, in_=pos1f), R, W_ + [d_pos])
            dve.op(lambda e: e.tensor_copy(out=pos2i, in_=pos2f), R, W_ + [d_pos])
            dve.op(lambda e: e.tensor_tensor(out=r3(es32, NSLOT), in0=bsl.unsqueeze(1).to_broadcast([128, NSLOT, 16]),
                                             in1=siota[:, 0:NSLOT].unsqueeze(2).to_broadcast([128, NSLOT, 16]), op=ALU.is_le), R, W_)
            dve.op(lambda e: e.tensor_reduce(out=esf, in_=r3(es32, NSLOT), axis=AX.X, op=ALU.add), R, W_)
            dve.op(lambda e: e.tensor_scalar(out=unused, in0=siota[:, 0:NSLOT], scalar1=total[:, 0:1], scalar2=1.0e6, op0=ALU.is_ge, op1=ALU.mult), R, W_)
            dve.op(lambda e: e.tensor_scalar(out=wbase, in0=esf, scalar1=-1.0, scalar2=512.0, op0=ALU.add, op1=ALU.mult), R, W_)
            dve.op(lambda e: e.tensor_tensor(out=wbase, in0=wbase, in1=unused, op=ALU.add), R, W_)
            dve.op(lambda e: e.tensor_scalar(out=wbase, in0=wbase, scalar1=iop[:, 0:1], scalar2=None, op0=ALU.add), R, W_)
            for q in range(4):
                dve.op(lambda e: e.tensor_scalar(out=widxf[:, :, q], in0=wbase, scalar1=float(128 * q), scalar2=None, op0=ALU.add), R, W_)
            dve.op(lambda e: e.tensor_copy(out=widx, in_=widxf.rearrange("p s q -> p (s q)")), R, W_ + [d_widx])
            if "route" in dbg_aps:
                sp.dma(dbg_aps["route"][:, 0:16], pos1f, reads=[d_rt])
                sp.dma(dbg_aps["route"][:, 16:32], pos2f, reads=[d_rt])
                sp.dma(dbg_aps["route"][:, 32:64], wbase, reads=[d_rt])
                sp.dma(dbg_aps["route"][:, 64:80], p1a, reads=[d_pp])
                sp.dma(dbg_aps["route"][:, 80:96], p2a, reads=[d_pp])
                sp.dma(dbg_aps["route"][:, 96:112], cnt, reads=[d_rt])
            for tt in range(16):
                for posi in (pos1i, pos2i):
                    pool.dma_fn(lambda e: e.indirect_dma_start(out=Xs, out_offset=IOA(ap=posi[:, tt:tt + 1], axis=0), in_=xnb_all[:, tt, :], in_offset=None,
                                                               bounds_check=BCV, oob_is_err=False),
                                reads=[d_xnb[tt], d_pos], writes=[d_Xs])
            fw.barrier()
            ald = Alloc(big, P8_TOP, WORDS)
            wbufs = [(ald.bf16(8192), [Dep() for _ in range(4)]) for _ in range(6)]
            xsls = [(r3(ald.bf16(NA * D), NA), Dep()) for _ in range(2)]
            XTs = [(r3(ald.bf16(16 * SL), 16), Dep()) for _ in range(2)]
            hids = [(r3(ald.bf16(4 * SL), 4), Dep()) for _ in range(2)]
            sbs = [(ald.bf16(SL), Dep()) for _ in range(2)]
            yos = [(ald.f32(D), Dep()) for _ in range(2)]
            cnt8 = dict(yi=0, ei=0)

            def wload(i, s):
                wsl = []
                for m, wl in enumerate((wg_l, wu_l, wd_l)):
                    buf, deps = wbufs[(3 * i + m) % 6]
                    for q in range(4):
                        pool.dma_fn(lambda e: e.indirect_dma_start(out=buf[:, q * 2048:(q + 1) * 2048], out_offset=None, in_=wl,
                                                                   in_offset=IOA(ap=widx[:, s * 4 + q:s * 4 + q + 1], axis=0), bounds_check=BWV, oob_is_err=False),
                                    reads=[d_widx], writes=[deps[q]])
                    wsl.append((buf, deps))
                return wsl

            def xload(i, s):
                xsl, dxs = xsls[i % 2]
                sp.dma(xsl, Xs[s * SL:(s + 1) * SL, :].rearrange("(a p) n -> p a n", p=128), reads=[d_Xs], writes=[dxs])

            def emit_T(i, s):
                xsl, dxs = xsls[i % 2]
                XT, dXT = XTs[i % 2]
                for a in range(NA):
                    for q4 in range(4):
                        pb, pd = nb()
                        for j in range(4):
                            kc = q4 * 4 + j
                            mm(pb[:, j * 128:(j + 1) * 128], xsl[:, a, kc * 128:(kc + 1) * 128], identb, True, True, [dxs, d_const], [pd])
                        cnt8["ei"] += 1
                        if cnt8["ei"] % 2 == 0:
                            act.op(lambda e: e.activation(out=XT[:, q4 * 4:q4 * 4 + 4, a * 128:(a + 1) * 128], in_=r3(pb, 4), func=AF.Copy), reads=[pd], pws=[dXT])
                        else:
                            dve.op(lambda e: e.tensor_copy(out=XT[:, q4 * 4:q4 * 4 + 4, a * 128:(a + 1) * 128], in_=r3(pb, 4)), reads=[pd], pws=[dXT])

            def emit_GU(i, s, wsl):
                wg, dwg = r3(wsl[0][0], 16), wsl[0][1]
                wu, dwu = r3(wsl[1][0], 16), wsl[1][1]
                XT, dXT = XTs[i % 2]
                hid, dhid = hids[i % 2]
                for ffc in range(4):
                    pg, pdg = nb()
                    for kc in range(KC):
                        mm(pg[:, 0:SL], wg[:, kc, ffc * 128:(ffc + 1) * 128], XT[:, kc, :], kc == 0, kc == KC - 1, [dwg[kc // 4], dXT], [pdg])
                    pu, pdu = nb()
                    for kc in range(KC):
                        mm(pu[:, 0:SL], wu[:, kc, ffc * 128:(ffc + 1) * 128], XT[:, kc, :], kc == 0, kc == KC - 1, [dwu[kc // 4], dXT], [pdu])
                    sb_, dsb = sbs[ffc % 2]
                    act.op(lambda e: e.activation(out=sb_, in_=pg[:, 0:SL], func=AF.Silu), reads=[pdg], writes=[dsb])
                    dve.op(lambda e: e.tensor_tensor(out=hid[:, ffc, :], in0=pu[:, 0:SL], in1=sb_, op=ALU.mult), reads=[pdu, dsb], writes=[dhid])

            def emit_D(i, s, wsl):
                wd, dwd = r3(wsl[2][0], 4), wsl[2][1]
                hid, dhid = hids[i % 2]
                for a in range(NA):
                    yo, dyo = yos[cnt8["yi"] % 2]
                    cnt8["yi"] += 1
                    for dblk in range(4):
                        ds_ = slice(dblk * 512, (dblk + 1) * 512)
                        pb, pd = nb()
                        for ffc in range(4):
                            mm(pb, hid[:, ffc, a * 128:(a + 1) * 128], wd[:, ffc, ds_], ffc == 0, ffc == 3, [dhid, dwd[ffc]], [pd])
                        if dblk % 2 == 0:
                            act.op(lambda e: e.activation(out=yo[:, ds_], in_=pb, func=AF.Copy), reads=[pd], pws=[dyo])
                        else:
                            dve.op(lambda e: e.tensor_copy(out=yo[:, ds_], in_=pb), reads=[pd], pws=[dyo])
                    r0 = s * SL + a * 128
                    sp.dma(Ys[r0:r0 + 128, :], yo, reads=[dyo], writes=[d_Ys])

            lo_n = NSLOT - NSLOT // 3
            lo, hi = list(range(lo_n)), list(range(NSLOT - 1, lo_n - 1, -1))
            order = []
            while lo or hi:
                order += lo[:2]
                lo = lo[2:]
                if hi:
                    order.append(hi.pop(0))
            assert sorted(order) == list(range(NSLOT))
            xload(0, order[0])
            emit_T(0, order[0])
            for i, s in enumerate(order):
                wsl = wload(i, s)
                if i + 1 < NSLOT:
                    xload(i + 1, order[i + 1])
                emit_GU(i, s, wsl)
                if i + 1 < NSLOT:
                    emit_T(i + 1, order[i + 1])
                emit_D(i, s, wsl)
            fw.barrier()
            ale = Alloc(big, P8_TOP, WORDS)
            cts = [(ale.f32(D), Dep()) for _ in range(2)]
            y1s = [(ale.f32(D), Dep()) for _ in range(2)]
            y2s = [(ale.f32(D), Dep()) for _ in range(2)]
            junk2 = ale.bf16(D)
            sts2 = [(ale.f32(8), Dep()) for _ in range(4)]
            load_gB(4)
            for tt in range(16):
                xt, dx = cts[tt % 2]
                y1, dy1 = y1s[tt % 2]
                y2, dy2 = y2s[tt % 2]
                st, d_st = sts2[tt % 4]
                pool.dma(xt, h2_s[tt * 128:(tt + 1) * 128, :], writes=[dx])
                pool.dma_fn(lambda e: e.indirect_dma_start(out=y1, out_offset=None, in_=Ys, in_offset=IOA(ap=pos1i[:, tt:tt + 1], axis=0),
                                                           bounds_check=BCV, oob_is_err=False), reads=[d_Ys, d_pos], writes=[dy1])
                pool.dma_fn(lambda e: e.indirect_dma_start(out=y2, out_offset=None, in_=Ys, in_offset=IOA(ap=pos2i[:, tt:tt + 1], axis=0),
                                                           bounds_check=BCV, oob_is_err=False), reads=[d_Ys, d_pos], writes=[dy2])
                dve.op(lambda e: e.scalar_tensor_tensor(out=xt, in0=y1, scalar=p1a[:, tt:tt + 1], in1=xt, op0=ALU.mult, op1=ALU.add),
                       reads=[dy1, dx, d_pp], writes=[dx])
                dve.op(lambda e: e.scalar_tensor_tensor(out=xt, in0=y2, scalar=p2a[:, tt:tt + 1], in1=xt, op0=ALU.mult, op1=ALU.add),
                       reads=[dy2, dx, d_pp], writes=[dx])
                if "h3" in dbg_aps:
                    sp.dma(dbg_aps["h3"][tt * 128:(tt + 1) * 128, :], xt, reads=[dx])
                act.op(lambda e: e.activation(out=junk2, in_=xt, func=AF.Square, accum_out=st[:, 0:1]), reads=[dx], writes=[d_junk, d_st])
                dve.op(lambda e: e.tensor_scalar(out=st[:, 1:2], in0=st[:, 0:1], scalar1=1.0 / D, scalar2=1e-6, op0=ALU.mult, op1=ALU.add),
                       reads=[d_st], writes=[d_st])
                act.op(lambda e: e.activation(out=st[:, 2:3], in_=st[:, 1:2], func=AF.Sqrt), reads=[d_st], writes=[d_st])
                dve.op(lambda e: e.reciprocal(out=st[:, 3:4], in_=st[:, 2:3]), reads=[d_st], writes=[d_st])
                dve.op(lambda e: e.scalar_tensor_tensor(out=xt, in0=xt, scalar=st[:, 3:4], in1=gBt, op0=ALU.mult, op1=ALU.mult),
                       reads=[dx, d_st, d_gB], writes=[dx])
                sp.dma(out[tt * 128:(tt + 1) * 128, :], xt, reads=[dx])
            fw.barrier()

        fw.barrier()
    return nc


def host_consts(inp):
    l = 0
    f = np.float32
    gBh = np.stack([np.broadcast_to(v, (128, D)) for v in (inp["norm_mix_g"][l], inp["norm_xattn_g"][l], inp["norm_ffn_g"][l],
                                                            inp["norm_mem_g"][l], inp["norm_final_g"])]).astype(f)
    pvh = np.zeros((128, NPV), f)

    def col(v, n):
        return np.ascontiguousarray(np.asarray(v, f).reshape(n, 128).T)
    pvh[:, 0:8] = col(inp["pool_scale"][l], 8)
    mu = np.asarray(inp["rwkv_mu"][l], f)
    pvh[:, 8:32] = col(mu[0:3072], 24)
    pvh[:, 32] = mu[3072:3200]
    pvh[:, 33] = mu[3200:3328]
    pvh[0:32, 34] = mu[3328:3360]
    pvh[:, 35:43] = col(inp["rwkv_w0"][l], 8)
    pvh[:, 43:51] = col(inp["rwkv_a0"][l], 8)
    pvh[:, 51:59] = col(inp["rwkv_k_k"][l], 8)
    pvh[:, 59:67] = col(inp["rwkv_k_a"][l], 8)
    pvh[:, 67:75] = col(inp["rwkv_ln_w"][l], 8)
    pvh[:, 75:83] = col(inp["rwkv_ln_b"][l], 8)
    pvh[:, 83:91] = col(np.asarray(inp["rwkv_r_k"][l]).reshape(-1), 8)
    cst = np.zeros((128, 1024), f)
    p = np.arange(128)
    cst[:, 0:128] = np.eye(128, dtype=f)
    cst[:, 128:256] = (p[:, None] // 64 == p[None, :] // 64).astype(f)
    s = p % 64
    tcol = np.arange(64)
    strict = (s[:, None] < tcol[None, :]).astype(f)
    incl = (s[:, None] <= tcol[None, :]).astype(f)
    one = np.concatenate([strict, incl], 1)
    cst[:, 256:512] = np.concatenate([one, one], 1)
    low = (s[:, None] > tcol[None, :]).astype(f)
    cst[:, 512:640] = np.concatenate([low, low], 1)
    cst[:, 640:704] = (s[:, None] == tcol[None, :]).astype(f)
    tt = np.arange(16)
    for gi, w in enumerate((2, 4, 8, 16)):
        cst[:, 704 + gi * 16:704 + (gi + 1) * 16] = (1.0 / np.minimum(tt + 1, w)).astype(f)[None, :]
    cst[:, 768:896] = (p[:, None] < p[None, :]).astype(f)
    cst[:, 896] = p.astype(f)
    cst[:, 897:929] = np.arange(32, dtype=f)[None, :]
    cst[:, 929:937] = (float(SL) * np.arange(8, dtype=f))[None, :]
    rm = np.ones((128, T), f)
    rm[:, ::64] = 0.0
    w_r = np.concatenate([inp["moe_w_group"][l], inp["moe_w_expert"][l]], 1).astype(f)
    b_r = np.concatenate([inp["moe_b_group"][l], inp["moe_b_expert"][l]])[None, :].astype(f)
    return dict(gB=gBh, pv=pvh, cst=cst, rmask=rm, w_r=np.ascontiguousarray(w_r), b_r=b_r)


def make_in_maps(inp, cores):
    l = 0
    c = host_consts(inp)
    shared = dict(
        w_in=inp["w_in"][l], pool_w=inp["pool_w"][l], w2=inp["rwkv_w2"][l], a2=inp["rwkv_a2"][l], g2=inp["rwkv_g2"][l],
        w_out=inp["w_out"][l], w_q=inp["xattn_w_q"][l], w_kv=inp["xattn_w_kv"][l], w_o=inp["xattn_w_o"][l],
        wg_l=np.asarray(inp["moe_w_gate"][l], np.float32).reshape(16, 4, 4, 128, 512).transpose(0, 1, 3, 2, 4).reshape(8192, 2048),
        wu_l=np.asarray(inp["moe_w_up"][l], np.float32).reshape(16, 4, 4, 128, 512).transpose(0, 1, 3, 2, 4).reshape(8192, 2048),
        wd_l=np.asarray(inp["moe_w_down"][l], np.float32).reshape(8192, 2048), **c)
    shared = {k: np.ascontiguousarray(np.asarray(v, np.float32)) for k, v in shared.items()}
    maps = []
    for b in cores:
        m = dict(shared)
        m["x"] = np.ascontiguousarray(inp["x"][b])
        m["mem"] = np.ascontiguousarray(inp["mem"][b])
        maps.append(m)
    return maps


def kernel(**inputs):
    inp = {k: np.asarray(v) for k, v in inputs.items()}
    nc = build()
    maps = make_in_maps(inp, list(range(8)))
    res = run_bass_kernel_spmd(nc, maps, core_ids=list(range(8)))
    return np.stack([np.asarray(r["out"]) for r in res.results], 0).astype(np.float32)
```

```python
import numpy as np
import concourse.bass as bass
import concourse.mybir as mybir
from concourse.bass_utils import run_bass_kernel_spmd
from contextlib import ExitStack

F32 = mybir.dt.float32
BF16 = mybir.dt.bfloat16
I32 = mybir.dt.int32
AF = mybir.ActivationFunctionType
ALU = mybir.AluOpType
AX = mybir.AxisListType

D = 2048
KC = 16
T = 2048
NTT = T // 128
NTQ = T // 512
NCH = T // 64
PAD = 16
CDEC = float(np.exp(-0.5))
WORDS = 51200
NSLOT, SL = 26, 384
NA = SL // 128


class Dep:
    __slots__ = ("w", "r", "p")

    def __init__(self):
        self.w = None
        self.r = {}
        self.p = {}


class Eng:
    def __init__(self, fw, name, b, is_pe=False):
        self.fw, self.name, self.b, self.is_pe = fw, name, b, is_pe
        self.sem = fw.new_sem(name)
        self.cnt = 0
        self.waited = {}
        self.dma_slots = None
        self.dma_i = 0

    def _wait(self, tok):
        sem, val = tok
        if self.waited.get(id(sem), 0) < val:
            self.b.wait_ge(sem, val)
            self.waited[id(sem)] = val

    def _collect(self, reads, writes, pws=()):
        def w_(t):
            if t is not None and not (self.is_pe and t[0] is self.sem):
                self._wait(t)
        for d in reads:
            w_(d.w)
            for t in d.p.values():
                w_(t)
        for d in writes:
            w_(d.w)
            for t in d.p.values():
                w_(t)
            for t in d.r.values():
                w_(t)
        for d in pws:
            w_(d.w)
            for t in d.r.values():
                w_(t)

    def op(self, fn, reads=(), writes=(), pws=()):
        self._collect(reads, writes, pws)
        inst = fn(self.b)
        self.cnt += 1
        inst.then_inc(self.sem, 1)
        tok = (self.sem, self.cnt)
        for d in reads:
            d.r[id(self.sem)] = tok
        for d in writes:
            d.w = tok
            d.r = {}
            d.p = {}
        for d in pws:
            d.p[id(self.sem)] = tok
        return tok

    def dma(self, out, in_, reads=(), writes=(), **kw):
        return self.dma_fn(lambda e: e.dma_start(out=out, in_=in_, **kw), reads, writes)

    def dma_fn(self, fn, reads=(), writes=()):
        if self.dma_slots is None:
            self.dma_slots = [[self.fw.new_sem(f"{self.name}_d{i}"), 0] for i in range(8)]
        self._collect(reads, writes)
        slot = self.dma_slots[self.dma_i % len(self.dma_slots)]
        self.dma_i += 1
        if slot[1] > 0:
            self._wait((slot[0], slot[1]))
        inst = fn(self.b)
        slot[1] += 16
        inst.then_inc(slot[0], 16)
        tok = (slot[0], slot[1])
        for d in reads:
            d.r[id(slot[0])] = tok
        for d in writes:
            d.w = tok
            d.r = {}
            d.p = {}
        return tok


class FW:
    def __init__(self, nc, es):
        self.nc, self.es = nc, es
        self.pe = Eng(self, "pe", nc.tensor, True)
        self.act = Eng(self, "act", nc.scalar)
        self.dve = Eng(self, "dve", nc.vector)
        self.pool = Eng(self, "pool", nc.gpsimd)
        self.sp = Eng(self, "sp", nc.sync)
        self.engs = [self.pe, self.act, self.dve, self.pool, self.sp]

    def new_sem(self, name):
        return self.es.enter_context(self.nc.semaphore(name))

    def barrier(self):
        toks = []
        for e in self.engs:
            if e.cnt > 0:
                toks.append((e.sem, e.cnt))
            if e.dma_slots:
                for s in e.dma_slots:
                    if s[1] > 0:
                        toks.append((s[0], s[1]))
        for e in self.engs:
            for t in toks:
                if t[0] is not e.sem:
                    e._wait(t)


class Alloc:
    def __init__(self, big, start, end):
        self.big, self.top, self.end = big, start, end

    def f32(self, n):
        a = self.big[:, self.top:self.top + n]
        self.top += n
        assert self.top <= self.end, (self.top, self.end)
        return a

    def bf16(self, n):
        w = (n + 1) // 2
        a = self.big[:, self.top:self.top + w].bitcast(BF16)
        self.top += w
        assert self.top <= self.end, (self.top, self.end)
        return a[:, 0:n]


def r3(ap, a):
    return ap.rearrange("p (a b) -> p a b", a=a)


PV = dict(pool_scale=0, mu_rkv=8, mu_lo=32, w0=35, a0=43, k_k=51, k_a=59, ln_w=67, ln_b=75, r_k=83, omka=91)
NPV = 99


def build(stop=99, dbg=()):
    nc = bass.Bass("TRN2", target_bir_lowering=False)

    def din(name, shape):
        return nc.dram_tensor(name, list(shape), F32, kind="ExternalInput").ap()

    x = din("x", [T, D])
    mem = din("mem", [256, D])
    w_in = din("w_in", [D, 4384])
    pool_w = din("pool_w", [4, 256, 256])
    w2 = din("w2", [64, 1024])
    a2 = din("a2", [64, 1024])
    g2 = din("g2", [160, 1024])
    w_out = din("w_out", [D, D])
    w_q = din("w_q", [D, D])
    w_kv = din("w_kv", [D, 2 * D])
    w_o = din("w_o", [D, D])
    w_r = din("w_r", [D, 20])
    b_r = din("b_r", [1, 20])
    wg_l = din("wg_l", [8192, 2048])
    wu_l = din("wu_l", [8192, 2048])
    wd_l = din("wd_l", [8192, 2048])
    gB = din("gB", [5, 128, D])
    pvd = din("pv", [128, NPV])
    cst = din("cst", [128, 1024])
    rmask_d = din("rmask", [128, T])
    out = nc.dram_tensor("out", [T, D], F32, kind="ExternalOutput").ap()
    dbg_aps = {}
    for name, shape in dbg:
        dbg_aps[name] = nc.dram_tensor(name, list(shape), F32, kind="ExternalOutput").ap()
    rkv_s = nc.dram_tensor("rkv_s", [24, 128, T], F32, kind="Internal").ap()
    h1_s = nc.dram_tensor("h1_s", [T, D], F32, kind="Internal").ap()
    h2_s = nc.dram_tensor("h2_s", [T, D], F32, kind="Internal").ap()
    NROW = NSLOT * SL
    Xs = nc.dram_tensor("Xs", [NROW, D], BF16, kind="Internal").ap()
    Ys = nc.dram_tensor("Ys", [NROW, D], F32, kind="Internal").ap()

    with ExitStack() as es:
        fw = FW(nc, es)
        pe, act, dve, pool, sp = fw.pe, fw.act, fw.dve, fw.pool, fw.sp
        big = es.enter_context(nc.sbuf_tensor("big", [128, WORDS], F32))[:]
        banks = [(es.enter_context(nc.psum_tensor(f"bk{i}", [128, 512], F32))[:], Dep()) for i in range(8)]
        bki = [0]

        def nb():
            b = banks[bki[0] % 8]
            bki[0] += 1
            return b

        def mm(o, lhsT, rhs, start, stop, reads, writes):
            pe.op(lambda e: e.matmul(o, lhsT=lhsT, rhs=rhs, start=start, stop=stop), reads, writes)

        CONST_W = 7424
        ca = Alloc(big, 0, CONST_W)
        identf = ca.f32(128)
        blk1 = ca.f32(128)
        mSI = ca.f32(256)
        mL = ca.f32(128)
        i64 = ca.f32(64)
        rcnt = ca.f32(64)
        pv = ca.f32(NPV + 1)
        identb = ca.bf16(128)
        onesb = ca.bf16(128)
        rmask = ca.bf16(T)
        gBt = ca.f32(D)
        lo1 = ca.bf16(T)
        sg1 = ca.bf16(T)
        sg2 = ca.bf16(T)
        ones1 = ca.f32(128)
        brow = ca.f32(20)
        d_const, d_gB, d_lo1, d_sg1, d_sg2 = Dep(), Dep(), Dep(), Dep(), Dep()
        B0_OFF = CONST_W
        B1_OFF = B0_OFF + 8192
        M_OFF = B1_OFF + 8192
        A_OFF = WORDS - 16384
        bufA = r3(big[:, A_OFF:WORDS].bitcast(BF16), 16)
        bufB = r3(big[:, B0_OFF:M_OFF].bitcast(BF16), 16)
        d_A, d_B = Dep(), Dep()

        sp.dma(identf, cst[:, 0:128], writes=[d_const])
        sp.dma(blk1, cst[:, 128:256], writes=[d_const])
        sp.dma(mSI, cst[:, 256:512], writes=[d_const])
        sp.dma(mL, cst[:, 512:640], writes=[d_const])
        sp.dma(i64, cst[:, 640:704], writes=[d_const])
        sp.dma(rcnt, cst[:, 704:768], writes=[d_const])
        sp.dma(pv[:, 0:NPV], pvd, writes=[d_const])
        sp.dma(brow[0:1, :], b_r, writes=[d_const])
        pool.dma(identb, cst[:, 0:128], writes=[d_const])
        pool.dma(rmask, rmask_d, writes=[d_const])
        pool.op(lambda e: e.memset(onesb, 1.0), writes=[d_const])
        pool.op(lambda e: e.memset(ones1, 1.0), writes=[d_const])
        dve.op(lambda e: e.tensor_scalar(out=pv[:, PV["omka"]:PV["omka"] + 8], in0=pv[:, PV["k_a"]:PV["k_a"] + 8],
                                         scalar1=-1.0, scalar2=1.0, op0=ALU.mult, op1=ALU.add), reads=[d_const], writes=[d_const])
        fw.barrier()

        def pvc(name, j):
            c = PV[name] + j
            return pv[:, c:c + 1]

        d_Xs, d_Ys = Dep(), Dep()
        zf = [0]

        def zero_fill(n, zt, dz):
            while n > 0 and zf[0] < NROW // 128 and stop >= 8:
                c = zf[0]
                pool.dma(Xs[c * 128:(c + 1) * 128, :], zt, reads=[dz])
                zf[0] += 1
                n -= 1

        def load_gB(i):
            sp.dma(gBt, gB[i], writes=[d_gB])

        def norm_tiles(al, ntiles, src_fn, dstT, d_dst, tok_off=0, keep=None, nbuf=3):
            xts = [(al.f32(D), Dep()) for _ in range(nbuf)]
            xns = [(al.bf16(D), Dep()) for _ in range(nbuf)]
            junk = al.bf16(D)
            d_junk = Dep()
            sts = [(al.f32(8), Dep()) for _ in range(4)]

            def stage_a(i):
                xt, dx = xts[i % nbuf]
                xn, dn = xns[i % nbuf]
                st, d_st = sts[i % 4]
                sp.dma(xt, src_fn(i), writes=[dx])
                if keep is not None:
                    keep(i, xt, dx)
                act.op(lambda e: e.activation(out=junk, in_=xt, func=AF.Square, accum_out=st[:, 0:1]), reads=[dx], writes=[d_junk, d_st])
                dve.op(lambda e: e.tensor_scalar(out=st[:, 1:2], in0=st[:, 0:1], scalar1=1.0 / D, scalar2=1e-6, op0=ALU.mult, op1=ALU.add),
                       reads=[d_st], writes=[d_st])
                act.op(lambda e: e.activation(out=st[:, 2:3], in_=st[:, 1:2], func=AF.Sqrt), reads=[d_st], writes=[d_st])
                dve.op(lambda e: e.reciprocal(out=st[:, 3:4], in_=st[:, 2:3]), reads=[d_st], writes=[d_st])
                dve.op(lambda e: e.scalar_tensor_tensor(out=xn, in0=xt, scalar=st[:, 3:4], in1=gBt, op0=ALU.mult, op1=ALU.mult),
                       reads=[dx, d_st, d_gB], writes=[dn])

            def stage_b(i):
                xn, dn = xns[i % nbuf]
                for q in range(4):
                    pb, pd = nb()
                    for j in range(4):
                        kc = q * 4 + j
                        mm(pb[:, j * 128:(j + 1) * 128], xn[:, kc * 128:(kc + 1) * 128], identb, True, True, [dn, d_const], [pd])
                    t0 = tok_off + i * 128
                    if q % 2 == 0:
                        act.op(lambda e: e.activation(out=dstT[:, q * 4:q * 4 + 4, t0:t0 + 128], in_=r3(pb, 4), func=AF.Copy), reads=[pd], pws=[d_dst])
                    else:
                        dve.op(lambda e: e.tensor_copy(out=dstT[:, q * 4:q * 4 + 4, t0:t0 + 128], in_=r3(pb, 4)), reads=[pd], pws=[d_dst])

            for i in range(ntiles + 1):
                if i < ntiles:
                    stage_a(i)
                if i >= 1:
                    stage_b(i - 1)

        def dump(name, ap_sb, dep, dst=None):
            if name in dbg_aps:
                sp.dma(dbg_aps[name] if dst is None else dst, ap_sb, reads=[dep])

        load_gB(0)
        al = Alloc(big, M_OFF, A_OFF)
        norm_tiles(al, NTT, lambda i: x[i * 128:(i + 1) * 128, :], bufA, d_A)
        fw.barrier()
        if "hnT" in dbg_aps:
            tmp = Alloc(big, M_OFF, A_OFF).f32(T)
            dtmp = Dep()
            for kc in range(16):
                dve.op(lambda e: e.tensor_copy(out=tmp, in_=bufA[:, kc, :]), reads=[d_A], writes=[dtmp])
                sp.dma(dbg_aps["hnT"][kc * 128:(kc + 1) * 128, :], tmp, reads=[dtmp])
            fw.barrier()

        if stop >= 2:
            al = Alloc(big, B1_OFF, A_OFF)
            wbs = [(r3(al.bf16(16 * 128), 16), Dep()) for _ in range(4)]
            wbi = [0]
            pbufs = [(al.f32(PAD + T), Dep()) for _ in range(2)]
            fbs = [(al.f32(PAD + T), Dep()) for _ in range(3)]
            pooled = [(al.bf16(T), Dep()) for _ in range(2)]
            pwb = r3(al.bf16(8 * 256), 8)
            d_pw = Dep()
            ztile = al.bf16(D)
            d_zt = Dep()
            dve.op(lambda e: e.memset(ztile, 0.0), writes=[d_zt])
            for pbf, dp in pbufs + fbs:
                dve.op(lambda e: e.memset(pbf[:, 0:PAD], 0.0), writes=[dp])
            pool.dma(pwb, pool_w.rearrange("g (cc p) d -> p (g cc) d", p=128), writes=[d_pw])
            pbi = [0]

            def proj_block(col0, n):
                ws, dw = wbs[wbi[0] % 4]
                wbi[0] += 1
                pool.dma(ws[:, :, 0:n], w_in[:, col0:col0 + n].rearrange("(kc p) n -> p kc n", p=128), writes=[dw])
                if wbi[0] > 4:
                    zero_fill(3, ztile, d_zt)
                pbf, dp = pbufs[pbi[0] % 2]
                pbi[0] += 1
                for tq in range(NTQ):
                    pb, pd = nb()
                    for kc in range(KC):
                        mm(pb[0:n, :], ws[:, kc, 0:n], bufA[:, kc, tq * 512:(tq + 1) * 512], kc == 0, kc == KC - 1, [dw, d_A], [pd])
                    act.op(lambda e: e.activation(out=pbf[0:n, PAD + tq * 512:PAD + (tq + 1) * 512], in_=pb[0:n, :], func=AF.Copy), reads=[pd], writes=[dp])
                return pbf, dp

            def tshift(pbf, dp, n, mu_ap, zout, dz):
                f0, df0 = fbs[0]
                dve.op(lambda e: e.tensor_tensor(out=f0[0:n, 0:T], in0=pbf[0:n, PAD - 1:PAD - 1 + T], in1=pbf[0:n, PAD:PAD + T], op=ALU.subtract),
                       reads=[dp], writes=[df0])
                dve.op(lambda e: e.scalar_tensor_tensor(out=zout, in0=f0[0:n, 0:T], scalar=mu_ap, in1=pbf[0:n, PAD:PAD + T], op0=ALU.mult, op1=ALU.add),
                       reads=[df0, dp, d_const], writes=[dz])

            z1f, dz1 = fbs[1]
            z1 = z1f[:, PAD:PAD + T]
            pbf, dp = proj_block(4096, 128)
            tshift(pbf, dp, 128, pv[:, PV["mu_lo"]:PV["mu_lo"] + 1], z1, dz1)
            act.op(lambda e: e.activation(out=lo1[0:64, :], in_=z1[0:64, :], func=AF.Tanh), reads=[dz1], writes=[d_lo1])
            act.op(lambda e: e.activation(out=lo1[64:128, :], in_=z1[64:128, :], func=AF.Copy), reads=[dz1], writes=[d_lo1])
            pbf, dp = proj_block(4224, 128)
            tshift(pbf, dp, 128, pv[:, PV["mu_lo"] + 1:PV["mu_lo"] + 2], z1, dz1)
            act.op(lambda e: e.activation(out=sg1, in_=z1, func=AF.Sigmoid), reads=[dz1], writes=[d_sg1])
            pbf, dp = proj_block(4352, 32)
            tshift(pbf, dp, 32, pv[0:32, PV["mu_lo"] + 2:PV["mu_lo"] + 3], z1[0:32, :], dz1)
            act.op(lambda e: e.activation(out=sg2[0:32, :], in_=z1[0:32, :], func=AF.Sigmoid), reads=[dz1], writes=[d_sg2])
            for j in range(24):
                pbf, dp = proj_block(1024 + j * 128, 128)
                tshift(pbf, dp, 128, pvc("mu_rkv", j), z1, dz1)
                sp.dma(rkv_s[j], z1, reads=[dz1])
            for cb in range(8):
                gi = cb // 2
                w = (2, 4, 8, 16)[gi]
                pbf, dp = proj_block(cb * 128, 128)
                (fa, dfa), (fb_, dfb) = fbs[1], fbs[2]
                src, dsrc = pbf, dp
                sh = 1
                k = 0
                while sh < w:
                    dst, ddst = (fa, dfa) if k % 2 == 0 else (fb_, dfb)
                    dve.op(lambda e: e.tensor_tensor(out=dst[:, PAD:PAD + T], in0=src[:, PAD:PAD + T], in1=src[:, PAD - sh:PAD - sh + T], op=ALU.add),
                           reads=[dsrc], writes=[ddst])
                    src, dsrc = dst, ddst
                    sh *= 2
                    k += 1
                po, dpo = pooled[cb % 2]
                dve.op(lambda e: e.scalar_tensor_tensor(out=po, in0=src[:, PAD:PAD + T], scalar=1.0 / w, in1=pbf[:, PAD:PAD + T], op0=ALU.mult, op1=ALU.subtract),
                       reads=[dsrc, dp], writes=[dpo])
                f0, df0 = fbs[0]
                dve.op(lambda e: e.tensor_tensor(out=f0[:, 0:16], in0=src[:, PAD:PAD + 16], in1=rcnt[:, gi * 16:(gi + 1) * 16], op=ALU.mult),
                       reads=[dsrc, d_const], writes=[df0])
                dve.op(lambda e: e.tensor_tensor(out=po[:, 0:16], in0=f0[:, 0:16], in1=pbf[:, PAD:PAD + 16], op=ALU.subtract),
                       reads=[df0, dp], writes=[dpo])
                if cb % 2 == 1:
                    for db in range(2):
                        for tq in range(NTQ):
                            pb, pd = nb()
                            for cc in range(2):
                                mm(pb, pwb[:, gi * 2 + cc, db * 128:(db + 1) * 128], pooled[cc][0][:, tq * 512:(tq + 1) * 512], cc == 0, cc == 1,
                                   [d_pw, pooled[cc][1]], [pd])
                            blk = gi * 2 + db
                            act.op(lambda e: e.activation(out=bufB[:, blk, tq * 512:(tq + 1) * 512], in_=pb, func=AF.Copy, scale=pvc("pool_scale", blk)),
                                   reads=[pd, d_const], writes=[d_B])
            zero_fill(NROW, ztile, d_zt)
            fw.barrier()
            if "mixT" in dbg_aps:
                tmp = Alloc(big, B1_OFF, A_OFF).f32(T)
                dtmp = Dep()
                for kc in range(8):
                    dve.op(lambda e: e.tensor_copy(out=tmp, in_=bufB[:, kc, :]), reads=[d_B], writes=[dtmp])
                    sp.dma(dbg_aps["mixT"][kc * 128:(kc + 1) * 128, :], tmp, reads=[dtmp])
                fw.barrier()


        if stop >= 3:
            QTK = 512
            NQ = T // QTK
            al = Alloc(big, M_OFF, WORDS)
            lw = al.bf16(1024)
            g2a = al.bf16(1024)
            g2b = al.bf16(1024)
            S32 = al.f32(64)
            Sbf = al.bf16(64)
            d_lw, d_S32, d_Sbf = Dep(), Dep(), Dep()
            sets = []
            for i in range(3):
                sets.append(dict(AR=r3(al.bf16(8 * 128), 8), BK=r3(al.bf16(8 * 128), 8), BKh=r3(al.bf16(8 * 128), 8), vb=al.bf16(QTK),
                                 GL=al.f32(8), gbuf=al.bf16(QTK), bonus=al.f32(QTK), ybuf=al.f32(QTK),
                                 d_AR=Dep(), d_BK=Dep(), d_BKh=Dep(), d_vb=Dep(), d_GL=Dep(), d_g=Dep(), d_bonus=Dep(), d_y=Dep(),
                                 d_ARr=[Dep() for _ in range(4)]))
            Fq = [al.f32(QTK) for _ in range(8)]
            dFq = [Dep() for _ in range(8)]
            cbs = []
            for i in range(2):
                cbs.append(dict(NBall=r3(al.bf16(8 * 128), 8), KBall=r3(al.bf16(8 * 128), 8), TM=r3(al.bf16(8 * 320), 8), APU=r3(al.bf16(8 * 128), 8),
                                Mc=r3(al.f32(8 * 64), 8), CcT=r3(al.f32(8 * 64), 8),
                                d_NB=[Dep() for _ in range(4)], d_KB=[Dep() for _ in range(4)], d_TM=[Dep() for _ in range(4)],
                                d_APU=[Dep() for _ in range(4)], d_Mc=[Dep() for _ in range(4)], d_Cc=[Dep() for _ in range(4)]))
            TT = r3(al.bf16(8 * 64), 8)
            Wg = [[r3(al.bf16(2 * 128), 2) for _ in range(2)] for _ in range(4)]
            NTg = [[r3(al.bf16(2 * 64), 2) for _ in range(2)] for _ in range(4)]
            d_TT = [Dep() for _ in range(4)]
            dWg = [[Dep(), Dep()] for _ in range(4)]
            dNTg = [[Dep(), Dep()] for _ in range(4)]
            yc, sq, rs = al.f32(QTK), al.f32(QTK), al.f32(QTK)
            dyc, dsq, drs = Dep(), Dep(), Dep()
            pool.dma(lw[0:64, :], w2, writes=[d_lw])
            pool.dma(lw[64:128, :], a2, writes=[d_lw])
            pool.dma(g2a, g2[0:128, :], writes=[d_lw])
            pool.dma(g2b[0:32, :], g2[128:160, :], writes=[d_lw])
            HS = [slice(0, 64), slice(64, 128)]
            i64b = i64.unsqueeze(1).to_broadcast([128, 2, 64])
            pyb, pdyb = banks[7]
            nbm = [0]

            def nb7():
                b = banks[nbm[0] % 7]
                nbm[0] += 1
                return b

            def prep_gen(u):
                hp, q = divmod(u, NQ)
                S_ = sets[u % 3]
                cs = slice(hp * 128, (hp + 1) * 128)
                tsl = slice(q * QTK, (q + 1) * QTK)
                k_, sgw, alr, cum, kk, f5, f6, f7 = Fq
                dk, dsgw, dalr, dcum, dkk, df5, df6, df7 = dFq
                AR, BK, BKh, vb, GL, gbuf, bonus = S_["AR"], S_["BK"], S_["BKh"], S_["vb"], S_["GL"], S_["gbuf"], S_["bonus"]
                d_AR, d_BK, d_BKh, d_vb, d_GL, d_g, d_bonus = S_["d_AR"], S_["d_BK"], S_["d_BKh"], S_["d_vb"], S_["d_GL"], S_["d_g"], S_["d_bonus"]
                sp.dma(k_, rkv_s[8 + hp][:, tsl], writes=[dk])
                pb, pd = nb7()
                mm(pb, lw[0:64, cs], lo1[0:64, tsl], True, True, [d_lw, d_lo1], [pd])
                act.op(lambda e: e.activation(out=sgw, in_=pb, func=AF.Sigmoid, bias=pvc("w0", hp)), reads=[pd, d_const], writes=[dsgw])
                pb, pd = nb7()
                mm(pb, lw[64:128, cs], lo1[64:128, tsl], True, True, [d_lw, d_lo1], [pd])
                act.op(lambda e: e.activation(out=alr, in_=pb, func=AF.Sigmoid, bias=pvc("a0", hp)), reads=[pd, d_const], writes=[dalr])
                yield
                pb, pd = nb7()
                mm(pb, g2a[:, cs], sg1[:, tsl], True, False, [d_lw, d_sg1], [pd])
                mm(pb, g2b[0:32, cs], sg2[0:32, tsl], False, True, [d_lw, d_sg2], [pd])
                act.op(lambda e: e.activation(out=gbuf, in_=pb, func=AF.Copy), reads=[pd], writes=[d_g])
                dve.op(lambda e: e.tensor_tensor_scan(out=cum, data0=rmask[:, 0:QTK], data1=sgw, initial=0.0, op0=ALU.mult, op1=ALU.add),
                       reads=[d_const, dsgw], writes=[dcum])
                yield
                act.op(lambda e: e.activation(out=kk, in_=k_, func=AF.Copy, scale=pvc("k_k", hp)), reads=[dk, d_const], writes=[dkk])
                pool.op(lambda e: e.tensor_tensor(out=f5, in0=kk, in1=kk, op=ALU.mult), reads=[dkk], writes=[df5])
                yield
                pb, pd = nb7()
                mm(pb, blk1, f5, True, True, [d_const, df5], [pd])
                dve.op(lambda e: e.tensor_scalar(out=f6, in0=pb, scalar1=1e-24, scalar2=None, op0=ALU.max), reads=[pd], writes=[df6])
                act.op(lambda e: e.activation(out=f6, in_=f6, func=AF.Sqrt), reads=[df6], writes=[df6])
                yield
                dve.op(lambda e: e.reciprocal(out=f6, in_=f6), reads=[df6], writes=[df6])
                yield
                pool.op(lambda e: e.tensor_tensor(out=kk, in0=kk, in1=f6, op=ALU.mult), reads=[dkk, df6], writes=[dkk])
                dve.op(lambda e: e.tensor_scalar(out=f5, in0=alr, scalar1=pvc("k_a", hp), scalar2=pvc("omka", hp), op0=ALU.mult, op1=ALU.add),
                       reads=[dalr, d_const], writes=[df5])
                yield
                pool.op(lambda e: e.tensor_tensor(out=f5, in0=f5, in1=k_, op=ALU.mult), reads=[df5, dk], writes=[df5])
                pool.op(lambda e: e.tensor_tensor(out=alr, in0=alr, in1=kk, op=ALU.mult), reads=[dalr, dkk], writes=[dalr])
                yield
                act.op(lambda e: e.activation(out=f6, in_=cum, func=AF.Exp, scale=CDEC), reads=[dcum], writes=[df6])
                dve.op(lambda e: e.tensor_tensor(out=BK[:, :, 0:64], in0=r3(alr, 8), in1=r3(f6, 8), op=ALU.mult), reads=[dalr, df6], writes=[d_BK])
                pool.op(lambda e: e.tensor_tensor(out=BK[:, :, 64:128], in0=r3(f5, 8), in1=r3(f6, 8), op=ALU.mult), reads=[df5, df6], writes=[d_BK])
                yield
                dve.op(lambda e: e.tensor_tensor(out=r3(f6, 8), in0=r3(cum, 8), in1=r3(cum, 8)[:, :, 63:64].to_broadcast([128, 8, 64]), op=ALU.subtract),
                       reads=[dcum], writes=[df6])
                act.op(lambda e: e.activation(out=f6, in_=f6, func=AF.Exp, scale=CDEC), reads=[df6], writes=[df6])
                yield
                dve.op(lambda e: e.tensor_tensor(out=BKh[:, :, 0:64], in0=r3(alr, 8), in1=r3(f6, 8), op=ALU.mult), reads=[dalr, df6], writes=[d_BKh])
                pool.op(lambda e: e.tensor_tensor(out=BKh[:, :, 64:128], in0=r3(f5, 8), in1=r3(f6, 8), op=ALU.mult), reads=[df5, df6], writes=[d_BKh])
                yield
                pool.op(lambda e: e.tensor_tensor(out=f6, in0=cum, in1=sgw, op=ALU.subtract), reads=[dcum, dsgw], writes=[df6])
                act.op(lambda e: e.activation(out=f6, in_=f6, func=AF.Exp, scale=-CDEC), reads=[df6], writes=[df6])
                yield
                dve.op(lambda e: e.scalar_tensor_tensor(out=AR[:, :, 0:64], in0=r3(kk, 8), scalar=-1.0, in1=r3(f6, 8), op0=ALU.mult, op1=ALU.mult),
                       reads=[dkk, df6], writes=[d_AR])
                act.op(lambda e: e.activation(out=GL, in_=r3(cum, 8)[:, :, 63], func=AF.Exp, scale=-CDEC), reads=[dcum], writes=[d_GL])
                yield
                act.op(lambda e: e.activation(out=f6, in_=cum, func=AF.Exp, scale=-CDEC), reads=[dcum], writes=[df6])
                sp.dma(k_, rkv_s[hp][:, tsl], writes=[dk])
                pool.op(lambda e: e.tensor_tensor(out=AR[:, :, 64:128], in0=r3(k_, 8), in1=r3(f6, 8), op=ALU.mult), reads=[dk, df6], writes=[d_AR] + S_["d_ARr"])
                yield
                dve.op(lambda e: e.scalar_tensor_tensor(out=f6, in0=k_, scalar=pvc("r_k", hp), in1=f5, op0=ALU.mult, op1=ALU.mult),
                       reads=[dk, df5, d_const], writes=[df6])
                sp.dma(sgw, rkv_s[16 + hp][:, tsl], writes=[dsgw])
                yield
                pb, pd = nb7()
                mm(pb, blk1, f6, True, True, [d_const, df6], [pd])
                dve.op(lambda e: e.tensor_tensor(out=bonus, in0=pb, in1=sgw, op=ALU.mult), reads=[pd, dsgw], writes=[d_bonus])
                act.op(lambda e: e.activation(out=vb, in_=sgw, func=AF.Copy), reads=[dsgw], writes=[d_vb])
                yield

            def front_gen(u):
                hp, q = divmod(u, NQ)
                S_ = sets[u % 3]
                C_ = cbs[u % 2]
                tsl = slice(q * QTK, (q + 1) * QTK)
                AR, BK, BKh, vb, GL, gbuf, bonus, ybuf = S_["AR"], S_["BK"], S_["BKh"], S_["vb"], S_["GL"], S_["gbuf"], S_["bonus"], S_["ybuf"]
                d_AR, d_BK, d_BKh, d_vb, d_GL, d_g, d_bonus, dy = S_["d_AR"], S_["d_BK"], S_["d_BKh"], S_["d_vb"], S_["d_GL"], S_["d_g"], S_["d_bonus"], S_["d_y"]
                d_ARr = S_["d_ARr"]
                NBall, KBall, TM, APU, Mc, CcT = C_["NBall"], C_["KBall"], C_["TM"], C_["APU"], C_["Mc"], C_["CcT"]
                d_NB, d_KB, d_TM, d_APU, d_Mc, d_Cc = C_["d_NB"], C_["d_KB"], C_["d_TM"], C_["d_APU"], C_["d_Mc"], C_["d_Cc"]
                pool.op(lambda e: e.tensor_tensor(out=Mc, in0=i64.unsqueeze(1).to_broadcast([128, 8, 64]),
                                                  in1=GL.unsqueeze(2).to_broadcast([128, 8, 64]), op=ALU.mult),
                        reads=[d_const, d_GL], writes=d_Mc)
                for g in range(4):
                    l0 = 2 * g
                    pa, pda = nb7()
                    pb_, pdb = nb7()
                    pt, pdt = nb7()
                    pv_, pdv = nb7()
                    for ci in range(2):
                        c = l0 + ci
                        for h in range(2):
                            hs = HS[h]
                            mm(pa[hs, ci * 128:(ci + 1) * 128], BK[hs, c, 0:64], AR[hs, c, :], True, True, [d_BK, d_AR, d_ARr[g]], [pda])
                            mm(pb_[hs, ci * 128:(ci + 1) * 128], BK[hs, c, 64:128], AR[hs, c, :], True, True, [d_BK, d_AR, d_ARr[g]], [pdb])
                            mm(pt[hs, ci * 64:(ci + 1) * 64], AR[hs, c, 0:64], BK[hs, c, 0:64], True, True, [d_BK, d_AR], [pdt])
                            idh = identb[hs, 64 * h:64 * h + 64]
                            mm(pv_[hs, ci * 256:ci * 256 + 64], vb[hs, c * 64:(c + 1) * 64], idh, True, True, [d_vb, d_const], [pdv])
                            mm(pv_[hs, ci * 256 + 64:ci * 256 + 128], BKh[hs, c, 0:64], idh, True, True, [d_BKh, d_const], [pdv])
                            mm(pv_[hs, ci * 256 + 128:ci * 256 + 192], BKh[hs, c, 64:128], idh, True, True, [d_BKh, d_const], [pdv])
                            mm(pv_[hs, ci * 256 + 192:ci * 256 + 256], AR[hs, c, 0:64], idh, True, True, [d_AR, d_const], [pdv])
                    dve.op(lambda e: e.tensor_tensor(out=NBall[:, l0:l0 + 2, :], in0=r3(pa[:, 0:256], 2), in1=r3(mSI, 2), op=ALU.mult),
                           reads=[pda, d_const], writes=[d_NB[g]])
                    dve.op(lambda e: e.tensor_tensor(out=KBall[:, l0:l0 + 2, :], in0=r3(pb_[:, 0:256], 2), in1=r3(mSI, 2), op=ALU.mult),
                           reads=[pdb, d_const], writes=[d_KB[g]])
                    dve.op(lambda e: e.tensor_tensor(out=NTg[g][0], in0=r3(pt[:, 0:128], 2), in1=r3(mL, 2), op=ALU.mult),
                           reads=[pdt, d_const], writes=[dNTg[g][0]])
                    act.op(lambda e: e.activation(out=TM[:, l0:l0 + 2, 0:256], in_=r3(pv_, 2), func=AF.Copy), reads=[pdv], writes=[d_TM[g]])
                    pool.op(lambda e: e.tensor_tensor(out=Wg[g][0][:, :, 64:128], in0=NBall[:, l0:l0 + 2, 0:64], in1=i64b, op=ALU.add),
                            reads=[d_NB[g], d_const], writes=[dWg[g][0]])
                    yield
                for g in range(4):
                    l0 = 2 * g
                    p0, pd0 = nb7()
                    q0, qd0 = nb7()
                    NT, dNT = NTg[g][0], dNTg[g][0]
                    for ci in range(2):
                        for h in range(2):
                            hs = HS[h]
                            mm(p0[hs, ci * 64:(ci + 1) * 64], NT[hs, ci, :], NBall[hs, l0 + ci, 0:64], True, True, [dNT, d_NB[g]], [pd0])
                            mm(q0[hs, ci * 64:(ci + 1) * 64], NBall[hs, l0 + ci, 0:64], NT[hs, ci, :], True, True, [dNT, d_NB[g]], [qd0])
                    act.op(lambda e: e.activation(out=Wg[g][0][:, :, 0:64], in_=r3(p0[:, 0:128], 2), func=AF.Copy), reads=[pd0], writes=[dWg[g][0]])
                    act.op(lambda e: e.activation(out=NTg[g][1], in_=r3(q0[:, 0:128], 2), func=AF.Copy), reads=[qd0], writes=[dNTg[g][1]])
                    yield
                cur, ntc = 0, 1
                for lvl in range(1, 6):
                    last = lvl == 5
                    for g in range(4):
                        l0 = 2 * g
                        Wc, dWc = Wg[g][cur], dWg[g][cur]
                        NTc, dNTc = NTg[g][ntc], dNTg[g][ntc]
                        p1, pd1 = nb7()
                        if not last:
                            q1, qd1 = nb7()
                        for ci in range(2):
                            for h in range(2):
                                hs = HS[h]
                                if not last:
                                    mm(p1[hs, ci * 128:(ci + 1) * 128], NTc[hs, ci, :], Wc[hs, ci, :], True, True, [dNTc, dWc], [pd1])
                                    mm(q1[hs, ci * 64:(ci + 1) * 64], Wc[hs, ci, 0:64], NTc[hs, ci, :], True, True, [dNTc, dWc], [qd1])
                                else:
                                    mm(p1[hs, ci * 64:(ci + 1) * 64], NTc[hs, ci, :], Wc[hs, ci, 64:128], True, True, [dNTc, dWc], [pd1])
                        if not last:
                            Wn, dWn = Wg[g][1 - cur], dWg[g][1 - cur]
                            NTn, dNTn = NTg[g][1 - ntc], dNTg[g][1 - ntc]
                            act.op(lambda e: e.activation(out=Wn[:, :, 0:64], in_=r3(p1[:, 0:256], 2)[:, :, 0:64], func=AF.Copy), reads=[pd1], writes=[dWn])
                            dve.op(lambda e: e.tensor_tensor(out=Wn[:, :, 64:128], in0=r3(p1[:, 0:256], 2)[:, :, 64:128], in1=Wc[:, :, 64:128], op=ALU.add),
                                   reads=[pd1, dWc], writes=[dWn])
                            act.op(lambda e: e.activation(out=NTn, in_=r3(q1[:, 0:128], 2), func=AF.Copy), reads=[qd1], writes=[dNTn])
                        else:
                            dve.op(lambda e: e.tensor_tensor(out=TT[:, l0:l0 + 2, :], in0=r3(p1[:, 0:128], 2), in1=Wc[:, :, 64:128], op=ALU.add),
                                   reads=[pd1, dWc], writes=[d_TT[g]])
                        if g % 2 == 1:
                            yield
                    cur, ntc = 1 - cur, 1 - ntc
                for g in range(4):
                    l0 = 2 * g
                    pw, pdw = nb7()
                    for ci in range(2):
                        for h in range(2):
                            hs = HS[h]
                            mm(pw[hs, ci * 64:(ci + 1) * 64], KBall[hs, l0 + ci, 0:64], TM[hs, l0 + ci, 0:64], True, True, [d_KB[g], d_TM[g]], [pdw])
                    act.op(lambda e: e.activation(out=TM[:, l0:l0 + 2, 256:320], in_=r3(pw[:, 0:128], 2), func=AF.Copy), reads=[pdw], writes=[d_TM[g]])
                yield
                for g in range(4):
                    l0 = 2 * g
                    pq, pdq = nb7()
                    for ci in range(2):
                        for h in range(2):
                            hs = HS[h]
                            mm(pq[hs, ci * 128:(ci + 1) * 128], TT[hs, l0 + ci, :], TM[hs, l0 + ci, 192:320], True, True, [d_TT[g], d_TM[g]], [pdq])
                    dve.op(lambda e: e.tensor_copy(out=APU[:, l0:l0 + 2, :], in_=r3(pq[:, 0:256], 2)), reads=[pdq], writes=[d_APU[g]])
                yield
                for g in range(4):
                    l0 = 2 * g
                    pm, pdm = nb7()
                    pc, pdc = nb7()
                    pr, pdr = nb7()
                    for ci in range(2):
                        l = l0 + ci
                        for h in range(2):
                            hs = HS[h]
                            mm(pm[hs, ci * 64:(ci + 1) * 64], APU[hs, l, 0:64], TM[hs, l, 64:128], True, True, [d_APU[g], d_TM[g]], [pdm])
                            mm(pc[hs, ci * 64:(ci + 1) * 64], TM[hs, l, 64:128], APU[hs, l, 64:128], True, False, [d_APU[g], d_TM[g]], [pdc])
                            mm(pc[hs, ci * 64:(ci + 1) * 64], TM[hs, l, 128:192], TM[hs, l, 0:64], False, True, [d_TM[g]], [pdc])
                            mm(pr[hs, ci * 64:(ci + 1) * 64], APU[hs, l, 0:64], NBall[hs, l, 64:128], True, True, [d_APU[g], d_NB[g]], [pdr])
                    dve.op(lambda e: e.tensor_tensor(out=Mc[:, l0:l0 + 2, :], in0=r3(pm[:, 0:128], 2), in1=Mc[:, l0:l0 + 2, :], op=ALU.add),
                           reads=[pdm, d_Mc[g]], writes=[d_Mc[g]])
                    act.op(lambda e: e.activation(out=CcT[:, l0:l0 + 2, :], in_=r3(pc[:, 0:128], 2), func=AF.Copy), reads=[pdc], writes=[d_Cc[g]])
                    dve.op(lambda e: e.tensor_tensor(out=AR[:, l0:l0 + 2, 64:128], in0=r3(pr[:, 0:128], 2), in1=AR[:, l0:l0 + 2, 64:128], op=ALU.add),
                           reads=[pdr, d_ARr[g], d_AR], writes=[d_ARr[g]])
                    if g % 2 == 1:
                        yield
            def back_gen(u):
                hp, q = divmod(u, NQ)
                S_ = sets[u % 3]
                C_ = cbs[u % 2]
                tsl = slice(q * QTK, (q + 1) * QTK)
                AR, BK, BKh, vb, GL, gbuf, bonus, ybuf = S_["AR"], S_["BK"], S_["BKh"], S_["vb"], S_["GL"], S_["gbuf"], S_["bonus"], S_["ybuf"]
                d_AR, d_BK, d_BKh, d_vb, d_GL, d_g, d_bonus, dy = S_["d_AR"], S_["d_BK"], S_["d_BKh"], S_["d_vb"], S_["d_GL"], S_["d_g"], S_["d_bonus"], S_["d_y"]
                d_ARr = S_["d_ARr"]
                NBall, KBall, TM, APU, Mc, CcT = C_["NBall"], C_["KBall"], C_["TM"], C_["APU"], C_["Mc"], C_["CcT"]
                d_NB, d_KB, d_TM, d_APU, d_Mc, d_Cc = C_["d_NB"], C_["d_KB"], C_["d_TM"], C_["d_APU"], C_["d_Mc"], C_["d_Cc"]
                if q == 0:
                    dve.op(lambda e: e.memset(S32, 0.0), writes=[d_S32])
                    dve.op(lambda e: e.memset(Sbf, 0.0), writes=[d_Sbf])
                for l in range(8):
                    g = l // 2
                    ps_, pds = nb7()
                    for h in range(2):
                        hs = HS[h]
                        mm(ps_[hs, 0:64], Mc[hs, l, :], S32[hs, :], True, True, [d_Mc[g], d_S32], [pds])
                    for h in range(2):
                        hs = HS[h]
                        mm(pyb[hs, l * 64:(l + 1) * 64], Sbf[hs, :], AR[hs, l, 64:128], True, False, [d_Sbf, d_ARr[g]], [pdyb])
                        mm(pyb[hs, l * 64:(l + 1) * 64], APU[hs, l, 64:128], NBall[hs, l, 64:128], False, False, [d_APU[g], d_NB[g]], [pdyb])
                        mm(pyb[hs, l * 64:(l + 1) * 64], TM[hs, l, 0:64], KBall[hs, l, 64:128], False, True, [d_TM[g], d_KB[g]], [pdyb])
                    dve.op(lambda e: e.tensor_tensor(out=S32, in0=ps_[:, 0:64], in1=CcT[:, l, :], op=ALU.add), reads=[pds, d_Cc[g], d_S32], writes=[d_S32])
                    act.op(lambda e: e.activation(out=Sbf, in_=S32, func=AF.Copy), reads=[d_S32], writes=[d_Sbf])
                    yield
                act.op(lambda e: e.activation(out=ybuf, in_=pyb, func=AF.Copy), reads=[pdyb], writes=[dy])
                pb, pd = nb7()
                mm(pb, blk1, ybuf, True, True, [d_const, dy], [pd])
                dve.op(lambda e: e.scalar_tensor_tensor(out=yc, in0=pb, scalar=-1.0 / 64, in1=ybuf, op0=ALU.mult, op1=ALU.add),
                       reads=[pd, dy], writes=[dyc])
                pool.op(lambda e: e.tensor_tensor(out=sq, in0=yc, in1=yc, op=ALU.mult), reads=[dyc], writes=[dsq])
                yield
                pb, pd = nb7()
                mm(pb, blk1, sq, True, True, [d_const, dsq], [pd])
                dve.op(lambda e: e.tensor_scalar(out=rs, in0=pb, scalar1=1.0 / 64, scalar2=64e-5, op0=ALU.mult, op1=ALU.add), reads=[pd], writes=[drs])
                act.op(lambda e: e.activation(out=rs, in_=rs, func=AF.Sqrt), reads=[drs], writes=[drs])
                yield
                dve.op(lambda e: e.reciprocal(out=rs, in_=rs), reads=[drs], writes=[drs])
                pool.op(lambda e: e.tensor_tensor(out=yc, in0=yc, in1=rs, op=ALU.mult), reads=[dyc, drs], writes=[dyc])
                yield
                dve.op(lambda e: e.tensor_scalar(out=yc, in0=yc, scalar1=pvc("ln_w", hp), scalar2=pvc("ln_b", hp), op0=ALU.mult, op1=ALU.add),
                       reads=[dyc, d_const], writes=[dyc])
                pool.op(lambda e: e.tensor_tensor(out=yc, in0=yc, in1=bonus, op=ALU.add), reads=[dyc, d_bonus], writes=[dyc])
                dve.op(lambda e: e.tensor_tensor(out=bufB[:, 8 + hp, tsl], in0=yc, in1=gbuf, op=ALU.mult), reads=[dyc, d_g], writes=[d_B])
                yield

            def run_interleaved(gens, steps=None):
                steps = steps or [1] * len(gens)
                gens = [[g, k] for g, k in zip(gens, steps) if g is not None]
                while gens:
                    for gk in list(gens):
                        for _ in range(gk[1]):
                            try:
                                next(gk[0])
                            except StopIteration:
                                gens.remove(gk)
                                break

            NU = 8 * NQ
            PIPE_STEPS = [1, 1, 1]
            run_interleaved([prep_gen(0)])
            run_interleaved([front_gen(0), prep_gen(1)])
            for u in range(NU):
                run_interleaved([back_gen(u), front_gen(u + 1) if u + 1 < NU else None, prep_gen(u + 2) if u + 2 < NU else None], PIPE_STEPS)
            fw.barrier()
            if "rwT" in dbg_aps:
                tmp = Alloc(big, M_OFF, WORDS).f32(T)
                dtmp = Dep()
                for kc in range(8):
                    dve.op(lambda e: e.tensor_copy(out=tmp, in_=bufB[:, 8 + kc, :]), reads=[d_B], writes=[dtmp])
                    sp.dma(dbg_aps["rwT"][kc * 128:(kc + 1) * 128, :], tmp, reads=[dtmp])
                fw.barrier()

        def out_proj(srcT, d_src, wmat, res_fn, dst_dram, al, pre=None):
            wbs2 = [(r3(al.bf16(16 * 512), 16), Dep()) for _ in range(2)]
            xts = [(al.f32(512), Dep()) for _ in range(3)]
            hos = [(al.f32(512), Dep()) for _ in range(2)]
            i = 0
            for dblk in range(4):
                ws, dw = wbs2[dblk % 2]
                ds_ = slice(dblk * 512, (dblk + 1) * 512)
                if dblk == 0 and pre is not None:
                    ws, dw = pre
                else:
                    pool.dma(ws, wmat[:, ds_].rearrange("(kc p) n -> p kc n", p=128), writes=[dw])
                for tt in range(NTT):
                    rows = slice(tt * 128, (tt + 1) * 128)
                    xt, dx = xts[i % 3]
                    ho, dh = hos[i % 2]
                    i += 1
                    sp.dma(xt, res_fn(rows, ds_), writes=[dx])
                    pb, pd = nb()
                    for kc in range(KC):
                        mm(pb, srcT[:, kc, rows], ws[:, kc, :], kc == 0, kc == KC - 1, [d_src, dw], [pd])
                    dve.op(lambda e: e.tensor_tensor(out=ho, in0=pb, in1=xt, op=ALU.add), reads=[pd, dx], writes=[dh])
                    act.dma(dst_dram[rows, ds_], ho, reads=[dh])

        if stop >= 4:
            out_proj(bufB, d_B, w_out, lambda rows, cols: x[rows, cols], h1_s, Alloc(big, M_OFF, A_OFF))
            fw.barrier()
            if stop >= 5:
                d_memT = Dep()
                alB = Alloc(big, B0_OFF, M_OFF)
                memT = r3(alB.bf16(16 * 256), 16)
                wbs5_0 = (r3(alB.bf16(16 * 512), 16), Dep())
                load_gB(3)
                norm_tiles(alB, 2, lambda i: mem[i * 128:(i + 1) * 128, :], memT, d_memT, nbuf=2)
                pool.dma(wbs5_0[0], w_kv[:, 0:512].rearrange("(kc p) n -> p kc n", p=128), writes=[wbs5_0[1]])
            load_gB(1)
            norm_tiles(Alloc(big, M_OFF, A_OFF), NTT, lambda i: h1_s[i * 128:(i + 1) * 128, :], bufA, d_A)
            fw.barrier()
            if "h1" in dbg_aps:
                tmp = Alloc(big, M_OFF, A_OFF).f32(D)
                dtmp = Dep()
                for tt in range(NTT):
                    sp.dma(tmp, h1_s[tt * 128:(tt + 1) * 128, :], writes=[dtmp])
                    sp.dma(dbg_aps["h1"][tt * 128:(tt + 1) * 128, :], tmp, reads=[dtmp])
                fw.barrier()


        if stop >= 5:
            kv_al = Alloc(big, M_OFF, M_OFF + 4096)
            KT = r3(kv_al.bf16(16 * 256), 16)
            Vb = r3(kv_al.bf16(2 * D), 2)
            d_KT, d_Vb = Dep(), Dep()
            alM = Alloc(big, M_OFF + 4096, A_OFF)
            wbs5 = [wbs5_0, (r3(alM.bf16(16 * 512), 16), Dep())]
            wq_pre = (r3(alM.bf16(16 * 256), 16), Dep())
            for g in range(8):
                ws, dw = wbs5[g % 2]
                if g > 0:
                    pool.dma(ws, w_kv[:, g * 512:(g + 1) * 512].rearrange("(kc p) n -> p kc n", p=128), writes=[dw])
                if g == 1:
                    pool.dma(wq_pre[0], w_q[:, 0:256].rearrange("(kc p) n -> p kc n", p=128), writes=[wq_pre[1]])
                if g < 4:
                    for j in range(4):
                        cb = g * 4 + j
                        pb, pd = nb()
                        for kc in range(KC):
                            mm(pb[:, 0:256], ws[:, kc, j * 128:(j + 1) * 128], memT[:, kc, :], kc == 0, kc == KC - 1, [dw, d_memT], [pd])
                        act.op(lambda e: e.activation(out=KT[:, cb, :], in_=pb[:, 0:256], func=AF.Copy), reads=[pd], writes=[d_KT])
                else:
                    for mc in range(2):
                        pb, pd = nb()
                        for kc in range(KC):
                            mm(pb, memT[:, kc, mc * 128:(mc + 1) * 128], ws[:, kc, :], kc == 0, kc == KC - 1, [dw, d_memT], [pd])
                        dve.op(lambda e: e.tensor_copy(out=Vb[:, mc, (g - 4) * 512:(g - 3) * 512], in_=pb), reads=[pd], writes=[d_Vb])
            fw.barrier()
            alM = Alloc(big, M_OFF + 4096, A_OFF)
            wbs6 = [(r3(alM.bf16(16 * 256), 16), Dep()) for _ in range(2)]
            qscale = float(512 ** -0.5)
            for g in range(8):
                if g == 0:
                    ws, dw = wq_pre
                else:
                    ws, dw = wbs6[g % 2]
                    pool.dma(ws, w_q[:, g * 256:(g + 1) * 256].rearrange("(kc p) n -> p kc n", p=128), writes=[dw])
                for j in range(2):
                    cb = g * 2 + j
                    for tq in range(NTQ):
                        ts_ = slice(tq * 512, (tq + 1) * 512)
                        pb, pd = nb()
                        for kc in range(KC):
                            mm(pb, ws[:, kc, j * 128:(j + 1) * 128], bufA[:, kc, ts_], kc == 0, kc == KC - 1, [dw, d_A], [pd])
                        if tq % 2 == 0:
                            act.op(lambda e: e.activation(out=bufB[:, cb, ts_], in_=pb, func=AF.Copy, scale=qscale), reads=[pd], writes=[d_B])
                        else:
                            dve.op(lambda e: e.tensor_scalar(out=bufB[:, cb, ts_], in0=pb, scalar1=qscale, scalar2=None, op0=ALU.mult), reads=[pd], writes=[d_B])
            fw.barrier()
            alM = Alloc(big, M_OFF + 4096, A_OFF)
            Es = [(r3(alM.bf16(2 * 512), 2), Dep()) for _ in range(2)]
            rinvs = [(alM.f32(512), Dep()) for _ in range(2)]
            wo_pre = (r3(alM.bf16(16 * 512), 16), Dep())
            pool.dma(wo_pre[0], w_o[:, 0:512].rearrange("(kc p) n -> p kc n", p=128), writes=[wo_pre[1]])
            it = 0
            for h in range(4):
                for tq in range(NTQ):
                    ts_ = slice(tq * 512, (tq + 1) * 512)
                    E, dE = Es[it % 2]
                    rinv, dri = rinvs[it % 2]
                    it += 1
                    for mc in range(2):
                        pb, pd = nb()
                        for c in range(4):
                            mm(pb, KT[:, h * 4 + c, mc * 128:(mc + 1) * 128], bufB[:, h * 4 + c, ts_], c == 0, c == 3, [d_KT, d_B], [pd])
                        act.op(lambda e: e.activation(out=E[:, mc, :], in_=pb, func=AF.Exp), reads=[pd], writes=[dE])
                    pb, pd = nb()
                    for mc in range(2):
                        mm(pb, onesb, E[:, mc, :], mc == 0, mc == 1, [d_const, dE], [pd])
                    act.op(lambda e: e.activation(out=rinv, in_=pb, func=AF.Ln), reads=[pd], writes=[dri])
                    act.op(lambda e: e.activation(out=rinv, in_=rinv, func=AF.Exp, scale=-1.0), reads=[dri], writes=[dri])
                    for c in range(4):
                        pb, pd = nb()
                        for mc in range(2):
                            mm(pb, Vb[:, mc, h * 512 + c * 128:h * 512 + (c + 1) * 128], E[:, mc, :], mc == 0, mc == 1, [d_Vb, dE], [pd])
                        dve.op(lambda e: e.tensor_tensor(out=bufA[:, h * 4 + c, ts_], in0=pb, in1=rinv, op=ALU.mult), reads=[pd, dri], writes=[d_A])
            fw.barrier()
            out_proj(bufA, d_A, w_o, lambda rows, cols: h1_s[rows, cols], h2_s, Alloc(big, B0_OFF, M_OFF), pre=wo_pre)
            fw.barrier()
            if "h2" in dbg_aps:
                tmp = Alloc(big, B0_OFF, M_OFF).f32(D)
                dtmp = Dep()
                for tt in range(NTT):
                    sp.dma(tmp, h2_s[tt * 128:(tt + 1) * 128, :], writes=[dtmp])
                    sp.dma(dbg_aps["h2"][tt * 128:(tt + 1) * 128, :], tmp, reads=[dtmp])
                fw.barrier()

        if stop >= 8:
            IOA = bass.IndirectOffsetOnAxis
            bc_reg = es.enter_context(nc.gpsimd.register("bc"))
            nc.gpsimd.reg_mov(bc_reg, NROW - 1)
            BCV = nc.gpsimd.snap(bc_reg)
            bw_reg = es.enter_context(nc.gpsimd.register("bw"))
            nc.gpsimd.reg_mov(bw_reg, 8191)
            BWV = nc.gpsimd.snap(bw_reg)
            al8 = Alloc(big, B0_OFF, WORDS)
            LT = al8.f32(128)
            iop = al8.f32(1)
            siota = al8.f32(32)
            thr8 = al8.f32(8)
            p1a, p2a = al8.f32(16), al8.f32(16)
            pos1i = al8.f32(16).bitcast(I32)
            pos2i = al8.f32(16).bitcast(I32)
            widx = al8.f32(NSLOT * 4).bitcast(I32)
            d_c8, d_pos, d_widx, d_pp = Dep(), Dep(), Dep(), Dep()
            P8_TOP = al8.top
            sp.dma(LT, cst[:, 768:896], writes=[d_c8])
            sp.dma(iop, cst[:, 896:897], writes=[d_c8], allow_slow_non_contiguous=True)
            sp.dma(siota, cst[:, 897:929], writes=[d_c8])
            sp.dma(thr8, cst[:, 929:937], writes=[d_c8])
            fw.barrier()
            xnb_all = r3(al8.bf16(16 * D), 16)
            d_xnb = [Dep() for _ in range(16)]
            xts = [(al8.f32(D), Dep()) for _ in range(3)]
            xn32s = [(al8.f32(D), Dep()) for _ in range(2)]
            junk = al8.bf16(D)
            d_junk = Dep()
            h32s = [(r3(al8.f32(16 * 128), 16), Dep()) for _ in range(2)]
            wr32 = r3(al8.f32(16 * 20), 16)
            d_wr = Dep()
            logits = r3(al8.f32(16 * 20), 16)
            d_log = Dep()
            sts = [(al8.f32(8), Dep()) for _ in range(4)]
            sp.dma(wr32, w_r.rearrange("(kc p) n -> p kc n", p=128), writes=[d_wr])
            load_gB(2)

            def a8_stage_a(tt):
                xt, dx = xts[tt % 3]
                xn32, dxn = xn32s[tt % 2]
                st, d_st = sts[tt % 4]
                sp.dma(xt, h2_s[tt * 128:(tt + 1) * 128, :], writes=[dx])
                act.op(lambda e: e.activation(out=junk, in_=xt, func=AF.Square, accum_out=st[:, 0:1]), reads=[dx], writes=[d_junk, d_st])
                dve.op(lambda e: e.tensor_scalar(out=st[:, 1:2], in0=st[:, 0:1], scalar1=1.0 / D, scalar2=1e-6, op0=ALU.mult, op1=ALU.add),
                       reads=[d_st], writes=[d_st])
                act.op(lambda e: e.activation(out=st[:, 2:3], in_=st[:, 1:2], func=AF.Sqrt), reads=[d_st], writes=[d_st])
                dve.op(lambda e: e.reciprocal(out=st[:, 3:4], in_=st[:, 2:3]), reads=[d_st], writes=[d_st])
                dve.op(lambda e: e.scalar_tensor_tensor(out=xn32, in0=xt, scalar=st[:, 3:4], in1=gBt, op0=ALU.mult, op1=ALU.mult),
                       reads=[dx, d_st, d_gB], writes=[dxn])
                act.op(lambda e: e.activation(out=xnb_all[:, tt, :], in_=xn32, func=AF.Copy), reads=[dxn], writes=[d_xnb[tt]])

            def a8_stage_b(tt):
                xn32, dxn = xn32s[tt % 2]
                h32, d_h32 = h32s[tt % 2]
                for q in range(4):
                    pf, pdf = nb()
                    for j in range(4):
                        kc = q * 4 + j
                        mm(pf[:, j * 128:(j + 1) * 128], xn32[:, kc * 128:(kc + 1) * 128], identf, True, True, [dxn, d_const], [pdf])
                    if q % 2 == 0:
                        dve.op(lambda e: e.tensor_copy(out=h32[:, q * 4:q * 4 + 4, :], in_=r3(pf, 4)), reads=[pdf], pws=[d_h32])
                    else:
                        act.op(lambda e: e.activation(out=h32[:, q * 4:q * 4 + 4, :], in_=r3(pf, 4), func=AF.Copy), reads=[pdf], pws=[d_h32])
                pb, pd = nb()
                for kc in range(KC):
                    mm(pb[:, 0:20], h32[:, kc, :], wr32[:, kc, :], kc == 0, False, [d_h32, d_wr], [pd])
                mm(pb[:, 0:20], ones1[0:1, 0:128], brow[0:1, 0:20], False, True, [d_const], [pd])
                dve.op(lambda e: e.tensor_copy(out=logits[:, tt, :], in_=pb[:, 0:20]), reads=[pd], writes=[d_log])

            for tt in range(17):
                if tt < 16:
                    a8_stage_a(tt)
                if tt >= 1:
                    a8_stage_b(tt - 1)
            NT_ = 16
            rt = [al8.f32(NT_ * 4) for _ in range(12)]
            rt4 = al8.f32(NT_ * 16)
            sel1 = al8.f32(NT_ * 16)
            sel2 = al8.f32(NT_ * 16)
            ind = al8.f32(NT_ * 16)
            tot = r3(al8.f32(NT_ * 16), NT_)
            tcum = r3(al8.f32(NT_ * 16), NT_)
            posall = al8.f32(NT_ * 16)
            ptmp = al8.f32(NT_ * 16)
            c8 = al8.f32(16 * 8)
            cnt, nsl, bsl, bsl256 = al8.f32(16), al8.f32(16), al8.f32(16), al8.f32(16)
            total = al8.f32(1)
            es32 = al8.f32(NSLOT * 16)
            esf, unused, wbase = al8.f32(NSLOT), al8.f32(NSLOT), al8.f32(NSLOT)
            pos1f, pos2f = al8.f32(16), al8.f32(16)
            widxf = al8.f32(NSLOT * 4).rearrange("p (s q) -> p s q", q=4)
            d_rt = Dep()
            lg = logits[:, :, 0:4]
            le = logits[:, :, 4:20].rearrange("p t (g e) -> p t g e", g=4)
            gmax, gsum, gw, m1, m2 = [rt[i][:, 0:NT_] for i in range(5)]
            goh, gsh, esel, oh1, e2 = [r3(rt[7 + i], NT_) for i in range(5)]
            t4 = rt4.rearrange("p (t g e) -> p t g e", t=NT_, g=4)

            def bc3(v):
                return v.unsqueeze(2).to_broadcast([128, NT_, 4])

            def v4(a):
                return a.rearrange("p (t g e) -> p t g e", t=NT_, g=4)
            R = [d_log, d_rt, d_c8]
            W_ = [d_rt]
            dve.op(lambda e: e.tensor_reduce(out=gmax, in_=lg, axis=AX.X, op=ALU.max), R, W_)
            dve.op(lambda e: e.tensor_tensor(out=goh, in0=lg, in1=bc3(gmax), op=ALU.is_equal), R, W_)
            dve.op(lambda e: e.tensor_tensor(out=gsh, in0=lg, in1=bc3(gmax), op=ALU.subtract), R, W_)
            act.op(lambda e: e.activation(out=gsh, in_=gsh, func=AF.Exp), R, W_)
            dve.op(lambda e: e.tensor_reduce(out=gsum, in_=gsh, axis=AX.X, op=ALU.add), R, W_)
            dve.op(lambda e: e.reciprocal(out=gw, in_=gsum), R, W_)
            dve.op(lambda e: e.tensor_tensor(out=t4, in0=le, in1=goh.unsqueeze(3).to_broadcast([128, NT_, 4, 4]), op=ALU.mult), R, W_)
            dve.op(lambda e: e.tensor_reduce(out=esel, in_=t4.rearrange("p t g e -> p t e g"), axis=AX.X, op=ALU.add), R, W_)
            dve.op(lambda e: e.tensor_reduce(out=m1, in_=esel, axis=AX.X, op=ALU.max), R, W_)
            dve.op(lambda e: e.tensor_tensor(out=oh1, in0=esel, in1=bc3(m1), op=ALU.is_equal), R, W_)
            dve.op(lambda e: e.scalar_tensor_tensor(out=e2, in0=oh1, scalar=-1e30, in1=esel, op0=ALU.mult, op1=ALU.add), R, W_)
            dve.op(lambda e: e.tensor_reduce(out=m2, in_=e2, axis=AX.X, op=ALU.max), R, W_)
            dve.op(lambda e: e.tensor_tensor(out=e2, in0=e2, in1=bc3(m2), op=ALU.is_equal), R, W_)
            dve.op(lambda e: e.tensor_tensor(out=p1a, in0=m1, in1=m2, op=ALU.subtract), R, W_ + [d_pp])
            act.op(lambda e: e.activation(out=p1a, in_=p1a, func=AF.Sigmoid), R + [d_pp], W_ + [d_pp])
            dve.op(lambda e: e.tensor_scalar(out=p2a, in0=p1a, scalar1=-1.0, scalar2=1.0, op0=ALU.mult, op1=ALU.add), R + [d_pp], W_ + [d_pp])
            dve.op(lambda e: e.tensor_tensor(out=p1a, in0=p1a, in1=gw, op=ALU.mult), R + [d_pp], W_ + [d_pp])
            dve.op(lambda e: e.tensor_tensor(out=p2a, in0=p2a, in1=gw, op=ALU.mult), R + [d_pp], W_ + [d_pp])
            dve.op(lambda e: e.tensor_tensor(out=v4(sel1), in0=goh.unsqueeze(3).to_broadcast([128, NT_, 4, 4]),
                                             in1=oh1.unsqueeze(2).to_broadcast([128, NT_, 4, 4]), op=ALU.mult), R, W_)
            dve.op(lambda e: e.tensor_tensor(out=v4(sel2), in0=goh.unsqueeze(3).to_broadcast([128, NT_, 4, 4]),
                                             in1=e2.unsqueeze(2).to_broadcast([128, NT_, 4, 4]), op=ALU.mult), R, W_)
            dve.op(lambda e: e.tensor_tensor(out=ind, in0=sel1, in1=sel2, op=ALU.add), R, W_)
            pw, pdw = nb()
            mm(pw[:, 0:256], LT, ind, True, True, [d_rt, d_c8], [pdw])
            pt_, pdt = nb()
            mm(pt_[:, 0:256], ones1, ind, True, True, [d_rt, d_const], [pdt])
            dve.op(lambda e: e.tensor_copy(out=tot, in_=r3(pt_[:, 0:256], NT_)), R + [pdt], W_)
            dve.op(lambda e: e.memset(tcum[:, 0, :], 0.0), R, W_)
            for tt in range(1, NT_):
                dve.op(lambda e: e.tensor_tensor(out=tcum[:, tt, :], in0=tcum[:, tt - 1, :], in1=tot[:, tt - 1, :], op=ALU.add), R, W_)
            dve.op(lambda e: e.tensor_tensor(out=cnt, in0=tcum[:, NT_ - 1, :], in1=tot[:, NT_ - 1, :], op=ALU.add), R, W_)
            dve.op(lambda e: e.tensor_tensor(out=r3(c8, 16), in0=cnt.unsqueeze(2).to_broadcast([128, 16, 8]),
                                             in1=thr8.unsqueeze(1).to_broadcast([128, 16, 8]), op=ALU.is_gt), R, W_)
            dve.op(lambda e: e.tensor_reduce(out=nsl, in_=r3(c8, 16), axis=AX.X, op=ALU.add), R, W_)
            dve.op(lambda e: e.memset(bsl[:, 0:1], 0.0), R, W_)
            for ex in range(1, 16):
                dve.op(lambda e: e.tensor_tensor(out=bsl[:, ex:ex + 1], in0=bsl[:, ex - 1:ex], in1=nsl[:, ex - 1:ex], op=ALU.add), R, W_)
            dve.op(lambda e: e.tensor_tensor(out=total, in0=bsl[:, 15:16], in1=nsl[:, 15:16], op=ALU.add), R, W_)
            dve.op(lambda e: e.tensor_scalar(out=bsl256, in0=bsl, scalar1=float(SL), scalar2=None, op0=ALU.mult), R, W_)
            dve.op(lambda e: e.tensor_tensor(out=posall, in0=pw[:, 0:256], in1=tcum.rearrange("p t e -> p (t e)"), op=ALU.add), R + [pdw], W_)
            dve.op(lambda e: e.tensor_tensor(out=r3(posall, NT_), in0=r3(posall, NT_), in1=bsl256.unsqueeze(1).to_broadcast([128, NT_, 16]), op=ALU.add), R, W_)
            dve.op(lambda e: e.tensor_tensor(out=ptmp, in0=posall, in1=sel1, op=ALU.mult), R, W_)
            dve.op(lambda e: e.tensor_reduce(out=pos1f, in_=r3(ptmp, NT_), axis=AX.X, op=ALU.add), R, W_)
            dve.op(lambda e: e.tensor_tensor(out=ptmp, in0=posall, in1=sel2, op=ALU.mult), R, W_)
            dve.op(lambda e: e.tensor_reduce(out=pos2f, in_=r3(ptmp, NT_), axis=AX.X, op=ALU.add), R, W_)
            dve.op(lambda e: e.tensor_copy(out=pos1i, in_=pos1f), R, W_ + [d_pos])
            dve.op(lambda e: e.tensor_copy(out=pos2i, in_=pos2f), R, W_ + [d_pos])
            dve.op(lambda e: e.tensor_tensor(out=r3(es32, NSLOT), in0=bsl.unsqueeze(1).to_broadcast([128, NSLOT, 16]),
                                             in1=siota[:, 0:NSLOT].unsqueeze(2).to_broadcast([128, NSLOT, 16]), op=ALU.is_le), R, W_)
            dve.op(lambda e: e.tensor_reduce(out=esf, in_=r3(es32, NSLOT), axis=AX.X, op=ALU.add), R, W_)
            dve.op(lambda e: e.tensor_scalar(out=unused, in0=siota[:, 0:NSLOT], scalar1=total[:, 0:1], scalar2=1.0e6, op0=ALU.is_ge, op1=ALU.mult), R, W_)
            dve.op(lambda e: e.tensor_scalar(out=wbase, in0=esf, scalar1=-1.0, scalar2=512.0, op0=ALU.add, op1=ALU.mult), R, W_)
            dve.op(lambda e: e.tensor_tensor(out=wbase, in0=wbase, in1=unused, op=ALU.add), R, W_)
            dve.op(lambda e: e.tensor_scalar(out=wbase, in0=wbase, scalar1=iop[:, 0:1], scalar2=None, op0=ALU.add), R, W_)
            for q in range(4):
                dve.op(lambda e: e.tensor_scalar(out=widxf[:, :, q], in0=wbase, scalar1=float(128 * q), scalar2=None, op0=ALU.add), R, W_)
            dve.op(lambda e: e.tensor_copy(out=widx, in_=widxf.rearrange("p s q -> p (s q)")), R, W_ + [d_widx])
            if "route" in dbg_aps:
                sp.dma(dbg_aps["route"][:, 0:16], pos1f, reads=[d_rt])
                sp.dma(dbg_aps["route"][:, 16:32], pos2f, reads=[d_rt])
                sp.dma(dbg_aps["route"][:, 32:64], wbase, reads=[d_rt])
                sp.dma(dbg_aps["route"][:, 64:80], p1a, reads=[d_pp])
                sp.dma(dbg_aps["route"][:, 80:96], p2a, reads=[d_pp])
                sp.dma(dbg_aps["route"][:, 96:112], cnt, reads=[d_rt])
            for tt in range(16):
                for posi in (pos1i, pos2i):
                    pool.dma_fn(lambda e: e.indirect_dma_start(out=Xs, out_offset=IOA(ap=posi[:, tt:tt + 1], axis=0), in_=xnb_all[:, tt, :], in_offset=None,
                                                               bounds_check=BCV, oob_is_err=False),
                                reads=[d_xnb[tt], d_pos], writes=[d_Xs])
            fw.barrier()
            ald = Alloc(big, P8_TOP, WORDS)
            wbufs = [(ald.bf16(8192), [Dep() for _ in range(4)]) for _ in range(6)]
            xsls = [(r3(ald.bf16(NA * D), NA), Dep()) for _ in range(2)]
            XTs = [(r3(ald.bf16(16 * SL), 16), Dep()) for _ in range(2)]
            hids = [(r3(ald.bf16(4 * SL), 4), Dep()) for _ in range(2)]
            sbs = [(ald.bf16(SL), Dep()) for _ in range(2)]
            yos = [(ald.f32(D), Dep()) for _ in range(2)]
            cnt8 = dict(yi=0, ei=0)

            def wload(i, s):
                wsl = []
                for m, wl in enumerate((wg_l, wu_l, wd_l)):
                    buf, deps = wbufs[(3 * i + m) % 6]
                    for q in range(4):
                        pool.dma_fn(lambda e: e.indirect_dma_start(out=buf[:, q * 2048:(q + 1) * 2048], out_offset=None, in_=wl,
                                                                   in_offset=IOA(ap=widx[:, s * 4 + q:s * 4 + q + 1], axis=0), bounds_check=BWV, oob_is_err=False),
                                    reads=[d_widx], writes=[deps[q]])
                    wsl.append((buf, deps))
                return wsl

            def xload(i, s):
                xsl, dxs = xsls[i % 2]
                sp.dma(xsl, Xs[s * SL:(s + 1) * SL, :].rearrange("(a p) n -> p a n", p=128), reads=[d_Xs], writes=[dxs])

            def emit_T(i, s):
                xsl, dxs = xsls[i % 2]
                XT, dXT = XTs[i % 2]
                for a in range(NA):
                    for q4 in range(4):
                        pb, pd = nb()
                        for j in range(4):
                            kc = q4 * 4 + j
                            mm(pb[:, j * 128:(j + 1) * 128], xsl[:, a, kc * 128:(kc + 1) * 128], identb, True, True, [dxs, d_const], [pd])
                        cnt8["ei"] += 1
                        if cnt8["ei"] % 2 == 0:
                            act.op(lambda e: e.activation(out=XT[:, q4 * 4:q4 * 4 + 4, a * 128:(a + 1) * 128], in_=r3(pb, 4), func=AF.Copy), reads=[pd], pws=[dXT])
                        else:
                            dve.op(lambda e: e.tensor_copy(out=XT[:, q4 * 4:q4 * 4 + 4, a * 128:(a + 1) * 128], in_=r3(pb, 4)), reads=[pd], pws=[dXT])

            def emit_GU(i, s, wsl):
                wg, dwg = r3(wsl[0][0], 16), wsl[0][1]
                wu, dwu = r3(wsl[1][0], 16), wsl[1][1]
                XT, dXT = XTs[i % 2]
                hid, dhid = hids[i % 2]
                for ffc in range(4):
                    pg, pdg = nb()
                    for kc in range(KC):
                        mm(pg[:, 0:SL], wg[:, kc, ffc * 128:(ffc + 1) * 128], XT[:, kc, :], kc == 0, kc == KC - 1, [dwg[kc // 4], dXT], [pdg])
                    pu, pdu = nb()
                    for kc in range(KC):
                        mm(pu[:, 0:SL], wu[:, kc, ffc * 128:(ffc + 1) * 128], XT[:, kc, :], kc == 0, kc == KC - 1, [dwu[kc // 4], dXT], [pdu])
                    sb_, dsb = sbs[ffc % 2]
                    act.op(lambda e: e.activation(out=sb_, in_=pg[:, 0:SL], func=AF.Silu), reads=[pdg], writes=[dsb])
                    dve.op(lambda e: e.tensor_tensor(out=hid[:, ffc, :], in0=pu[:, 0:SL], in1=sb_, op=ALU.mult), reads=[pdu, dsb], writes=[dhid])

            def emit_D(i, s, wsl):
                wd, dwd = r3(wsl[2][0], 4), wsl[2][1]
                hid, dhid = hids[i % 2]
                for a in range(NA):
                    yo, dyo = yos[cnt8["yi"] % 2]
                    cnt8["yi"] += 1
                    for dblk in range(4):
                        ds_ = slice(dblk * 512, (dblk + 1) * 512)
                        pb, pd = nb()
                        for ffc in range(4):
                            mm(pb, hid[:, ffc, a * 128:(a + 1) * 128], wd[:, ffc, ds_], ffc == 0, ffc == 3, [dhid, dwd[ffc]], [pd])
                        if dblk % 2 == 0:
                            act.op(lambda e: e.activation(out=yo[:, ds_], in_=pb, func=AF.Copy), reads=[pd], pws=[dyo])
                        else:
                            dve.op(lambda e: e.tensor_copy(out=yo[:, ds_], in_=pb), reads=[pd], pws=[dyo])
                    r0 = s * SL + a * 128
                    sp.dma(Ys[r0:r0 + 128, :], yo, reads=[dyo], writes=[d_Ys])

            lo_n = NSLOT - NSLOT // 3
            lo, hi = list(range(lo_n)), list(range(NSLOT - 1, lo_n - 1, -1))
            order = []
            while lo or hi:
                order += lo[:2]
                lo = lo[2:]
                if hi:
                    order.append(hi.pop(0))
            assert sorted(order) == list(range(NSLOT))
            xload(0, order[0])
            emit_T(0, order[0])
            for i, s in enumerate(order):
                wsl = wload(i, s)
                if i + 1 < NSLOT:
                    xload(i + 1, order[i + 1])
                emit_GU(i, s, wsl)
                if i + 1 < NSLOT:
                    emit_T(i + 1, order[i + 1])
                emit_D(i, s, wsl)
            fw.barrier()
            ale = Alloc(big, P8_TOP, WORDS)
            cts = [(ale.f32(D), Dep()) for _ in range(2)]
            y1s = [(ale.f32(D), Dep()) for _ in range(2)]
            y2s = [(ale.f32(D), Dep()) for _ in range(2)]
            junk2 = ale.bf16(D)
            sts2 = [(ale.f32(8), Dep()) for _ in range(4)]
            load_gB(4)
            for tt in range(16):
                xt, dx = cts[tt % 2]
                y1, dy1 = y1s[tt % 2]
                y2, dy2 = y2s[tt % 2]
                st, d_st = sts2[tt % 4]
                pool.dma(xt, h2_s[tt * 128:(tt + 1) * 128, :], writes=[dx])
                pool.dma_fn(lambda e: e.indirect_dma_start(out=y1, out_offset=None, in_=Ys, in_offset=IOA(ap=pos1i[:, tt:tt + 1], axis=0),
                                                           bounds_check=BCV, oob_is_err=False), reads=[d_Ys, d_pos], writes=[dy1])
                pool.dma_fn(lambda e: e.indirect_dma_start(out=y2, out_offset=None, in_=Ys, in_offset=IOA(ap=pos2i[:, tt:tt + 1], axis=0),
                                                           bounds_check=BCV, oob_is_err=False), reads=[d_Ys, d_pos], writes=[dy2])
                dve.op(lambda e: e.scalar_tensor_tensor(out=xt, in0=y1, scalar=p1a[:, tt:tt + 1], in1=xt, op0=ALU.mult, op1=ALU.add),
                       reads=[dy1, dx, d_pp], writes=[dx])
                dve.op(lambda e: e.scalar_tensor_tensor(out=xt, in0=y2, scalar=p2a[:, tt:tt + 1], in1=xt, op0=ALU.mult, op1=ALU.add),
                       reads=[dy2, dx, d_pp], writes=[dx])
                if "h3" in dbg_aps:
                    sp.dma(dbg_aps["h3"][tt * 128:(tt + 1) * 128, :], xt, reads=[dx])
                act.op(lambda e: e.activation(out=junk2, in_=xt, func=AF.Square, accum_out=st[:, 0:1]), reads=[dx], writes=[d_junk, d_st])
                dve.op(lambda e: e.tensor_scalar(out=st[:, 1:2], in0=st[:, 0:1], scalar1=1.0 / D, scalar2=1e-6, op0=ALU.mult, op1=ALU.add),
                       reads=[d_st], writes=[d_st])
                act.op(lambda e: e.activation(out=st[:, 2:3], in_=st[:, 1:2], func=AF.Sqrt), reads=[d_st], writes=[d_st])
                dve.op(lambda e: e.reciprocal(out=st[:, 3:4], in_=st[:, 2:3]), reads=[d_st], writes=[d_st])
                dve.op(lambda e: e.scalar_tensor_tensor(out=xt, in0=xt, scalar=st[:, 3:4], in1=gBt, op0=ALU.mult, op1=ALU.mult),
                       reads=[dx, d_st, d_gB], writes=[dx])
                sp.dma(out[tt * 128:(tt + 1) * 128, :], xt, reads=[dx])
            fw.barrier()

        fw.barrier()
    return nc


def host_consts(inp):
    l = 0
    f = np.float32
    gBh = np.stack([np.broadcast_to(v, (128, D)) for v in (inp["norm_mix_g"][l], inp["norm_xattn_g"][l], inp["norm_ffn_g"][l],
                                                            inp["norm_mem_g"][l], inp["norm_final_g"])]).astype(f)
    pvh = np.zeros((128, NPV), f)

    def col(v, n):
        return np.ascontiguousarray(np.asarray(v, f).reshape(n, 128).T)
    pvh[:, 0:8] = col(inp["pool_scale"][l], 8)
    mu = np.asarray(inp["rwkv_mu"][l], f)
    pvh[:, 8:32] = col(mu[0:3072], 24)
    pvh[:, 32] = mu[3072:3200]
    pvh[:, 33] = mu[3200:3328]
    pvh[0:32, 34] = mu[3328:3360]
    pvh[:, 35:43] = col(inp["rwkv_w0"][l], 8)
    pvh[:, 43:51] = col(inp["rwkv_a0"][l], 8)
    pvh[:, 51:59] = col(inp["rwkv_k_k"][l], 8)
    pvh[:, 59:67] = col(inp["rwkv_k_a"][l], 8)
    pvh[:, 67:75] = col(inp["rwkv_ln_w"][l], 8)
    pvh[:, 75:83] = col(inp["rwkv_ln_b"][l], 8)
    pvh[:, 83:91] = col(np.asarray(inp["rwkv_r_k"][l]).reshape(-1), 8)
    cst = np.zeros((128, 1024), f)
    p = np.arange(128)
    cst[:, 0:128] = np.eye(128, dtype=f)
    cst[:, 128:256] = (p[:, None] // 64 == p[None, :] // 64).astype(f)
    s = p % 64
    tcol = np.arange(64)
    strict = (s[:, None] < tcol[None, :]).astype(f)
    incl = (s[:, None] <= tcol[None, :]).astype(f)
    one = np.concatenate([strict, incl], 1)
    cst[:, 256:512] = np.concatenate([one, one], 1)
    low = (s[:, None] > tcol[None, :]).astype(f)
    cst[:, 512:640] = np.concatenate([low, low], 1)
    cst[:, 640:704] = (s[:, None] == tcol[None, :]).astype(f)
    tt = np.arange(16)
    for gi, w in enumerate((2, 4, 8, 16)):
        cst[:, 704 + gi * 16:704 + (gi + 1) * 16] = (1.0 / np.minimum(tt + 1, w)).astype(f)[None, :]
    cst[:, 768:896] = (p[:, None] < p[None, :]).astype(f)
    cst[:, 896] = p.astype(f)
    cst[:, 897:929] = np.arange(32, dtype=f)[None, :]
    cst[:, 929:937] = (float(SL) * np.arange(8, dtype=f))[None, :]
    rm = np.ones((128, T), f)
    rm[:, ::64] = 0.0
    w_r = np.concatenate([inp["moe_w_group"][l], inp["moe_w_expert"][l]], 1).astype(f)
    b_r = np.concatenate([inp["moe_b_group"][l], inp["moe_b_expert"][l]])[None, :].astype(f)
    return dict(gB=gBh, pv=pvh, cst=cst, rmask=rm, w_r=np.ascontiguousarray(w_r), b_r=b_r)


def make_in_maps(inp, cores):
    l = 0
    c = host_consts(inp)
    shared = dict(
        w_in=inp["w_in"][l], pool_w=inp["pool_w"][l], w2=inp["rwkv_w2"][l], a2=inp["rwkv_a2"][l], g2=inp["rwkv_g2"][l],
        w_out=inp["w_out"][l], w_q=inp["xattn_w_q"][l], w_kv=inp["xattn_w_kv"][l], w_o=inp["xattn_w_o"][l],
        wg_l=np.asarray(inp["moe_w_gate"][l], np.float32).reshape(16, 4, 4, 128, 512).transpose(0, 1, 3, 2, 4).reshape(8192, 2048),
        wu_l=np.asarray(inp["moe_w_up"][l], np.float32).reshape(16, 4, 4, 128, 512).transpose(0, 1, 3, 2, 4).reshape(8192, 2048),
        wd_l=np.asarray(inp["moe_w_down"][l], np.float32).reshape(8192, 2048), **c)
    shared = {k: np.ascontiguousarray(np.asarray(v, np.float32)) for k, v in shared.items()}
    maps = []
    for b in cores:
        m = dict(shared)
        m["x"] = np.ascontiguousarray(inp["x"][b])
        m["mem"] = np.ascontiguousarray(inp["mem"][b])
        maps.append(m)
    return maps


def kernel(**inputs):
    inp = {k: np.asarray(v) for k, v in inputs.items()}
    nc = build()
    maps = make_in_maps(inp, list(range(8)))
    res = run_bass_kernel_spmd(nc, maps, core_ids=list(range(8)))
    return np.stack([np.asarray(r["out"]) for r in res.results], 0).astype(np.float32)
```

```python
import numpy as np
import concourse.bass as bass
import concourse.mybir as mybir
from concourse.bass_utils import run_bass_kernel_spmd
from contextlib import ExitStack

F32 = mybir.dt.float32
BF16 = mybir.dt.bfloat16
I32 = mybir.dt.int32
AF = mybir.ActivationFunctionType
ALU = mybir.AluOpType
AX = mybir.AxisListType

D = 2048
KC = 16
T = 2048
NTT = T // 128
NTQ = T // 512
NCH = T // 64
PAD = 16
CDEC = float(np.exp(-0.5))
WORDS = 51200
NSLOT, SL = 26, 384
NA = SL // 128


class Dep:
    __slots__ = ("w", "r", "p")

    def __init__(self):
        self.w = None
        self.r = {}
        self.p = {}


class Eng:
    def __init__(self, fw, name, b, is_pe=False):
        self.fw, self.name, self.b, self.is_pe = fw, name, b, is_pe
        self.sem = fw.new_sem(name)
        self.cnt = 0
        self.waited = {}
        self.dma_slots = None
        self.dma_i = 0

    def _wait(self, tok):
        sem, val = tok
        if self.waited.get(id(sem), 0) < val:
            self.b.wait_ge(sem, val)
            self.waited[id(sem)] = val

    def _collect(self, reads, writes, pws=()):
        def w_(t):
            if t is not None and not (self.is_pe and t[0] is self.sem):
                self._wait(t)
        for d in reads:
            w_(d.w)
            for t in d.p.values():
                w_(t)
        for d in writes:
            w_(d.w)
            for t in d.p.values():
                w_(t)
            for t in d.r.values():
                w_(t)
        for d in pws:
            w_(d.w)
            for t in d.r.values():
                w_(t)

    def op(self, fn, reads=(), writes=(), pws=()):
        self._collect(reads, writes, pws)
        inst = fn(self.b)
        self.cnt += 1
        inst.then_inc(self.sem, 1)
        tok = (self.sem, self.cnt)
        for d in reads:
            d.r[id(self.sem)] = tok
        for d in writes:
            d.w = tok
            d.r = {}
            d.p = {}
        for d in pws:
            d.p[id(self.sem)] = tok
        return tok

    def dma(self, out, in_, reads=(), writes=(), **kw):
        return self.dma_fn(lambda e: e.dma_start(out=out, in_=in_, **kw), reads, writes)

    def dma_fn(self, fn, reads=(), writes=()):
        if self.dma_slots is None:
            self.dma_slots = [[self.fw.new_sem(f"{self.name}_d{i}"), 0] for i in range(8)]
        self._collect(reads, writes)
        slot = self.dma_slots[self.dma_i % len(self.dma_slots)]
        self.dma_i += 1
        if slot[1] > 0:
            self._wait((slot[0], slot[1]))
        inst = fn(self.b)
        slot[1] += 16
        inst.then_inc(slot[0], 16)
        tok = (slot[0], slot[1])
        for d in reads:
            d.r[id(slot[0])] = tok
        for d in writes:
            d.w = tok
            d.r = {}
            d.p = {}
        return tok


class FW:
    def __init__(self, nc, es):
        self.nc, self.es = nc, es
        self.pe = Eng(self, "pe", nc.tensor, True)
        self.act = Eng(self, "act", nc.scalar)
        self.dve = Eng(self, "dve", nc.vector)
        self.pool = Eng(self, "pool", nc.gpsimd)
        self.sp = Eng(self, "sp", nc.sync)
        self.engs = [self.pe, self.act, self.dve, self.pool, self.sp]

    def new_sem(self, name):
        return self.es.enter_context(self.nc.semaphore(name))

    def barrier(self):
        toks = []
        for e in self.engs:
            if e.cnt > 0:
                toks.append((e.sem, e.cnt))
            if e.dma_slots:
                for s in e.dma_slots:
                    if s[1] > 0:
                        toks.append((s[0], s[1]))
        for e in self.engs:
            for t in toks:
                if t[0] is not e.sem:
                    e._wait(t)


class Alloc:
    def __init__(self, big, start, end):
        self.big, self.top, self.end = big, start, end

    def f32(self, n):
        a = self.big[:, self.top:self.top + n]
        self.top += n
        assert self.top <= self.end, (self.top, self.end)
        return a

    def bf16(self, n):
        w = (n + 1) // 2
        a = self.big[:, self.top:self.top + w].bitcast(BF16)
        self.top += w
        assert self.top <= self.end, (self.top, self.end)
        return a[:, 0:n]


def r3(ap, a):
    return ap.rearrange("p (a b) -> p a b", a=a)


PV = dict(pool_scale=0, mu_rkv=8, mu_lo=32, w0=35, a0=43, k_k=51, k_a=59, ln_w=67, ln_b=75, r_k=83, omka=91)
NPV = 99


def build(stop=99, dbg=()):
    nc = bass.Bass("TRN2", target_bir_lowering=False)

    def din(name, shape):
        return nc.dram_tensor(name, list(shape), F32, kind="ExternalInput").ap()

    x = din("x", [T, D])
    mem = din("mem", [256, D])
    w_in = din("w_in", [D, 4384])
    pool_w = din("pool_w", [4, 256, 256])
    w2 = din("w2", [64, 1024])
    a2 = din("a2", [64, 1024])
    g2 = din("g2", [160, 1024])
    w_out = din("w_out", [D, D])
    w_q = din("w_q", [D, D])
    w_kv = din("w_kv", [D, 2 * D])
    w_o = din("w_o", [D, D])
    w_r = din("w_r", [D, 20])
    b_r = din("b_r", [1, 20])
    wg_l = din("wg_l", [8192, 2048])
    wu_l = din("wu_l", [8192, 2048])
    wd_l = din("wd_l", [8192, 2048])
    gB = din("gB", [5, 128, D])
    pvd = din("pv", [128, NPV])
    cst = din("cst", [128, 1024])
    rmask_d = din("rmask", [128, T])
    out = nc.dram_tensor("out", [T, D], F32, kind="ExternalOutput").ap()
    dbg_aps = {}
    for name, shape in dbg:
        dbg_aps[name] = nc.dram_tensor(name, list(shape), F32, kind="ExternalOutput").ap()
    rkv_s = nc.dram_tensor("rkv_s", [24, 128, T], F32, kind="Internal").ap()
    h1_s = nc.dram_tensor("h1_s", [T, D], F32, kind="Internal").ap()
    h2_s = nc.dram_tensor("h2_s", [T, D], F32, kind="Internal").ap()
    NROW = NSLOT * SL
    Xs = nc.dram_tensor("Xs", [NROW, D], BF16, kind="Internal").ap()
    Ys = nc.dram_tensor("Ys", [NROW, D], F32, kind="Internal").ap()

    with ExitStack() as es:
        fw = FW(nc, es)
        pe, act, dve, pool, sp = fw.pe, fw.act, fw.dve, fw.pool, fw.sp
        big = es.enter_context(nc.sbuf_tensor("big", [128, WORDS], F32))[:]
        banks = [(es.enter_context(nc.psum_tensor(f"bk{i}", [128, 512], F32))[:], Dep()) for i in range(8)]
        bki = [0]

        def nb():
            b = banks[bki[0] % 8]
            bki[0] += 1
            return b

        def mm(o, lhsT, rhs, start, stop, reads, writes):
            pe.op(lambda e: e.matmul(o, lhsT=lhsT, rhs=rhs, start=start, stop=stop), reads, writes)

        CONST_W = 7424
        ca = Alloc(big, 0, CONST_W)
        identf = ca.f32(128)
        blk1 = ca.f32(128)
        mSI = ca.f32(256)
        mL = ca.f32(128)
        i64 = ca.f32(64)
        rcnt = ca.f32(64)
        pv = ca.f32(NPV + 1)
        identb = ca.bf16(128)
        onesb = ca.bf16(128)
        rmask = ca.bf16(T)
        gBt = ca.f32(D)
        lo1 = ca.bf16(T)
        sg1 = ca.bf16(T)
        sg2 = ca.bf16(T)
        ones1 = ca.f32(128)
        brow = ca.f32(20)
        d_const, d_gB, d_lo1, d_sg1, d_sg2 = Dep(), Dep(), Dep(), Dep(), Dep()
        B0_OFF = CONST_W
        B1_OFF = B0_OFF + 8192
        M_OFF = B1_OFF + 8192
        A_OFF = WORDS - 16384
        bufA = r3(big[:, A_OFF:WORDS].bitcast(BF16), 16)
        bufB = r3(big[:, B0_OFF:M_OFF].bitcast(BF16), 16)
        d_A, d_B = Dep(), Dep()

        sp.dma(identf, cst[:, 0:128], writes=[d_const])
        sp.dma(blk1, cst[:, 128:256], writes=[d_const])
        sp.dma(mSI, cst[:, 256:512], writes=[d_const])
        sp.dma(mL, cst[:, 512:640], writes=[d_const])
        sp.dma(i64, cst[:, 640:704], writes=[d_const])
        sp.dma(rcnt, cst[:, 704:768], writes=[d_const])
        sp.dma(pv[:, 0:NPV], pvd, writes=[d_const])
        sp.dma(brow[0:1, :], b_r, writes=[d_const])
        pool.dma(identb, cst[:, 0:128], writes=[d_const])
        pool.dma(rmask, rmask_d, writes=[d_const])
        pool.op(lambda e: e.memset(onesb, 1.0), writes=[d_const])
        pool.op(lambda e: e.memset(ones1, 1.0), writes=[d_const])
        dve.op(lambda e: e.tensor_scalar(out=pv[:, PV["omka"]:PV["omka"] + 8], in0=pv[:, PV["k_a"]:PV["k_a"] + 8],
                                         scalar1=-1.0, scalar2=1.0, op0=ALU.mult, op1=ALU.add), reads=[d_const], writes=[d_const])
        fw.barrier()

        def pvc(name, j):
            c = PV[name] + j
            return pv[:, c:c + 1]

        d_Xs, d_Ys = Dep(), Dep()
        zf = [0]

        def zero_fill(n, zt, dz):
            while n > 0 and zf[0] < NROW // 128 and stop >= 8:
                c = zf[0]
                pool.dma(Xs[c * 128:(c + 1) * 128, :], zt, reads=[dz])
                zf[0] += 1
                n -= 1

        def load_gB(i):
            sp.dma(gBt, gB[i], writes=[d_gB])

        def norm_tiles(al, ntiles, src_fn, dstT, d_dst, tok_off=0, keep=None, nbuf=3):
            xts = [(al.f32(D), Dep()) for _ in range(nbuf)]
            xns = [(al.bf16(D), Dep()) for _ in range(nbuf)]
            junk = al.bf16(D)
            d_junk = Dep()
            sts = [(al.f32(8), Dep()) for _ in range(4)]

            def stage_a(i):
                xt, dx = xts[i % nbuf]
                xn, dn = xns[i % nbuf]
                st, d_st = sts[i % 4]
                sp.dma(xt, src_fn(i), writes=[dx])
                if keep is not None:
                    keep(i, xt, dx)
                act.op(lambda e: e.activation(out=junk, in_=xt, func=AF.Square, accum_out=st[:, 0:1]), reads=[dx], writes=[d_junk, d_st])
                dve.op(lambda e: e.tensor_scalar(out=st[:, 1:2], in0=st[:, 0:1], scalar1=1.0 / D, scalar2=1e-6, op0=ALU.mult, op1=ALU.add),
                       reads=[d_st], writes=[d_st])
                act.op(lambda e: e.activation(out=st[:, 2:3], in_=st[:, 1:2], func=AF.Sqrt), reads=[d_st], writes=[d_st])
                dve.op(lambda e: e.reciprocal(out=st[:, 3:4], in_=st[:, 2:3]), reads=[d_st], writes=[d_st])
                dve.op(lambda e: e.scalar_tensor_tensor(out=xn, in0=xt, scalar=st[:, 3:4], in1=gBt, op0=ALU.mult, op1=ALU.mult),
                       reads=[dx, d_st, d_gB], writes=[dn])

            def stage_b(i):
                xn, dn = xns[i % nbuf]
                for q in range(4):
                    pb, pd = nb()
                    for j in range(4):
                        kc = q * 4 + j
                        mm(pb[:, j * 128:(j + 1) * 128], xn[:, kc * 128:(kc + 1) * 128], identb, True, True, [dn, d_const], [pd])
                    t0 = tok_off + i * 128
                    if q % 2 == 0:
                        act.op(lambda e: e.activation(out=dstT[:, q * 4:q * 4 + 4, t0:t0 + 128], in_=r3(pb, 4), func=AF.Copy), reads=[pd], pws=[d_dst])
                    else:
                        dve.op(lambda e: e.tensor_copy(out=dstT[:, q * 4:q * 4 + 4, t0:t0 + 128], in_=r3(pb, 4)), reads=[pd], pws=[d_dst])

            for i in range(ntiles + 1):
                if i < ntiles:
                    stage_a(i)
                if i >= 1:
                    stage_b(i - 1)

        def dump(name, ap_sb, dep, dst=None):
            if name in dbg_aps:
                sp.dma(dbg_aps[name] if dst is None else dst, ap_sb, reads=[dep])

        load_gB(0)
        al = Alloc(big, M_OFF, A_OFF)
        norm_tiles(al, NTT, lambda i: x[i * 128:(i + 1) * 128, :], bufA, d_A)
        fw.barrier()
        if "hnT" in dbg_aps:
            tmp = Alloc(big, M_OFF, A_OFF).f32(T)
            dtmp = Dep()
            for kc in range(16):
                dve.op(lambda e: e.tensor_copy(out=tmp, in_=bufA[:, kc, :]), reads=[d_A], writes=[dtmp])
                sp.dma(dbg_aps["hnT"][kc * 128:(kc + 1) * 128, :], tmp, reads=[dtmp])
            fw.barrier()

        if stop >= 2:
            al = Alloc(big, B1_OFF, A_OFF)
            wbs = [(r3(al.bf16(16 * 128), 16), Dep()) for _ in range(4)]
            wbi = [0]
            pbufs = [(al.f32(PAD + T), Dep()) for _ in range(2)]
            fbs = [(al.f32(PAD + T), Dep()) for _ in range(3)]
            pooled = [(al.bf16(T), Dep()) for _ in range(2)]
            pwb = r3(al.bf16(8 * 256), 8)
            d_pw = Dep()
            ztile = al.bf16(D)
            d_zt = Dep()
            dve.op(lambda e: e.memset(ztile, 0.0), writes=[d_zt])
            for pbf, dp in pbufs + fbs:
                dve.op(lambda e: e.memset(pbf[:, 0:PAD], 0.0), writes=[dp])
            pool.dma(pwb, pool_w.rearrange("g (cc p) d -> p (g cc) d", p=128), writes=[d_pw])
            pbi = [0]

            def proj_block(col0, n):
                ws, dw = wbs[wbi[0] % 4]
                wbi[0] += 1
                pool.dma(ws[:, :, 0:n], w_in[:, col0:col0 + n].rearrange("(kc p) n -> p kc n", p=128), writes=[dw])
                if wbi[0] > 4:
                    zero_fill(3, ztile, d_zt)
                pbf, dp = pbufs[pbi[0] % 2]
                pbi[0] += 1
                for tq in range(NTQ):
                    pb, pd = nb()
                    for kc in range(KC):
                        mm(pb[0:n, :], ws[:, kc, 0:n], bufA[:, kc, tq * 512:(tq + 1) * 512], kc == 0, kc == KC - 1, [dw, d_A], [pd])
                    act.op(lambda e: e.activation(out=pbf[0:n, PAD + tq * 512:PAD + (tq + 1) * 512], in_=pb[0:n, :], func=AF.Copy), reads=[pd], writes=[dp])
                return pbf, dp

            def tshift(pbf, dp, n, mu_ap, zout, dz):
                f0, df0 = fbs[0]
                dve.op(lambda e: e.tensor_tensor(out=f0[0:n, 0:T], in0=pbf[0:n, PAD - 1:PAD - 1 + T], in1=pbf[0:n, PAD:PAD + T], op=ALU.subtract),
                       reads=[dp], writes=[df0])
                dve.op(lambda e: e.scalar_tensor_tensor(out=zout, in0=f0[0:n, 0:T], scalar=mu_ap, in1=pbf[0:n, PAD:PAD + T], op0=ALU.mult, op1=ALU.add),
                       reads=[df0, dp, d_const], writes=[dz])

            z1f, dz1 = fbs[1]
            z1 = z1f[:, PAD:PAD + T]
            pbf, dp = proj_block(4096, 128)
            tshift(pbf, dp, 128, pv[:, PV["mu_lo"]:PV["mu_lo"] + 1], z1, dz1)
            act.op(lambda e: e.activation(out=lo1[0:64, :], in_=z1[0:64, :], func=AF.Tanh), reads=[dz1], writes=[d_lo1])
            act.op(lambda e: e.activation(out=lo1[64:128, :], in_=z1[64:128, :], func=AF.Copy), reads=[dz1], writes=[d_lo1])
            pbf, dp = proj_block(4224, 128)
            tshift(pbf, dp, 128, pv[:, PV["mu_lo"] + 1:PV["mu_lo"] + 2], z1, dz1)
            act.op(lambda e: e.activation(out=sg1, in_=z1, func=AF.Sigmoid), reads=[dz1], writes=[d_sg1])
            pbf, dp = proj_block(4352, 32)
            tshift(pbf, dp, 32, pv[0:32, PV["mu_lo"] + 2:PV["mu_lo"] + 3], z1[0:32, :], dz1)
            act.op(lambda e: e.activation(out=sg2[0:32, :], in_=z1[0:32, :], func=AF.Sigmoid), reads=[dz1], writes=[d_sg2])
            for j in range(24):
                pbf, dp = proj_block(1024 + j * 128, 128)
                tshift(pbf, dp, 128, pvc("mu_rkv", j), z1, dz1)
                sp.dma(rkv_s[j], z1, reads=[dz1])
            for cb in range(8):
                gi = cb // 2
                w = (2, 4, 8, 16)[gi]
                pbf, dp = proj_block(cb * 128, 128)
                (fa, dfa), (fb_, dfb) = fbs[1], fbs[2]
                src, dsrc = pbf, dp
                sh = 1
                k = 0
                while sh < w:
                    dst, ddst = (fa, dfa) if k % 2 == 0 else (fb_, dfb)
                    dve.op(lambda e: e.tensor_tensor(out=dst[:, PAD:PAD + T], in0=src[:, PAD:PAD + T], in1=src[:, PAD - sh:PAD - sh + T], op=ALU.add),
                           reads=[dsrc], writes=[ddst])
                    src, dsrc = dst, ddst
                    sh *= 2
                    k += 1
                po, dpo = pooled[cb % 2]
                dve.op(lambda e: e.scalar_tensor_tensor(out=po, in0=src[:, PAD:PAD + T], scalar=1.0 / w, in1=pbf[:, PAD:PAD + T], op0=ALU.mult, op1=ALU.subtract),
                       reads=[dsrc, dp], writes=[dpo])
                f0, df0 = fbs[0]
                dve.op(lambda e: e.tensor_tensor(out=f0[:, 0:16], in0=src[:, PAD:PAD + 16], in1=rcnt[:, gi * 16:(gi + 1) * 16], op=ALU.mult),
                       reads=[dsrc, d_const], writes=[df0])
                dve.op(lambda e: e.tensor_tensor(out=po[:, 0:16], in0=f0[:, 0:16], in1=pbf[:, PAD:PAD + 16], op=ALU.subtract),
                       reads=[df0, dp], writes=[dpo])
                if cb % 2 == 1:
                    for db in range(2):
                        for tq in range(NTQ):
                            pb, pd = nb()
                            for cc in range(2):
                                mm(pb, pwb[:, gi * 2 + cc, db * 128:(db + 1) * 128], pooled[cc][0][:, tq * 512:(tq + 1) * 512], cc == 0, cc == 1,
                                   [d_pw, pooled[cc][1]], [pd])
                            blk = gi * 2 + db
                            act.op(lambda e: e.activation(out=bufB[:, blk, tq * 512:(tq + 1) * 512], in_=pb, func=AF.Copy, scale=pvc("pool_scale", blk)),
                                   reads=[pd, d_const], writes=[d_B])
            zero_fill(NROW, ztile, d_zt)
            fw.barrier()
            if "mixT" in dbg_aps:
                tmp = Alloc(big, B1_OFF, A_OFF).f32(T)
                dtmp = Dep()
                for kc in range(8):
                    dve.op(lambda e: e.tensor_copy(out=tmp, in_=bufB[:, kc, :]), reads=[d_B], writes=[dtmp])
                    sp.dma(dbg_aps["mixT"][kc * 128:(kc + 1) * 128, :], tmp, reads=[dtmp])
                fw.barrier()


        if stop >= 3:
            QTK = 512
            NQ = T // QTK
            al = Alloc(big, M_OFF, WORDS)
            lw = al.bf16(1024)
            g2a = al.bf16(1024)
            g2b = al.bf16(1024)
            S32 = al.f32(64)
            Sbf = al.bf16(64)
            d_lw, d_S32, d_Sbf = Dep(), Dep(), Dep()
            sets = []
            for i in range(3):
                sets.append(dict(AR=r3(al.bf16(8 * 128), 8), BK=r3(al.bf16(8 * 128), 8), BKh=r3(al.bf16(8 * 128), 8), vb=al.bf16(QTK),
                                 GL=al.f32(8), gbuf=al.bf16(QTK), bonus=al.f32(QTK), ybuf=al.f32(QTK),
                                 d_AR=Dep(), d_BK=Dep(), d_BKh=Dep(), d_vb=Dep(), d_GL=Dep(), d_g=Dep(), d_bonus=Dep(), d_y=Dep(),
                                 d_ARr=[Dep() for _ in range(4)]))
            Fq = [al.f32(QTK) for _ in range(8)]
            dFq = [Dep() for _ in range(8)]
            cbs = []
            for i in range(2):
                cbs.append(dict(NBall=r3(al.bf16(8 * 128), 8), KBall=r3(al.bf16(8 * 128), 8), TM=r3(al.bf16(8 * 320), 8), APU=r3(al.bf16(8 * 128), 8),
                                Mc=r3(al.f32(8 * 64), 8), CcT=r3(al.f32(8 * 64), 8),
                                d_NB=[Dep() for _ in range(4)], d_KB=[Dep() for _ in range(4)], d_TM=[Dep() for _ in range(4)],
                                d_APU=[Dep() for _ in range(4)], d_Mc=[Dep() for _ in range(4)], d_Cc=[Dep() for _ in range(4)]))
            TT = r3(al.bf16(8 * 64), 8)
            Wg = [[r3(al.bf16(2 * 128), 2) for _ in range(2)] for _ in range(4)]
            NTg = [[r3(al.bf16(2 * 64), 2) for _ in range(2)] for _ in range(4)]
            d_TT = [Dep() for _ in range(4)]
            dWg = [[Dep(), Dep()] for _ in range(4)]
            dNTg = [[Dep(), Dep()] for _ in range(4)]
            yc, sq, rs = al.f32(QTK), al.f32(QTK), al.f32(QTK)
            dyc, dsq, drs = Dep(), Dep(), Dep()
            pool.dma(lw[0:64, :], w2, writes=[d_lw])
            pool.dma(lw[64:128, :], a2, writes=[d_lw])
            pool.dma(g2a, g2[0:128, :], writes=[d_lw])
            pool.dma(g2b[0:32, :], g2[128:160, :], writes=[d_lw])
            HS = [slice(0, 64), slice(64, 128)]
            i64b = i64.unsqueeze(1).to_broadcast([128, 2, 64])
            pyb, pdyb = banks[7]
            nbm = [0]

            def nb7():
                b = banks[nbm[0] % 7]
                nbm[0] += 1
                return b

            def prep_gen(u):
                hp, q = divmod(u, NQ)
                S_ = sets[u % 3]
                cs = slice(hp * 128, (hp + 1) * 128)
                tsl = slice(q * QTK, (q + 1) * QTK)
                k_, sgw, alr, cum, kk, f5, f6, f7 = Fq
                dk, dsgw, dalr, dcum, dkk, df5, df6, df7 = dFq
                AR, BK, BKh, vb, GL, gbuf, bonus = S_["AR"], S_["BK"], S_["BKh"], S_["vb"], S_["GL"], S_["gbuf"], S_["bonus"]
                d_AR, d_BK, d_BKh, d_vb, d_GL, d_g, d_bonus = S_["d_AR"], S_["d_BK"], S_["d_BKh"], S_["d_vb"], S_["d_GL"], S_["d_g"], S_["d_bonus"]
                sp.dma(k_, rkv_s[8 + hp][:, tsl], writes=[dk])
                pb, pd = nb7()
                mm(pb, lw[0:64, cs], lo1[0:64, tsl], True, True, [d_lw, d_lo1], [pd])
                act.op(lambda e: e.activation(out=sgw, in_=pb, func=AF.Sigmoid, bias=pvc("w0", hp)), reads=[pd, d_const], writes=[dsgw])
                pb, pd = nb7()
                mm(pb, lw[64:128, cs], lo1[64:128, tsl], True, True, [d_lw, d_lo1], [pd])
                act.op(lambda e: e.activation(out=alr, in_=pb, func=AF.Sigmoid, bias=pvc("a0", hp)), reads=[pd, d_const], writes=[dalr])
                yield
                pb, pd = nb7()
                mm(pb, g2a[:, cs], sg1[:, tsl], True, False, [d_lw, d_sg1], [pd])
                mm(pb, g2b[0:32, cs], sg2[0:32, tsl], False, True, [d_lw, d_sg2], [pd])
                act.op(lambda e: e.activation(out=gbuf, in_=pb, func=AF.Copy), reads=[pd], writes=[d_g])
                dve.op(lambda e: e.tensor_tensor_scan(out=cum, data0=rmask[:, 0:QTK], data1=sgw, initial=0.0, op0=ALU.mult, op1=ALU.add),
                       reads=[d_const, dsgw], writes=[dcum])
                yield
                act.op(lambda e: e.activation(out=kk, in_=k_, func=AF.Copy, scale=pvc("k_k", hp)), reads=[dk, d_const], writes=[dkk])
                pool.op(lambda e: e.tensor_tensor(out=f5, in0=kk, in1=kk, op=ALU.mult), reads=[dkk], writes=[df5])
                yield
                pb, pd = nb7()
                mm(pb, blk1, f5, True, True, [d_const, df5], [pd])
                dve.op(lambda e: e.tensor_scalar(out=f6, in0=pb, scalar1=1e-24, scalar2=None, op0=ALU.max), reads=[pd], writes=[df6])
                act.op(lambda e: e.activation(out=f6, in_=f6, func=AF.Sqrt), reads=[df6], writes=[df6])
                yield
                dve.op(lambda e: e.reciprocal(out=f6, in_=f6), reads=[df6], writes=[df6])
                yield
                pool.op(lambda e: e.tensor_tensor(out=kk, in0=kk, in1=f6, op=ALU.mult), reads=[dkk, df6], writes=[dkk])
                dve.op(lambda e: e.tensor_scalar(out=f5, in0=alr, scalar1=pvc("k_a", hp), scalar2=pvc("omka", hp), op0=ALU.mult, op1=ALU.add),
                       reads=[dalr, d_const], writes=[df5])
                yield
                pool.op(lambda e: e.tensor_tensor(out=f5, in0=f5, in1=k_, op=ALU.mult), reads=[df5, dk], writes=[df5])
                pool.op(lambda e: e.tensor_tensor(out=alr, in0=alr, in1=kk, op=ALU.mult), reads=[dalr, dkk], writes=[dalr])
                yield
                act.op(lambda e: e.activation(out=f6, in_=cum, func=AF.Exp, scale=CDEC), reads=[dcum], writes=[df6])
                dve.op(lambda e: e.tensor_tensor(out=BK[:, :, 0:64], in0=r3(alr, 8), in1=r3(f6, 8), op=ALU.mult), reads=[dalr, df6], writes=[d_BK])
                pool.op(lambda e: e.tensor_tensor(out=BK[:, :, 64:128], in0=r3(f5, 8), in1=r3(f6, 8), op=ALU.mult), reads=[df5, df6], writes=[d_BK])
                yield
                dve.op(lambda e: e.tensor_tensor(out=r3(f6, 8), in0=r3(cum, 8), in1=r3(cum, 8)[:, :, 63:64].to_broadcast([128, 8, 64]), op=ALU.subtract),
                       reads=[dcum], writes=[df6])
                act.op(lambda e: e.activation(out=f6, in_=f6, func=AF.Exp, scale=CDEC), reads=[df6], writes=[df6])
                yield
                dve.op(lambda e: e.tensor_tensor(out=BKh[:, :, 0:64], in0=r3(alr, 8), in1=r3(f6, 8), op=ALU.mult), reads=[dalr, df6], writes=[d_BKh])
                pool.op(lambda e: e.tensor_tensor(out=BKh[:, :, 64:128], in0=r3(f5, 8), in1=r3(f6, 8), op=ALU.mult), reads=[df5, df6], writes=[d_BKh])
                yield
                pool.op(lambda e: e.tensor_tensor(out=f6, in0=cum, in1=sgw, op=ALU.subtract), reads=[dcum, dsgw], writes=[df6])
                act.op(lambda e: e.activation(out=f6, in_=f6, func=AF.Exp, scale=-CDEC), reads=[df6], writes=[df6])
                yield
                dve.op(lambda e: e.scalar_tensor_tensor(out=AR[:, :, 0:64], in0=r3(kk, 8), scalar=-1.0, in1=r3(f6, 8), op0=ALU.mult, op1=ALU.mult),
                       reads=[dkk, df6], writes=[d_AR])
                act.op(lambda e: e.activation(out=GL, in_=r3(cum, 8)[:, :, 63], func=AF.Exp, scale=-CDEC), reads=[dcum], writes=[d_GL])
                yield
                act.op(lambda e: e.activation(out=f6, in_=cum, func=AF.Exp, scale=-CDEC), reads=[dcum], writes=[df6])
                sp.dma(k_, rkv_s[hp][:, tsl], writes=[dk])
                pool.op(lambda e: e.tensor_tensor(out=AR[:, :, 64:128], in0=r3(k_, 8), in1=r3(f6, 8), op=ALU.mult), reads=[dk, df6], writes=[d_AR] + S_["d_ARr"])
                yield
                dve.op(lambda e: e.scalar_tensor_tensor(out=f6, in0=k_, scalar=pvc("r_k", hp), in1=f5, op0=ALU.mult, op1=ALU.mult),
                       reads=[dk, df5, d_const], writes=[df6])
                sp.dma(sgw, rkv_s[16 + hp][:, tsl], writes=[dsgw])
                yield
                pb, pd = nb7()
                mm(pb, blk1, f6, True, True, [d_const, df6], [pd])
                dve.op(lambda e: e.tensor_tensor(out=bonus, in0=pb, in1=sgw, op=ALU.mult), reads=[pd, dsgw], writes=[d_bonus])
                act.op(lambda e: e.activation(out=vb, in_=sgw, func=AF.Copy), reads=[dsgw], writes=[d_vb])
                yield

            def front_gen(u):
                hp, q = divmod(u, NQ)
                S_ = sets[u % 3]
                C_ = cbs[u % 2]
                tsl = slice(q * QTK, (q + 1) * QTK)
                AR, BK, BKh, vb, GL, gbuf, bonus, ybuf = S_["AR"], S_["BK"], S_["BKh"], S_["vb"], S_["GL"], S_["gbuf"], S_["bonus"], S_["ybuf"]
                d_AR, d_BK, d_BKh, d_vb, d_GL, d_g, d_bonus, dy = S_["d_AR"], S_["d_BK"], S_["d_BKh"], S_["d_vb"], S_["d_GL"], S_["d_g"], S_["d_bonus"], S_["d_y"]
                d_ARr = S_["d_ARr"]
                NBall, KBall, TM, APU, Mc, CcT = C_["NBall"], C_["KBall"], C_["TM"], C_["APU"], C_["Mc"], C_["CcT"]
                d_NB, d_KB, d_TM, d_APU, d_Mc, d_Cc = C_["d_NB"], C_["d_KB"], C_["d_TM"], C_["d_APU"], C_["d_Mc"], C_["d_Cc"]
                pool.op(lambda e: e.tensor_tensor(out=Mc, in0=i64.unsqueeze(1).to_broadcast([128, 8, 64]),
                                                  in1=GL.unsqueeze(2).to_broadcast([128, 8, 64]), op=ALU.mult),
                        reads=[d_const, d_GL], writes=d_Mc)
                for g in range(4):
                    l0 = 2 * g
                    pa, pda = nb7()
                    pb_, pdb = nb7()
                    pt, pdt = nb7()
                    pv_, pdv = nb7()
                    for ci in range(2):
                        c = l0 + ci
                        for h in range(2):
                            hs = HS[h]
                            mm(pa[hs, ci * 128:(ci + 1) * 128], BK[hs, c, 0:64], AR[hs, c, :], True, True, [d_BK, d_AR, d_ARr[g]], [pda])
                            mm(pb_[hs, ci * 128:(ci + 1) * 128], BK[hs, c, 64:128], AR[hs, c, :], True, True, [d_BK, d_AR, d_ARr[g]], [pdb])
                            mm(pt[hs, ci * 64:(ci + 1) * 64], AR[hs, c, 0:64], BK[hs, c, 0:64], True, True, [d_BK, d_AR], [pdt])
                            idh = identb[hs, 64 * h:64 * h + 64]
                            mm(pv_[hs, ci * 256:ci * 256 + 64], vb[hs, c * 64:(c + 1) * 64], idh, True, True, [d_vb, d_const], [pdv])
                            mm(pv_[hs, ci * 256 + 64:ci * 256 + 128], BKh[hs, c, 0:64], idh, True, True, [d_BKh, d_const], [pdv])
                            mm(pv_[hs, ci * 256 + 128:ci * 256 + 192], BKh[hs, c, 64:128], idh, True, True, [d_BKh, d_const], [pdv])
                            mm(pv_[hs, ci * 256 + 192:ci * 256 + 256], AR[hs, c, 0:64], idh, True, True, [d_AR, d_const], [pdv])
                    dve.op(lambda e: e.tensor_tensor(out=NBall[:, l0:l0 + 2, :], in0=r3(pa[:, 0:256], 2), in1=r3(mSI, 2), op=ALU.mult),
                           reads=[pda, d_const], writes=[d_NB[g]])
                    dve.op(lambda e: e.tensor_tensor(out=KBall[:, l0:l0 + 2, :], in0=r3(pb_[:, 0:256], 2), in1=r3(mSI, 2), op=ALU.mult),
                           reads=[pdb, d_const], writes=[d_KB[g]])
                    dve.op(lambda e: e.tensor_tensor(out=NTg[g][0], in0=r3(pt[:, 0:128], 2), in1=r3(mL, 2), op=ALU.mult),
                           reads=[pdt, d_const], writes=[dNTg[g][0]])
                    act.op(lambda e: e.activation(out=TM[:, l0:l0 + 2, 0:256], in_=r3(pv_, 2), func=AF.Copy), reads=[pdv], writes=[d_TM[g]])
                    pool.op(lambda e: e.tensor_tensor(out=Wg[g][0][:, :, 64:128], in0=NBall[:, l0:l0 + 2, 0:64], in1=i64b, op=ALU.add),
                            reads=[d_NB[g], d_const], writes=[dWg[g][0]])
                    yield
                for g in range(4):
                    l0 = 2 * g
                    p0, pd0 = nb7()
                    q0, qd0 = nb7()
                    NT, dNT = NTg[g][0], dNTg[g][0]
                    for ci in range(2):
                        for h in range(2):
                            hs = HS[h]
                            mm(p0[hs, ci * 64:(ci + 1) * 64], NT[hs, ci, :], NBall[hs, l0 + ci, 0:64], True, True, [dNT, d_NB[g]], [pd0])
                            mm(q0[hs, ci * 64:(ci + 1) * 64], NBall[hs, l0 + ci, 0:64], NT[hs, ci, :], True, True, [dNT, d_NB[g]], [qd0])
                    act.op(lambda e: e.activation(out=Wg[g][0][:, :, 0:64], in_=r3(p0[:, 0:128], 2), func=AF.Copy), reads=[pd0], writes=[dWg[g][0]])
                    act.op(lambda e: e.activation(out=NTg[g][1], in_=r3(q0[:, 0:128], 2), func=AF.Copy), reads=[qd0], writes=[dNTg[g][1]])
                    yield
                cur, ntc = 0, 1
                for lvl in range(1, 6):
                    last = lvl == 5
                    for g in range(4):
                        l0 = 2 * g
                        Wc, dWc = Wg[g][cur], dWg[g][cur]
                        NTc, dNTc = NTg[g][ntc], dNTg[g][ntc]
                        p1, pd1 = nb7()
                        if not last:
                            q1, qd1 = nb7()
                        for ci in range(2):
                            for h in range(2):
                                hs = HS[h]
                                if not last:
                                    mm(p1[hs, ci * 128:(ci + 1) * 128], NTc[hs, ci, :], Wc[hs, ci, :], True, True, [dNTc, dWc], [pd1])
                                    mm(q1[hs, ci * 64:(ci + 1) * 64], Wc[hs, ci, 0:64], NTc[hs, ci, :], True, True, [dNTc, dWc], [qd1])
                                else:
                                    mm(p1[hs, ci * 64:(ci + 1) * 64], NTc[hs, ci, :], Wc[hs, ci, 64:128], True, True, [dNTc, dWc], [pd1])
                        if not last:
                            Wn, dWn = Wg[g][1 - cur], dWg[g][1 - cur]
                            NTn, dNTn = NTg[g][1 - ntc], dNTg[g][1 - ntc]
                            act.op(lambda e: e.activation(out=Wn[:, :, 0:64], in_=r3(p1[:, 0:256], 2)[:, :, 0:64], func=AF.Copy), reads=[pd1], writes=[dWn])
                            dve.op(lambda e: e.tensor_tensor(out=Wn[:, :, 64:128], in0=r3(p1[:, 0:256], 2)[:, :, 64:128], in1=Wc[:, :, 64:128], op=ALU.add),
                                   reads=[pd1, dWc], writes=[dWn])
                            act.op(lambda e: e.activation(out=NTn, in_=r3(q1[:, 0:128], 2), func=AF.Copy), reads=[qd1], writes=[dNTn])
                        else:
                            dve.op(lambda e: e.tensor_tensor(out=TT[:, l0:l0 + 2, :], in0=r3(p1[:, 0:128], 2), in1=Wc[:, :, 64:128], op=ALU.add),
                                   reads=[pd1, dWc], writes=[d_TT[g]])
                        if g % 2 == 1:
                            yield
                    cur, ntc = 1 - cur, 1 - ntc
                for g in range(4):
                    l0 = 2 * g
                    pw, pdw = nb7()
                    for ci in range(2):
                        for h in range(2):
                            hs = HS[h]
                            mm(pw[hs, ci * 64:(ci + 1) * 64], KBall[hs, l0 + ci, 0:64], TM[hs, l0 + ci, 0:64], True, True, [d_KB[g], d_TM[g]], [pdw])
                    act.op(lambda e: e.activation(out=TM[:, l0:l0 + 2, 256:320], in_=r3(pw[:, 0:128], 2), func=AF.Copy), reads=[pdw], writes=[d_TM[g]])
                yield
                for g in range(4):
                    l0 = 2 * g
                    pq, pdq = nb7()
                    for ci in range(2):
                        for h in range(2):
                            hs = HS[h]
                            mm(pq[hs, ci * 128:(ci + 1) * 128], TT[hs, l0 + ci, :], TM[hs, l0 + ci, 192:320], True, True, [d_TT[g], d_TM[g]], [pdq])
                    dve.op(lambda e: e.tensor_copy(out=APU[:, l0:l0 + 2, :], in_=r3(pq[:, 0:256], 2)), reads=[pdq], writes=[d_APU[g]])
                yield
                for g in range(4):
                    l0 = 2 * g
                    pm, pdm = nb7()
                    pc, pdc = nb7()
                    pr, pdr = nb7()
                    for ci in range(2):
                        l = l0 + ci
                        for h in range(2):
                            hs = HS[h]
                            mm(pm[hs, ci * 64:(ci + 1) * 64], APU[hs, l, 0:64], TM[hs, l, 64:128], True, True, [d_APU[g], d_TM[g]], [pdm])
                            mm(pc[hs, ci * 64:(ci + 1) * 64], TM[hs, l, 64:128], APU[hs, l, 64:128], True, False, [d_APU[g], d_TM[g]], [pdc])
                            mm(pc[hs, ci * 64:(ci + 1) * 64], TM[hs, l, 128:192], TM[hs, l, 0:64], False, True, [d_TM[g]], [pdc])
                            mm(pr[hs, ci * 64:(ci + 1) * 64], APU[hs, l, 0:64], NBall[hs, l, 64:128], True, True, [d_APU[g], d_NB[g]], [pdr])
                    dve.op(lambda e: e.tensor_tensor(out=Mc[:, l0:l0 + 2, :], in0=r3(pm[:, 0:128], 2), in1=Mc[:, l0:l0 + 2, :], op=ALU.add),
                           reads=[pdm, d_Mc[g]], writes=[d_Mc[g]])
                    act.op(lambda e: e.activation(out=CcT[:, l0:l0 + 2, :], in_=r3(pc[:, 0:128], 2), func=AF.Copy), reads=[pdc], writes=[d_Cc[g]])
                    dve.op(lambda e: e.tensor_tensor(out=AR[:, l0:l0 + 2, 64:128], in0=r3(pr[:, 0:128], 2), in1=AR[:, l0:l0 + 2, 64:128], op=ALU.add),
                           reads=[pdr, d_ARr[g], d_AR], writes=[d_ARr[g]])
                    if g % 2 == 1:
                        yield
            def back_gen(u):
                hp, q = divmod(u, NQ)
                S_ = sets[u % 3]
                C_ = cbs[u % 2]
                tsl = slice(q * QTK, (q + 1) * QTK)
                AR, BK, BKh, vb, GL, gbuf, bonus, ybuf = S_["AR"], S_["BK"], S_["BKh"], S_["vb"], S_["GL"], S_["gbuf"], S_["bonus"], S_["ybuf"]
                d_AR, d_BK, d_BKh, d_vb, d_GL, d_g, d_bonus, dy = S_["d_AR"], S_["d_BK"], S_["d_BKh"], S_["d_vb"], S_["d_GL"], S_["d_g"], S_["d_bonus"], S_["d_y"]
                d_ARr = S_["d_ARr"]
                NBall, KBall, TM, APU, Mc, CcT = C_["NBall"], C_["KBall"], C_["TM"], C_["APU"], C_["Mc"], C_["CcT"]
                d_NB, d_KB, d_TM, d_APU, d_Mc, d_Cc = C_["d_NB"], C_["d_KB"], C_["d_TM"], C_["d_APU"], C_["d_Mc"], C_["d_Cc"]
                if q == 0:
                    dve.op(lambda e: e.memset(S32, 0.0), writes=[d_S32])
                    dve.op(lambda e: e.memset(Sbf, 0.0), writes=[d_Sbf])
                for l in range(8):
                    g = l // 2
                    ps_, pds = nb7()
                    for h in range(2):
                        hs = HS[h]
                        mm(ps_[hs, 0:64], Mc[hs, l, :], S32[hs, :], True, True, [d_Mc[g], d_S32], [pds])
                    for h in range(2):
                        hs = HS[h]
                        mm(pyb[hs, l * 64:(l + 1) * 64], Sbf[hs, :], AR[hs, l, 64:128], True, False, [d_Sbf, d_ARr[g]], [pdyb])
                        mm(pyb[hs, l * 64:(l + 1) * 64], APU[hs, l, 64:128], NBall[hs, l, 64:128], False, False, [d_APU[g], d_NB[g]], [pdyb])
                        mm(pyb[hs, l * 64:(l + 1) * 64], TM[hs, l, 0:64], KBall[hs, l, 64:128], False, True, [d_TM[g], d_KB[g]], [pdyb])
                    dve.op(lambda e: e.tensor_tensor(out=S32, in0=ps_[:, 0:64], in1=CcT[:, l, :], op=ALU.add), reads=[pds, d_Cc[g], d_S32], writes=[d_S32])
                    act.op(lambda e: e.activation(out=Sbf, in_=S32, func=AF.Copy), reads=[d_S32], writes=[d_Sbf])
                    yield
                act.op(lambda e: e.activation(out=ybuf, in_=pyb, func=AF.Copy), reads=[pdyb], writes=[dy])
                pb, pd = nb7()
                mm(pb, blk1, ybuf, True, True, [d_const, dy], [pd])
                dve.op(lambda e: e.scalar_tensor_tensor(out=yc, in0=pb, scalar=-1.0 / 64, in1=ybuf, op0=ALU.mult, op1=ALU.add),
                       reads=[pd, dy], writes=[dyc])
                pool.op(lambda e: e.tensor_tensor(out=sq, in0=yc, in1=yc, op=ALU.mult), reads=[dyc], writes=[dsq])
                yield
                pb, pd = nb7()
                mm(pb, blk1, sq, True, True, [d_const, dsq], [pd])
                dve.op(lambda e: e.tensor_scalar(out=rs, in0=pb, scalar1=1.0 / 64, scalar2=64e-5, op0=ALU.mult, op1=ALU.add), reads=[pd], writes=[drs])
                act.op(lambda e: e.activation(out=rs, in_=rs, func=AF.Sqrt), reads=[drs], writes=[drs])
                yield
                dve.op(lambda e: e.reciprocal(out=rs, in_=rs), reads=[drs], writes=[drs])
                pool.op(lambda e: e.tensor_tensor(out=yc, in0=yc, in1=rs, op=ALU.mult), reads=[dyc, drs], writes=[dyc])
                yield
                dve.op(lambda e: e.tensor_scalar(out=yc, in0=yc, scalar1=pvc("ln_w", hp), scalar2=pvc("ln_b", hp), op0=ALU.mult, op1=ALU.add),
                       reads=[dyc, d_const], writes=[dyc])
                pool.op(lambda e: e.tensor_tensor(out=yc, in0=yc, in1=bonus, op=ALU.add), reads=[dyc, d_bonus], writes=[dyc])
                dve.op(lambda e: e.tensor_tensor(out=bufB[:, 8 + hp, tsl], in0=yc, in1=gbuf, op=ALU.mult), reads=[dyc, d_g], writes=[d_B])
                yield

            def run_interleaved(gens, steps=None):
                steps = steps or [1] * len(gens)
                gens = [[g, k] for g, k in zip(gens, steps) if g is not None]
                while gens:
                    for gk in list(gens):
                        for _ in range(gk[1]):
                            try:
                                next(gk[0])
                            except StopIteration:
                                gens.remove(gk)
                                break

            NU = 8 * NQ
            PIPE_STEPS = [1, 1, 1]
            run_interleaved([prep_gen(0)])
            run_interleaved([front_gen(0), prep_gen(1)])
            for u in range(NU):
                run_interleaved([back_gen(u), front_gen(u + 1) if u + 1 < NU else None, prep_gen(u + 2) if u + 2 < NU else None], PIPE_STEPS)
            fw.barrier()
            if "rwT" in dbg_aps:
                tmp = Alloc(big, M_OFF, WORDS).f32(T)
                dtmp = Dep()
                for kc in range(8):
                    dve.op(lambda e: e.tensor_copy(out=tmp, in_=bufB[:, 8 + kc, :]), reads=[d_B], writes=[dtmp])
                    sp.dma(dbg_aps["rwT"][kc * 128:(kc + 1) * 128, :], tmp, reads=[dtmp])
                fw.barrier()

        def out_proj(srcT, d_src, wmat, res_fn, dst_dram, al, pre=None):
            wbs2 = [(r3(al.bf16(16 * 512), 16), Dep()) for _ in range(2)]
            xts = [(al.f32(512), Dep()) for _ in range(3)]
            hos = [(al.f32(512), Dep()) for _ in range(2)]
            i = 0
            for dblk in range(4):
                ws, dw = wbs2[dblk % 2]
                ds_ = slice(dblk * 512, (dblk + 1) * 512)
                if dblk == 0 and pre is not None:
                    ws, dw = pre
                else:
                    pool.dma(ws, wmat[:, ds_].rearrange("(kc p) n -> p kc n", p=128), writes=[dw])
                for tt in range(NTT):
                    rows = slice(tt * 128, (tt + 1) * 128)
                    xt, dx = xts[i % 3]
                    ho, dh = hos[i % 2]
                    i += 1
                    sp.dma(xt, res_fn(rows, ds_), writes=[dx])
                    pb, pd = nb()
                    for kc in range(KC):
                        mm(pb, srcT[:, kc, rows], ws[:, kc, :], kc == 0, kc == KC - 1, [d_src, dw], [pd])
                    dve.op(lambda e: e.tensor_tensor(out=ho, in0=pb, in1=xt, op=ALU.add), reads=[pd, dx], writes=[dh])
                    act.dma(dst_dram[rows, ds_], ho, reads=[dh])

        if stop >= 4:
            out_proj(bufB, d_B, w_out, lambda rows, cols: x[rows, cols], h1_s, Alloc(big, M_OFF, A_OFF))
            fw.barrier()
            if stop >= 5:
                d_memT = Dep()
                alB = Alloc(big, B0_OFF, M_OFF)
                memT = r3(alB.bf16(16 * 256), 16)
                wbs5_0 = (r3(alB.bf16(16 * 512), 16), Dep())
                load_gB(3)
                norm_tiles(alB, 2, lambda i: mem[i * 128:(i + 1) * 128, :], memT, d_memT, nbuf=2)
                pool.dma(wbs5_0[0], w_kv[:, 0:512].rearrange("(kc p) n -> p kc n", p=128), writes=[wbs5_0[1]])
            load_gB(1)
            norm_tiles(Alloc(big, M_OFF, A_OFF), NTT, lambda i: h1_s[i * 128:(i + 1) * 128, :], bufA, d_A)
            fw.barrier()
            if "h1" in dbg_aps:
                tmp = Alloc(big, M_OFF, A_OFF).f32(D)
                dtmp = Dep()
                for tt in range(NTT):
                    sp.dma(tmp, h1_s[tt * 128:(tt + 1) * 128, :], writes=[dtmp])
                    sp.dma(dbg_aps["h1"][tt * 128:(tt + 1) * 128, :], tmp, reads=[dtmp])
                fw.barrier()


        if stop >= 5:
            kv_al = Alloc(big, M_OFF, M_OFF + 4096)
            KT = r3(kv_al.bf16(16 * 256), 16)
            Vb = r3(kv_al.bf16(2 * D), 2)
            d_KT, d_Vb = Dep(), Dep()
            alM = Alloc(big, M_OFF + 4096, A_OFF)
            wbs5 = [wbs5_0, (r3(alM.bf16(16 * 512), 16), Dep())]
            wq_pre = (r3(alM.bf16(16 * 256), 16), Dep())
            for g in range(8):
                ws, dw = wbs5[g % 2]
                if g > 0:
                    pool.dma(ws, w_kv[:, g * 512:(g + 1) * 512].rearrange("(kc p) n -> p kc n", p=128), writes=[dw])
                if g == 1:
                    pool.dma(wq_pre[0], w_q[:, 0:256].rearrange("(kc p) n -> p kc n", p=128), writes=[wq_pre[1]])
                if g < 4:
                    for j in range(4):
                        cb = g * 4 + j
                        pb, pd = nb()
                        for kc in range(KC):
                            mm(pb[:, 0:256], ws[:, kc, j * 128:(j + 1) * 128], memT[:, kc, :], kc == 0, kc == KC - 1, [dw, d_memT], [pd])
                        act.op(lambda e: e.activation(out=KT[:, cb, :], in_=pb[:, 0:256], func=AF.Copy), reads=[pd], writes=[d_KT])
                else:
                    for mc in range(2):
                        pb, pd = nb()
                        for kc in range(KC):
                            mm(pb, memT[:, kc, mc * 128:(mc + 1) * 128], ws[:, kc, :], kc == 0, kc == KC - 1, [dw, d_memT], [pd])
                        dve.op(lambda e: e.tensor_copy(out=Vb[:, mc, (g - 4) * 512:(g - 3) * 512], in_=pb), reads=[pd], writes=[d_Vb])
            fw.barrier()
            alM = Alloc(big, M_OFF + 4096, A_OFF)
            wbs6 = [(r3(alM.bf16(16 * 256), 16), Dep()) for _ in range(2)]
            qscale = float(512 ** -0.5)
            for g in range(8):
                if g == 0:
                    ws, dw = wq_pre
                else:
                    ws, dw = wbs6[g % 2]
                    pool.dma(ws, w_q[:, g * 256:(g + 1) * 256].rearrange("(kc p) n -> p kc n", p=128), writes=[dw])
                for j in range(2):
                    cb = g * 2 + j
                    for tq in range(NTQ):
                        ts_ = slice(tq * 512, (tq + 1) * 512)
                        pb, pd = nb()
                        for kc in range(KC):
                            mm(pb, ws[:, kc, j * 128:(j + 1) * 128], bufA[:, kc, ts_], kc == 0, kc == KC - 1, [dw, d_A], [pd])
                        if tq % 2 == 0:
                            act.op(lambda e: e.activation(out=bufB[:, cb, ts_], in_=pb, func=AF.Copy, scale=qscale), reads=[pd], writes=[d_B])
                        else:
                            dve.op(lambda e: e.tensor_scalar(out=bufB[:, cb, ts_], in0=pb, scalar1=qscale, scalar2=None, op0=ALU.mult), reads=[pd], writes=[d_B])
            fw.barrier()
            alM = Alloc(big, M_OFF + 4096, A_OFF)
            Es = [(r3(alM.bf16(2 * 512), 2), Dep()) for _ in range(2)]
            rinvs = [(alM.f32(512), Dep()) for _ in range(2)]
            wo_pre = (r3(alM.bf16(16 * 512), 16), Dep())
            pool.dma(wo_pre[0], w_o[:, 0:512].rearrange("(kc p) n -> p kc n", p=128), writes=[wo_pre[1]])
            it = 0
            for h in range(4):
                for tq in range(NTQ):
                    ts_ = slice(tq * 512, (tq + 1) * 512)
                    E, dE = Es[it % 2]
                    rinv, dri = rinvs[it % 2]
                    it += 1
                    for mc in range(2):
                        pb, pd = nb()
                        for c in range(4):
                            mm(pb, KT[:, h * 4 + c, mc * 128:(mc + 1) * 128], bufB[:, h * 4 + c, ts_], c == 0, c == 3, [d_KT, d_B], [pd])
                        act.op(lambda e: e.activation(out=E[:, mc, :], in_=pb, func=AF.Exp), reads=[pd], writes=[dE])
                    pb, pd = nb()
                    for mc in range(2):
                        mm(pb, onesb, E[:, mc, :], mc == 0, mc == 1, [d_const, dE], [pd])
                    dve.op(lambda e: e.reciprocal(out=rinv, in_=pb), reads=[pd], writes=[dri])
                    for c in range(4):
                        pb, pd = nb()
                        for mc in range(2):
                            mm(pb, Vb[:, mc, h * 512 + c * 128:h * 512 + (c + 1) * 128], E[:, mc, :], mc == 0, mc == 1, [d_Vb, dE], [pd])
                        dve.op(lambda e: e.tensor_tensor(out=bufA[:, h * 4 + c, ts_], in0=pb, in1=rinv, op=ALU.mult), reads=[pd, dri], writes=[d_A])
            fw.barrier()
            out_proj(bufA, d_A, w_o, lambda rows, cols: h1_s[rows, cols], h2_s, Alloc(big, B0_OFF, M_OFF), pre=wo_pre)
            fw.barrier()
            if "h2" in dbg_aps:
                tmp = Alloc(big, B0_OFF, M_OFF).f32(D)
                dtmp = Dep()
                for tt in range(NTT):
                    sp.dma(tmp, h2_s[tt * 128:(tt + 1) * 128, :], writes=[dtmp])
                    sp.dma(dbg_aps["h2"][tt * 128:(tt + 1) * 128, :], tmp, reads=[dtmp])
                fw.barrier()

        if stop >= 8:
            IOA = bass.IndirectOffsetOnAxis
            bc_reg = es.enter_context(nc.gpsimd.register("bc"))
            nc.gpsimd.reg_mov(bc_reg, NROW - 1)
            BCV = nc.gpsimd.snap(bc_reg)
            bw_reg = es.enter_context(nc.gpsimd.register("bw"))
            nc.gpsimd.reg_mov(bw_reg, 8191)
            BWV = nc.gpsimd.snap(bw_reg)
            al8 = Alloc(big, B0_OFF, WORDS)
            LT = al8.f32(128)
            iop = al8.f32(1)
            siota = al8.f32(32)
            thr8 = al8.f32(8)
            p1a, p2a = al8.f32(16), al8.f32(16)
            pos1i = al8.f32(16).bitcast(I32)
            pos2i = al8.f32(16).bitcast(I32)
            widx = al8.f32(NSLOT * 4).bitcast(I32)
            d_c8, d_pos, d_widx, d_pp = Dep(), Dep(), Dep(), Dep()
            P8_TOP = al8.top
            sp.dma(LT, cst[:, 768:896], writes=[d_c8])
            sp.dma(iop, cst[:, 896:897], writes=[d_c8], allow_slow_non_contiguous=True)
            sp.dma(siota, cst[:, 897:929], writes=[d_c8])
            sp.dma(thr8, cst[:, 929:937], writes=[d_c8])
            fw.barrier()
            xnb_all = r3(al8.bf16(16 * D), 16)
            d_xnb = [Dep() for _ in range(16)]
            xts = [(al8.f32(D), Dep()) for _ in range(3)]
            xn32s = [(al8.f32(D), Dep()) for _ in range(2)]
            junk = al8.bf16(D)
            d_junk = Dep()
            h32s = [(r3(al8.f32(16 * 128), 16), Dep()) for _ in range(2)]
            wr32 = r3(al8.f32(16 * 20), 16)
            d_wr = Dep()
            logits = r3(al8.f32(16 * 20), 16)
            d_log = Dep()
            sts = [(al8.f32(8), Dep()) for _ in range(4)]
            sp.dma(wr32, w_r.rearrange("(kc p) n -> p kc n", p=128), writes=[d_wr])
            load_gB(2)

            def a8_stage_a(tt):
                xt, dx = xts[tt % 3]
                xn32, dxn = xn32s[tt % 2]
                st, d_st = sts[tt % 4]
                sp.dma(xt, h2_s[tt * 128:(tt + 1) * 128, :], writes=[dx])
                act.op(lambda e: e.activation(out=junk, in_=xt, func=AF.Square, accum_out=st[:, 0:1]), reads=[dx], writes=[d_junk, d_st])
                dve.op(lambda e: e.tensor_scalar(out=st[:, 1:2], in0=st[:, 0:1], scalar1=1.0 / D, scalar2=1e-6, op0=ALU.mult, op1=ALU.add),
                       reads=[d_st], writes=[d_st])
                act.op(lambda e: e.activation(out=st[:, 2:3], in_=st[:, 1:2], func=AF.Sqrt), reads=[d_st], writes=[d_st])
                dve.op(lambda e: e.reciprocal(out=st[:, 3:4], in_=st[:, 2:3]), reads=[d_st], writes=[d_st])
                dve.op(lambda e: e.scalar_tensor_tensor(out=xn32, in0=xt, scalar=st[:, 3:4], in1=gBt, op0=ALU.mult, op1=ALU.mult),
                       reads=[dx, d_st, d_gB], writes=[dxn])
                act.op(lambda e: e.activation(out=xnb_all[:, tt, :], in_=xn32, func=AF.Copy), reads=[dxn], writes=[d_xnb[tt]])

            def a8_stage_b(tt):
                xn32, dxn = xn32s[tt % 2]
                h32, d_h32 = h32s[tt % 2]
                for q in range(4):
                    pf, pdf = nb()
                    for j in range(4):
                        kc = q * 4 + j
                        mm(pf[:, j * 128:(j + 1) * 128], xn32[:, kc * 128:(kc + 1) * 128], identf, True, True, [dxn, d_const], [pdf])
                    if q % 2 == 0:
                        dve.op(lambda e: e.tensor_copy(out=h32[:, q * 4:q * 4 + 4, :], in_=r3(pf, 4)), reads=[pdf], pws=[d_h32])
                    else:
                        act.op(lambda e: e.activation(out=h32[:, q * 4:q * 4 + 4, :], in_=r3(pf, 4), func=AF.Copy), reads=[pdf], pws=[d_h32])
                pb, pd = nb()
                for kc in range(KC):
                    mm(pb[:, 0:20], h32[:, kc, :], wr32[:, kc, :], kc == 0, False, [d_h32, d_wr], [pd])
                mm(pb[:, 0:20], ones1[0:1, 0:128], brow[0:1, 0:20], False, True, [d_const], [pd])
                dve.op(lambda e: e.tensor_copy(out=logits[:, tt, :], in_=pb[:, 0:20]), reads=[pd], writes=[d_log])

            for tt in range(17):
                if tt < 16:
                    a8_stage_a(tt)
                if tt >= 1:
                    a8_stage_b(tt - 1)
            NT_ = 16
            rt = [al8.f32(NT_ * 4) for _ in range(12)]
            rt4 = al8.f32(NT_ * 16)
            sel1 = al8.f32(NT_ * 16)
            sel2 = al8.f32(NT_ * 16)
            ind = al8.f32(NT_ * 16)
            tot = r3(al8.f32(NT_ * 16), NT_)
            tcum = r3(al8.f32(NT_ * 16), NT_)
            posall = al8.f32(NT_ * 16)
            ptmp = al8.f32(NT_ * 16)
            c8 = al8.f32(16 * 8)
            cnt, nsl, bsl, bsl256 = al8.f32(16), al8.f32(16), al8.f32(16), al8.f32(16)
            total = al8.f32(1)
            es32 = al8.f32(NSLOT * 16)
            esf, unused, wbase = al8.f32(NSLOT), al8.f32(NSLOT), al8.f32(NSLOT)
            pos1f, pos2f = al8.f32(16), al8.f32(16)
            widxf = al8.f32(NSLOT * 4).rearrange("p (s q) -> p s q", q=4)
            d_rt = Dep()
            lg = logits[:, :, 0:4]
            le = logits[:, :, 4:20].rearrange("p t (g e) -> p t g e", g=4)
            gmax, gsum, gw, m1, m2 = [rt[i][:, 0:NT_] for i in range(5)]
            goh, gsh, esel, oh1, e2 = [r3(rt[7 + i], NT_) for i in range(5)]
            t4 = rt4.rearrange("p (t g e) -> p t g e", t=NT_, g=4)

            def bc3(v):
                return v.unsqueeze(2).to_broadcast([128, NT_, 4])

            def v4(a):
                return a.rearrange("p (t g e) -> p t g e", t=NT_, g=4)
            R = [d_log, d_rt, d_c8]
            W_ = [d_rt]
            dve.op(lambda e: e.tensor_reduce(out=gmax, in_=lg, axis=AX.X, op=ALU.max), R, W_)
            dve.op(lambda e: e.tensor_tensor(out=goh, in0=lg, in1=bc3(gmax), op=ALU.is_equal), R, W_)
            dve.op(lambda e: e.tensor_tensor(out=gsh, in0=lg, in1=bc3(gmax), op=ALU.subtract), R, W_)
            act.op(lambda e: e.activation(out=gsh, in_=gsh, func=AF.Exp), R, W_)
            dve.op(lambda e: e.tensor_reduce(out=gsum, in_=gsh, axis=AX.X, op=ALU.add), R, W_)
            dve.op(lambda e: e.reciprocal(out=gw, in_=gsum), R, W_)
            dve.op(lambda e: e.tensor_tensor(out=t4, in0=le, in1=goh.unsqueeze(3).to_broadcast([128, NT_, 4, 4]), op=ALU.mult), R, W_)
            dve.op(lambda e: e.tensor_reduce(out=esel, in_=t4.rearrange("p t g e -> p t e g"), axis=AX.X, op=ALU.add), R, W_)
            dve.op(lambda e: e.tensor_reduce(out=m1, in_=esel, axis=AX.X, op=ALU.max), R, W_)
            dve.op(lambda e: e.tensor_tensor(out=oh1, in0=esel, in1=bc3(m1), op=ALU.is_equal), R, W_)
            dve.op(lambda e: e.scalar_tensor_tensor(out=e2, in0=oh1, scalar=-1e30, in1=esel, op0=ALU.mult, op1=ALU.add), R, W_)
            dve.op(lambda e: e.tensor_reduce(out=m2, in_=e2, axis=AX.X, op=ALU.max), R, W_)
            dve.op(lambda e: e.tensor_tensor(out=e2, in0=e2, in1=bc3(m2), op=ALU.is_equal), R, W_)
            dve.op(lambda e: e.tensor_tensor(out=p1a, in0=m1, in1=m2, op=ALU.subtract), R, W_ + [d_pp])
            act.op(lambda e: e.activation(out=p1a, in_=p1a, func=AF.Sigmoid), R + [d_pp], W_ + [d_pp])
            dve.op(lambda e: e.tensor_scalar(out=p2a, in0=p1a, scalar1=-1.0, scalar2=1.0, op0=ALU.mult, op1=ALU.add), R + [d_pp], W_ + [d_pp])
            dve.op(lambda e: e.tensor_tensor(out=p1a, in0=p1a, in1=gw, op=ALU.mult), R + [d_pp], W_ + [d_pp])
            dve.op(lambda e: e.tensor_tensor(out=p2a, in0=p2a, in1=gw, op=ALU.mult), R + [d_pp], W_ + [d_pp])
            dve.op(lambda e: e.tensor_tensor(out=v4(sel1), in0=goh.unsqueeze(3).to_broadcast([128, NT_, 4, 4]),
                                             in1=oh1.unsqueeze(2).to_broadcast([128, NT_, 4, 4]), op=ALU.mult), R, W_)
            dve.op(lambda e: e.tensor_tensor(out=v4(sel2), in0=goh.unsqueeze(3).to_broadcast([128, NT_, 4, 4]),
                                             in1=e2.unsqueeze(2).to_broadcast([128, NT_, 4, 4]), op=ALU.mult), R, W_)
            dve.op(lambda e: e.tensor_tensor(out=ind, in0=sel1, in1=sel2, op=ALU.add), R, W_)
            pw, pdw = nb()
            mm(pw[:, 0:256], LT, ind, True, True, [d_rt, d_c8], [pdw])
            pt_, pdt = nb()
            mm(pt_[:, 0:256], ones1, ind, True, True, [d_rt, d_const], [pdt])
            dve.op(lambda e: e.tensor_copy(out=tot, in_=r3(pt_[:, 0:256], NT_)), R + [pdt], W_)
            dve.op(lambda e: e.memset(tcum[:, 0, :], 0.0), R, W_)
            for tt in range(1, NT_):
                dve.op(lambda e: e.tensor_tensor(out=tcum[:, tt, :], in0=tcum[:, tt - 1, :], in1=tot[:, tt - 1, :], op=ALU.add), R, W_)
            dve.op(lambda e: e.tensor_tensor(out=cnt, in0=tcum[:, NT_ - 1, :], in1=tot[:, NT_ - 1, :], op=ALU.add), R, W_)
            dve.op(lambda e: e.tensor_tensor(out=r3(c8, 16), in0=cnt.unsqueeze(2).to_broadcast([128, 16, 8]),
                                             in1=thr8.unsqueeze(1).to_broadcast([128, 16, 8]), op=ALU.is_gt), R, W_)
            dve.op(lambda e: e.tensor_reduce(out=nsl, in_=r3(c8, 16), axis=AX.X, op=ALU.add), R, W_)
            dve.op(lambda e: e.memset(bsl[:, 0:1], 0.0), R, W_)
            for ex in range(1, 16):
                dve.op(lambda e: e.tensor_tensor(out=bsl[:, ex:ex + 1], in0=bsl[:, ex - 1:ex], in1=nsl[:, ex - 1:ex], op=ALU.add), R, W_)
            dve.op(lambda e: e.tensor_tensor(out=total, in0=bsl[:, 15:16], in1=nsl[:, 15:16], op=ALU.add), R, W_)
            dve.op(lambda e: e.tensor_scalar(out=bsl256, in0=bsl, scalar1=float(SL), scalar2=None, op0=ALU.mult), R, W_)
            dve.op(lambda e: e.tensor_tensor(out=posall, in0=pw[:, 0:256], in1=tcum.rearrange("p t e -> p (t e)"), op=ALU.add), R + [pdw], W_)
            dve.op(lambda e: e.tensor_tensor(out=r3(posall, NT_), in0=r3(posall, NT_), in1=bsl256.unsqueeze(1).to_broadcast([128, NT_, 16]), op=ALU.add), R, W_)
            dve.op(lambda e: e.tensor_tensor(out=ptmp, in0=posall, in1=sel1, op=ALU.mult), R, W_)
            dve.op(lambda e: e.tensor_reduce(out=pos1f, in_=r3(ptmp, NT_), axis=AX.X, op=ALU.add), R, W_)
            dve.op(lambda e: e.tensor_tensor(out=ptmp, in0=posall, in1=sel2, op=ALU.mult), R, W_)
            dve.op(lambda e: e.tensor_reduce(out=pos2f, in_=r3(ptmp, NT_), axis=AX.X, op=ALU.add), R, W_)
            dve.op(lambda e: e.tensor_copy(out=pos1i, in_=pos1f), R, W_ + [d_pos])
            dve.op(lambda e: e.tensor_copy(out=pos2i, in_=pos2f), R, W_ + [d_pos])
            dve.op(lambda e: e.tensor_tensor(out=r3(es32, NSLOT), in0=bsl.unsqueeze(1).to_broadcast([128, NSLOT, 16]),
                                             in1=siota[:, 0:NSLOT].unsqueeze(2).to_broadcast([128, NSLOT, 16]), op=ALU.is_le), R, W_)
            dve.op(lambda e: e.tensor_reduce(out=esf, in_=r3(es32, NSLOT), axis=AX.X, op=ALU.add), R, W_)
            dve.op(lambda e: e.tensor_scalar(out=unused, in0=siota[:, 0:NSLOT], scalar1=total[:, 0:1], scalar2=1.0e6, op0=ALU.is_ge, op1=ALU.mult), R, W_)
            dve.op(lambda e: e.tensor_scalar(out=wbase, in0=esf, scalar1=-1.0, scalar2=512.0, op0=ALU.add, op1=ALU.mult), R, W_)
            dve.op(lambda e: e.tensor_tensor(out=wbase, in0=wbase, in1=unused, op=ALU.add), R, W_)
            dve.op(lambda e: e.tensor_scalar(out=wbase, in0=wbase, scalar1=iop[:, 0:1], scalar2=None, op0=ALU.add), R, W_)
            for q in range(4):
                dve.op(lambda e: e.tensor_scalar(out=widxf[:, :, q], in0=wbase, scalar1=float(128 * q), scalar2=None, op0=ALU.add), R, W_)
            dve.op(lambda e: e.tensor_copy(out=widx, in_=widxf.rearrange("p s q -> p (s q)")), R, W_ + [d_widx])
            if "route" in dbg_aps:
                sp.dma(dbg_aps["route"][:, 0:16], pos1f, reads=[d_rt])
                sp.dma(dbg_aps["route"][:, 16:32], pos2f, reads=[d_rt])
                sp.dma(dbg_aps["route"][:, 32:64], wbase, reads=[d_rt])
                sp.dma(dbg_aps["route"][:, 64:80], p1a, reads=[d_pp])
                sp.dma(dbg_aps["route"][:, 80:96], p2a, reads=[d_pp])
                sp.dma(dbg_aps["route"][:, 96:112], cnt, reads=[d_rt])
            for tt in range(16):
                for posi in (pos1i, pos2i):
                    pool.dma_fn(lambda e: e.indirect_dma_start(out=Xs, out_offset=IOA(ap=posi[:, tt:tt + 1], axis=0), in_=xnb_all[:, tt, :], in_offset=None,
                                                               bounds_check=BCV, oob_is_err=False),
                                reads=[d_xnb[tt], d_pos], writes=[d_Xs])
            fw.barrier()
            ald = Alloc(big, P8_TOP, WORDS)
            wbufs = [(ald.bf16(8192), [Dep() for _ in range(4)]) for _ in range(6)]
            xsls = [(r3(ald.bf16(NA * D), NA), Dep()) for _ in range(2)]
            XTs = [(r3(ald.bf16(16 * SL), 16), Dep()) for _ in range(2)]
            hids = [(r3(ald.bf16(4 * SL), 4), Dep()) for _ in range(2)]
            sbs = [(ald.bf16(SL), Dep()) for _ in range(2)]
            yos = [(ald.f32(D), Dep()) for _ in range(2)]
            cnt8 = dict(yi=0, ei=0)

            def wload(i, s):
                wsl = []
                for m, wl in enumerate((wg_l, wu_l, wd_l)):
                    buf, deps = wbufs[(3 * i + m) % 6]
                    for q in range(4):
                        pool.dma_fn(lambda e: e.indirect_dma_start(out=buf[:, q * 2048:(q + 1) * 2048], out_offset=None, in_=wl,
                                                                   in_offset=IOA(ap=widx[:, s * 4 + q:s * 4 + q + 1], axis=0), bounds_check=BWV, oob_is_err=False),
                                    reads=[d_widx], writes=[deps[q]])
                    wsl.append((buf, deps))
                return wsl

            def xload(i, s):
                xsl, dxs = xsls[i % 2]
                sp.dma(xsl, Xs[s * SL:(s + 1) * SL, :].rearrange("(a p) n -> p a n", p=128), reads=[d_Xs], writes=[dxs])

            def emit_T(i, s):
                xsl, dxs = xsls[i % 2]
                XT, dXT = XTs[i % 2]
                for a in range(NA):
                    for q4 in range(4):
                        pb, pd = nb()
                        for j in range(4):
                            kc = q4 * 4 + j
                            mm(pb[:, j * 128:(j + 1) * 128], xsl[:, a, kc * 128:(kc + 1) * 128], identb, True, True, [dxs, d_const], [pd])
                        cnt8["ei"] += 1
                        if cnt8["ei"] % 2 == 0:
                            act.op(lambda e: e.activation(out=XT[:, q4 * 4:q4 * 4 + 4, a * 128:(a + 1) * 128], in_=r3(pb, 4), func=AF.Copy), reads=[pd], pws=[dXT])
                        else:
                            dve.op(lambda e: e.tensor_copy(out=XT[:, q4 * 4:q4 * 4 + 4, a * 128:(a + 1) * 128], in_=r3(pb, 4)), reads=[pd], pws=[dXT])

            def emit_GU(i, s, wsl):
                wg, dwg = r3(wsl[0][0], 16), wsl[0][1]
                wu, dwu = r3(wsl[1][0], 16), wsl[1][1]
                XT, dXT = XTs[i % 2]
                hid, dhid = hids[i % 2]
                for ffc in range(4):
                    pg, pdg = nb()
                    for kc in range(KC):
                        mm(pg[:, 0:SL], wg[:, kc, ffc * 128:(ffc + 1) * 128], XT[:, kc, :], kc == 0, kc == KC - 1, [dwg[kc // 4], dXT], [pdg])
                    pu, pdu = nb()
                    for kc in range(KC):
                        mm(pu[:, 0:SL], wu[:, kc, ffc * 128:(ffc + 1) * 128], XT[:, kc, :], kc == 0, kc == KC - 1, [dwu[kc // 4], dXT], [pdu])
                    sb_, dsb = sbs[ffc % 2]
                    act.op(lambda e: e.activation(out=sb_, in_=pg[:, 0:SL], func=AF.Silu), reads=[pdg], writes=[dsb])
                    dve.op(lambda e: e.tensor_tensor(out=hid[:, ffc, :], in0=pu[:, 0:SL], in1=sb_, op=ALU.mult), reads=[pdu, dsb], writes=[dhid])

            def emit_D(i, s, wsl):
                wd, dwd = r3(wsl[2][0], 4), wsl[2][1]
                hid, dhid = hids[i % 2]
                for a in range(NA):
                    yo, dyo = yos[cnt8["yi"] % 2]
                    cnt8["yi"] += 1
                    for dblk in range(4):
                        ds_ = slice(dblk * 512, (dblk + 1) * 512)
                        pb, pd = nb()
                        for ffc in range(4):
                            mm(pb, hid[:, ffc, a * 128:(a + 1) * 128], wd[:, ffc, ds_], ffc == 0, ffc == 3, [dhid, dwd[ffc]], [pd])
                        if dblk % 2 == 0:
                            act.op(lambda e: e.activation(out=yo[:, ds_], in_=pb, func=AF.Copy), reads=[pd], pws=[dyo])
                        else:
                            dve.op(lambda e: e.tensor_copy(out=yo[:, ds_], in_=pb), reads=[pd], pws=[dyo])
                    r0 = s * SL + a * 128
                    sp.dma(Ys[r0:r0 + 128, :], yo, reads=[dyo], writes=[d_Ys])

            lo_n = NSLOT - NSLOT // 3
            lo, hi = list(range(lo_n)), list(range(NSLOT - 1, lo_n - 1, -1))
            order = []
            while lo or hi:
                order += lo[:2]
                lo = lo[2:]
                if hi:
                    order.append(hi.pop(0))
            assert sorted(order) == list(range(NSLOT))
            xload(0, order[0])
            emit_T(0, order[0])
            for i, s in enumerate(order):
                wsl = wload(i, s)
                if i + 1 < NSLOT:
                    xload(i + 1, order[i + 1])
                emit_GU(i, s, wsl)
                if i + 1 < NSLOT:
                    emit_T(i + 1, order[i + 1])
                emit_D(i, s, wsl)
            fw.barrier()
            ale = Alloc(big, P8_TOP, WORDS)
            NCB = 4
            cts = [(ale.f32(D), Dep()) for _ in range(NCB)]
            y1s = [(ale.f32(D), Dep()) for _ in range(NCB)]
            y2s = [(ale.f32(D), Dep()) for _ in range(NCB)]
            junk2 = ale.bf16(D)
            sts2 = [(ale.f32(8), Dep()) for _ in range(4)]
            load_gB(4)
            for tt in range(16):
                xt, dx = cts[tt % NCB]
                y1, dy1 = y1s[tt % NCB]
                y2, dy2 = y2s[tt % NCB]
                st, d_st = sts2[tt % 4]
                pool.dma(xt, h2_s[tt * 128:(tt + 1) * 128, :], writes=[dx])
                pool.dma_fn(lambda e: e.indirect_dma_start(out=y1, out_offset=None, in_=Ys, in_offset=IOA(ap=pos1i[:, tt:tt + 1], axis=0),
                                                           bounds_check=BCV, oob_is_err=False), reads=[d_Ys, d_pos], writes=[dy1])
                pool.dma_fn(lambda e: e.indirect_dma_start(out=y2, out_offset=None, in_=Ys, in_offset=IOA(ap=pos2i[:, tt:tt + 1], axis=0),
                                                           bounds_check=BCV, oob_is_err=False), reads=[d_Ys, d_pos], writes=[dy2])
                dve.op(lambda e: e.scalar_tensor_tensor(out=xt, in0=y1, scalar=p1a[:, tt:tt + 1], in1=xt, op0=ALU.mult, op1=ALU.add),
                       reads=[dy1, dx, d_pp], writes=[dx])
                dve.op(lambda e: e.scalar_tensor_tensor(out=xt, in0=y2, scalar=p2a[:, tt:tt + 1], in1=xt, op0=ALU.mult, op1=ALU.add),
                       reads=[dy2, dx, d_pp], writes=[dx])
                if "h3" in dbg_aps:
                    sp.dma(dbg_aps["h3"][tt * 128:(tt + 1) * 128, :], xt, reads=[dx])
                act.op(lambda e: e.activation(out=junk2, in_=xt, func=AF.Square, accum_out=st[:, 0:1]), reads=[dx], writes=[d_junk, d_st])
                dve.op(lambda e: e.tensor_scalar(out=st[:, 1:2], in0=st[:, 0:1], scalar1=1.0 / D, scalar2=1e-6, op0=ALU.mult, op1=ALU.add),
                       reads=[d_st], writes=[d_st])
                act.op(lambda e: e.activation(out=st[:, 2:3], in_=st[:, 1:2], func=AF.Sqrt), reads=[d_st], writes=[d_st])
                dve.op(lambda e: e.reciprocal(out=st[:, 3:4], in_=st[:, 2:3]), reads=[d_st], writes=[d_st])
                dve.op(lambda e: e.scalar_tensor_tensor(out=xt, in0=xt, scalar=st[:, 3:4], in1=gBt, op0=ALU.mult, op1=ALU.mult),
                       reads=[dx, d_st, d_gB], writes=[dx])
                sp.dma(out[tt * 128:(tt + 1) * 128, :], xt, reads=[dx])
            fw.barrier()

        fw.barrier()
    return nc


def host_consts(inp):
    l = 0
    f = np.float32
    gBh = np.stack([np.broadcast_to(v, (128, D)) for v in (inp["norm_mix_g"][l], inp["norm_xattn_g"][l], inp["norm_ffn_g"][l],
                                                            inp["norm_mem_g"][l], inp["norm_final_g"])]).astype(f)
    pvh = np.zeros((128, NPV), f)

    def col(v, n):
        return np.ascontiguousarray(np.asarray(v, f).reshape(n, 128).T)
    pvh[:, 0:8] = col(inp["pool_scale"][l], 8)
    mu = np.asarray(inp["rwkv_mu"][l], f)
    pvh[:, 8:32] = col(mu[0:3072], 24)
    pvh[:, 32] = mu[3072:3200]
    pvh[:, 33] = mu[3200:3328]
    pvh[0:32, 34] = mu[3328:3360]
    pvh[:, 35:43] = col(inp["rwkv_w0"][l], 8)
    pvh[:, 43:51] = col(inp["rwkv_a0"][l], 8)
    pvh[:, 51:59] = col(inp["rwkv_k_k"][l], 8)
    pvh[:, 59:67] = col(inp["rwkv_k_a"][l], 8)
    pvh[:, 67:75] = col(inp["rwkv_ln_w"][l], 8)
    pvh[:, 75:83] = col(inp["rwkv_ln_b"][l], 8)
    pvh[:, 83:91] = col(np.asarray(inp["rwkv_r_k"][l]).reshape(-1), 8)
    cst = np.zeros((128, 1024), f)
    p = np.arange(128)
    cst[:, 0:128] = np.eye(128, dtype=f)
    cst[:, 128:256] = (p[:, None] // 64 == p[None, :] // 64).astype(f)
    s = p % 64
    tcol = np.arange(64)
    strict = (s[:, None] < tcol[None, :]).astype(f)
    incl = (s[:, None] <= tcol[None, :]).astype(f)
    one = np.concatenate([strict, incl], 1)
    cst[:, 256:512] = np.concatenate([one, one], 1)
    low = (s[:, None] > tcol[None, :]).astype(f)
    cst[:, 512:640] = np.concatenate([low, low], 1)
    cst[:, 640:704] = (s[:, None] == tcol[None, :]).astype(f)
    tt = np.arange(16)
    for gi, w in enumerate((2, 4, 8, 16)):
        cst[:, 704 + gi * 16:704 + (gi + 1) * 16] = (1.0 / np.minimum(tt + 1, w)).astype(f)[None, :]
    cst[:, 768:896] = (p[:, None] < p[None, :]).astype(f)
    cst[:, 896] = p.astype(f)
    cst[:, 897:929] = np.arange(32, dtype=f)[None, :]
    cst[:, 929:937] = (float(SL) * np.arange(8, dtype=f))[None, :]
    rm = np.ones((128, T), f)
    rm[:, ::64] = 0.0
    w_r = np.concatenate([inp["moe_w_group"][l], inp["moe_w_expert"][l]], 1).astype(f)
    b_r = np.concatenate([inp["moe_b_group"][l], inp["moe_b_expert"][l]])[None, :].astype(f)
    return dict(gB=gBh, pv=pvh, cst=cst, rmask=rm, w_r=np.ascontiguousarray(w_r), b_r=b_r)


def make_in_maps(inp, cores):
    l = 0
    c = host_consts(inp)
    shared = dict(
        w_in=inp["w_in"][l], pool_w=inp["pool_w"][l], w2=inp["rwkv_w2"][l], a2=inp["rwkv_a2"][l], g2=inp["rwkv_g2"][l],
        w_out=inp["w_out"][l], w_q=inp["xattn_w_q"][l], w_kv=inp["xattn_w_kv"][l], w_o=inp["xattn_w_o"][l],
        wg_l=np.asarray(inp["moe_w_gate"][l], np.float32).reshape(16, 4, 4, 128, 512).transpose(0, 1, 3, 2, 4).reshape(8192, 2048),
        wu_l=np.asarray(inp["moe_w_up"][l], np.float32).reshape(16, 4, 4, 128, 512).transpose(0, 1, 3, 2, 4).reshape(8192, 2048),
        wd_l=np.asarray(inp["moe_w_down"][l], np.float32).reshape(8192, 2048), **c)
    shared = {k: np.ascontiguousarray(np.asarray(v, np.float32)) for k, v in shared.items()}
    maps = []
    for b in cores:
        m = dict(shared)
        m["x"] = np.ascontiguousarray(inp["x"][b])
        m["mem"] = np.ascontiguousarray(inp["mem"][b])
        maps.append(m)
    return maps


def kernel(**inputs):
    inp = {k: np.asarray(v) for k, v in inputs.items()}
    nc = build()
    maps = make_in_maps(inp, list(range(8)))
    res = run_bass_kernel_spmd(nc, maps, core_ids=list(range(8)))
    return np.stack([np.asarray(r["out"]) for r in res.results], 0).astype(np.float32)
```
